# Optimizing a Trainium2 kernel written in Bass

```python
import jax, jax.numpy as jnp
from jax import lax
import numpy as np

D_MODEL = 1024
BATCH = 4
SEQ = 8192
DEPTH = 2

GRID_W = 64
CTX_LEN = 256
HEAD_DIM = 64
N_HEADS_A = 8
N_KV_A = 2
N_HEADS_B = 8
N_KV_B = 2
BRANCH_WIDTH = 512
CONV_K = 3
WINDOW = 128
Q_BLOCK = 128
N_BRANCH = 3
N_EXPERTS = 16
EXPERT_HIDDEN = 1024
CAPACITY_FACTOR = 2
ROPE_BASE = 10000.0
EPS = 1e-6
NEG_INF = -1e30

KV_A = N_KV_A * HEAD_DIM
KV_B = N_KV_B * HEAD_DIM
KV_END = 2 * KV_A + 2 * KV_B
OFF_QA = KV_END
OFF_QB = OFF_QA + N_HEADS_A * HEAD_DIM
OFF_CONV = OFF_QB + N_HEADS_B * HEAD_DIM
OFF_GATE = OFF_CONV + 3 * BRANCH_WIDTH
PROJ_WIDTH = OFF_GATE + N_BRANCH * D_MODEL

kernel_name = "hybrid_parallel_gqa_window_shortconv_ecmoe_dit"


def rmsnorm(x, g):
    x32 = x.astype(jnp.float32)
    y = x32 * lax.rsqrt(jnp.mean(x32 * x32, axis=-1, keepdims=True) + EPS)
    return (y * g.astype(jnp.float32)).astype(x.dtype)


def modulate(h, shift, scale):
    return h * (1 + scale) + shift


def rope_tables(n):
    rows = n // GRID_W
    row = jnp.repeat(jnp.arange(rows, dtype=jnp.float32), GRID_W)
    col = jnp.tile(jnp.arange(GRID_W, dtype=jnp.float32), rows)
    n_freq = HEAD_DIM // 4
    inv = ROPE_BASE ** (-jnp.arange(n_freq, dtype=jnp.float32) / n_freq)
    ang = jnp.concatenate([row[:, None] * inv, col[:, None] * inv], axis=-1)
    return jnp.cos(ang), jnp.sin(ang)


def apply_rope(x, cos, sin):
    half = HEAD_DIM // 2
    c = cos[None, :, None, :].astype(x.dtype)
    s = sin[None, :, None, :].astype(x.dtype)
    x1, x2 = x[..., :half], x[..., half:]
    return jnp.concatenate([x1 * c - x2 * s, x2 * c + x1 * s], axis=-1)


def q_heads(p_q, qg, n_heads, n_kv, rope):
    B, n, _ = p_q.shape
    q = rmsnorm(p_q.reshape(B, n, n_heads, HEAD_DIM), qg)
    if rope is not None:
        q = apply_rope(q, *rope)
    return q.reshape(B, n, n_kv, n_heads // n_kv, HEAD_DIM)


def kv_heads(p_kv, kg_a, kg_b, rope):
    B, n, _ = p_kv.shape
    ka, va, kb, vb = jnp.split(p_kv, [KV_A, 2 * KV_A, 2 * KV_A + KV_B], axis=-1)
    ka = rmsnorm(ka.reshape(B, n, N_KV_A, HEAD_DIM), kg_a)
    kb = rmsnorm(kb.reshape(B, n, N_KV_B, HEAD_DIM), kg_b)
    va = va.reshape(B, n, N_KV_A, HEAD_DIM)
    vb = vb.reshape(B, n, N_KV_B, HEAD_DIM)
    if rope is not None:
        ka = apply_rope(ka, *rope)
        kb = apply_rope(kb, *rope)
    return ka, va, kb, vb


def attend(q, k, v, mask, sink):
    s = jnp.einsum('bqkgd,bskd->bkgqs', q, k, preferred_element_type=jnp.float32) * (HEAD_DIM ** -0.5)
    if mask is not None:
        s = jnp.where(mask, s, NEG_INF)
    if sink is not None:
        sk = jnp.broadcast_to(sink.astype(jnp.float32)[None, :, :, None, None], s.shape[:-1] + (1,))
        p = jax.nn.softmax(jnp.concatenate([s, sk], axis=-1), axis=-1)[..., :-1]
    else:
        p = jax.nn.softmax(s, axis=-1)
    return jnp.einsum('bkgqs,bskd->bqkgd', p.astype(v.dtype), v)


def to_blocks(q):
    B, n, KV, G, d = q.shape
    return q.reshape(B, n // Q_BLOCK, Q_BLOCK, KV, G, d).transpose(1, 0, 2, 3, 4, 5)


def from_blocks(o, n):
    nblk, B, qb, KV, G, d = o.shape
    return o.transpose(1, 0, 2, 3, 4, 5).reshape(B, n, KV * G * d)


def global_attention(q, k_lat, v_lat, k_ctx, v_ctx):
    k = jnp.concatenate([k_lat, k_ctx], axis=1)
    v = jnp.concatenate([v_lat, v_ctx], axis=1)
    o = lax.map(lambda qb: attend(qb, k, v, None, None), to_blocks(q))
    return from_blocks(o, q.shape[1])


def window_attention(q, k_lat, v_lat, k_ctx, v_ctx, sink):
    n = q.shape[1]
    nblk = n // Q_BLOCK
    pad = ((0, 0), (WINDOW, WINDOW), (0, 0), (0, 0))
    k_pad = jnp.pad(k_lat, pad)
    v_pad = jnp.pad(v_lat, pad)
    span = Q_BLOCK + 2 * WINDOW
    ctx_mask = jnp.ones((Q_BLOCK, k_ctx.shape[1]), dtype=bool)

    def block(args):
        i, qb = args
        start = i * Q_BLOCK
        kb = lax.dynamic_slice_in_dim(k_pad, start, span, axis=1)
        vb = lax.dynamic_slice_in_dim(v_pad, start, span, axis=1)
        qpos = start + jnp.arange(Q_BLOCK)
        kpos = start - WINDOW + jnp.arange(span)
        local = (jnp.abs(qpos[:, None] - kpos[None, :]) <= WINDOW) & (kpos >= 0)[None, :] & (kpos < n)[None, :]
        mask = jnp.concatenate([local, ctx_mask], axis=1)
        return attend(qb, jnp.concatenate([kb, k_ctx], axis=1), jnp.concatenate([vb, v_ctx], axis=1), mask, sink)

    o = lax.map(block, (jnp.arange(nblk), to_blocks(q)))
    return from_blocks(o, n)


def short_conv(p_conv, w):
    b_gate, c_gate, x_in = jnp.split(p_conv, 3, axis=-1)
    u = c_gate * x_in
    n = u.shape[1]
    up = jnp.pad(u, ((0, 0), (CONV_K // 2, CONV_K // 2), (0, 0)))
    conv = sum(w[j] * up[:, j:j + n] for j in range(CONV_K))
    return b_gate * conv


def merge_branches(o_a, o_b, o_c, gate_logits, w_branch, w_out):
    g_a, g_b, g_c = jnp.split(jax.nn.sigmoid(gate_logits), 3, axis=-1)
    merged = g_a * (o_a @ w_branch[0]) + g_b * (o_b @ w_branch[1]) + g_c * (o_c @ w_branch[2])
    return merged @ w_out


def latent_mixer(p, ctx_kv, rope, qg_a, kg_a, qg_b, kg_b, sink, conv_w, w_branch, w_out):
    ka, va, kb, vb = kv_heads(p[..., :KV_END], kg_a, kg_b, rope)
    ka_c, va_c, kb_c, vb_c = ctx_kv
    qa = q_heads(p[..., OFF_QA:OFF_QB], qg_a, N_HEADS_A, N_KV_A, rope)
    qb = q_heads(p[..., OFF_QB:OFF_CONV], qg_b, N_HEADS_B, N_KV_B, rope)
    o_a = global_attention(qa, ka, va, ka_c, va_c)
    o_b = window_attention(qb, kb, vb, kb_c, vb_c, sink.reshape(N_KV_B, N_HEADS_B // N_KV_B))
    o_c = short_conv(p[..., OFF_CONV:OFF_GATE], conv_w)
    return merge_branches(o_a, o_b, o_c, p[..., OFF_GATE:], w_branch, w_out)


def context_mixer(pc, ctx_kv, qg_a, qg_b, sink, conv_w, w_branch, w_out):
    B, L, _ = pc.shape
    ka_c, va_c, kb_c, vb_c = ctx_kv
    qa = q_heads(pc[..., OFF_QA:OFF_QB], qg_a, N_HEADS_A, N_KV_A, None)
    qb = q_heads(pc[..., OFF_QB:OFF_CONV], qg_b, N_HEADS_B, N_KV_B, None)
    o_a = attend(qa, ka_c, va_c, None, None).reshape(B, L, -1)
    o_b = attend(qb, kb_c, vb_c, None, sink.reshape(N_KV_B, N_HEADS_B // N_KV_B)).reshape(B, L, -1)
    o_c = short_conv(pc[..., OFF_CONV:OFF_GATE], conv_w)
    return merge_branches(o_a, o_b, o_c, pc[..., OFF_GATE:], w_branch, w_out)


def expert_choice_ffn(h, w_router, w_g, w_u, w_d):
    B, n, D = h.shape
    cap = CAPACITY_FACTOR * n // N_EXPERTS
    logits = jnp.einsum('bnd,de->ben', h, w_router, preferred_element_type=jnp.float32)
    aff = jax.nn.softmax(logits, axis=1)
    gate, idx = lax.top_k(aff, cap)
    bi = jnp.arange(B)[:, None, None]
    xs = h[bi, idx]
    a = jnp.einsum('becd,edf->becf', xs, w_g)
    u = jnp.einsum('becd,edf->becf', xs, w_u)
    y = jnp.einsum('becf,efd->becd', jax.nn.silu(a) * u, w_d)
    return jnp.zeros_like(h).at[bi, idx].add(y * gate[..., None].astype(y.dtype))


def setup_inputs(seed: int = 0) -> dict:
    key = jax.random.key(seed)
    ks = jax.random.split(key, 22)
    D = D_MODEL

    def nrm(k, shape, scale):
        return jax.random.normal(k, shape, jnp.float32) * scale

    return {
        "x": nrm(ks[0], (BATCH, SEQ, D), 1.0),
        "c": nrm(ks[1], (BATCH, D), 1.0),
        "ctx": nrm(ks[2], (BATCH, CTX_LEN, D), 1.0),
        "c_ctx": nrm(ks[3], (D,), 1.0),
        "w_ada": nrm(ks[4], (DEPTH, D, 6 * D), 0.5 * D ** -0.5),
        "b_ada": nrm(ks[5], (DEPTH, 6 * D), 0.02),
        "g_mix": 1.0 + nrm(ks[6], (DEPTH, D), 0.02),
        "g_ffn": 1.0 + nrm(ks[7], (DEPTH, D), 0.02),
        "w_in": nrm(ks[8], (DEPTH, D, PROJ_WIDTH), D ** -0.5),
        "qg_a": 1.0 + nrm(ks[9], (DEPTH, HEAD_DIM), 0.02),
        "kg_a": 1.0 + nrm(ks[10], (DEPTH, HEAD_DIM), 0.02),
        "qg_b": 1.0 + nrm(ks[11], (DEPTH, HEAD_DIM), 0.02),
        "kg_b": 1.0 + nrm(ks[12], (DEPTH, HEAD_DIM), 0.02),
        "sink_b": nrm(ks[13], (DEPTH, N_HEADS_B), 0.5),
        "conv_w": nrm(ks[14], (DEPTH, CONV_K, BRANCH_WIDTH), CONV_K ** -0.5),
        "w_branch": nrm(ks[15], (DEPTH, N_BRANCH, BRANCH_WIDTH, D), BRANCH_WIDTH ** -0.5),
        "w_out": nrm(ks[16], (DEPTH, D, D), D ** -0.5),
        "w_router": nrm(ks[17], (DEPTH, D, N_EXPERTS), D ** -0.5),
        "w_e_gate": nrm(ks[18], (DEPTH, N_EXPERTS, D, EXPERT_HIDDEN), D ** -0.5),
        "w_e_up": nrm(ks[19], (DEPTH, N_EXPERTS, D, EXPERT_HIDDEN), D ** -0.5),
        "w_e_down": nrm(ks[20], (DEPTH, N_EXPERTS, EXPERT_HIDDEN, D), EXPERT_HIDDEN ** -0.5),
    }


def reference(x, c, ctx, c_ctx, w_ada, b_ada, g_mix, g_ffn, w_in, qg_a, kg_a, qg_b, kg_b, sink_b,
              conv_w, w_branch, w_out, w_router, w_e_gate, w_e_up, w_e_down):
    n = x.shape[1]
    rope = rope_tables(n)
    silu_c = jax.nn.silu(c)
    silu_cc = jax.nn.silu(c_ctx)
    xc = ctx
    for l in range(DEPTH):
        last = l == DEPTH - 1
        mod = silu_c @ w_ada[l] + b_ada[l]
        sh1, sc1, gt1, sh2, sc2, gt2 = [m[:, None, :] for m in jnp.split(mod, 6, axis=-1)]
        modc = silu_cc @ w_ada[l] + b_ada[l]
        shc1, scc1, gtc1, shc2, scc2, gtc2 = jnp.split(modc, 6)

        hc = modulate(rmsnorm(xc, g_mix[l]), shc1, scc1)
        pc = hc @ (w_in[l][:, :KV_END] if last else w_in[l])
        ctx_kv = kv_heads(pc[..., :KV_END], kg_a[l], kg_b[l], None)

        h = modulate(rmsnorm(x, g_mix[l]), sh1, sc1)
        p = h @ w_in[l]
        x = x + gt1 * latent_mixer(p, ctx_kv, rope, qg_a[l], kg_a[l], qg_b[l], kg_b[l], sink_b[l],
                                   conv_w[l], w_branch[l], w_out[l])
        h2 = modulate(rmsnorm(x, g_ffn[l]), sh2, sc2)
        x = x + gt2 * expert_choice_ffn(h2, w_router[l], w_e_gate[l], w_e_up[l], w_e_down[l])

        if not last:
            xc = xc + gtc1 * context_mixer(pc, ctx_kv, qg_a[l], qg_b[l], sink_b[l], conv_w[l],
                                           w_branch[l], w_out[l])
            hc2 = modulate(rmsnorm(xc, g_ffn[l]), shc2, scc2)
            xc = xc + gtc2 * expert_choice_ffn(hc2, w_router[l], w_e_gate[l], w_e_up[l], w_e_down[l])
    return x
```

```python
import numpy as np
from contextlib import ExitStack
import concourse.bass as bass
import concourse.mybir as mybir
from concourse.bass_utils import run_bass_kernel_spmd

F32 = mybir.dt.float32
BF16 = mybir.dt.bfloat16
I32 = mybir.dt.int32
AF = mybir.ActivationFunctionType
ALU = mybir.AluOpType
AX = mybir.AxisListType

NL, NCX, NT, D = 8192, 256, 8448, 1024
NTL = NT // 128
EPS = 1e-6
NE = 16
N_CORES = 4


class K:
    def __init__(self, nc, stack):
        self.nc = nc
        self.stack = stack
        self.eng = {'pe': nc.tensor, 'act': nc.scalar, 'dve': nc.vector, 'pool': nc.gpsimd, 'sp': nc.sync}
        self.sem = {n: stack.enter_context(nc.semaphore("s_" + n)) for n in self.eng}
        self.cnt = {n: 0 for n in self.eng}
        self.seen = {n: {} for n in self.eng}
        self.dsem = {}
        self.dval = {}
        self.lastw = {}
        self.readers = {}
        self.nwaits = 0
        self.nins = 0
        self.drr = {}

    def _key(self, t):
        if isinstance(t, (str, int)):
            return t
        if isinstance(t, tuple):
            return tuple(self._key(x) for x in t)
        return ('id', id(t))

    def _deps(self, R, W):
        deps = []
        for t in list(R) + list(W):
            d = self.lastw.get(self._key(t))
            if d is not None:
                deps.append(d)
        for t in W:
            deps.extend(self.readers.get(self._key(t), {}).items())
        return deps

    def _wait(self, e, deps):
        h = self.eng[e]
        seen = self.seen[e]
        need = {}
        for key, val in deps:
            if key == ('E', 'pe') and e == 'pe':
                continue
            if seen.get(key, 0) >= val:
                continue
            if need.get(key, 0) < val:
                need[key] = val
        for key, val in need.items():
            s = self.sem[key[1]] if key[0] == 'E' else self.dsem[key[1]]
            h.wait_ge(s, val)
            seen[key] = val
            self.nwaits += 1

    def _record(self, tok, R, W):
        key, val = tok
        for t in R:
            r = self.readers.setdefault(self._key(t), {})
            if r.get(key, 0) < val:
                r[key] = val
        for t in W:
            self.lastw[self._key(t)] = tok
            self.readers[self._key(t)] = {}

    def op(self, e, fn, R=(), W=()):
        self._wait(e, self._deps(R, W))
        ins = fn(self.eng[e])
        ins.then_inc(self.sem[e], 1)
        self.cnt[e] += 1
        self.nins += 1
        self._record((('E', e), self.cnt[e]), R, W)
        return ins

    NDS = 8

    def dma(self, e, stream, fn, R=(), W=()):
        i = self.drr.get(e, 0)
        self.drr[e] = i + 1
        stream = "%s%d" % (e, i % self.NDS)
        if stream not in self.dsem:
            self.dsem[stream] = self.stack.enter_context(self.nc.semaphore("d_" + stream))
            self.dval[stream] = 0
        deps = self._deps(R, W)
        if self.dval[stream] > 0:
            deps.append((('D', stream), self.dval[stream]))
        self._wait(e, deps)
        ins = fn(self.eng[e])
        ins.then_inc(self.dsem[stream], 16)
        self.dval[stream] += 16
        self.nins += 1
        self._record((('D', stream), self.dval[stream]), R, W)
        return ins

    def barrier(self, engines=None):
        deps = [(('E', n), c) for n, c in self.cnt.items() if c > 0]
        deps += [(('D', s), v) for s, v in self.dval.items() if v > 0]
        for e in (engines or self.eng):
            self._wait(e, deps)


def build(nlayers=2, debug=False, stop=None, cut=99, force_ctx=False):
    nc = bass.Bass("TRN2", target_bir_lowering=False)
    st = ExitStack()
    with st:
        k = K(nc, st)

        def din(name, shape, dt=F32):
            return nc.dram_tensor(name, list(shape), dt, kind="ExternalInput")

        def dsc(name, shape, dt, out=False):
            return nc.dram_tensor(name, list(shape), dt, kind="ExternalOutput" if (out or debug) else "Internal")

        L = nlayers
        xin = din("xin", [NT, D])
        cvec = din("cvec", [128, 8, 2])
        w_ada = din("w_ada", [L, 128, 8, 6144])
        b_col2 = din("b_col2", [L, 128, 96])
        b_bc = din("b_bc", [L, 128, 4, D])
        gcol2 = din("gcol2", [L, 128, 2, 8, 2])
        gffn_bc = din("gffn_bc", [L, 128, D])
        w1d = din("w1", [L, 128, 8, 768])
        w2d = din("w2", [L, 128, 8, 2048])
        wcxd = din("wcx", [L, 128, 8, 1024])
        w3d = din("w3", [L, 128, 8, 3584])
        wbrd = din("wbr", [L, 128, 12, D])
        woutd = din("wout", [L, 128, 8, D])
        hgd = din("hg", [L, 128, 8])
        sinkd = din("sink", [L, 128, 8])
        convwd = din("convw", [L, 128, 4, 3])
        wrd = din("wr", [L, 128, 8, NE])
        wegd = din("weg", [L, NE, 128, 8, D])
        weud = din("weu", [L, NE, 128, 8, D])
        wedd = din("wed", [L, NE, 128, 8, D])
        ropeCd = din("ropeC", [128, NT])
        ropeSd = din("ropeS", [128, NT])
        cmisc = din("cmisc", [128, 5, 128])
        iotad = din("iota", [128, 1024])
        combd = din("comb", [128, NTL, NE, 5])
        xs = dsc("xs", [NT, D], F32, out=True)
        hTd = dsc("hTd", [128, 8, NT], BF16)
        uTd = dsc("uTd", [128, 4, NT + 4], F32)
        oaTd = dsc("oaTd", [128, 4, NT], BF16)
        obTd = dsc("obTd", [128, 4, NT], BF16)
        mTd = dsc("mTd", [128, 8, NT], BF16)
        h2d = dsc("h2d", [NT, D], BF16)
        affd = dsc("affd", [NT, NE], F32)
        bcd = dsc("bcd", [2, 4, 128, D], F32)
        xgd = dsc("xgd", [NE * 9 * 128, D], BF16)
        Yd = dsc("Yd", [NE * 9 * 128, D], F32)

        def sb(name, shape, dt, stack=None):
            return (stack or st).enter_context(nc.sbuf_tensor(name, list(shape), dt))

        PS = [st.enter_context(nc.psum_tensor("ps%d" % i, [128, 512], F32)) for i in range(7)]
        PSB = st.enter_context(nc.psum_tensor("psb", [128, 1024], BF16))
        psrr = {}

        def ps_get(pool, banks):
            i = psrr.get(pool, 0)
            psrr[pool] = i + 1
            return PS[banks[i % len(banks)]]

        ident = sb("ident", [128, 128], F32)
        blockones = sb("blockones", [128, 128], F32)
        ones32 = sb("ones32", [128, 128], F32)
        onesb = sb("onesb", [128, 128], BF16)
        triL = sb("triL", [128, 128], BF16)
        maskP = sb("maskP", [128, 128], BF16)
        maskN = sb("maskN", [128, 128], BF16)
        identb = sb("identb", [128, 128], BF16)
        sc = sb("sc", [128, 8, 2], F32)
        lbc = sb("lbc", [128, 8, 2, 128], F32)
        modcol = sb("modcol", [128, 48, 2], F32)
        A1c = sb("A1c", [128, 8, 2], F32)
        hg = sb("hgs", [128, 8], F32)
        esink = sb("esink", [128, 8], F32)
        cw = sb("cw", [128, 4, 3], F32)
        ss = sb("ss", [128, 4], F32)
        rsd = sb("rsd", [128, 4], F32)

        g = nc.gpsimd
        lr1 = st.enter_context(g.register("lr1"))
        lr2 = st.enter_context(g.register("lr2"))
        licur = sb("licur", [128, 1], I32)

        def gather_loop(idx_all, src, dst, xg_, nj, tag):
            s1 = st.enter_context(nc.semaphore("lc" + tag))
            s2 = st.enter_context(nc.semaphore("ld" + tag))
            with g.Fori(0, nj) as j:
                g.tensor_copy(out=licur[:, 0:1], in_=idx_all[:, bass.ds(j, 1)]).then_inc(s1, 1)
                g.reg_mov(lr1, 1)
                g.reg_add(lr1, lr1, j)
                g.wait_ge(s1, lr1)
                g.indirect_dma_start(out=xg_[:, :], out_offset=None, in_=src[:, :], in_offset=bass.IndirectOffsetOnAxis(ap=licur[:, 0:1], axis=0),
                                     bounds_check=NT - 1, oob_is_err=False).then_inc(s2, 16)
                g.reg_mov(lr1, 32)
                g.reg_mul(lr1, lr1, j)
                g.reg_add(lr1, lr1, 16)
                g.wait_ge(s2, lr1)
                g.reg_mov(lr2, 128 * D)
                g.reg_mul(lr2, lr2, j)
                g.dma_start(out=bass.AP(dst, lr2, [[D, 128], [1, D]]), in_=xg_[:, :]).then_inc(s2, 16)
                g.reg_add(lr1, lr1, 16)
                g.wait_ge(s2, lr1)

        def scatter_loop(idx_all, src, dst, yt_, nj, tag):
            s1 = st.enter_context(nc.semaphore("sc" + tag))
            s2 = st.enter_context(nc.semaphore("sd" + tag))
            with g.Fori(0, nj) as j:
                g.tensor_copy(out=licur[:, 0:1], in_=idx_all[:, bass.ds(j, 1)]).then_inc(s1, 1)
                g.reg_mov(lr1, 1)
                g.reg_add(lr1, lr1, j)
                g.wait_ge(s1, lr1)
                g.reg_mov(lr2, 128 * D)
                g.reg_mul(lr2, lr2, j)
                g.dma_start(out=yt_[:, :], in_=bass.AP(src, lr2, [[D, 128], [1, D]])).then_inc(s2, 16)
                g.reg_mov(lr1, 32)
                g.reg_mul(lr1, lr1, j)
                g.reg_add(lr1, lr1, 16)
                g.wait_ge(s2, lr1)
                g.indirect_dma_start(out=dst[:, :], out_offset=bass.IndirectOffsetOnAxis(ap=licur[:, 0:1], axis=0), in_=yt_[:, :], in_offset=None,
                                     bounds_check=NT - 1, oob_is_err=False, compute_op=ALU.add).then_inc(s2, 16)
                g.reg_add(lr1, lr1, 16)
                g.wait_ge(s2, lr1)

        def ld(dst, src, eng='sp', stream='misc', R=(), W=None, slow=False):
            if slow:
                return k.dma(eng, stream, lambda e: e.dma_start(out=dst, in_=src, allow_slow_non_contiguous=True), R=R, W=W)
            return k.dma(eng, stream, lambda e: e.dma_start(out=dst, in_=src), R=R, W=W)

        ld(ident[:], cmisc[:, 0, :], W=[ident])
        ld(blockones[:], cmisc[:, 1, :], W=[blockones])
        ld(triL[:], cmisc[:, 2, :], eng='pool', stream='miscp', W=[triL])
        ld(maskP[:], cmisc[:, 3, :], eng='pool', stream='miscp', W=[maskP])
        ld(maskN[:], cmisc[:, 4, :], eng='pool', stream='miscp', W=[maskN])
        ld(identb[:], cmisc[:, 0, :], eng='pool', stream='miscp', W=[identb])
        ld(sc[:], cvec.ap(), W=[sc])
        k.op('dve', lambda e: e.memset(ones32[:], 1.0), W=[ones32])
        k.op('dve', lambda e: e.memset(onesb[:], 1.0), W=[onesb])
        for i in range(4):
            r0, r1 = i * (NT // 4), (i + 1) * (NT // 4)
            ld(xs[r0:r1, :], xin[r0:r1, :], stream='xcopy', W=[('xs', 'init')])
        k.op('act', lambda e: e.activation(out=sc[:], in_=sc[:], func=AF.Silu), R=[sc], W=[sc])
        for kc in range(8):
            for v in range(2):
                k.op('dve', lambda e, kc=kc, v=v: e.tensor_scalar(out=lbc[:, kc, v, :], in0=ones32[:], scalar1=sc[:, kc, v:v + 1],
                                                                   scalar2=None, op0=ALU.mult), R=[sc, ones32], W=[lbc])
        k.barrier()

        blocks = [(i * 512, 512, 0) for i in range(16)] + [(NL, NCX, 1)]

        def rstd_of(xt, junk, col):
            k.op('pool', lambda e: e.memset(ss[:, col:col + 1], 0.0), W=[('ss', col)])
            k.op('act', lambda e: e.activation(out=junk[:], in_=xt[:], func=AF.Square, accum_out=ss[:, col:col + 1]),
                 R=[xt], W=[junk, ('ss', col)])
            k.op('act', lambda e: e.activation(out=rsd[:, col:col + 1], in_=ss[:, col:col + 1], func=AF.Sqrt, scale=1.0 / D, bias=EPS),
                 R=[('ss', col)], W=[('rsd', col)])
            k.op('dve', lambda e: e.reciprocal(out=rsd[:, col:col + 1], in_=rsd[:, col:col + 1]), R=[('rsd', col)], W=[('rsd', col)])

        for l in range(L):
            last = (l == L - 1) and not force_ctx
            with ExitStack() as p0:
                wa = [sb("wa%d_%d" % (l, i), [128, 8, 512], F32, p0) for i in range(2)]
                bb = sb("bb%d" % l, [128, 4, D], F32, p0)
                gfb = sb("gfb%d" % l, [128, D], F32, p0)
                rowt = sb("rowt%d" % l, [128, D], F32, p0)
                bcol = sb("bcol%d" % l, [128, 96], F32, p0)
                gc2 = sb("gc2%d" % l, [128, 2, 8, 2], F32, p0)
                ld(bb[:], b_bc[l], W=[bb])
                ld(gfb[:], gffn_bc[l], W=[gfb])
                ld(bcol[:], b_col2[l], W=[bcol])
                ld(gc2[:], gcol2[l], W=[gc2])
                ld(hg[:], hgd[l], W=[hg])
                ld(esink[:], sinkd[l], W=[esink])
                ld(cw[:], convwd[l], W=[cw])
                k.op('act', lambda e: e.activation(out=esink[:], in_=esink[:], func=AF.Exp), R=[esink], W=[esink])
                psmod = PS[0]
                rowsel = {4: (0, 0), 5: (0, 1), 6: (1, 0), 7: (1, 1), 8: (2, 0), 9: (2, 1), 10: (3, 0), 11: (3, 1)}
                for ch in range(12):
                    w = wa[ch % 2]
                    ld(w[:], w_ada[l, :, :, ch * 512:(ch + 1) * 512], stream='wa%d' % (ch % 2), W=[w])
                    for sub in range(4):
                        j = ch * 4 + sub
                        for kc in range(8):
                            k.op('pe', lambda e, w=w, j=j, kc=kc, sub=sub: e.matmul(
                                psmod[:, 2 * j:2 * j + 2], lhsT=w[:, kc, sub * 128:(sub + 1) * 128], rhs=sc[:, kc, :],
                                start=(kc == 0), stop=(kc == 7)), R=[w, sc], W=[psmod])
                    if ch in rowsel:
                        vi, half = rowsel[ch]
                        for v in range(2):
                            pr = PS[1 + v]
                            for kc in range(8):
                                k.op('pe', lambda e, w=w, kc=kc, v=v, pr=pr: e.matmul(
                                    pr[:, :], lhsT=lbc[:, kc, v, :], rhs=w[:, kc, :], start=(kc == 0), stop=(kc == 7)),
                                    R=[w, lbc], W=[pr])
                            hs = slice(half * 512, (half + 1) * 512)
                            k.op('dve', lambda e, pr=pr, vi=vi, hs=hs: e.tensor_tensor(
                                out=rowt[:, hs], in0=pr[:, :], in1=bb[:, vi, hs], op=ALU.add), R=[pr, bb], W=[rowt])
                            if vi == 2:
                                k.op('dve', lambda e, hs=hs: e.scalar_tensor_tensor(
                                    out=rowt[:, hs], in0=rowt[:, hs], scalar=1.0, in1=gfb[:, hs], op0=ALU.add, op1=ALU.mult),
                                    R=[rowt, gfb], W=[rowt])
                            ld(bcd[v, vi, :, hs], rowt[:, hs], stream='bcst', R=[rowt], W=[('bcd', v, vi, half)])
                mc = modcol[:].rearrange("p a b -> p (a b)")
                k.op('dve', lambda e: e.tensor_tensor(out=mc, in0=psmod[:, 0:96], in1=bcol[:], op=ALU.add), R=[psmod, bcol], W=[modcol])
                k.op('dve', lambda e: e.scalar_tensor_tensor(out=A1c[:], in0=modcol[:, 8:16, :], scalar=1.0, in1=gc2[:, 0, :, :],
                                                            op0=ALU.add, op1=ALU.mult), R=[modcol, gc2], W=[A1c])
                k.barrier()
            if stop == 'p0':
                break

            with ExitStack() as pkv:
                KTA = sb("KTA%d" % l, [128, NT], BF16, pkv)
                KTB = sb("KTB%d" % l, [128, NT], BF16, pkv)
                VA = sb("VA%d" % l, [128, NTL, 2, 80], BF16, pkv)
                VB = sb("VB%d" % l, [128, NTL, 2, 80], BF16, pkv)
                KT = {'A': KTA, 'B': KTB}
                VV = {'A': VA, 'B': VB}
                k.op('pool', lambda e: e.memset(VA[:], 0.0), W=[VA])
                k.op('pool', lambda e: e.memset(VB[:], 0.0), W=[VB])
                k.op('pool', lambda e: e.memset(VA[:, :, :, 64:65], 1.0), W=[VA])
                k.op('pool', lambda e: e.memset(VB[:, :, :, 64:65], 1.0), W=[VB])

                def normrope(psp, psr, gi, Cb, Sb, out_ap, outres, n, tmp):
                    sq, rt, t1, t2 = tmp
                    k.op('act', lambda e: e.activation(out=sq[:, :n], in_=psp[:, :n], func=AF.Square), R=[psp], W=[sq])
                    pq = ps_get('nr', [6])
                    k.op('pe', lambda e: e.matmul(pq[:, :n], lhsT=blockones[:], rhs=sq[:, :n], start=True, stop=True),
                         R=[sq, blockones], W=[pq])
                    k.op('act', lambda e: e.activation(out=rt[:, :n], in_=pq[:, :n], func=AF.Sqrt, scale=1.0 / 64, bias=EPS),
                         R=[pq], W=[rt])
                    k.op('dve', lambda e: e.reciprocal(out=rt[:, :n], in_=rt[:, :n]), R=[rt], W=[rt])
                    k.op('dve', lambda e: e.scalar_tensor_tensor(out=t1[:, :n], in0=psp[:, :n], scalar=hg[:, gi:gi + 1], in1=Cb[:, :n],
                                                                op0=ALU.mult, op1=ALU.mult), R=[psp, Cb, hg], W=[t1])
                    k.op('dve', lambda e: e.scalar_tensor_tensor(out=t2[:, :n], in0=psr[:, :n], scalar=hg[:, gi + 1:gi + 2], in1=Sb[:, :n],
                                                                op0=ALU.mult, op1=ALU.mult), R=[psr, Sb, hg], W=[t2])
                    k.op('pool', lambda e: e.tensor_tensor(out=t1[:, :n], in0=t1[:, :n], in1=t2[:, :n], op=ALU.add), R=[t1, t2], W=[t1])
                    k.op('pool', lambda e: e.tensor_tensor(out=out_ap, in0=t1[:, :n], in1=rt[:, :n], op=ALU.mult), R=[t1, rt], W=[outres])

                with ExitStack() as p1:
                    W1 = sb("W1_%d" % l, [128, 8, 768], BF16, p1)
                    ld(W1[:], w1d[l], eng='pool', stream='wbig', W=[W1])
                    xt2 = [sb("xt%d_%d" % (l, i), [128, D], F32, p1) for i in range(2)]
                    xn2 = [sb("xn%d_%d" % (l, i), [128, D], F32, p1) for i in range(2)]
                    junk = sb("junk%d" % l, [128, D], F32, p1)
                    hTb2 = [sb("hTb%d_%d" % (l, i), [128, 8, 512], BF16, p1) for i in range(2)]
                    Cb2 = [sb("Cb%d_%d" % (l, i), [128, 512], F32, p1) for i in range(2)]
                    Sb2 = [sb("Sb%d_%d" % (l, i), [128, 512], F32, p1) for i in range(2)]
                    tmps = [[sb("nt%d_%d_%d" % (l, i, j), [128, 512], F32, p1) for j in range(4)] for i in range(2)]
                    ti_glob = 0
                    for bi, (t0, ntok, v) in enumerate(blocks if cut >= 5 else blocks[:1]):
                        if cut < 2:
                            break
                        hTb = hTb2[bi % 2]
                        Cb, Sb = Cb2[bi % 2], Sb2[bi % 2]
                        ld(Cb[:, :ntok], ropeCd[:, t0:t0 + ntok], stream='rope%d' % (bi % 2), W=[Cb])
                        ld(Sb[:, :ntok], ropeSd[:, t0:t0 + ntok], stream='rope%d' % (bi % 2), W=[Sb])
                        for tt in range(ntok // 128):
                            xt = xt2[ti_glob % 2]
                            xn = xn2[ti_glob % 2]
                            col = ti_glob % 2
                            r0 = t0 + tt * 128
                            ld(xt[:], xs[r0:r0 + 128, :], stream='xt%d' % (ti_glob % 2), R=[('xs', 'init')], W=[xt])
                            rstd_of(xt, junk, col)
                            k.op('dve', lambda e, xn=xn, xt=xt, col=col: e.tensor_scalar(out=xn[:], in0=xt[:], scalar1=rsd[:, col:col + 1],
                                                                                      scalar2=None, op0=ALU.mult),
                                 R=[xt, ('rsd', col)], W=[xn])
                            for half in range(2):
                                pt = ps_get('tr', [0, 1])
                                for q in range(4):
                                    kc = half * 4 + q
                                    k.op('pe', lambda e, pt=pt, q=q, kc=kc, xn=xn: e.transpose(pt[:, q * 128:(q + 1) * 128],
                                                                                               xn[:, kc * 128:(kc + 1) * 128], ident[:]),
                                         R=[xn, ident], W=[pt])
                                for q in range(4):
                                    kc = half * 4 + q
                                    eng = 'act' if q % 2 == 0 else 'dve'
                                    dst = hTb[:, kc, tt * 128:(tt + 1) * 128]
                                    if eng == 'act':
                                        k.op('act', lambda e, pt=pt, q=q, kc=kc, dst=dst: e.activation(
                                            out=dst, in_=pt[:, q * 128:(q + 1) * 128], func=AF.Identity,
                                            scale=A1c[:, kc, v:v + 1], bias=modcol[:, kc, v:v + 1]), R=[pt, A1c, modcol], W=[hTb])
                                    else:
                                        k.op('dve', lambda e, pt=pt, q=q, kc=kc, dst=dst: e.tensor_scalar(
                                            out=dst, in0=pt[:, q * 128:(q + 1) * 128], scalar1=A1c[:, kc, v:v + 1],
                                            scalar2=modcol[:, kc, v:v + 1], op0=ALU.mult, op1=ALU.add), R=[pt, A1c, modcol], W=[hTb])
                            ti_glob += 1
                        ld(hTd[:, :, t0:t0 + ntok], hTb[:, :, :ntok], stream='hst%d' % (bi % 2), R=[hTb], W=[('hTd', bi)])
                        for ai, (nm, c0, gi) in enumerate((('A', 0, 2), ('B', 128, 6)) if cut >= 3 else ()):
                            psp = ps_get('kp', [2, 3])
                            psr = ps_get('kr', [4, 5])
                            for kc in range(8):
                                k.op('pe', lambda e, kc=kc, psp=psp, c0=c0: e.matmul(psp[:, :ntok], lhsT=W1[:, kc, c0:c0 + 128],
                                                                                    rhs=hTb[:, kc, :ntok], start=(kc == 0), stop=(kc == 7)),
                                     R=[W1, hTb], W=[psp])
                            for kc in range(8):
                                k.op('pe', lambda e, kc=kc, psr=psr, c0=c0: e.matmul(psr[:, :ntok], lhsT=W1[:, kc, 256 + c0:256 + c0 + 128],
                                                                                    rhs=hTb[:, kc, :ntok], start=(kc == 0), stop=(kc == 7)),
                                     R=[W1, hTb], W=[psr])
                            normrope(psp, psr, gi, Cb, Sb, KT[nm][:, t0:t0 + ntok], KT[nm], ntok, tmps[ai])
                        for tt in range(ntok // 128 if cut >= 4 else 0):
                            ti = (t0 + tt * 128) // 128
                            pv = ps_get('kp', [2, 3]) if cut != 41 else ps_get('tr', [0, 1])
                            for kc in range(8):
                                k.op('pe', lambda e, kc=kc, pv=pv, tt=tt: e.matmul(pv[:, 0:256], lhsT=hTb[:, kc, tt * 128:(tt + 1) * 128],
                                                                                  rhs=W1[:, kc, 512:768], start=(kc == 0), stop=(kc == 7)),
                                     R=[W1, hTb], W=[pv])
                            for g_ in range(2):
                                k.op('act', lambda e, pv=pv, ti=ti, g_=g_: e.copy(out=VA[:, ti, g_, 0:64], in_=pv[:, g_ * 64:(g_ + 1) * 64]),
                                     R=[pv], W=[VA])
                                k.op('dve', lambda e, pv=pv, ti=ti, g_=g_: e.tensor_copy(out=VB[:, ti, g_, 0:64], in_=pv[:, 128 + g_ * 64:128 + (g_ + 1) * 64]),
                                     R=[pv], W=[VB])
                    k.barrier()
                if stop == 'p1':
                    if debug:
                        dk = nc.dram_tensor("dbgKTA", [128, NT], BF16, kind="ExternalOutput")
                        dkb = nc.dram_tensor("dbgKTB", [128, NT], BF16, kind="ExternalOutput")
                        dv = nc.dram_tensor("dbgVA", [128, NTL, 2, 80], BF16, kind="ExternalOutput")
                        ld(dk.ap(), KTA[:], stream='dbg', R=[KTA], W=['dbgo'])
                        ld(dkb.ap(), KTB[:], stream='dbg', R=[KTB], W=['dbgo'])
                        ld(dv.ap(), VA[:], stream='dbg', R=[VA], W=['dbgo'])
                        k.barrier()
                    break

                with ExitStack() as p2:
                    W2 = sb("W2_%d" % l, [128, 8, 2048], BF16, p2)
                    ld(W2[:], w2d[l], eng='pool', stream='wbig', W=[W2])
                    hTb2 = [sb("hTq%d_%d" % (l, i), [128, 8, 512], BF16, p2) for i in range(2)]
                    Cb2 = [sb("Cq%d_%d" % (l, i), [128, 512], F32, p2) for i in range(2)]
                    Sb2 = [sb("Sq%d_%d" % (l, i), [128, 512], F32, p2) for i in range(2)]
                    tmps = [[sb("qt%d_%d_%d" % (l, i, j), [128, 512], F32, p2) for j in range(4)] for i in range(2)]
                    QT = {'A': sb("QTA%d" % l, [128, 4, 512], BF16, p2), 'B': sb("QTB%d" % l, [128, 4, 512], BF16, p2)}
                    OT2 = {'A': [sb("OaT%d_%d" % (l, i), [128, 4, 512], BF16, p2) for i in range(2)],
                           'B': [sb("ObT%d_%d" % (l, i), [128, 4, 512], BF16, p2) for i in range(2)]}
                    PT = [sb("PT%d_%d" % (l, i), [128, 512], BF16, p2) for i in range(4)]
                    rec = [sb("rec%d_%d" % (l, i), [128, 512], F32, p2) for i in range(2)]
                    bcs = [sb("bcs%d_%d" % (l, i), [64, 512], F32, p2) for i in range(2)]
                    ptc = [0]
                    fin = [0]

                    def finalize(pso, n, dst_ap, dstres, sink_col=None):
                        r = rec[fin[0] % 2]
                        bc = bcs[fin[0] % 2]
                        fin[0] += 1
                        if sink_col is not None:
                            k.op('dve', lambda e: e.tensor_scalar(out=r[64:65, :n], in0=pso[64:65, :n], scalar1=esink[64:65, sink_col:sink_col + 1],
                                                                  scalar2=None, op0=ALU.add), R=[pso, esink], W=[r])
                            k.op('dve', lambda e: e.reciprocal(out=r[64:65, :n], in_=r[64:65, :n]), R=[r], W=[r])
                        else:
                            k.op('dve', lambda e: e.reciprocal(out=r[64:65, :n], in_=pso[64:65, :n]), R=[pso], W=[r])
                        pb = ps_get('nr', [6])
                        k.op('pe', lambda e: e.matmul(pb[0:64, :n], lhsT=ones32[64:65, 0:64], rhs=r[64:65, :n], start=True, stop=True),
                             R=[r, ones32], W=[pb])
                        k.op('act', lambda e: e.copy(out=bc[0:64, :n], in_=pb[0:64, :n]), R=[pb], W=[bc])
                        k.op('dve', lambda e: e.tensor_tensor(out=dst_ap, in0=pso[0:64, :n], in1=bc[0:64, :n], op=ALU.mult),
                             R=[pso, bc], W=[dstres])

                    qblocks = blocks if not last else blocks[:16]
                    for bi, (t0, ntok, v) in enumerate(qblocks):
                        latent = (v == 0)
                        hTb = hTb2[bi % 2]
                        Cb, Sb = Cb2[bi % 2], Sb2[bi % 2]
                        ld(hTb[:, :, :ntok], hTd[:, :, t0:t0 + ntok], stream='hq%d' % (bi % 2), R=[('hTd', bi)], W=[hTb])
                        ld(Cb[:, :ntok], ropeCd[:, t0:t0 + ntok], stream='rq%d' % (bi % 2), W=[Cb])
                        ld(Sb[:, :ntok], ropeSd[:, t0:t0 + ntok], stream='rq%d' % (bi % 2), W=[Sb])
                        qi = 0
                        for nm, base, gi in (('A', 0, 0), ('B', 1024, 4)):
                            for j in range(4):
                                psp = ps_get('kp', [2, 3])
                                psr = ps_get('kr', [4, 5])
                                for kc in range(8):
                                    k.op('pe', lambda e, kc=kc, psp=psp, c0=base + j * 128: e.matmul(
                                        psp[:, :ntok], lhsT=W2[:, kc, c0:c0 + 128], rhs=hTb[:, kc, :ntok], start=(kc == 0), stop=(kc == 7)),
                                        R=[W2, hTb], W=[psp])
                                for kc in range(8):
                                    k.op('pe', lambda e, kc=kc, psr=psr, c0=base + 512 + j * 128: e.matmul(
                                        psr[:, :ntok], lhsT=W2[:, kc, c0:c0 + 128], rhs=hTb[:, kc, :ntok], start=(kc == 0), stop=(kc == 7)),
                                        R=[W2, hTb], W=[psr])
                                normrope(psp, psr, gi, Cb, Sb, QT[nm][:, j, :ntok], (QT[nm], j), ntok, tmps[qi % 2])
                                qi += 1
                        OaT = OT2['A'][bi % 2]
                        ObT = OT2['B'][bi % 2]
                        kts = list(range(NTL)) if latent else [64, 65]
                        for j in range(4):
                            for hh in range(2):
                                rows = slice(64 * hh, 64 * hh + 64)
                                pso = ps_get('o', [4, 5])
                                for n_, kt in enumerate(kts):
                                    pss = ps_get('s', [0, 1, 2, 3])
                                    pt = PT[ptc[0] % 4]
                                    ptc[0] += 1
                                    k.op('pe', lambda e, pss=pss, kt=kt: e.matmul(pss[:, :ntok], lhsT=KTA[rows, kt * 128:(kt + 1) * 128],
                                                                                 rhs=QT['A'][rows, j, :ntok], start=True, stop=True),
                                         R=[KTA, (QT['A'], j)], W=[pss])
                                    k.op('act', lambda e, pss=pss, pt=pt: e.activation(out=pt[:, :ntok], in_=pss[:, :ntok], func=AF.Exp, scale=0.125),
                                         R=[pss], W=[pt])
                                    k.op('pe', lambda e, pso=pso, pt=pt, kt=kt, n_=n_: e.matmul(
                                        pso[0:65, :ntok], lhsT=VA[:, kt, hh, 0:65], rhs=pt[:, :ntok], start=(n_ == 0), stop=(n_ == len(kts) - 1)),
                                        R=[VA, pt], W=[pso])
                                finalize(pso, ntok, OaT[rows, j, :ntok], (OaT, j, hh))
                        for j in range(4):
                            for hh in range(2):
                                rows = slice(64 * hh, 64 * hh + 64)
                                head = 4 * hh + j
                                pso = ps_get('o', [4, 5])
                                for qt in range(ntok // 128):
                                    cols = slice(qt * 128, (qt + 1) * 128)
                                    I = (t0 + qt * 128) // 128
                                    kl = [(64, None), (65, None)]
                                    if latent:
                                        if I - 1 >= 0:
                                            kl.append((I - 1, maskP))
                                        kl.append((I, None))
                                        if I + 1 < 64:
                                            kl.append((I + 1, maskN))
                                    for n_, (kt, msk) in enumerate(kl):
                                        pss = ps_get('s', [0, 1, 2, 3])
                                        pt = PT[ptc[0] % 4]
                                        ptc[0] += 1
                                        k.op('pe', lambda e, pss=pss, kt=kt: e.matmul(pss[:, 0:128], lhsT=KTB[rows, kt * 128:(kt + 1) * 128],
                                                                                     rhs=QT['B'][rows, j, cols], start=True, stop=True),
                                             R=[KTB, (QT['B'], j)], W=[pss])
                                        k.op('act', lambda e, pss=pss, pt=pt: e.activation(out=pt[:, 0:128], in_=pss[:, 0:128], func=AF.Exp, scale=0.125),
                                             R=[pss], W=[pt])
                                        if msk is not None:
                                            k.op('pool', lambda e, pt=pt, msk=msk: e.tensor_tensor(out=pt[:, 0:128], in0=pt[:, 0:128], in1=msk[:],
                                                                                                    op=ALU.mult), R=[pt, msk], W=[pt])
                                        k.op('pe', lambda e, pso=pso, pt=pt, kt=kt, n_=n_, kl=kl: e.matmul(
                                            pso[0:65, cols], lhsT=VB[:, kt, hh, 0:65], rhs=pt[:, 0:128], start=(n_ == 0), stop=(n_ == len(kl) - 1)),
                                            R=[VB, pt], W=[pso])
                                finalize(pso, ntok, ObT[rows, j, :ntok], (ObT, j, hh), sink_col=head)
                        ld(oaTd[:, :, t0:t0 + ntok], OaT[:, :, :ntok], stream='ost%d' % (bi % 2), R=[(OaT, j, hh) for j in range(4) for hh in range(2)],
                           W=[('oaTd', bi)])
                        ld(obTd[:, :, t0:t0 + ntok], ObT[:, :, :ntok], stream='ost%d' % (bi % 2), R=[(ObT, j, hh) for j in range(4) for hh in range(2)],
                           W=[('obTd', bi)])
                    k.barrier()
            if stop == 'p2':
                break
            mblocks = blocks if not last else blocks[:16]

            def uoff(t0):
                return t0 + 1 if t0 < NL else t0 + 3

            with ExitStack() as p3a:
                Wcx = sb("Wcx%d" % l, [128, 8, 1024], BF16, p3a)
                ld(Wcx[:], wcxd[l], eng='pool', stream='wbig', W=[Wcx])
                hTb2 = [sb("hTu%d_%d" % (l, i), [128, 8, 512], BF16, p3a) for i in range(2)]
                ub2 = [sb("ub%d_%d" % (l, i), [128, 4, 512], F32, p3a) for i in range(2)]
                cs2 = [sb("cs%d_%d" % (l, i), [128, 512], F32, p3a) for i in range(2)]
                zt = sb("zt%d" % l, [128, 4, 2], F32, p3a)
                k.op('dve', lambda e: e.memset(zt[:], 0.0), W=[zt])
                ld(uTd[:, :, 0:1], zt[:, :, 0:1], stream='zp', R=[zt], W=['uzp'], slow=True)
                ld(uTd[:, :, NL + 1:NL + 3], zt[:, :, 0:2], stream='zp', R=[zt], W=['uzp'], slow=True)
                ld(uTd[:, :, NT + 3:NT + 4], zt[:, :, 0:1], stream='zp', R=[zt], W=['uzp'], slow=True)
                cc = 0
                for bi, (t0, ntok, v) in enumerate(mblocks):
                    hTb = hTb2[bi % 2]
                    ub = ub2[bi % 2]
                    ld(hTb[:, :, :ntok], hTd[:, :, t0:t0 + ntok], stream='hu%d' % (bi % 2), W=[hTb])
                    for c in range(4):
                        psc = ps_get('kp', [2, 3])
                        psx = ps_get('kr', [4, 5])
                        cs = cs2[cc % 2]
                        cc += 1
                        for kc in range(8):
                            k.op('pe', lambda e, kc=kc, psc=psc, c=c: e.matmul(psc[:, :ntok], lhsT=Wcx[:, kc, c * 128:(c + 1) * 128],
                                                                              rhs=hTb[:, kc, :ntok], start=(kc == 0), stop=(kc == 7)),
                                 R=[Wcx, hTb], W=[psc])
                        for kc in range(8):
                            k.op('pe', lambda e, kc=kc, psx=psx, c=c: e.matmul(psx[:, :ntok], lhsT=Wcx[:, kc, 512 + c * 128:512 + (c + 1) * 128],
                                                                              rhs=hTb[:, kc, :ntok], start=(kc == 0), stop=(kc == 7)),
                                 R=[Wcx, hTb], W=[psx])
                        k.op('act', lambda e, cs=cs, psc=psc: e.copy(out=cs[:, :ntok], in_=psc[:, :ntok]), R=[psc], W=[cs])
                        k.op('dve', lambda e, cs=cs, psx=psx, c=c, ub=ub: e.tensor_tensor(out=ub[:, c, :ntok], in0=psx[:, :ntok], in1=cs[:, :ntok],
                                                                                       op=ALU.mult), R=[psx, cs], W=[(ub, c)])
                    o0 = uoff(t0)
                    ld(uTd[:, :, o0:o0 + ntok], ub[:, :, :ntok], stream='ust%d' % (bi % 2), R=[(ub, c) for c in range(4)], W=[('uTd', bi)])
                k.barrier()

            with ExitStack() as p3b:
                W3 = sb("W3_%d" % l, [128, 8, 3584], BF16, p3b)
                Wbr = sb("Wbr%d" % l, [128, 12, D], BF16, p3b)
                ld(W3[:], w3d[l], eng='pool', stream='wbig', W=[W3])
                ld(Wbr[:], wbrd[l], eng='pool', stream='wbig', W=[Wbr])
                hTb2 = [sb("hTm%d_%d" % (l, i), [128, 8, 512], BF16, p3b) for i in range(2)]
                Oa2 = [sb("Oam%d_%d" % (l, i), [128, 4, 512], BF16, p3b) for i in range(2)]
                Ob2 = [sb("Obm%d_%d" % (l, i), [128, 4, 512], BF16, p3b) for i in range(2)]
                u2 = [sb("um%d_%d" % (l, i), [128, 4, 514], F32, p3b) for i in range(2)]
                OcT = sb("OcT%d" % l, [128, 4, 512], BF16, p3b)
                mT2 = [sb("mT%d_%d" % (l, i), [128, 8, 512], BF16, p3b) for i in range(2)]
                cva = [sb("cva%d_%d" % (l, i), [128, 512], F32, p3b) for i in range(2)]
                cvb = [sb("cvb%d_%d" % (l, i), [128, 512], F32, p3b) for i in range(2)]
                sg2 = [sb("sg%d_%d" % (l, i), [128, 512], F32, p3b) for i in range(3)]
                acc2 = [sb("acc%d_%d" % (l, i), [128, 512], F32, p3b) for i in range(2)]
                tm2 = [sb("tm%d_%d" % (l, i), [128, 512], F32, p3b) for i in range(2)]
                cnt = [0, 0, 0]
                for bi, (t0, ntok, v) in enumerate(mblocks):
                    hTb, Oa, Ob, ub, mT = hTb2[bi % 2], Oa2[bi % 2], Ob2[bi % 2], u2[bi % 2], mT2[bi % 2]
                    o0 = uoff(t0)
                    ld(hTb[:, :, :ntok], hTd[:, :, t0:t0 + ntok], stream='hm%d' % (bi % 2), W=[hTb])
                    ld(Oa[:, :, :ntok], oaTd[:, :, t0:t0 + ntok], stream='hm%d' % (bi % 2), W=[Oa])
                    ld(Ob[:, :, :ntok], obTd[:, :, t0:t0 + ntok], stream='hm%d' % (bi % 2), W=[Ob])
                    ld(ub[:, :, :ntok + 2], uTd[:, :, o0 - 1:o0 + ntok + 1], stream='hm%d' % (bi % 2), W=[ub])
                    for c in range(4):
                        psb = ps_get('kp', [2, 3])
                        for kc in range(8):
                            k.op('pe', lambda e, kc=kc, psb=psb, c=c: e.matmul(psb[:, :ntok], lhsT=W3[:, kc, c * 128:(c + 1) * 128],
                                                                              rhs=hTb[:, kc, :ntok], start=(kc == 0), stop=(kc == 7)),
                                 R=[W3, hTb], W=[psb])
                        a = cva[cnt[0] % 2]
                        cnt[0] += 1
                        k.op('pool', lambda e, a=a, c=c: e.tensor_scalar(out=a[:, :ntok], in0=ub[:, c, 0:ntok], scalar1=cw[:, c, 0:1], scalar2=None,
                                                                        op0=ALU.mult), R=[ub, cw], W=[a])
                        a2 = cvb[cnt[0] % 2]
                        for tap in (1, 2):
                            k.op('pool', lambda e, a2=a2, c=c, tap=tap: e.tensor_scalar(out=a2[:, :ntok], in0=ub[:, c, tap:ntok + tap], scalar1=cw[:, c, tap:tap + 1],
                                                                                       scalar2=None, op0=ALU.mult), R=[ub, cw], W=[a2])
                            k.op('pool', lambda e, a=a, a2=a2: e.tensor_tensor(out=a[:, :ntok], in0=a[:, :ntok], in1=a2[:, :ntok], op=ALU.add), R=[a, a2], W=[a])
                        k.op('dve', lambda e, a=a, c=c, psb=psb: e.tensor_tensor(out=OcT[:, c, :ntok], in0=psb[:, :ntok], in1=a[:, :ntok], op=ALU.mult),
                             R=[psb, a], W=[(OcT, c)])
                    Obr = [Oa, Ob, OcT]
                    for m in range(8):
                        acc = acc2[m % 2]
                        for br in range(3):
                            psg = ps_get('s', [0, 1])
                            psr = ps_get('kr', [4, 5])
                            sg = sg2[cnt[1] % 3]
                            cnt[1] += 1
                            c0 = 512 + br * 1024 + m * 128
                            for kc in range(8):
                                k.op('pe', lambda e, kc=kc, psg=psg, c0=c0: e.matmul(psg[:, :ntok], lhsT=W3[:, kc, c0:c0 + 128],
                                                                                    rhs=hTb[:, kc, :ntok], start=(kc == 0), stop=(kc == 7)),
                                     R=[W3, hTb], W=[psg])
                            k.op('act', lambda e, sg=sg, psg=psg: e.activation(out=sg[:, :ntok], in_=psg[:, :ntok], func=AF.Sigmoid), R=[psg], W=[sg])
                            Rr = [Wbr] + ([Oa] if br == 0 else [Ob] if br == 1 else [(OcT, c) for c in range(4)])
                            for c in range(4):
                                k.op('pe', lambda e, c=c, psr=psr, br=br, m=m: e.matmul(psr[:, :ntok], lhsT=Wbr[:, br * 4 + c, m * 128:(m + 1) * 128],
                                                                                       rhs=Obr[br][:, c, :ntok], start=(c == 0), stop=(c == 3)),
                                     R=Rr, W=[psr])
                            if br == 0:
                                k.op('dve', lambda e, psr=psr, sg=sg, acc=acc: e.tensor_tensor(out=acc[:, :ntok], in0=psr[:, :ntok], in1=sg[:, :ntok],
                                                                                            op=ALU.mult), R=[psr, sg], W=[acc])
                            else:
                                tm = tm2[cnt[2] % 2]
                                cnt[2] += 1
                                k.op('dve', lambda e, psr=psr, sg=sg, tm=tm: e.tensor_tensor(out=tm[:, :ntok], in0=psr[:, :ntok], in1=sg[:, :ntok],
                                                                                          op=ALU.mult), R=[psr, sg], W=[tm])
                                if br == 1:
                                    k.op('pool', lambda e, tm=tm, acc=acc: e.tensor_tensor(out=acc[:, :ntok], in0=acc[:, :ntok], in1=tm[:, :ntok],
                                                                                        op=ALU.add), R=[acc, tm], W=[acc])
                                else:
                                    k.op('pool', lambda e, tm=tm, acc=acc, m=m, mT=mT: e.tensor_tensor(out=mT[:, m, :ntok], in0=acc[:, :ntok], in1=tm[:, :ntok],
                                                                                                    op=ALU.add), R=[acc, tm], W=[(mT, m)])
                    ld(mTd[:, :, t0:t0 + ntok], mT[:, :, :ntok], stream='mst%d' % (bi % 2), R=[(mT, m) for m in range(8)], W=[('mTd', bi)])
                k.barrier()
            if stop == 'p3b':
                break

            pmoe = ExitStack()
            aff = sb("aff%d" % l, [128, NTL, NE], F32, pmoe)
            with ExitStack() as p3c:
                Wout = sb("Wout%d" % l, [128, 8, D], BF16, p3c)
                ld(Wout[:], woutd[l], eng='pool', stream='wbig', W=[Wout])
                Wr = sb("Wr%d" % l, [128, 8, NE], F32, p3c)
                ld(Wr[:], wrd[l], W=[Wr])
                bct = {}
                for vv in range(2):
                    for vi, nm in ((0, 'gt1'), (1, 'sh2'), (2, 'A2')):
                        t_ = sb("bc_%s%d_%d" % (nm, vv, l), [128, D], F32, p3c)
                        ld(t_[:], bcd[vv, vi], W=[t_])
                        bct[(vv, nm)] = t_
                mT2 = [sb("mTo%d_%d" % (l, i), [128, 8, 512], BF16, p3c) for i in range(2)]
                xt2 = [sb("xo%d_%d" % (l, i), [128, D], F32, p3c) for i in range(2)]
                xw2 = [sb("xw%d_%d" % (l, i), [128, D], F32, p3c) for i in range(2)]
                h22 = [sb("h2%d_%d" % (l, i), [128, D], F32, p3c) for i in range(2)]
                h2b2 = [sb("h2b%d_%d" % (l, i), [128, D], BF16, p3c) for i in range(2)]
                junk = sb("junk3%d" % l, [128, D], F32, p3c)
                h2T = [sb("h2T%d_%d" % (l, i), [128, 8, 128], F32, p3c) for i in range(2)]
                ex2 = [sb("ex%d_%d" % (l, i), [128, NE], F32, p3c) for i in range(2)]
                sm = sb("sm%d" % l, [128, 8], F32, p3c)
                tg = 0
                for bi, (t0, ntok, v) in enumerate(mblocks):
                    mT = mT2[bi % 2]
                    ld(mT[:, :, :ntok], mTd[:, :, t0:t0 + ntok], stream='mo%d' % (bi % 2), W=[mT])
                    for tt in range(ntok // 128):
                        i2 = tg % 2
                        tg += 1
                        xt, xw, h2, h2b, hT_, ex = xt2[i2], xw2[i2], h22[i2], h2b2[i2], h2T[i2], ex2[i2]
                        r0 = t0 + tt * 128
                        ti = r0 // 128
                        ld(xt[:], xs[r0:r0 + 128, :], stream='xo%d' % i2, W=[xt])
                        for half in range(2):
                            hs = slice(half * 512, (half + 1) * 512)
                            po = ps_get('kp', [2, 3])
                            for m in range(8):
                                k.op('pe', lambda e, m=m, po=po, hs=hs, tt=tt: e.matmul(po[:, :], lhsT=mT[:, m, tt * 128:(tt + 1) * 128], rhs=Wout[:, m, hs],
                                                                                       start=(m == 0), stop=(m == 7)), R=[Wout, mT], W=[po])
                            k.op('dve', lambda e, po=po, hs=hs, xw=xw: e.tensor_tensor(out=xw[:, hs], in0=po[:, :], in1=bct[(v, 'gt1')][:, hs], op=ALU.mult),
                                 R=[po, bct[(v, 'gt1')]], W=[(xw, half)])
                            k.op('pool', lambda e, hs=hs, xw=xw, xt=xt: e.tensor_tensor(out=xw[:, hs], in0=xw[:, hs], in1=xt[:, hs], op=ALU.add),
                                 R=[(xw, half), xt], W=[(xw, half)])
                        ld(xs[r0:r0 + 128, :], xw[:], stream='xst%d' % i2, R=[(xw, 0), (xw, 1)], W=[('xs', ti)])
                        col = 2 + i2
                        k.op('pool', lambda e, col=col: e.memset(ss[:, col:col + 1], 0.0), W=[('ss', col)])
                        k.op('act', lambda e, xw=xw, col=col: e.activation(out=junk[:], in_=xw[:], func=AF.Square, accum_out=ss[:, col:col + 1]),
                             R=[(xw, 0), (xw, 1)], W=[junk, ('ss', col)])
                        k.op('act', lambda e, col=col: e.activation(out=rsd[:, col:col + 1], in_=ss[:, col:col + 1], func=AF.Sqrt, scale=1.0 / D, bias=EPS),
                             R=[('ss', col)], W=[('rsd', col)])
                        k.op('dve', lambda e, col=col: e.reciprocal(out=rsd[:, col:col + 1], in_=rsd[:, col:col + 1]), R=[('rsd', col)], W=[('rsd', col)])
                        k.op('dve', lambda e, xw=xw, h2=h2, col=col: e.scalar_tensor_tensor(out=h2[:], in0=xw[:], scalar=rsd[:, col:col + 1], in1=bct[(v, 'A2')][:],
                                                                                         op0=ALU.mult, op1=ALU.mult),
                             R=[(xw, 0), (xw, 1), ('rsd', col), bct[(v, 'A2')]], W=[h2])
                        k.op('pool', lambda e, h2=h2: e.tensor_tensor(out=h2[:], in0=h2[:], in1=bct[(v, 'sh2')][:], op=ALU.add), R=[h2, bct[(v, 'sh2')]], W=[h2])
                        k.op('act', lambda e, h2=h2, h2b=h2b: e.copy(out=h2b[:], in_=h2[:]), R=[h2], W=[h2b])
                        ld(h2d[r0:r0 + 128, :], h2b[:], stream='h2st%d' % i2, R=[h2b], W=[('h2d', ti)])
                        for half in range(2):
                            pt = ps_get('tr', [0, 1])
                            for q in range(4):
                                kc = half * 4 + q
                                k.op('pe', lambda e, pt=pt, q=q, kc=kc, h2=h2: e.transpose(pt[:, q * 128:(q + 1) * 128], h2[:, kc * 128:(kc + 1) * 128], ident[:]),
                                     R=[h2, ident], W=[pt])
                            eng = 'act' if half == 0 else 'dve'
                            src = pt[:, :]
                            dst = hT_[:].rearrange("p a b -> p (a b)")[:, half * 512:(half + 1) * 512]
                            if eng == 'act':
                                k.op('act', lambda e, dst=dst, src=src: e.copy(out=dst, in_=src), R=[pt], W=[(hT_, half)])
                            else:
                                k.op('dve', lambda e, dst=dst, src=src: e.tensor_copy(out=dst, in_=src), R=[pt], W=[(hT_, half)])
                        pl = ps_get('nr', [6])
                        for kc in range(8):
                            k.op('pe', lambda e, kc=kc, pl=pl, hT_=hT_: e.matmul(pl[:, 0:NE], lhsT=hT_[:, kc, :], rhs=Wr[:, kc, :], start=(kc == 0), stop=(kc == 7)),
                                 R=[(hT_, 0), (hT_, 1), Wr], W=[pl])
                        c2 = 4 * i2
                        k.op('dve', lambda e, pl=pl, c2=c2: e.tensor_reduce(out=sm[:, c2:c2 + 1], in_=pl[:, 0:NE], axis=AX.X, op=ALU.max), R=[pl], W=[('sm', i2)])
                        k.op('dve', lambda e, c2=c2: e.tensor_scalar(out=sm[:, c2 + 1:c2 + 2], in0=sm[:, c2:c2 + 1], scalar1=-1.0, scalar2=None, op0=ALU.mult),
                             R=[('sm', i2)], W=[('sm', i2)])
                        k.op('pool', lambda e, c2=c2: e.memset(sm[:, c2 + 2:c2 + 3], 0.0), R=[('sm', i2)], W=[('sm', i2)])
                        k.op('act', lambda e, pl=pl, ex=ex, c2=c2: e.activation(out=ex[:], in_=pl[:, 0:NE], func=AF.Exp, bias=sm[:, c2 + 1:c2 + 2],
                                                                              accum_out=sm[:, c2 + 2:c2 + 3]), R=[pl, ('sm', i2)], W=[ex, ('sm', i2)])
                        k.op('dve', lambda e, c2=c2: e.reciprocal(out=sm[:, c2 + 3:c2 + 4], in_=sm[:, c2 + 2:c2 + 3]), R=[('sm', i2)], W=[('sm', i2)])
                        k.op('dve', lambda e, ex=ex, ti=ti, c2=c2: e.tensor_scalar(out=aff[:, ti, :], in0=ex[:], scalar1=sm[:, c2 + 3:c2 + 4], scalar2=None, op0=ALU.mult),
                             R=[ex, ('sm', i2)], W=[(aff, ti)])
                        ld(affd[r0:r0 + 128, :], aff[:, ti, :], stream='afst%d' % i2, R=[(aff, ti)], W=[('affd', ti)])
                k.barrier()
            if stop == 'p3c':
                pmoe.close()
                break

            with pmoe:
                gt2 = [sb("gt2_%d_%d" % (l, vv), [128, D], F32, pmoe) for vv in range(2)]
                for vv in range(2):
                    ld(gt2[vv][:], bcd[vv, 3], W=[gt2[vv]])
                NSC = 9
                NJ = NE * NSC
                idx_all = sb("idx_all%d" % l, [128, NJ], I32, pmoe)
                gate_all = sb("gate_all%d" % l, [128, NJ], F32, pmoe)
                k.op('pool', lambda e: e.memset(idx_all[:], 1 << 20), W=[idx_all])
                k.op('pool', lambda e: e.memset(gate_all[:], 0.0), W=[gate_all])
                sets = [(0, 64, 1024, 0)] + ([] if last else [(64, 2, 32, 1)])
                nst = 8 if last else 9
                with ExitStack() as pth:
                    iota = sb("iota%d" % l, [128, 1024], F32, pth)
                    ld(iota[:], iotad.ap(), W=[iota])
                    comb = sb("comb%d" % l, [128, NTL, NE, 5], BF16, pth)
                    ld(comb[:], combd.ap(), eng='pool', W=[comb])
                    posm = sb("posm%d" % l, [128, NTL, NE], F32, pth)
                    lo = sb("lo%d" % l, [128, NE], F32, pth)
                    hi = sb("hi%d" % l, [128, NE], F32, pth)
                    mid = sb("mid%d" % l, [128, NE], F32, pth)
                    ge = sb("ge%d" % l, [128, NE], F32, pth)
                    g2 = sb("g2%d" % l, [128, NE], F32, pth)
                    cntp = sb("cntp%d" % l, [128, NE], F32, pth)
                    cmp_ = sb("cmp%d" % l, [128, 64, NE], F32, pth)
                    maskb = sb("maskb%d" % l, [128, 64, NE], BF16, pth)
                    inc = [sb("inc%d_%d" % (l, i), [128, 64, NE], F32, pth) for i in range(2)]
                    tot = sb("tot%d" % l, [128, 64, NE], F32, pth)
                    r1 = sb("r1_%d" % l, [128, NTL, NE], F32, pth)
                    r2 = sb("r2_%d" % l, [128, NTL, NE], F32, pth)
                    k.op('pool', lambda e: e.tensor_copy(out=comb[:, :, :, 2], in_=aff[:]), R=[aff], W=[comb])
                    k.op('dve', lambda e: e.tensor_tensor(out=r1[:], in0=aff[:], in1=comb[:, :, :, 2], op=ALU.subtract), R=[aff, comb], W=[r1])
                    k.op('pool', lambda e: e.tensor_copy(out=comb[:, :, :, 3], in_=r1[:]), R=[r1], W=[comb])
                    k.op('dve', lambda e: e.tensor_tensor(out=r2[:], in0=r1[:], in1=comb[:, :, :, 3], op=ALU.subtract), R=[r1, comb], W=[r2])
                    k.op('pool', lambda e: e.tensor_copy(out=comb[:, :, :, 4], in_=r2[:]), R=[r2], W=[comb])
                    for (ti0, T, cap, vv) in sets:
                        affs = aff[:, ti0:ti0 + T, :]
                        k.op('dve', lambda e: e.memset(lo[:], 0.0), W=[lo])
                        k.op('dve', lambda e: e.memset(hi[:], 1.0), W=[hi])
                        for it in range(32):
                            k.op('dve', lambda e: e.tensor_tensor(out=mid[:], in0=lo[:], in1=hi[:], op=ALU.add), R=[lo, hi], W=[mid])
                            k.op('dve', lambda e: e.tensor_scalar(out=mid[:], in0=mid[:], scalar1=0.5, scalar2=None, op0=ALU.mult), R=[mid], W=[mid])
                            k.op('dve', lambda e, T=T, affs=affs: e.tensor_tensor(out=cmp_[:, :T, :], in0=affs, in1=mid[:].unsqueeze(1).to_broadcast([128, T, NE]),
                                                                                 op=ALU.is_ge), R=[aff, mid], W=[cmp_])
                            k.op('dve', lambda e, T=T: e.tensor_reduce(out=cntp[:], in_=cmp_[:, :T, :].rearrange("p t e -> p e t"), axis=AX.X, op=ALU.add),
                                 R=[cmp_], W=[cntp])
                            pc = ps_get('nr', [6])
                            k.op('pe', lambda e, pc=pc: e.matmul(pc[:, 0:NE], lhsT=ones32[:], rhs=cntp[:], start=True, stop=True), R=[ones32, cntp], W=[pc])
                            k.op('dve', lambda e, pc=pc, cap=cap: e.tensor_scalar(out=ge[:], in0=pc[:, 0:NE], scalar1=float(cap) - 0.5, scalar2=None, op0=ALU.is_ge),
                                 R=[pc], W=[ge])
                            k.op('dve', lambda e: e.tensor_tensor(out=g2[:], in0=ge[:], in1=mid[:], op=ALU.mult), R=[ge, mid], W=[g2])
                            k.op('dve', lambda e: e.tensor_tensor(out=lo[:], in0=lo[:], in1=g2[:], op=ALU.max), R=[lo, g2], W=[lo])
                            k.op('dve', lambda e: e.scalar_tensor_tensor(out=g2[:], in0=ge[:], scalar=2.0, in1=mid[:], op0=ALU.mult, op1=ALU.add),
                                 R=[ge, mid, g2], W=[g2])
                            k.op('dve', lambda e: e.tensor_tensor(out=hi[:], in0=hi[:], in1=g2[:], op=ALU.min), R=[hi, g2], W=[hi])
                        k.op('dve', lambda e, T=T, affs=affs: e.tensor_tensor(out=cmp_[:, :T, :], in0=affs, in1=lo[:].unsqueeze(1).to_broadcast([128, T, NE]),
                                                                             op=ALU.is_ge), R=[aff, lo], W=[cmp_])
                        k.op('pool', lambda e, T=T: e.tensor_copy(out=maskb[:, :T, :], in_=cmp_[:, :T, :]), R=[cmp_], W=[maskb])
                        ncol = T * NE
                        mb = maskb[:].rearrange("p t e -> p (t e)")
                        totf = tot[:].rearrange("p t e -> p (t e)")
                        pp = [PS[2], PS[3]]
                        pq = [PS[4], PS[5]]
                        nhf = (ncol + 511) // 512
                        for hf in range(nhf):
                            n_ = min(512, ncol - hf * 512)
                            k.op('pe', lambda e, hf=hf, n_=n_: e.matmul(pp[hf][:, :n_], lhsT=triL[:], rhs=mb[:, hf * 512:hf * 512 + n_], start=True, stop=True),
                                 R=[triL, maskb], W=[pp[hf]])
                            k.op('pe', lambda e, hf=hf, n_=n_: e.matmul(pq[hf][:, :n_], lhsT=onesb[:], rhs=mb[:, hf * 512:hf * 512 + n_], start=True, stop=True),
                                 R=[onesb, maskb], W=[pq[hf]])
                            k.op('act', lambda e, hf=hf, n_=n_: e.copy(out=totf[:, hf * 512:hf * 512 + n_], in_=pq[hf][:, :n_]), R=[pq[hf]], W=[tot])
                        k.op('pool', lambda e, T=T: e.tensor_copy(out=inc[0][:, :T, :], in_=tot[:, :T, :]), R=[tot], W=[inc[0]])
                        cur = 0
                        s_ = 1
                        while s_ < T:
                            a_, b_ = inc[cur], inc[1 - cur]
                            k.op('dve', lambda e, a_=a_, b_=b_, s_=s_, T=T: e.tensor_tensor(out=b_[:, s_:T, :], in0=a_[:, s_:T, :], in1=a_[:, 0:T - s_, :], op=ALU.add),
                                 R=[a_], W=[b_])
                            k.op('pool', lambda e, a_=a_, b_=b_, s_=s_: e.tensor_copy(out=b_[:, 0:s_, :], in_=a_[:, 0:s_, :]), R=[a_], W=[b_])
                            cur = 1 - cur
                            s_ *= 2
                        incf = inc[cur]
                        oth = inc[1 - cur]
                        othf = oth[:].rearrange("p t e -> p (t e)")
                        k.op('dve', lambda e, T=T: e.tensor_tensor(out=oth[:, :T, :], in0=incf[:, :T, :], in1=tot[:, :T, :], op=ALU.subtract), R=[incf, tot], W=[oth])
                        for hf in range(nhf):
                            n_ = min(512, ncol - hf * 512)
                            k.op('dve', lambda e, hf=hf, n_=n_: e.tensor_tensor(out=othf[:, hf * 512:hf * 512 + n_], in0=othf[:, hf * 512:hf * 512 + n_],
                                                                              in1=pp[hf][:, :n_], op=ALU.add), R=[oth, pp[hf]], W=[oth])
                        k.op('dve', lambda e, T=T, ti0=ti0: e.scalar_tensor_tensor(out=posm[:, ti0:ti0 + T, :], in0=oth[:, :T, :], scalar=1.0, in1=cmp_[:, :T, :],
                                                                                  op0=ALU.add, op1=ALU.mult), R=[oth, cmp_], W=[posm])
                        k.op('dve', lambda e, T=T, ti0=ti0: e.tensor_scalar(out=posm[:, ti0:ti0 + T, :], in0=posm[:, ti0:ti0 + T, :], scalar1=-1.0, scalar2=None,
                                                                           op0=ALU.add), R=[posm], W=[posm])
                    k.barrier()
                    if debug and stop == 'p4a':
                        dpos = nc.dram_tensor("dbgposm", [128, NTL, NE], F32, kind="ExternalOutput")
                        ld(dpos.ap(), posm[:], R=[posm], W=['dbgo'])
                        k.barrier()
                        break

                    Sb_ = [sb("Sone%d_%d" % (l, i), [128, 1024], BF16, pth) for i in range(3)]
                    rows5 = sb("rows5_%d" % l, [8, 1056], F32, pth)
                    t5 = sb("t5_%d" % l, [128, 48], F32, pth)
                    idxf = sb("idxf%d" % l, [128, NSC], F32, pth)
                    g1 = sb("g1_%d" % l, [128, NSC], F32, pth)
                    rr = 0
                    for e_ in range(NE):
                        for (ti0, T, cap, vv) in sets:
                            off = 0 if vv == 0 else 1024
                            nh = (cap + 511) // 512
                            pi = [PS[0], PS[1]]
                            for i_ in range(T):
                                S_ = Sb_[rr % 3]
                                rr += 1
                                ti = ti0 + i_
                                k.op('dve', lambda e, S_=S_, ti=ti, e_=e_, cap=cap: e.tensor_scalar(out=S_[:, :cap], in0=iota[:, :cap], scalar1=posm[:, ti, e_:e_ + 1],
                                                                                                   scalar2=None, op0=ALU.is_equal), R=[iota, posm], W=[S_])
                                for hf in range(nh):
                                    n_ = min(512, cap - hf * 512)
                                    k.op('pe', lambda e, S_=S_, ti=ti, hf=hf, n_=n_, i_=i_, T=T, e_=e_: e.matmul(pi[hf][0:5, :n_], lhsT=comb[:, ti, e_, :],
                                                                                                              rhs=S_[:, hf * 512:hf * 512 + n_],
                                                                                                              start=(i_ == 0), stop=(i_ == T - 1)), R=[comb, S_], W=[pi[hf]])
                            for hf in range(nh):
                                n_ = min(512, cap - hf * 512)
                                k.op('act', lambda e, hf=hf, n_=n_, off=off: e.copy(out=rows5[0:5, off + hf * 512:off + hf * 512 + n_], in_=pi[hf][0:5, :n_]),
                                     R=[pi[hf]], W=[rows5])
                        ptx = ps_get('nr', [6])
                        for s in range(nst):
                            prow = 128 if s < 8 else 32
                            k.op('pe', lambda e, s=s, prow=prow: e.transpose(ptx[0:prow, 5 * s:5 * s + 5], rows5[0:5, s * 128:s * 128 + prow], ident[0:5, 0:5]),
                                 R=[rows5, ident], W=[ptx])
                        k.op('act', lambda e: e.copy(out=t5[:, 0:40], in_=ptx[:, 0:40]), R=[ptx], W=[t5])
                        if nst == 9:
                            k.op('act', lambda e: e.copy(out=t5[0:32, 40:45], in_=ptx[0:32, 40:45]), R=[ptx], W=[t5])
                        for (c0, c1, prow) in ((0, 8, 128),) + (((8, 9, 32),) if nst == 9 else ()):
                            n5 = slice(5 * c0, 5 * c1, 5)
                            k.op('dve', lambda e, c0=c0, c1=c1, prow=prow: e.scalar_tensor_tensor(
                                out=idxf[0:prow, c0:c1], in0=t5[0:prow, 5 * c0:5 * c1:5], scalar=128.0, in1=t5[0:prow, 5 * c0 + 1:5 * c1:5], op0=ALU.mult, op1=ALU.add),
                                R=[t5], W=[idxf])
                            k.op('dve', lambda e, c0=c0, c1=c1, prow=prow, e_=e_: e.tensor_copy(out=idx_all[0:prow, e_ * NSC + c0:e_ * NSC + c1], in_=idxf[0:prow, c0:c1]),
                                 R=[idxf], W=[idx_all])
                            k.op('dve', lambda e, c0=c0, c1=c1, prow=prow: e.tensor_tensor(out=g1[0:prow, c0:c1], in0=t5[0:prow, 5 * c0 + 2:5 * c1:5],
                                                                                         in1=t5[0:prow, 5 * c0 + 3:5 * c1:5], op=ALU.add), R=[t5], W=[g1])
                            k.op('dve', lambda e, c0=c0, c1=c1, prow=prow, e_=e_: e.tensor_tensor(out=gate_all[0:prow, e_ * NSC + c0:e_ * NSC + c1], in0=g1[0:prow, c0:c1],
                                                                                               in1=t5[0:prow, 5 * c0 + 4:5 * c1:5], op=ALU.add), R=[g1, t5], W=[gate_all])
                    k.barrier()
                if debug and stop == 'p4a':
                    break
                if debug and stop == 'p4b':
                    di = nc.dram_tensor("dbgidx", [128, NJ], I32, kind="ExternalOutput")
                    dg = nc.dram_tensor("dbggate", [128, NJ], F32, kind="ExternalOutput")
                    ld(di.ap(), idx_all[:], R=[idx_all], W=['dbgo'])
                    ld(dg.ap(), gate_all[:], R=[gate_all], W=['dbgo'])
                    k.barrier()
                    break

                with ExitStack() as pgl:
                    xgt = sb("xgt%d" % l, [128, D], BF16, pgl)
                    k.op('pool', lambda e: e.memset(xgt[:], 0.0), W=[xgt])
                    k.barrier()
                    gather_loop(idx_all, h2d, xgd, xgt, NJ, "g%d" % l)
                    k.op('pool', lambda e: e.memset(xgt[:, 0:2], 0.0), W=[xgt])
                    k.barrier()

                with ExitStack() as pex:
                    Wg2 = [sb("Wg%d_%d" % (l, i), [128, 8, D], BF16, pex) for i in range(2)]
                    Wu2 = [sb("Wu%d_%d" % (l, i), [128, 8, D], BF16, pex) for i in range(2)]
                    Wd2 = [sb("Wd%d_%d" % (l, i), [128, 8, D], BF16, pex) for i in range(2)]
                    xg = sb("xg%d" % l, [128, NSC, D], BF16, pex)
                    xsT = sb("xsT%d" % l, [128, 8, 1056], BF16, pex)
                    hd = sb("hd%d" % l, [128, 8, 1056], BF16, pex)
                    sa2 = [sb("sa%d_%d" % (l, i), [128, 512], F32, pex) for i in range(2)]
                    yo2 = [sb("yo%d_%d" % (l, i), [128, D], F32, pex) for i in range(2)]
                    cnt_ = [0]

                    def load_expert(e_, slot):
                        ld(Wg2[slot][:], wegd[l, e_], eng='pool', W=[Wg2[slot]])
                        ld(Wu2[slot][:], weud[l, e_], eng='pool', W=[Wu2[slot]])
                        ld(Wd2[slot][:], wedd[l, e_], eng='pool', W=[Wd2[slot]])

                    load_expert(0, 0)
                    groups = [(0, 512), (512, 512)] + ([(1024, 32)] if nst == 9 else [])
                    for e_ in range(NE):
                        slot = e_ % 2
                        if e_ + 1 < NE:
                            load_expert(e_ + 1, 1 - slot)
                        Wg, Wu, Wd = Wg2[slot], Wu2[slot], Wd2[slot]
                        j0 = e_ * NSC
                        ld(xg[:, 0:nst, :], xgd[j0 * 128:(j0 + nst) * 128, :].rearrange("(s p) d -> p s d", p=128), W=[xg])
                        for kc in range(8):
                            for s in range(8):
                                k.op('pe', lambda e, s=s, kc=kc: e.transpose(PSB[:, s * 128:(s + 1) * 128], xg[:, s, kc * 128:(kc + 1) * 128], identb[:]),
                                     R=[xg, identb], W=[PSB])
                            if kc % 2 == 0:
                                k.op('act', lambda e, kc=kc: e.copy(out=xsT[:, kc, 0:1024], in_=PSB[:, 0:1024]), R=[PSB], W=[(xsT, kc)])
                            else:
                                k.op('dve', lambda e, kc=kc: e.tensor_copy(out=xsT[:, kc, 0:1024], in_=PSB[:, 0:1024]), R=[PSB], W=[(xsT, kc)])
                        if nst == 9:
                            for kc in range(8):
                                k.op('pe', lambda e, kc=kc: e.transpose(PSB[:, kc * 32:(kc + 1) * 32], xg[0:32, 8, kc * 128:(kc + 1) * 128], identb[0:32, 0:32]),
                                     R=[xg, identb], W=[PSB])
                            for kc in range(8):
                                k.op('act', lambda e, kc=kc: e.copy(out=xsT[:, kc, 1024:1056], in_=PSB[:, kc * 32:(kc + 1) * 32]), R=[PSB], W=[(xsT, kc)])
                        xr = [(xsT, kc) for kc in range(8)]
                        for fc in range(8):
                            for (c0, n_) in groups:
                                pa = ps_get('s', [0, 1])
                                pu = ps_get('kr', [4, 5])
                                sa = sa2[cnt_[0] % 2]
                                cnt_[0] += 1
                                for kc in range(8):
                                    k.op('pe', lambda e, kc=kc, fc=fc, c0=c0, n_=n_, pa=pa: e.matmul(pa[:, :n_], lhsT=Wg[:, kc, fc * 128:(fc + 1) * 128],
                                                                                                    rhs=xsT[:, kc, c0:c0 + n_], start=(kc == 0), stop=(kc == 7)),
                                         R=[Wg] + xr, W=[pa])
                                for kc in range(8):
                                    k.op('pe', lambda e, kc=kc, fc=fc, c0=c0, n_=n_, pu=pu: e.matmul(pu[:, :n_], lhsT=Wu[:, kc, fc * 128:(fc + 1) * 128],
                                                                                                    rhs=xsT[:, kc, c0:c0 + n_], start=(kc == 0), stop=(kc == 7)),
                                         R=[Wu] + xr, W=[pu])
                                k.op('act', lambda e, sa=sa, pa=pa, n_=n_: e.activation(out=sa[:, :n_], in_=pa[:, :n_], func=AF.Silu), R=[pa], W=[sa])
                                k.op('dve', lambda e, sa=sa, pu=pu, n_=n_, fc=fc, c0=c0: e.tensor_tensor(out=hd[:, fc, c0:c0 + n_], in0=pu[:, :n_], in1=sa[:, :n_],
                                                                                                      op=ALU.mult), R=[pu, sa], W=[(hd, fc)])
                        hr = [(hd, fc) for fc in range(8)]
                        for s in range(nst):
                            prow = 128 if s < 8 else 32
                            vv = 0 if s < 8 else 1
                            yo = yo2[s % 2]
                            for half in range(2):
                                hs = slice(half * 512, (half + 1) * 512)
                                py = ps_get('kp', [2, 3]) if half == 0 else ps_get('nr', [6])
                                for fc in range(8):
                                    k.op('pe', lambda e, fc=fc, s=s, py=py, hs=hs, prow=prow: e.matmul(py[0:prow, :], lhsT=hd[:, fc, s * 128:s * 128 + prow], rhs=Wd[:, fc, hs],
                                                                                                      start=(fc == 0), stop=(fc == 7)), R=[Wd] + hr, W=[py])
                                k.op('dve', lambda e, py=py, yo=yo, hs=hs, s=s, vv=vv, prow=prow, j0=j0: e.scalar_tensor_tensor(
                                    out=yo[0:prow, hs], in0=py[0:prow, :], scalar=gate_all[0:prow, j0 + s:j0 + s + 1], in1=gt2[vv][0:prow, hs], op0=ALU.mult, op1=ALU.mult),
                                    R=[py, gate_all, gt2[vv]], W=[(yo, half)])
                            ld(Yd[(j0 + s) * 128:(j0 + s) * 128 + prow, :], yo[0:prow, :], R=[(yo, 0), (yo, 1)], W=[('Yd', j0 + s)])
                    k.barrier()

                with ExitStack() as psl:
                    yt = sb("yt%d" % l, [128, D], F32, psl)
                    k.op('pool', lambda e: e.memset(yt[:], 0.0), W=[yt])
                    k.barrier()
                    scatter_loop(idx_all, Yd, xs, yt, NJ, "s%d" % l)
                    k.op('pool', lambda e: e.memset(yt[:, 0:2], 0.0), W=[yt])
                    k.barrier()
        k.barrier()
    return nc


def _colmajor(w):
    return np.ascontiguousarray(w.reshape(8, 128, -1).transpose(1, 0, 2))


def _consts():
    ident = np.eye(128, dtype=np.float32)
    blk = (np.arange(128)[:, None] // 64 == np.arange(128)[None, :] // 64).astype(np.float32)
    p = np.arange(128)
    triL = (p[:, None] < p[None, :]).astype(np.float32)
    maskP = (p[:, None] >= p[None, :]).astype(np.float32)
    maskN = (p[:, None] <= p[None, :]).astype(np.float32)
    cm = np.ascontiguousarray(np.stack([ident, blk, triL, maskP, maskN], axis=1))
    iota = np.ascontiguousarray(np.broadcast_to(np.arange(1024, dtype=np.float32), (128, 1024)))
    tidc = np.zeros((128, NTL, NE, 5), np.float32)
    tidc[:, :, :, 0] = np.arange(NTL)[None, :, None]
    tidc[:, :, :, 1] = np.arange(128)[:, None, None]
    t = np.arange(NL)
    row = (t // 64).astype(np.float32)
    colp = (t % 64).astype(np.float32)
    inv = np.power(np.float32(10000.0), -(np.arange(16, dtype=np.float32) / np.float32(16))).astype(np.float32)
    ang = np.concatenate([row[:, None] * inv[None, :], colp[:, None] * inv[None, :]], axis=-1).astype(np.float32)
    cos = np.cos(ang).astype(np.float32)
    sin = np.sin(ang).astype(np.float32)
    C = np.ones((128, NT), np.float32)
    S = np.zeros((128, NT), np.float32)
    for r in range(128):
        i = r % 64
        f = i % 32
        C[r, :NL] = cos[:, f]
        S[r, :NL] = sin[:, f] * (-1.0 if i < 32 else 1.0)
    return cm, iota, tidc, C, S


def _swap_halves(cols):
    cols = np.asarray(cols)
    return (cols // 64) * 64 + ((cols % 64) + 32) % 64


def prep_inputs(inp):
    L = inp['w_ada'].shape[0]
    cm, iota, tidc, C, S = _consts()
    f = lambda a: np.ascontiguousarray(a, dtype=np.float32)
    w_in = inp['w_in']
    kA, vA, kB, vB = np.arange(0, 128), np.arange(128, 256), np.arange(256, 384), np.arange(384, 512)
    qa0, qb0, cv0, gt0 = 512, 1024, 1536, 3072
    qorder = np.concatenate([np.concatenate([np.arange(j * 64, j * 64 + 64), np.arange((4 + j) * 64, (4 + j) * 64 + 64)]) for j in range(4)])
    c1 = np.concatenate([kA, kB, _swap_halves(kA), _swap_halves(kB), vA, vB])
    c2 = np.concatenate([qa0 + qorder, qa0 + _swap_halves(qorder), qb0 + qorder, qb0 + _swap_halves(qorder)])
    ccx = np.concatenate([np.arange(cv0 + 512, cv0 + 1024), np.arange(cv0 + 1024, cv0 + 1536)])
    c3 = np.concatenate([np.arange(cv0, cv0 + 512), np.arange(gt0, gt0 + 3072)])
    shared = {}
    shared['w_ada'] = f(np.stack([_colmajor(inp['w_ada'][l]) for l in range(L)]))
    bcol = np.stack([inp['b_ada'][l].reshape(48, 128).T for l in range(L)])
    shared['b_col2'] = f(np.repeat(bcol, 2, axis=2))
    sel = [slice(2048, 3072), slice(3072, 4096), slice(4096, 5120), slice(5120, 6144)]
    shared['b_bc'] = f(np.stack([np.stack([np.broadcast_to(inp['b_ada'][l][s], (128, 1024)) for s in sel], axis=1) for l in range(L)]))
    gc = np.zeros((L, 128, 2, 8, 2), np.float32)
    for l in range(L):
        gc[l, :, 0, :, :] = inp['g_mix'][l].reshape(8, 128).T[:, :, None]
        gc[l, :, 1, :, :] = inp['g_ffn'][l].reshape(8, 128).T[:, :, None]
    shared['gcol2'] = gc
    shared['gffn_bc'] = f(np.stack([np.broadcast_to(inp['g_ffn'][l], (128, 1024)) for l in range(L)]))
    shared['w1'] = f(np.stack([_colmajor(w_in[l][:, c1]) for l in range(L)]))
    shared['w2'] = f(np.stack([_colmajor(w_in[l][:, c2]) for l in range(L)]))
    shared['wcx'] = f(np.stack([_colmajor(w_in[l][:, ccx]) for l in range(L)]))
    shared['w3'] = f(np.stack([_colmajor(w_in[l][:, c3]) for l in range(L)]))
    wbr = np.zeros((L, 128, 12, 1024), np.float32)
    for l in range(L):
        for br in range(3):
            wb = inp['w_branch'][l, br]
            if br < 2:
                wb = wb[qorder]
            wbr[l, :, br * 4:(br + 1) * 4, :] = wb.reshape(4, 128, 1024).transpose(1, 0, 2)
    shared['wbr'] = wbr
    shared['wout'] = f(np.stack([_colmajor(inp['w_out'][l]) for l in range(L)]))
    hgv = np.zeros((L, 128, 8), np.float32)
    sw = (np.arange(64) + 32) % 64
    for l in range(L):
        for i, g in enumerate((inp['qg_a'][l], inp['kg_a'][l], inp['qg_b'][l], inp['kg_b'][l])):
            hgv[l, :, 2 * i] = np.tile(g, 2)
            hgv[l, :, 2 * i + 1] = np.tile(g[sw], 2)
    shared['hg'] = hgv
    shared['sink'] = f(np.stack([np.broadcast_to(inp['sink_b'][l], (128, 8)) for l in range(L)]))
    shared['convw'] = f(np.stack([inp['conv_w'][l].reshape(3, 4, 128).transpose(2, 1, 0) for l in range(L)]))
    shared['wr'] = f(np.stack([_colmajor(inp['w_router'][l]) for l in range(L)]))
    for nm, key in (('weg', 'w_e_gate'), ('weu', 'w_e_up'), ('wed', 'w_e_down')):
        w = inp[key]
        shared[nm] = f(w.reshape(L, NE, 8, 128, 1024).transpose(0, 1, 3, 2, 4))
    shared['ropeC'] = C
    shared['ropeS'] = S
    shared['cmisc'] = cm
    shared['iota'] = iota
    shared['comb'] = tidc
    maps = []
    B = inp['x'].shape[0]
    for b in range(B):
        m = dict(shared)
        m['xin'] = f(np.concatenate([inp['x'][b], inp['ctx'][b]], axis=0))
        cv = np.zeros((128, 8, 2), np.float32)
        cv[:, :, 0] = inp['c'][b].reshape(8, 128).T
        cv[:, :, 1] = inp['c_ctx'].reshape(8, 128).T
        m['cvec'] = cv
        maps.append(m)
    return maps


_NC_CACHE = {}
_PER_LAYER = ('w_ada', 'b_col2', 'b_bc', 'gcol2', 'gffn_bc', 'w1', 'w2', 'wcx', 'w3', 'wbr', 'wout', 'hg', 'sink', 'convw', 'wr',
              'weg', 'weu', 'wed')
FUSED = False


def _layer_slice(m, l):
    out = {}
    for k_, v in m.items():
        out[k_] = np.ascontiguousarray(v[l:l + 1]) if k_ in _PER_LAYER else v
    return out


def kernel(**inputs):
    inp = {k_: np.asarray(v) for k_, v in inputs.items()}
    maps = prep_inputs(inp)
    L = inp['w_ada'].shape[0]
    cores = list(range(len(maps)))
    if FUSED:
        if 'nc' not in _NC_CACHE:
            _NC_CACHE['nc'] = build(nlayers=L)
        res = run_bass_kernel_spmd(_NC_CACHE['nc'], maps, core_ids=cores)
        xs = [np.asarray(r["xs"]) for r in res.results]
    else:
        xs = [m['xin'] for m in maps]
        for l in range(L):
            key = ('layer', l == L - 1)
            if key not in _NC_CACHE:
                _NC_CACHE[key] = build(nlayers=1, force_ctx=(l != L - 1))
            lm = []
            for m, x_ in zip(maps, xs):
                d = _layer_slice(m, l)
                d['xin'] = np.ascontiguousarray(x_, dtype=np.float32)
                lm.append(d)
            res = run_bass_kernel_spmd(_NC_CACHE[key], lm, core_ids=cores)
            xs = [np.asarray(r["xs"]) for r in res.results]
    out = np.stack([x_[:NL] for x_ in xs], axis=0)
    return out.astype(np.float32)
```

```python
import numpy as np
from contextlib import ExitStack
import concourse.bass as bass
import concourse.mybir as mybir
from concourse.bass_utils import run_bass_kernel_spmd

F32 = mybir.dt.float32
BF16 = mybir.dt.bfloat16
I32 = mybir.dt.int32
AF = mybir.ActivationFunctionType
ALU = mybir.AluOpType
AX = mybir.AxisListType

NL, NCX, NT, D = 8192, 256, 8448, 1024
NTL = NT // 128
EPS = 1e-6
NE = 16
N_CORES = 4


class K:
    def __init__(self, nc, stack):
        self.nc = nc
        self.stack = stack
        self.eng = {'pe': nc.tensor, 'act': nc.scalar, 'dve': nc.vector, 'pool': nc.gpsimd, 'sp': nc.sync}
        self.sem = {n: stack.enter_context(nc.semaphore("s_" + n)) for n in self.eng}
        self.cnt = {n: 0 for n in self.eng}
        self.seen = {n: {} for n in self.eng}
        self.dsem = {}
        self.dval = {}
        self.lastw = {}
        self.readers = {}
        self.nwaits = 0
        self.nins = 0
        self.drr = {}

    def _key(self, t):
        if isinstance(t, (str, int)):
            return t
        if isinstance(t, tuple):
            return tuple(self._key(x) for x in t)
        return ('id', id(t))

    def _deps(self, R, W):
        deps = []
        for t in list(R) + list(W):
            d = self.lastw.get(self._key(t))
            if d is not None:
                deps.append(d)
        for t in W:
            deps.extend(self.readers.get(self._key(t), {}).items())
        return deps

    def _wait(self, e, deps):
        h = self.eng[e]
        seen = self.seen[e]
        need = {}
        for key, val in deps:
            if key == ('E', 'pe') and e == 'pe':
                continue
            if seen.get(key, 0) >= val:
                continue
            if need.get(key, 0) < val:
                need[key] = val
        for key, val in need.items():
            s = self.sem[key[1]] if key[0] == 'E' else self.dsem[key[1]]
            h.wait_ge(s, val)
            seen[key] = val
            self.nwaits += 1

    def _record(self, tok, R, W):
        key, val = tok
        for t in R:
            r = self.readers.setdefault(self._key(t), {})
            if r.get(key, 0) < val:
                r[key] = val
        for t in W:
            self.lastw[self._key(t)] = tok
            self.readers[self._key(t)] = {}

    def op(self, e, fn, R=(), W=()):
        self._wait(e, self._deps(R, W))
        ins = fn(self.eng[e])
        ins.then_inc(self.sem[e], 1)
        self.cnt[e] += 1
        self.nins += 1
        self._record((('E', e), self.cnt[e]), R, W)
        return ins

    NDS = 8

    def dma(self, e, stream, fn, R=(), W=()):
        i = self.drr.get(e, 0)
        self.drr[e] = i + 1
        stream = "%s%d" % (e, i % self.NDS)
        if stream not in self.dsem:
            self.dsem[stream] = self.stack.enter_context(self.nc.semaphore("d_" + stream))
            self.dval[stream] = 0
        deps = self._deps(R, W)
        if self.dval[stream] > 0:
            deps.append((('D', stream), self.dval[stream]))
        self._wait(e, deps)
        ins = fn(self.eng[e])
        ins.then_inc(self.dsem[stream], 16)
        self.dval[stream] += 16
        self.nins += 1
        self._record((('D', stream), self.dval[stream]), R, W)
        return ins

    def barrier(self, engines=None):
        deps = [(('E', n), c) for n, c in self.cnt.items() if c > 0]
        deps += [(('D', s), v) for s, v in self.dval.items() if v > 0]
        for e in (engines or self.eng):
            self._wait(e, deps)


def build(nlayers=2, debug=False, stop=None, cut=99, force_ctx=False):
    nc = bass.Bass("TRN2", target_bir_lowering=False)
    st = ExitStack()
    with st:
        k = K(nc, st)

        def din(name, shape, dt=F32):
            return nc.dram_tensor(name, list(shape), dt, kind="ExternalInput")

        def dsc(name, shape, dt, out=False):
            return nc.dram_tensor(name, list(shape), dt, kind="ExternalOutput" if (out or debug) else "Internal")

        L = nlayers
        xin = din("xin", [NT, D])
        cvec = din("cvec", [128, 8, 2])
        w_ada = din("w_ada", [L, 128, 8, 6144])
        b_col2 = din("b_col2", [L, 128, 96])
        b_bc = din("b_bc", [L, 128, 4, D])
        gcol2 = din("gcol2", [L, 128, 2, 8, 2])
        gffn_bc = din("gffn_bc", [L, 128, D])
        w1d = din("w1", [L, 128, 8, 768])
        w2d = din("w2", [L, 128, 8, 2048])
        wcxd = din("wcx", [L, 128, 8, 1024])
        w3d = din("w3", [L, 128, 8, 3584])
        wbrd = din("wbr", [L, 128, 12, D])
        woutd = din("wout", [L, 128, 8, D])
        hgd = din("hg", [L, 128, 8])
        sinkd = din("sink", [L, 128, 8])
        convwd = din("convw", [L, 128, 4, 3])
        wrd = din("wr", [L, 128, 8, NE])
        wegd = din("weg", [L, NE, 128, 8, D])
        weud = din("weu", [L, NE, 128, 8, D])
        wedd = din("wed", [L, NE, 128, 8, D])
        ropeCd = din("ropeC", [128, NT])
        ropeSd = din("ropeS", [128, NT])
        cmisc = din("cmisc", [128, 5, 128])
        iotad = din("iota", [128, 1024])
        combd = din("comb", [128, NTL, NE, 5])
        xs = dsc("xs", [NT, D], F32, out=True)
        hTd = dsc("hTd", [128, 8, NT], BF16)
        uTd = dsc("uTd", [128, 4, NT + 4], F32)
        oaTd = dsc("oaTd", [128, 4, NT], BF16)
        obTd = dsc("obTd", [128, 4, NT], BF16)
        mTd = dsc("mTd", [128, 8, NT], BF16)
        h2d = dsc("h2d", [NT, D], BF16)
        affd = dsc("affd", [NT, NE], F32)
        bcd = dsc("bcd", [2, 4, 128, D], F32)
        xgd = dsc("xgd", [NE * 9 * 128, D], BF16)
        Yd = dsc("Yd", [NE * 9 * 128, D], F32)

        def sb(name, shape, dt, stack=None):
            return (stack or st).enter_context(nc.sbuf_tensor(name, list(shape), dt))

        PS = [st.enter_context(nc.psum_tensor("ps%d" % i, [128, 512], F32)) for i in range(7)]
        PSB = st.enter_context(nc.psum_tensor("psb", [128, 1024], BF16))
        psrr = {}

        def ps_get(pool, banks):
            i = psrr.get(pool, 0)
            psrr[pool] = i + 1
            return PS[banks[i % len(banks)]]

        ident = sb("ident", [128, 128], F32)
        blockones = sb("blockones", [128, 128], F32)
        ones32 = sb("ones32", [128, 128], F32)
        onesb = sb("onesb", [128, 128], BF16)
        triL = sb("triL", [128, 128], BF16)
        maskP = sb("maskP", [128, 128], BF16)
        maskN = sb("maskN", [128, 128], BF16)
        identb = sb("identb", [128, 128], BF16)
        sc = sb("sc", [128, 8, 2], F32)
        lbc = sb("lbc", [128, 8, 2, 128], F32)
        modcol = sb("modcol", [128, 48, 2], F32)
        A1c = sb("A1c", [128, 8, 2], F32)
        hg = sb("hgs", [128, 8], F32)
        esink = sb("esink", [128, 8], F32)
        cw = sb("cw", [128, 4, 3], F32)
        ss = sb("ss", [128, 4], F32)
        rsd = sb("rsd", [128, 4], F32)

        g = nc.gpsimd
        lr1 = st.enter_context(g.register("lr1"))
        lr2 = st.enter_context(g.register("lr2"))
        licur = sb("licur", [128, 1], I32)

        def gather_loop(idx_all, src, dst, xg_, nj, tag):
            s1 = st.enter_context(nc.semaphore("lc" + tag))
            s2 = st.enter_context(nc.semaphore("ld" + tag))
            with g.Fori(0, nj) as j:
                g.tensor_copy(out=licur[:, 0:1], in_=idx_all[:, bass.ds(j, 1)]).then_inc(s1, 1)
                g.reg_mov(lr1, 1)
                g.reg_add(lr1, lr1, j)
                g.wait_ge(s1, lr1)
                g.indirect_dma_start(out=xg_[:, :], out_offset=None, in_=src[:, :], in_offset=bass.IndirectOffsetOnAxis(ap=licur[:, 0:1], axis=0),
                                     bounds_check=NT - 1, oob_is_err=False).then_inc(s2, 16)
                g.reg_mov(lr1, 32)
                g.reg_mul(lr1, lr1, j)
                g.reg_add(lr1, lr1, 16)
                g.wait_ge(s2, lr1)
                g.reg_mov(lr2, 128 * D)
                g.reg_mul(lr2, lr2, j)
                g.dma_start(out=bass.AP(dst, lr2, [[D, 128], [1, D]]), in_=xg_[:, :]).then_inc(s2, 16)
                g.reg_add(lr1, lr1, 16)
                g.wait_ge(s2, lr1)

        def scatter_loop(idx_all, src, dst, yt_, nj, tag):
            s1 = st.enter_context(nc.semaphore("sc" + tag))
            s2 = st.enter_context(nc.semaphore("sd" + tag))
            with g.Fori(0, nj) as j:
                g.tensor_copy(out=licur[:, 0:1], in_=idx_all[:, bass.ds(j, 1)]).then_inc(s1, 1)
                g.reg_mov(lr1, 1)
                g.reg_add(lr1, lr1, j)
                g.wait_ge(s1, lr1)
                g.reg_mov(lr2, 128 * D)
                g.reg_mul(lr2, lr2, j)
                g.dma_start(out=yt_[:, :], in_=bass.AP(src, lr2, [[D, 128], [1, D]])).then_inc(s2, 16)
                g.reg_mov(lr1, 32)
                g.reg_mul(lr1, lr1, j)
                g.reg_add(lr1, lr1, 16)
                g.wait_ge(s2, lr1)
                g.indirect_dma_start(out=dst[:, :], out_offset=bass.IndirectOffsetOnAxis(ap=licur[:, 0:1], axis=0), in_=yt_[:, :], in_offset=None,
                                     bounds_check=NT - 1, oob_is_err=False, compute_op=ALU.add).then_inc(s2, 16)
                g.reg_add(lr1, lr1, 16)
                g.wait_ge(s2, lr1)

        def ld(dst, src, eng='sp', stream='misc', R=(), W=None, slow=False):
            if slow:
                return k.dma(eng, stream, lambda e: e.dma_start(out=dst, in_=src, allow_slow_non_contiguous=True), R=R, W=W)
            return k.dma(eng, stream, lambda e: e.dma_start(out=dst, in_=src), R=R, W=W)

        ld(ident[:], cmisc[:, 0, :], W=[ident])
        ld(blockones[:], cmisc[:, 1, :], W=[blockones])
        ld(triL[:], cmisc[:, 2, :], eng='pool', stream='miscp', W=[triL])
        ld(maskP[:], cmisc[:, 3, :], eng='pool', stream='miscp', W=[maskP])
        ld(maskN[:], cmisc[:, 4, :], eng='pool', stream='miscp', W=[maskN])
        ld(identb[:], cmisc[:, 0, :], eng='pool', stream='miscp', W=[identb])
        ld(sc[:], cvec.ap(), W=[sc])
        k.op('dve', lambda e: e.memset(ones32[:], 1.0), W=[ones32])
        k.op('dve', lambda e: e.memset(onesb[:], 1.0), W=[onesb])
        for i in range(4):
            r0, r1 = i * (NT // 4), (i + 1) * (NT // 4)
            ld(xs[r0:r1, :], xin[r0:r1, :], stream='xcopy', W=[('xs', 'init')])
        k.op('act', lambda e: e.activation(out=sc[:], in_=sc[:], func=AF.Silu), R=[sc], W=[sc])
        for kc in range(8):
            for v in range(2):
                k.op('dve', lambda e, kc=kc, v=v: e.tensor_scalar(out=lbc[:, kc, v, :], in0=ones32[:], scalar1=sc[:, kc, v:v + 1],
                                                                   scalar2=None, op0=ALU.mult), R=[sc, ones32], W=[lbc])
        k.barrier()

        blocks = [(i * 512, 512, 0) for i in range(16)] + [(NL, NCX, 1)]

        def rstd_of(xt, junk, col):
            k.op('pool', lambda e: e.memset(ss[:, col:col + 1], 0.0), W=[('ss', col)])
            k.op('act', lambda e: e.activation(out=junk[:], in_=xt[:], func=AF.Square, accum_out=ss[:, col:col + 1]),
                 R=[xt], W=[junk, ('ss', col)])
            k.op('act', lambda e: e.activation(out=rsd[:, col:col + 1], in_=ss[:, col:col + 1], func=AF.Sqrt, scale=1.0 / D, bias=EPS),
                 R=[('ss', col)], W=[('rsd', col)])
            k.op('dve', lambda e: e.reciprocal(out=rsd[:, col:col + 1], in_=rsd[:, col:col + 1]), R=[('rsd', col)], W=[('rsd', col)])

        for l in range(L):
            last = (l == L - 1) and not force_ctx
            with ExitStack() as p0:
                wa = [sb("wa%d_%d" % (l, i), [128, 8, 512], F32, p0) for i in range(2)]
                bb = sb("bb%d" % l, [128, 4, D], F32, p0)
                gfb = sb("gfb%d" % l, [128, D], F32, p0)
                rowt = sb("rowt%d" % l, [128, D], F32, p0)
                bcol = sb("bcol%d" % l, [128, 96], F32, p0)
                gc2 = sb("gc2%d" % l, [128, 2, 8, 2], F32, p0)
                ld(bb[:], b_bc[l], W=[bb])
                ld(gfb[:], gffn_bc[l], W=[gfb])
                ld(bcol[:], b_col2[l], W=[bcol])
                ld(gc2[:], gcol2[l], W=[gc2])
                ld(hg[:], hgd[l], W=[hg])
                ld(esink[:], sinkd[l], W=[esink])
                ld(cw[:], convwd[l], W=[cw])
                k.op('act', lambda e: e.activation(out=esink[:], in_=esink[:], func=AF.Exp), R=[esink], W=[esink])
                psmod = PS[0]
                rowsel = {4: (0, 0), 5: (0, 1), 6: (1, 0), 7: (1, 1), 8: (2, 0), 9: (2, 1), 10: (3, 0), 11: (3, 1)}
                for ch in range(12):
                    w = wa[ch % 2]
                    ld(w[:], w_ada[l, :, :, ch * 512:(ch + 1) * 512], stream='wa%d' % (ch % 2), W=[w])
                    for sub in range(4):
                        j = ch * 4 + sub
                        for kc in range(8):
                            k.op('pe', lambda e, w=w, j=j, kc=kc, sub=sub: e.matmul(
                                psmod[:, 2 * j:2 * j + 2], lhsT=w[:, kc, sub * 128:(sub + 1) * 128], rhs=sc[:, kc, :],
                                start=(kc == 0), stop=(kc == 7)), R=[w, sc], W=[psmod])
                    if ch in rowsel:
                        vi, half = rowsel[ch]
                        for v in range(2):
                            pr = PS[1 + v]
                            for kc in range(8):
                                k.op('pe', lambda e, w=w, kc=kc, v=v, pr=pr: e.matmul(
                                    pr[:, :], lhsT=lbc[:, kc, v, :], rhs=w[:, kc, :], start=(kc == 0), stop=(kc == 7)),
                                    R=[w, lbc], W=[pr])
                            hs = slice(half * 512, (half + 1) * 512)
                            k.op('dve', lambda e, pr=pr, vi=vi, hs=hs: e.tensor_tensor(
                                out=rowt[:, hs], in0=pr[:, :], in1=bb[:, vi, hs], op=ALU.add), R=[pr, bb], W=[rowt])
                            if vi == 2:
                                k.op('dve', lambda e, hs=hs: e.scalar_tensor_tensor(
                                    out=rowt[:, hs], in0=rowt[:, hs], scalar=1.0, in1=gfb[:, hs], op0=ALU.add, op1=ALU.mult),
                                    R=[rowt, gfb], W=[rowt])
                            ld(bcd[v, vi, :, hs], rowt[:, hs], stream='bcst', R=[rowt], W=[('bcd', v, vi, half)])
                mc = modcol[:].rearrange("p a b -> p (a b)")
                k.op('dve', lambda e: e.tensor_tensor(out=mc, in0=psmod[:, 0:96], in1=bcol[:], op=ALU.add), R=[psmod, bcol], W=[modcol])
                k.op('dve', lambda e: e.scalar_tensor_tensor(out=A1c[:], in0=modcol[:, 8:16, :], scalar=1.0, in1=gc2[:, 0, :, :],
                                                            op0=ALU.add, op1=ALU.mult), R=[modcol, gc2], W=[A1c])
                k.barrier()
            if stop == 'p0':
                break

            with ExitStack() as pkv:
                KTA = sb("KTA%d" % l, [128, NT], BF16, pkv)
                KTB = sb("KTB%d" % l, [128, NT], BF16, pkv)
                VA = sb("VA%d" % l, [128, NTL, 2, 80], BF16, pkv)
                VB = sb("VB%d" % l, [128, NTL, 2, 80], BF16, pkv)
                KT = {'A': KTA, 'B': KTB}
                VV = {'A': VA, 'B': VB}
                k.op('pool', lambda e: e.memset(VA[:], 0.0), W=[VA])
                k.op('pool', lambda e: e.memset(VB[:], 0.0), W=[VB])
                k.op('pool', lambda e: e.memset(VA[:, :, :, 64:65], 1.0), W=[VA])
                k.op('pool', lambda e: e.memset(VB[:, :, :, 64:65], 1.0), W=[VB])

                def normrope(psp, psr, gi, Cb, Sb, out_ap, outres, n, tmp):
                    sq, rt, t1, t2 = tmp
                    k.op('act', lambda e: e.activation(out=sq[:, :n], in_=psp[:, :n], func=AF.Square), R=[psp], W=[sq])
                    pq = ps_get('nr', [6])
                    k.op('pe', lambda e: e.matmul(pq[:, :n], lhsT=blockones[:], rhs=sq[:, :n], start=True, stop=True),
                         R=[sq, blockones], W=[pq])
                    k.op('act', lambda e: e.activation(out=rt[:, :n], in_=pq[:, :n], func=AF.Sqrt, scale=1.0 / 64, bias=EPS),
                         R=[pq], W=[rt])
                    k.op('dve', lambda e: e.reciprocal(out=rt[:, :n], in_=rt[:, :n]), R=[rt], W=[rt])
                    k.op('dve', lambda e: e.scalar_tensor_tensor(out=t1[:, :n], in0=psp[:, :n], scalar=hg[:, gi:gi + 1], in1=Cb[:, :n],
                                                                op0=ALU.mult, op1=ALU.mult), R=[psp, Cb, hg], W=[t1])
                    k.op('dve', lambda e: e.scalar_tensor_tensor(out=t2[:, :n], in0=psr[:, :n], scalar=hg[:, gi + 1:gi + 2], in1=Sb[:, :n],
                                                                op0=ALU.mult, op1=ALU.mult), R=[psr, Sb, hg], W=[t2])
                    k.op('pool', lambda e: e.tensor_tensor(out=t1[:, :n], in0=t1[:, :n], in1=t2[:, :n], op=ALU.add), R=[t1, t2], W=[t1])
                    k.op('pool', lambda e: e.tensor_tensor(out=out_ap, in0=t1[:, :n], in1=rt[:, :n], op=ALU.mult), R=[t1, rt], W=[outres])

                with ExitStack() as p1:
                    W1 = sb("W1_%d" % l, [128, 8, 768], BF16, p1)
                    ld(W1[:], w1d[l], eng='pool', stream='wbig', W=[W1])
                    xt2 = [sb("xt%d_%d" % (l, i), [128, D], F32, p1) for i in range(2)]
                    xn2 = [sb("xn%d_%d" % (l, i), [128, D], F32, p1) for i in range(2)]
                    junk = sb("junk%d" % l, [128, D], F32, p1)
                    hTb2 = [sb("hTb%d_%d" % (l, i), [128, 8, 512], BF16, p1) for i in range(2)]
                    Cb2 = [sb("Cb%d_%d" % (l, i), [128, 512], F32, p1) for i in range(2)]
                    Sb2 = [sb("Sb%d_%d" % (l, i), [128, 512], F32, p1) for i in range(2)]
                    tmps = [[sb("nt%d_%d_%d" % (l, i, j), [128, 512], F32, p1) for j in range(4)] for i in range(2)]
                    ti_glob = 0
                    for bi, (t0, ntok, v) in enumerate(blocks if cut >= 5 else blocks[:1]):
                        if cut < 2:
                            break
                        hTb = hTb2[bi % 2]
                        Cb, Sb = Cb2[bi % 2], Sb2[bi % 2]
                        ld(Cb[:, :ntok], ropeCd[:, t0:t0 + ntok], stream='rope%d' % (bi % 2), W=[Cb])
                        ld(Sb[:, :ntok], ropeSd[:, t0:t0 + ntok], stream='rope%d' % (bi % 2), W=[Sb])
                        for tt in range(ntok // 128):
                            xt = xt2[ti_glob % 2]
                            xn = xn2[ti_glob % 2]
                            col = ti_glob % 2
                            r0 = t0 + tt * 128
                            ld(xt[:], xs[r0:r0 + 128, :], stream='xt%d' % (ti_glob % 2), R=[('xs', 'init')], W=[xt])
                            rstd_of(xt, junk, col)
                            k.op('dve', lambda e, xn=xn, xt=xt, col=col: e.tensor_scalar(out=xn[:], in0=xt[:], scalar1=rsd[:, col:col + 1],
                                                                                      scalar2=None, op0=ALU.mult),
                                 R=[xt, ('rsd', col)], W=[xn])
                            for half in range(2):
                                pt = ps_get('tr', [0, 1])
                                for q in range(4):
                                    kc = half * 4 + q
                                    k.op('pe', lambda e, pt=pt, q=q, kc=kc, xn=xn: e.transpose(pt[:, q * 128:(q + 1) * 128],
                                                                                               xn[:, kc * 128:(kc + 1) * 128], ident[:]),
                                         R=[xn, ident], W=[pt])
                                for q in range(4):
                                    kc = half * 4 + q
                                    eng = 'act' if q % 2 == 0 else 'dve'
                                    dst = hTb[:, kc, tt * 128:(tt + 1) * 128]
                                    if eng == 'act':
                                        k.op('act', lambda e, pt=pt, q=q, kc=kc, dst=dst: e.activation(
                                            out=dst, in_=pt[:, q * 128:(q + 1) * 128], func=AF.Identity,
                                            scale=A1c[:, kc, v:v + 1], bias=modcol[:, kc, v:v + 1]), R=[pt, A1c, modcol], W=[hTb])
                                    else:
                                        k.op('dve', lambda e, pt=pt, q=q, kc=kc, dst=dst: e.tensor_scalar(
                                            out=dst, in0=pt[:, q * 128:(q + 1) * 128], scalar1=A1c[:, kc, v:v + 1],
                                            scalar2=modcol[:, kc, v:v + 1], op0=ALU.mult, op1=ALU.add), R=[pt, A1c, modcol], W=[hTb])
                            ti_glob += 1
                        ld(hTd[:, :, t0:t0 + ntok], hTb[:, :, :ntok], stream='hst%d' % (bi % 2), R=[hTb], W=[('hTd', bi)])
                        for ai, (nm, c0, gi) in enumerate((('A', 0, 2), ('B', 128, 6)) if cut >= 3 else ()):
                            psp = ps_get('kp', [2, 3])
                            psr = ps_get('kr', [4, 5])
                            for kc in range(8):
                                k.op('pe', lambda e, kc=kc, psp=psp, c0=c0: e.matmul(psp[:, :ntok], lhsT=W1[:, kc, c0:c0 + 128],
                                                                                    rhs=hTb[:, kc, :ntok], start=(kc == 0), stop=(kc == 7)),
                                     R=[W1, hTb], W=[psp])
                            for kc in range(8):
                                k.op('pe', lambda e, kc=kc, psr=psr, c0=c0: e.matmul(psr[:, :ntok], lhsT=W1[:, kc, 256 + c0:256 + c0 + 128],
                                                                                    rhs=hTb[:, kc, :ntok], start=(kc == 0), stop=(kc == 7)),
                                     R=[W1, hTb], W=[psr])
                            normrope(psp, psr, gi, Cb, Sb, KT[nm][:, t0:t0 + ntok], KT[nm], ntok, tmps[ai])
                        for tt in range(ntok // 128 if cut >= 4 else 0):
                            ti = (t0 + tt * 128) // 128
                            pv = ps_get('kp', [2, 3]) if cut != 41 else ps_get('tr', [0, 1])
                            for kc in range(8):
                                k.op('pe', lambda e, kc=kc, pv=pv, tt=tt: e.matmul(pv[:, 0:256], lhsT=hTb[:, kc, tt * 128:(tt + 1) * 128],
                                                                                  rhs=W1[:, kc, 512:768], start=(kc == 0), stop=(kc == 7)),
                                     R=[W1, hTb], W=[pv])
                            for g_ in range(2):
                                k.op('act', lambda e, pv=pv, ti=ti, g_=g_: e.copy(out=VA[:, ti, g_, 0:64], in_=pv[:, g_ * 64:(g_ + 1) * 64]),
                                     R=[pv], W=[VA])
                                k.op('dve', lambda e, pv=pv, ti=ti, g_=g_: e.tensor_copy(out=VB[:, ti, g_, 0:64], in_=pv[:, 128 + g_ * 64:128 + (g_ + 1) * 64]),
                                     R=[pv], W=[VB])
                    k.barrier()
                if stop == 'p1':
                    if debug:
                        dk = nc.dram_tensor("dbgKTA", [128, NT], BF16, kind="ExternalOutput")
                        dkb = nc.dram_tensor("dbgKTB", [128, NT], BF16, kind="ExternalOutput")
                        dv = nc.dram_tensor("dbgVA", [128, NTL, 2, 80], BF16, kind="ExternalOutput")
                        ld(dk.ap(), KTA[:], stream='dbg', R=[KTA], W=['dbgo'])
                        ld(dkb.ap(), KTB[:], stream='dbg', R=[KTB], W=['dbgo'])
                        ld(dv.ap(), VA[:], stream='dbg', R=[VA], W=['dbgo'])
                        k.barrier()
                    break

                with ExitStack() as p2:
                    W2 = sb("W2_%d" % l, [128, 8, 2048], BF16, p2)
                    ld(W2[:], w2d[l], eng='pool', stream='wbig', W=[W2])
                    hTb2 = [sb("hTq%d_%d" % (l, i), [128, 8, 512], BF16, p2) for i in range(2)]
                    Cb2 = [sb("Cq%d_%d" % (l, i), [128, 512], F32, p2) for i in range(2)]
                    Sb2 = [sb("Sq%d_%d" % (l, i), [128, 512], F32, p2) for i in range(2)]
                    tmps = [[sb("qt%d_%d_%d" % (l, i, j), [128, 512], F32, p2) for j in range(4)] for i in range(2)]
                    QT = {'A': sb("QTA%d" % l, [128, 4, 512], BF16, p2), 'B': sb("QTB%d" % l, [128, 4, 512], BF16, p2)}
                    OT2 = {'A': [sb("OaT%d_%d" % (l, i), [128, 4, 512], BF16, p2) for i in range(2)],
                           'B': [sb("ObT%d_%d" % (l, i), [128, 4, 512], BF16, p2) for i in range(2)]}
                    PT = [sb("PT%d_%d" % (l, i), [128, 512], BF16, p2) for i in range(4)]
                    rec = [sb("rec%d_%d" % (l, i), [128, 512], F32, p2) for i in range(2)]
                    bcs = [sb("bcs%d_%d" % (l, i), [64, 512], F32, p2) for i in range(2)]
                    ptc = [0]
                    fin = [0]

                    def finalize(pso, n, dst_ap, dstres, sink_col=None):
                        r = rec[fin[0] % 2]
                        bc = bcs[fin[0] % 2]
                        fin[0] += 1
                        if sink_col is not None:
                            k.op('dve', lambda e: e.tensor_scalar(out=r[64:65, :n], in0=pso[64:65, :n], scalar1=esink[64:65, sink_col:sink_col + 1],
                                                                  scalar2=None, op0=ALU.add), R=[pso, esink], W=[r])
                            k.op('dve', lambda e: e.reciprocal(out=r[64:65, :n], in_=r[64:65, :n]), R=[r], W=[r])
                        else:
                            k.op('dve', lambda e: e.reciprocal(out=r[64:65, :n], in_=pso[64:65, :n]), R=[pso], W=[r])
                        pb = ps_get('nr', [6])
                        k.op('pe', lambda e: e.matmul(pb[0:64, :n], lhsT=ones32[64:65, 0:64], rhs=r[64:65, :n], start=True, stop=True),
                             R=[r, ones32], W=[pb])
                        k.op('act', lambda e: e.copy(out=bc[0:64, :n], in_=pb[0:64, :n]), R=[pb], W=[bc])
                        k.op('dve', lambda e: e.tensor_tensor(out=dst_ap, in0=pso[0:64, :n], in1=bc[0:64, :n], op=ALU.mult),
                             R=[pso, bc], W=[dstres])

                    qblocks = blocks if not last else blocks[:16]
                    for bi, (t0, ntok, v) in enumerate(qblocks):
                        latent = (v == 0)
                        hTb = hTb2[bi % 2]
                        Cb, Sb = Cb2[bi % 2], Sb2[bi % 2]
                        ld(hTb[:, :, :ntok], hTd[:, :, t0:t0 + ntok], stream='hq%d' % (bi % 2), R=[('hTd', bi)], W=[hTb])
                        ld(Cb[:, :ntok], ropeCd[:, t0:t0 + ntok], stream='rq%d' % (bi % 2), W=[Cb])
                        ld(Sb[:, :ntok], ropeSd[:, t0:t0 + ntok], stream='rq%d' % (bi % 2), W=[Sb])
                        qi = 0
                        for nm, base, gi in (('A', 0, 0), ('B', 1024, 4)):
                            for j in range(4):
                                psp = ps_get('kp', [2, 3])
                                psr = ps_get('kr', [4, 5])
                                for kc in range(8):
                                    k.op('pe', lambda e, kc=kc, psp=psp, c0=base + j * 128: e.matmul(
                                        psp[:, :ntok], lhsT=W2[:, kc, c0:c0 + 128], rhs=hTb[:, kc, :ntok], start=(kc == 0), stop=(kc == 7)),
                                        R=[W2, hTb], W=[psp])
                                for kc in range(8):
                                    k.op('pe', lambda e, kc=kc, psr=psr, c0=base + 512 + j * 128: e.matmul(
                                        psr[:, :ntok], lhsT=W2[:, kc, c0:c0 + 128], rhs=hTb[:, kc, :ntok], start=(kc == 0), stop=(kc == 7)),
                                        R=[W2, hTb], W=[psr])
                                normrope(psp, psr, gi, Cb, Sb, QT[nm][:, j, :ntok], (QT[nm], j), ntok, tmps[qi % 2])
                                qi += 1
                        OaT = OT2['A'][bi % 2]
                        ObT = OT2['B'][bi % 2]
                        kts = list(range(NTL)) if latent else [64, 65]
                        for j in range(4):
                            for hh in range(2):
                                rows = slice(64 * hh, 64 * hh + 64)
                                pso = ps_get('o', [4, 5])
                                for n_, kt in enumerate(kts):
                                    pss = ps_get('s', [0, 1, 2, 3])
                                    pt = PT[ptc[0] % 4]
                                    ptc[0] += 1
                                    k.op('pe', lambda e, pss=pss, kt=kt: e.matmul(pss[:, :ntok], lhsT=KTA[rows, kt * 128:(kt + 1) * 128],
                                                                                 rhs=QT['A'][rows, j, :ntok], start=True, stop=True),
                                         R=[KTA, (QT['A'], j)], W=[pss])
                                    k.op('act', lambda e, pss=pss, pt=pt: e.activation(out=pt[:, :ntok], in_=pss[:, :ntok], func=AF.Exp, scale=0.125),
                                         R=[pss], W=[pt])
                                    k.op('pe', lambda e, pso=pso, pt=pt, kt=kt, n_=n_: e.matmul(
                                        pso[0:65, :ntok], lhsT=VA[:, kt, hh, 0:65], rhs=pt[:, :ntok], start=(n_ == 0), stop=(n_ == len(kts) - 1)),
                                        R=[VA, pt], W=[pso])
                                finalize(pso, ntok, OaT[rows, j, :ntok], (OaT, j, hh))
                        for j in range(4):
                            for hh in range(2):
                                rows = slice(64 * hh, 64 * hh + 64)
                                head = 4 * hh + j
                                pso = ps_get('o', [4, 5])
                                for qt in range(ntok // 128):
                                    cols = slice(qt * 128, (qt + 1) * 128)
                                    I = (t0 + qt * 128) // 128
                                    kl = [(64, None), (65, None)]
                                    if latent:
                                        if I - 1 >= 0:
                                            kl.append((I - 1, maskP))
                                        kl.append((I, None))
                                        if I + 1 < 64:
                                            kl.append((I + 1, maskN))
                                    for n_, (kt, msk) in enumerate(kl):
                                        pss = ps_get('s', [0, 1, 2, 3])
                                        pt = PT[ptc[0] % 4]
                                        ptc[0] += 1
                                        k.op('pe', lambda e, pss=pss, kt=kt: e.matmul(pss[:, 0:128], lhsT=KTB[rows, kt * 128:(kt + 1) * 128],
                                                                                     rhs=QT['B'][rows, j, cols], start=True, stop=True),
                                             R=[KTB, (QT['B'], j)], W=[pss])
                                        k.op('act', lambda e, pss=pss, pt=pt: e.activation(out=pt[:, 0:128], in_=pss[:, 0:128], func=AF.Exp, scale=0.125),
                                             R=[pss], W=[pt])
                                        if msk is not None:
                                            k.op('pool', lambda e, pt=pt, msk=msk: e.tensor_tensor(out=pt[:, 0:128], in0=pt[:, 0:128], in1=msk[:],
                                                                                                    op=ALU.mult), R=[pt, msk], W=[pt])
                                        k.op('pe', lambda e, pso=pso, pt=pt, kt=kt, n_=n_, kl=kl: e.matmul(
                                            pso[0:65, cols], lhsT=VB[:, kt, hh, 0:65], rhs=pt[:, 0:128], start=(n_ == 0), stop=(n_ == len(kl) - 1)),
                                            R=[VB, pt], W=[pso])
                                finalize(pso, ntok, ObT[rows, j, :ntok], (ObT, j, hh), sink_col=head)
                        ld(oaTd[:, :, t0:t0 + ntok], OaT[:, :, :ntok], stream='ost%d' % (bi % 2), R=[(OaT, j, hh) for j in range(4) for hh in range(2)],
                           W=[('oaTd', bi)])
                        ld(obTd[:, :, t0:t0 + ntok], ObT[:, :, :ntok], stream='ost%d' % (bi % 2), R=[(ObT, j, hh) for j in range(4) for hh in range(2)],
                           W=[('obTd', bi)])
                    k.barrier()
            if stop == 'p2':
                break
            mblocks = blocks if not last else blocks[:16]

            def uoff(t0):
                return t0 + 1 if t0 < NL else t0 + 3

            with ExitStack() as p3a:
                Wcx = sb("Wcx%d" % l, [128, 8, 1024], BF16, p3a)
                ld(Wcx[:], wcxd[l], eng='pool', stream='wbig', W=[Wcx])
                hTb2 = [sb("hTu%d_%d" % (l, i), [128, 8, 512], BF16, p3a) for i in range(2)]
                ub2 = [sb("ub%d_%d" % (l, i), [128, 4, 512], F32, p3a) for i in range(2)]
                cs2 = [sb("cs%d_%d" % (l, i), [128, 512], F32, p3a) for i in range(2)]
                zt = sb("zt%d" % l, [128, 4, 2], F32, p3a)
                k.op('dve', lambda e: e.memset(zt[:], 0.0), W=[zt])
                ld(uTd[:, :, 0:1], zt[:, :, 0:1], stream='zp', R=[zt], W=['uzp'], slow=True)
                ld(uTd[:, :, NL + 1:NL + 3], zt[:, :, 0:2], stream='zp', R=[zt], W=['uzp'], slow=True)
                ld(uTd[:, :, NT + 3:NT + 4], zt[:, :, 0:1], stream='zp', R=[zt], W=['uzp'], slow=True)
                cc = 0
                for bi, (t0, ntok, v) in enumerate(mblocks):
                    hTb = hTb2[bi % 2]
                    ub = ub2[bi % 2]
                    ld(hTb[:, :, :ntok], hTd[:, :, t0:t0 + ntok], stream='hu%d' % (bi % 2), W=[hTb])
                    for c in range(4):
                        psc = ps_get('kp', [2, 3])
                        psx = ps_get('kr', [4, 5])
                        cs = cs2[cc % 2]
                        cc += 1
                        for kc in range(8):
                            k.op('pe', lambda e, kc=kc, psc=psc, c=c: e.matmul(psc[:, :ntok], lhsT=Wcx[:, kc, c * 128:(c + 1) * 128],
                                                                              rhs=hTb[:, kc, :ntok], start=(kc == 0), stop=(kc == 7)),
                                 R=[Wcx, hTb], W=[psc])
                        for kc in range(8):
                            k.op('pe', lambda e, kc=kc, psx=psx, c=c: e.matmul(psx[:, :ntok], lhsT=Wcx[:, kc, 512 + c * 128:512 + (c + 1) * 128],
                                                                              rhs=hTb[:, kc, :ntok], start=(kc == 0), stop=(kc == 7)),
                                 R=[Wcx, hTb], W=[psx])
                        k.op('act', lambda e, cs=cs, psc=psc: e.copy(out=cs[:, :ntok], in_=psc[:, :ntok]), R=[psc], W=[cs])
                        k.op('dve', lambda e, cs=cs, psx=psx, c=c, ub=ub: e.tensor_tensor(out=ub[:, c, :ntok], in0=psx[:, :ntok], in1=cs[:, :ntok],
                                                                                       op=ALU.mult), R=[psx, cs], W=[(ub, c)])
                    o0 = uoff(t0)
                    ld(uTd[:, :, o0:o0 + ntok], ub[:, :, :ntok], stream='ust%d' % (bi % 2), R=[(ub, c) for c in range(4)], W=[('uTd', bi)])
                k.barrier()

            with ExitStack() as p3b:
                W3 = sb("W3_%d" % l, [128, 8, 3584], BF16, p3b)
                Wbr = sb("Wbr%d" % l, [128, 12, D], BF16, p3b)
                ld(W3[:], w3d[l], eng='pool', stream='wbig', W=[W3])
                ld(Wbr[:], wbrd[l], eng='pool', stream='wbig', W=[Wbr])
                hTb2 = [sb("hTm%d_%d" % (l, i), [128, 8, 512], BF16, p3b) for i in range(2)]
                Oa2 = [sb("Oam%d_%d" % (l, i), [128, 4, 512], BF16, p3b) for i in range(2)]
                Ob2 = [sb("Obm%d_%d" % (l, i), [128, 4, 512], BF16, p3b) for i in range(2)]
                u2 = [sb("um%d_%d" % (l, i), [128, 4, 514], F32, p3b) for i in range(2)]
                OcT = sb("OcT%d" % l, [128, 4, 512], BF16, p3b)
                mT2 = [sb("mT%d_%d" % (l, i), [128, 8, 512], BF16, p3b) for i in range(2)]
                cva = [sb("cva%d_%d" % (l, i), [128, 512], F32, p3b) for i in range(2)]
                cvb = [sb("cvb%d_%d" % (l, i), [128, 512], F32, p3b) for i in range(2)]
                sg2 = [sb("sg%d_%d" % (l, i), [128, 512], F32, p3b) for i in range(3)]
                acc2 = [sb("acc%d_%d" % (l, i), [128, 512], F32, p3b) for i in range(2)]
                tm2 = [sb("tm%d_%d" % (l, i), [128, 512], F32, p3b) for i in range(2)]
                cnt = [0, 0, 0]
                for bi, (t0, ntok, v) in enumerate(mblocks):
                    hTb, Oa, Ob, ub, mT = hTb2[bi % 2], Oa2[bi % 2], Ob2[bi % 2], u2[bi % 2], mT2[bi % 2]
                    o0 = uoff(t0)
                    ld(hTb[:, :, :ntok], hTd[:, :, t0:t0 + ntok], stream='hm%d' % (bi % 2), W=[hTb])
                    ld(Oa[:, :, :ntok], oaTd[:, :, t0:t0 + ntok], stream='hm%d' % (bi % 2), W=[Oa])
                    ld(Ob[:, :, :ntok], obTd[:, :, t0:t0 + ntok], stream='hm%d' % (bi % 2), W=[Ob])
                    ld(ub[:, :, :ntok + 2], uTd[:, :, o0 - 1:o0 + ntok + 1], stream='hm%d' % (bi % 2), W=[ub])
                    for c in range(4):
                        psb = ps_get('kp', [2, 3])
                        for kc in range(8):
                            k.op('pe', lambda e, kc=kc, psb=psb, c=c: e.matmul(psb[:, :ntok], lhsT=W3[:, kc, c * 128:(c + 1) * 128],
                                                                              rhs=hTb[:, kc, :ntok], start=(kc == 0), stop=(kc == 7)),
                                 R=[W3, hTb], W=[psb])
                        a = cva[cnt[0] % 2]
                        cnt[0] += 1
                        k.op('pool', lambda e, a=a, c=c: e.tensor_scalar(out=a[:, :ntok], in0=ub[:, c, 0:ntok], scalar1=cw[:, c, 0:1], scalar2=None,
                                                                        op0=ALU.mult), R=[ub, cw], W=[a])
                        a2 = cvb[cnt[0] % 2]
                        for tap in (1, 2):
                            k.op('pool', lambda e, a2=a2, c=c, tap=tap: e.tensor_scalar(out=a2[:, :ntok], in0=ub[:, c, tap:ntok + tap], scalar1=cw[:, c, tap:tap + 1],
                                                                                       scalar2=None, op0=ALU.mult), R=[ub, cw], W=[a2])
                            k.op('pool', lambda e, a=a, a2=a2: e.tensor_tensor(out=a[:, :ntok], in0=a[:, :ntok], in1=a2[:, :ntok], op=ALU.add), R=[a, a2], W=[a])
                        k.op('dve', lambda e, a=a, c=c, psb=psb: e.tensor_tensor(out=OcT[:, c, :ntok], in0=psb[:, :ntok], in1=a[:, :ntok], op=ALU.mult),
                             R=[psb, a], W=[(OcT, c)])
                    Obr = [Oa, Ob, OcT]
                    for m in range(8):
                        acc = acc2[m % 2]
                        for br in range(3):
                            psg = ps_get('s', [0, 1])
                            psr = ps_get('kr', [4, 5])
                            sg = sg2[cnt[1] % 3]
                            cnt[1] += 1
                            c0 = 512 + br * 1024 + m * 128
                            for kc in range(8):
                                k.op('pe', lambda e, kc=kc, psg=psg, c0=c0: e.matmul(psg[:, :ntok], lhsT=W3[:, kc, c0:c0 + 128],
                                                                                    rhs=hTb[:, kc, :ntok], start=(kc == 0), stop=(kc == 7)),
                                     R=[W3, hTb], W=[psg])
                            k.op('act', lambda e, sg=sg, psg=psg: e.activation(out=sg[:, :ntok], in_=psg[:, :ntok], func=AF.Sigmoid), R=[psg], W=[sg])
                            Rr = [Wbr] + ([Oa] if br == 0 else [Ob] if br == 1 else [(OcT, c) for c in range(4)])
                            for c in range(4):
                                k.op('pe', lambda e, c=c, psr=psr, br=br, m=m: e.matmul(psr[:, :ntok], lhsT=Wbr[:, br * 4 + c, m * 128:(m + 1) * 128],
                                                                                       rhs=Obr[br][:, c, :ntok], start=(c == 0), stop=(c == 3)),
                                     R=Rr, W=[psr])
                            if br == 0:
                                k.op('dve', lambda e, psr=psr, sg=sg, acc=acc: e.tensor_tensor(out=acc[:, :ntok], in0=psr[:, :ntok], in1=sg[:, :ntok],
                                                                                            op=ALU.mult), R=[psr, sg], W=[acc])
                            else:
                                tm = tm2[cnt[2] % 2]
                                cnt[2] += 1
                                k.op('dve', lambda e, psr=psr, sg=sg, tm=tm: e.tensor_tensor(out=tm[:, :ntok], in0=psr[:, :ntok], in1=sg[:, :ntok],
                                                                                          op=ALU.mult), R=[psr, sg], W=[tm])
                                if br == 1:
                                    k.op('pool', lambda e, tm=tm, acc=acc: e.tensor_tensor(out=acc[:, :ntok], in0=acc[:, :ntok], in1=tm[:, :ntok],
                                                                                        op=ALU.add), R=[acc, tm], W=[acc])
                                else:
                                    k.op('pool', lambda e, tm=tm, acc=acc, m=m, mT=mT: e.tensor_tensor(out=mT[:, m, :ntok], in0=acc[:, :ntok], in1=tm[:, :ntok],
                                                                                                    op=ALU.add), R=[acc, tm], W=[(mT, m)])
                    ld(mTd[:, :, t0:t0 + ntok], mT[:, :, :ntok], stream='mst%d' % (bi % 2), R=[(mT, m) for m in range(8)], W=[('mTd', bi)])
                k.barrier()
            if stop == 'p3b':
                break

            pmoe = ExitStack()
            aff = sb("aff%d" % l, [128, NTL, NE], F32, pmoe)
            with ExitStack() as p3c:
                Wout = sb("Wout%d" % l, [128, 8, D], BF16, p3c)
                ld(Wout[:], woutd[l], eng='pool', stream='wbig', W=[Wout])
                Wr = sb("Wr%d" % l, [128, 8, NE], F32, p3c)
                ld(Wr[:], wrd[l], W=[Wr])
                bct = {}
                for vv in range(2):
                    for vi, nm in ((0, 'gt1'), (1, 'sh2'), (2, 'A2')):
                        t_ = sb("bc_%s%d_%d" % (nm, vv, l), [128, D], F32, p3c)
                        ld(t_[:], bcd[vv, vi], W=[t_])
                        bct[(vv, nm)] = t_
                mT2 = [sb("mTo%d_%d" % (l, i), [128, 8, 512], BF16, p3c) for i in range(2)]
                xt2 = [sb("xo%d_%d" % (l, i), [128, D], F32, p3c) for i in range(2)]
                xw2 = [sb("xw%d_%d" % (l, i), [128, D], F32, p3c) for i in range(2)]
                h22 = [sb("h2%d_%d" % (l, i), [128, D], F32, p3c) for i in range(2)]
                h2b2 = [sb("h2b%d_%d" % (l, i), [128, D], BF16, p3c) for i in range(2)]
                junk = sb("junk3%d" % l, [128, D], F32, p3c)
                h2T = [sb("h2T%d_%d" % (l, i), [128, 8, 128], F32, p3c) for i in range(2)]
                ex2 = [sb("ex%d_%d" % (l, i), [128, NE], F32, p3c) for i in range(2)]
                sm = sb("sm%d" % l, [128, 8], F32, p3c)
                tg = 0
                for bi, (t0, ntok, v) in enumerate(mblocks):
                    mT = mT2[bi % 2]
                    ld(mT[:, :, :ntok], mTd[:, :, t0:t0 + ntok], stream='mo%d' % (bi % 2), W=[mT])
                    for tt in range(ntok // 128):
                        i2 = tg % 2
                        tg += 1
                        xt, xw, h2, h2b, hT_, ex = xt2[i2], xw2[i2], h22[i2], h2b2[i2], h2T[i2], ex2[i2]
                        r0 = t0 + tt * 128
                        ti = r0 // 128
                        ld(xt[:], xs[r0:r0 + 128, :], stream='xo%d' % i2, W=[xt])
                        for half in range(2):
                            hs = slice(half * 512, (half + 1) * 512)
                            po = ps_get('kp', [2, 3])
                            for m in range(8):
                                k.op('pe', lambda e, m=m, po=po, hs=hs, tt=tt: e.matmul(po[:, :], lhsT=mT[:, m, tt * 128:(tt + 1) * 128], rhs=Wout[:, m, hs],
                                                                                       start=(m == 0), stop=(m == 7)), R=[Wout, mT], W=[po])
                            k.op('dve', lambda e, po=po, hs=hs, xw=xw: e.tensor_tensor(out=xw[:, hs], in0=po[:, :], in1=bct[(v, 'gt1')][:, hs], op=ALU.mult),
                                 R=[po, bct[(v, 'gt1')]], W=[(xw, half)])
                            k.op('pool', lambda e, hs=hs, xw=xw, xt=xt: e.tensor_tensor(out=xw[:, hs], in0=xw[:, hs], in1=xt[:, hs], op=ALU.add),
                                 R=[(xw, half), xt], W=[(xw, half)])
                        ld(xs[r0:r0 + 128, :], xw[:], stream='xst%d' % i2, R=[(xw, 0), (xw, 1)], W=[('xs', ti)])
                        col = 2 + i2
                        k.op('pool', lambda e, col=col: e.memset(ss[:, col:col + 1], 0.0), W=[('ss', col)])
                        k.op('act', lambda e, xw=xw, col=col: e.activation(out=junk[:], in_=xw[:], func=AF.Square, accum_out=ss[:, col:col + 1]),
                             R=[(xw, 0), (xw, 1)], W=[junk, ('ss', col)])
                        k.op('act', lambda e, col=col: e.activation(out=rsd[:, col:col + 1], in_=ss[:, col:col + 1], func=AF.Sqrt, scale=1.0 / D, bias=EPS),
                             R=[('ss', col)], W=[('rsd', col)])
                        k.op('dve', lambda e, col=col: e.reciprocal(out=rsd[:, col:col + 1], in_=rsd[:, col:col + 1]), R=[('rsd', col)], W=[('rsd', col)])
                        k.op('dve', lambda e, xw=xw, h2=h2, col=col: e.scalar_tensor_tensor(out=h2[:], in0=xw[:], scalar=rsd[:, col:col + 1], in1=bct[(v, 'A2')][:],
                                                                                         op0=ALU.mult, op1=ALU.mult),
                             R=[(xw, 0), (xw, 1), ('rsd', col), bct[(v, 'A2')]], W=[h2])
                        k.op('pool', lambda e, h2=h2: e.tensor_tensor(out=h2[:], in0=h2[:], in1=bct[(v, 'sh2')][:], op=ALU.add), R=[h2, bct[(v, 'sh2')]], W=[h2])
                        k.op('act', lambda e, h2=h2, h2b=h2b: e.copy(out=h2b[:], in_=h2[:]), R=[h2], W=[h2b])
                        ld(h2d[r0:r0 + 128, :], h2b[:], stream='h2st%d' % i2, R=[h2b], W=[('h2d', ti)])
                        for half in range(2):
                            pt = ps_get('tr', [0, 1])
                            for q in range(4):
                                kc = half * 4 + q
                                k.op('pe', lambda e, pt=pt, q=q, kc=kc, h2=h2: e.transpose(pt[:, q * 128:(q + 1) * 128], h2[:, kc * 128:(kc + 1) * 128], ident[:]),
                                     R=[h2, ident], W=[pt])
                            eng = 'act' if half == 0 else 'dve'
                            src = pt[:, :]
                            dst = hT_[:].rearrange("p a b -> p (a b)")[:, half * 512:(half + 1) * 512]
                            if eng == 'act':
                                k.op('act', lambda e, dst=dst, src=src: e.copy(out=dst, in_=src), R=[pt], W=[(hT_, half)])
                            else:
                                k.op('dve', lambda e, dst=dst, src=src: e.tensor_copy(out=dst, in_=src), R=[pt], W=[(hT_, half)])
                        pl = ps_get('nr', [6])
                        for kc in range(8):
                            k.op('pe', lambda e, kc=kc, pl=pl, hT_=hT_: e.matmul(pl[:, 0:NE], lhsT=hT_[:, kc, :], rhs=Wr[:, kc, :], start=(kc == 0), stop=(kc == 7)),
                                 R=[(hT_, 0), (hT_, 1), Wr], W=[pl])
                        c2 = 4 * i2
                        k.op('dve', lambda e, pl=pl, c2=c2: e.tensor_reduce(out=sm[:, c2:c2 + 1], in_=pl[:, 0:NE], axis=AX.X, op=ALU.max), R=[pl], W=[('sm', i2)])
                        k.op('dve', lambda e, c2=c2: e.tensor_scalar(out=sm[:, c2 + 1:c2 + 2], in0=sm[:, c2:c2 + 1], scalar1=-1.0, scalar2=None, op0=ALU.mult),
                             R=[('sm', i2)], W=[('sm', i2)])
                        k.op('pool', lambda e, c2=c2: e.memset(sm[:, c2 + 2:c2 + 3], 0.0), R=[('sm', i2)], W=[('sm', i2)])
                        k.op('act', lambda e, pl=pl, ex=ex, c2=c2: e.activation(out=ex[:], in_=pl[:, 0:NE], func=AF.Exp, bias=sm[:, c2 + 1:c2 + 2],
                                                                              accum_out=sm[:, c2 + 2:c2 + 3]), R=[pl, ('sm', i2)], W=[ex, ('sm', i2)])
                        k.op('dve', lambda e, c2=c2: e.reciprocal(out=sm[:, c2 + 3:c2 + 4], in_=sm[:, c2 + 2:c2 + 3]), R=[('sm', i2)], W=[('sm', i2)])
                        k.op('dve', lambda e, ex=ex, ti=ti, c2=c2: e.tensor_scalar(out=aff[:, ti, :], in0=ex[:], scalar1=sm[:, c2 + 3:c2 + 4], scalar2=None, op0=ALU.mult),
                             R=[ex, ('sm', i2)], W=[(aff, ti)])
                        ld(affd[r0:r0 + 128, :], aff[:, ti, :], stream='afst%d' % i2, R=[(aff, ti)], W=[('affd', ti)])
                k.barrier()
            if stop == 'p3c':
                pmoe.close()
                break

            with pmoe:
                gt2 = [sb("gt2_%d_%d" % (l, vv), [128, D], F32, pmoe) for vv in range(2)]
                for vv in range(2):
                    ld(gt2[vv][:], bcd[vv, 3], W=[gt2[vv]])
                NSC = 9
                NJ = NE * NSC
                idx_all = sb("idx_all%d" % l, [128, NJ], I32, pmoe)
                gate_all = sb("gate_all%d" % l, [128, NJ], F32, pmoe)
                k.op('pool', lambda e: e.memset(idx_all[:], 1 << 20), W=[idx_all])
                k.op('pool', lambda e: e.memset(gate_all[:], 0.0), W=[gate_all])
                sets = [(0, 64, 1024, 0)] + ([] if last else [(64, 2, 32, 1)])
                nst = 8 if last else 9
                with ExitStack() as pth:
                    iota = sb("iota%d" % l, [128, 1024], F32, pth)
                    ld(iota[:], iotad.ap(), W=[iota])
                    comb = sb("comb%d" % l, [128, NTL, NE, 5], BF16, pth)
                    ld(comb[:], combd.ap(), eng='pool', W=[comb])
                    posm = sb("posm%d" % l, [128, NTL, NE], F32, pth)
                    lo = sb("lo%d" % l, [128, NE], F32, pth)
                    hi = sb("hi%d" % l, [128, NE], F32, pth)
                    mid = sb("mid%d" % l, [128, NE], F32, pth)
                    ge = sb("ge%d" % l, [128, NE], F32, pth)
                    g2 = sb("g2%d" % l, [128, NE], F32, pth)
                    cntp = sb("cntp%d" % l, [128, NE], F32, pth)
                    cmp_ = sb("cmp%d" % l, [128, 64, NE], F32, pth)
                    maskb = sb("maskb%d" % l, [128, 64, NE], BF16, pth)
                    inc = [sb("inc%d_%d" % (l, i), [128, 64, NE], F32, pth) for i in range(2)]
                    tot = sb("tot%d" % l, [128, 64, NE], F32, pth)
                    r1 = sb("r1_%d" % l, [128, NTL, NE], F32, pth)
                    r2 = sb("r2_%d" % l, [128, NTL, NE], F32, pth)
                    k.op('pool', lambda e: e.tensor_copy(out=comb[:, :, :, 2], in_=aff[:]), R=[aff], W=[comb])
                    k.op('dve', lambda e: e.tensor_tensor(out=r1[:], in0=aff[:], in1=comb[:, :, :, 2], op=ALU.subtract), R=[aff, comb], W=[r1])
                    k.op('pool', lambda e: e.tensor_copy(out=comb[:, :, :, 3], in_=r1[:]), R=[r1], W=[comb])
                    k.op('dve', lambda e: e.tensor_tensor(out=r2[:], in0=r1[:], in1=comb[:, :, :, 3], op=ALU.subtract), R=[r1, comb], W=[r2])
                    k.op('pool', lambda e: e.tensor_copy(out=comb[:, :, :, 4], in_=r2[:]), R=[r2], W=[comb])
                    for (ti0, T, cap, vv) in sets:
                        affs = aff[:, ti0:ti0 + T, :]
                        k.op('dve', lambda e: e.memset(lo[:], 0.0), W=[lo])
                        k.op('dve', lambda e: e.memset(hi[:], 1.0), W=[hi])
                        for it in range(32):
                            k.op('dve', lambda e: e.tensor_tensor(out=mid[:], in0=lo[:], in1=hi[:], op=ALU.add), R=[lo, hi], W=[mid])
                            k.op('dve', lambda e: e.tensor_scalar(out=mid[:], in0=mid[:], scalar1=0.5, scalar2=None, op0=ALU.mult), R=[mid], W=[mid])
                            k.op('dve', lambda e, T=T, affs=affs: e.tensor_tensor(out=cmp_[:, :T, :], in0=affs, in1=mid[:].unsqueeze(1).to_broadcast([128, T, NE]),
                                                                                 op=ALU.is_ge), R=[aff, mid], W=[cmp_])
                            k.op('dve', lambda e, T=T: e.tensor_reduce(out=cntp[:], in_=cmp_[:, :T, :].rearrange("p t e -> p e t"), axis=AX.X, op=ALU.add),
                                 R=[cmp_], W=[cntp])
                            pc = ps_get('nr', [6])
                            k.op('pe', lambda e, pc=pc: e.matmul(pc[:, 0:NE], lhsT=ones32[:], rhs=cntp[:], start=True, stop=True), R=[ones32, cntp], W=[pc])
                            k.op('dve', lambda e, pc=pc, cap=cap: e.tensor_scalar(out=ge[:], in0=pc[:, 0:NE], scalar1=float(cap) - 0.5, scalar2=None, op0=ALU.is_ge),
                                 R=[pc], W=[ge])
                            k.op('dve', lambda e: e.tensor_tensor(out=g2[:], in0=ge[:], in1=mid[:], op=ALU.mult), R=[ge, mid], W=[g2])
                            k.op('dve', lambda e: e.tensor_tensor(out=lo[:], in0=lo[:], in1=g2[:], op=ALU.max), R=[lo, g2], W=[lo])
                            k.op('dve', lambda e: e.scalar_tensor_tensor(out=g2[:], in0=ge[:], scalar=2.0, in1=mid[:], op0=ALU.mult, op1=ALU.add),
                                 R=[ge, mid, g2], W=[g2])
                            k.op('dve', lambda e: e.tensor_tensor(out=hi[:], in0=hi[:], in1=g2[:], op=ALU.min), R=[hi, g2], W=[hi])
                        k.op('dve', lambda e, T=T, affs=affs: e.tensor_tensor(out=cmp_[:, :T, :], in0=affs, in1=lo[:].unsqueeze(1).to_broadcast([128, T, NE]),
                                                                             op=ALU.is_ge), R=[aff, lo], W=[cmp_])
                        k.op('pool', lambda e, T=T: e.tensor_copy(out=maskb[:, :T, :], in_=cmp_[:, :T, :]), R=[cmp_], W=[maskb])
                        ncol = T * NE
                        mb = maskb[:].rearrange("p t e -> p (t e)")
                        totf = tot[:].rearrange("p t e -> p (t e)")
                        pp = [PS[2], PS[3]]
                        pq = [PS[4], PS[5]]
                        nhf = (ncol + 511) // 512
                        for hf in range(nhf):
                            n_ = min(512, ncol - hf * 512)
                            k.op('pe', lambda e, hf=hf, n_=n_: e.matmul(pp[hf][:, :n_], lhsT=triL[:], rhs=mb[:, hf * 512:hf * 512 + n_], start=True, stop=True),
                                 R=[triL, maskb], W=[pp[hf]])
                            k.op('pe', lambda e, hf=hf, n_=n_: e.matmul(pq[hf][:, :n_], lhsT=onesb[:], rhs=mb[:, hf * 512:hf * 512 + n_], start=True, stop=True),
                                 R=[onesb, maskb], W=[pq[hf]])
                            k.op('act', lambda e, hf=hf, n_=n_: e.copy(out=totf[:, hf * 512:hf * 512 + n_], in_=pq[hf][:, :n_]), R=[pq[hf]], W=[tot])
                        k.op('pool', lambda e, T=T: e.tensor_copy(out=inc[0][:, :T, :], in_=tot[:, :T, :]), R=[tot], W=[inc[0]])
                        cur = 0
                        s_ = 1
                        while s_ < T:
                            a_, b_ = inc[cur], inc[1 - cur]
                            k.op('dve', lambda e, a_=a_, b_=b_, s_=s_, T=T: e.tensor_tensor(out=b_[:, s_:T, :], in0=a_[:, s_:T, :], in1=a_[:, 0:T - s_, :], op=ALU.add),
                                 R=[a_], W=[b_])
                            k.op('pool', lambda e, a_=a_, b_=b_, s_=s_: e.tensor_copy(out=b_[:, 0:s_, :], in_=a_[:, 0:s_, :]), R=[a_], W=[b_])
                            cur = 1 - cur
                            s_ *= 2
                        incf = inc[cur]
                        oth = inc[1 - cur]
                        othf = oth[:].rearrange("p t e -> p (t e)")
                        k.op('dve', lambda e, T=T: e.tensor_tensor(out=oth[:, :T, :], in0=incf[:, :T, :], in1=tot[:, :T, :], op=ALU.subtract), R=[incf, tot], W=[oth])
                        for hf in range(nhf):
                            n_ = min(512, ncol - hf * 512)
                            k.op('dve', lambda e, hf=hf, n_=n_: e.tensor_tensor(out=othf[:, hf * 512:hf * 512 + n_], in0=othf[:, hf * 512:hf * 512 + n_],
                                                                              in1=pp[hf][:, :n_], op=ALU.add), R=[oth, pp[hf]], W=[oth])
                        k.op('dve', lambda e, T=T, ti0=ti0: e.scalar_tensor_tensor(out=posm[:, ti0:ti0 + T, :], in0=oth[:, :T, :], scalar=1.0, in1=cmp_[:, :T, :],
                                                                                  op0=ALU.add, op1=ALU.mult), R=[oth, cmp_], W=[posm])
                        k.op('dve', lambda e, T=T, ti0=ti0: e.tensor_scalar(out=posm[:, ti0:ti0 + T, :], in0=posm[:, ti0:ti0 + T, :], scalar1=-1.0, scalar2=None,
                                                                           op0=ALU.add), R=[posm], W=[posm])
                    k.barrier()
                    if debug and stop == 'p4a':
                        dpos = nc.dram_tensor("dbgposm", [128, NTL, NE], F32, kind="ExternalOutput")
                        ld(dpos.ap(), posm[:], R=[posm], W=['dbgo'])
                        k.barrier()
                        break

                    Sb_ = [sb("Sone%d_%d" % (l, i), [128, 1024], BF16, pth) for i in range(3)]
                    rows5 = sb("rows5_%d" % l, [8, 1056], F32, pth)
                    t5 = sb("t5_%d" % l, [128, 48], F32, pth)
                    idxf = sb("idxf%d" % l, [128, NSC], F32, pth)
                    g1 = sb("g1_%d" % l, [128, NSC], F32, pth)
                    rr = 0
                    for e_ in range(NE):
                        for (ti0, T, cap, vv) in sets:
                            off = 0 if vv == 0 else 1024
                            nh = (cap + 511) // 512
                            pi = [PS[0], PS[1]]
                            for i_ in range(T):
                                S_ = Sb_[rr % 3]
                                rr += 1
                                ti = ti0 + i_
                                k.op('dve', lambda e, S_=S_, ti=ti, e_=e_, cap=cap: e.tensor_scalar(out=S_[:, :cap], in0=iota[:, :cap], scalar1=posm[:, ti, e_:e_ + 1],
                                                                                                   scalar2=None, op0=ALU.is_equal), R=[iota, posm], W=[S_])
                                for hf in range(nh):
                                    n_ = min(512, cap - hf * 512)
                                    k.op('pe', lambda e, S_=S_, ti=ti, hf=hf, n_=n_, i_=i_, T=T, e_=e_: e.matmul(pi[hf][0:5, :n_], lhsT=comb[:, ti, e_, :],
                                                                                                              rhs=S_[:, hf * 512:hf * 512 + n_],
                                                                                                              start=(i_ == 0), stop=(i_ == T - 1)), R=[comb, S_], W=[pi[hf]])
                            for hf in range(nh):
                                n_ = min(512, cap - hf * 512)
                                k.op('act', lambda e, hf=hf, n_=n_, off=off: e.copy(out=rows5[0:5, off + hf * 512:off + hf * 512 + n_], in_=pi[hf][0:5, :n_]),
                                     R=[pi[hf]], W=[rows5])
                        ptx = ps_get('nr', [6])
                        for s in range(nst):
                            prow = 128 if s < 8 else 32
                            k.op('pe', lambda e, s=s, prow=prow: e.transpose(ptx[0:prow, 5 * s:5 * s + 5], rows5[0:5, s * 128:s * 128 + prow], ident[0:5, 0:5]),
                                 R=[rows5, ident], W=[ptx])
                        k.op('act', lambda e: e.copy(out=t5[:, 0:40], in_=ptx[:, 0:40]), R=[ptx], W=[t5])
                        if nst == 9:
                            k.op('act', lambda e: e.copy(out=t5[0:32, 40:45], in_=ptx[0:32, 40:45]), R=[ptx], W=[t5])
                        for (c0, c1, prow) in ((0, 8, 128),) + (((8, 9, 32),) if nst == 9 else ()):
                            n5 = slice(5 * c0, 5 * c1, 5)
                            k.op('dve', lambda e, c0=c0, c1=c1, prow=prow: e.scalar_tensor_tensor(
                                out=idxf[0:prow, c0:c1], in0=t5[0:prow, 5 * c0:5 * c1:5], scalar=128.0, in1=t5[0:prow, 5 * c0 + 1:5 * c1:5], op0=ALU.mult, op1=ALU.add),
                                R=[t5], W=[idxf])
                            k.op('dve', lambda e, c0=c0, c1=c1, prow=prow, e_=e_: e.tensor_copy(out=idx_all[0:prow, e_ * NSC + c0:e_ * NSC + c1], in_=idxf[0:prow, c0:c1]),
                                 R=[idxf], W=[idx_all])
                            k.op('dve', lambda e, c0=c0, c1=c1, prow=prow: e.tensor_tensor(out=g1[0:prow, c0:c1], in0=t5[0:prow, 5 * c0 + 2:5 * c1:5],
                                                                                         in1=t5[0:prow, 5 * c0 + 3:5 * c1:5], op=ALU.add), R=[t5], W=[g1])
                            k.op('dve', lambda e, c0=c0, c1=c1, prow=prow, e_=e_: e.tensor_tensor(out=gate_all[0:prow, e_ * NSC + c0:e_ * NSC + c1], in0=g1[0:prow, c0:c1],
                                                                                               in1=t5[0:prow, 5 * c0 + 4:5 * c1:5], op=ALU.add), R=[g1, t5], W=[gate_all])
                    k.barrier()
                if debug and stop == 'p4a':
                    break
                if debug and stop == 'p4b':
                    di = nc.dram_tensor("dbgidx", [128, NJ], I32, kind="ExternalOutput")
                    dg = nc.dram_tensor("dbggate", [128, NJ], F32, kind="ExternalOutput")
                    ld(di.ap(), idx_all[:], R=[idx_all], W=['dbgo'])
                    ld(dg.ap(), gate_all[:], R=[gate_all], W=['dbgo'])
                    k.barrier()
                    break

                with ExitStack() as pgl:
                    xgt = sb("xgt%d" % l, [128, D], BF16, pgl)
                    k.op('pool', lambda e: e.memset(xgt[:], 0.0), W=[xgt])
                    k.barrier()
                    gather_loop(idx_all, h2d, xgd, xgt, NJ, "g%d" % l)
                    k.op('pool', lambda e: e.memset(xgt[:, 0:2], 0.0), W=[xgt])
                    k.barrier()

                with ExitStack() as pex:
                    Wg2 = [sb("Wg%d_%d" % (l, i), [128, 8, D], BF16, pex) for i in range(2)]
                    Wu2 = [sb("Wu%d_%d" % (l, i), [128, 8, D], BF16, pex) for i in range(2)]
                    Wd2 = [sb("Wd%d_%d" % (l, i), [128, 8, D], BF16, pex) for i in range(2)]
                    xg = sb("xg%d" % l, [128, NSC, D], BF16, pex)
                    xsT = sb("xsT%d" % l, [128, 8, 1056], BF16, pex)
                    hd = sb("hd%d" % l, [128, 8, 1056], BF16, pex)
                    sa2 = [sb("sa%d_%d" % (l, i), [128, 512], F32, pex) for i in range(2)]
                    yo2 = [sb("yo%d_%d" % (l, i), [128, D], F32, pex) for i in range(2)]
                    cnt_ = [0]

                    def load_expert(e_, slot):
                        ld(Wg2[slot][:], wegd[l, e_], eng='pool', W=[Wg2[slot]])
                        ld(Wu2[slot][:], weud[l, e_], eng='pool', W=[Wu2[slot]])
                        ld(Wd2[slot][:], wedd[l, e_], eng='pool', W=[Wd2[slot]])

                    load_expert(0, 0)
                    groups = [(0, 512), (512, 512)] + ([(1024, 32)] if nst == 9 else [])
                    for e_ in range(NE):
                        slot = e_ % 2
                        if e_ + 1 < NE:
                            load_expert(e_ + 1, 1 - slot)
                        Wg, Wu, Wd = Wg2[slot], Wu2[slot], Wd2[slot]
                        j0 = e_ * NSC
                        ld(xg[:, 0:nst, :], xgd[j0 * 128:(j0 + nst) * 128, :].rearrange("(s p) d -> p s d", p=128), W=[xg])
                        for kc in range(8):
                            for s in range(8):
                                k.op('pe', lambda e, s=s, kc=kc: e.transpose(PSB[:, s * 128:(s + 1) * 128], xg[:, s, kc * 128:(kc + 1) * 128], identb[:]),
                                     R=[xg, identb], W=[PSB])
                            if kc % 2 == 0:
                                k.op('act', lambda e, kc=kc: e.copy(out=xsT[:, kc, 0:1024], in_=PSB[:, 0:1024]), R=[PSB], W=[(xsT, kc)])
                            else:
                                k.op('dve', lambda e, kc=kc: e.tensor_copy(out=xsT[:, kc, 0:1024], in_=PSB[:, 0:1024]), R=[PSB], W=[(xsT, kc)])
                        if nst == 9:
                            for kc in range(8):
                                k.op('pe', lambda e, kc=kc: e.transpose(PSB[:, kc * 32:(kc + 1) * 32], xg[0:32, 8, kc * 128:(kc + 1) * 128], identb[0:32, 0:32]),
                                     R=[xg, identb], W=[PSB])
                            for kc in range(8):
                                k.op('act', lambda e, kc=kc: e.copy(out=xsT[:, kc, 1024:1056], in_=PSB[:, kc * 32:(kc + 1) * 32]), R=[PSB], W=[(xsT, kc)])
                        xr = [(xsT, kc) for kc in range(8)]
                        for fc in range(8):
                            for (c0, n_) in groups:
                                pa = ps_get('s', [0, 1])
                                pu = ps_get('kr', [4, 5])
                                sa = sa2[cnt_[0] % 2]
                                cnt_[0] += 1
                                for kc in range(8):
                                    k.op('pe', lambda e, kc=kc, fc=fc, c0=c0, n_=n_, pa=pa: e.matmul(pa[:, :n_], lhsT=Wg[:, kc, fc * 128:(fc + 1) * 128],
                                                                                                    rhs=xsT[:, kc, c0:c0 + n_], start=(kc == 0), stop=(kc == 7)),
                                         R=[Wg] + xr, W=[pa])
                                for kc in range(8):
                                    k.op('pe', lambda e, kc=kc, fc=fc, c0=c0, n_=n_, pu=pu: e.matmul(pu[:, :n_], lhsT=Wu[:, kc, fc * 128:(fc + 1) * 128],
                                                                                                    rhs=xsT[:, kc, c0:c0 + n_], start=(kc == 0), stop=(kc == 7)),
                                         R=[Wu] + xr, W=[pu])
                                k.op('act', lambda e, sa=sa, pa=pa, n_=n_: e.activation(out=sa[:, :n_], in_=pa[:, :n_], func=AF.Silu), R=[pa], W=[sa])
                                k.op('dve', lambda e, sa=sa, pu=pu, n_=n_, fc=fc, c0=c0: e.tensor_tensor(out=hd[:, fc, c0:c0 + n_], in0=pu[:, :n_], in1=sa[:, :n_],
                                                                                                      op=ALU.mult), R=[pu, sa], W=[(hd, fc)])
                        hr = [(hd, fc) for fc in range(8)]
                        for s in range(nst):
                            prow = 128 if s < 8 else 32
                            vv = 0 if s < 8 else 1
                            yo = yo2[s % 2]
                            for half in range(2):
                                hs = slice(half * 512, (half + 1) * 512)
                                py = ps_get('kp', [2, 3]) if half == 0 else ps_get('nr', [6])
                                for fc in range(8):
                                    k.op('pe', lambda e, fc=fc, s=s, py=py, hs=hs, prow=prow: e.matmul(py[0:prow, :], lhsT=hd[:, fc, s * 128:s * 128 + prow], rhs=Wd[:, fc, hs],
                                                                                                      start=(fc == 0), stop=(fc == 7)), R=[Wd] + hr, W=[py])
                                k.op('dve', lambda e, py=py, yo=yo, hs=hs, s=s, vv=vv, prow=prow, j0=j0: e.scalar_tensor_tensor(
                                    out=yo[0:prow, hs], in0=py[0:prow, :], scalar=gate_all[0:prow, j0 + s:j0 + s + 1], in1=gt2[vv][0:prow, hs], op0=ALU.mult, op1=ALU.mult),
                                    R=[py, gate_all, gt2[vv]], W=[(yo, half)])
                            ld(Yd[(j0 + s) * 128:(j0 + s) * 128 + prow, :], yo[0:prow, :], R=[(yo, 0), (yo, 1)], W=[('Yd', j0 + s)])
                    k.barrier()

                with ExitStack() as psl:
                    yt = sb("yt%d" % l, [128, D], F32, psl)
                    k.op('pool', lambda e: e.memset(yt[:], 0.0), W=[yt])
                    k.barrier()
                    scatter_loop(idx_all, Yd, xs, yt, NJ, "s%d" % l)
                    k.op('pool', lambda e: e.memset(yt[:, 0:2], 0.0), W=[yt])
                    k.barrier()
        k.barrier()
    return nc


def _colmajor(w):
    return np.ascontiguousarray(w.reshape(8, 128, -1).transpose(1, 0, 2))


def _consts():
    ident = np.eye(128, dtype=np.float32)
    blk = (np.arange(128)[:, None] // 64 == np.arange(128)[None, :] // 64).astype(np.float32)
    p = np.arange(128)
    triL = (p[:, None] < p[None, :]).astype(np.float32)
    maskP = (p[:, None] >= p[None, :]).astype(np.float32)
    maskN = (p[:, None] <= p[None, :]).astype(np.float32)
    cm = np.ascontiguousarray(np.stack([ident, blk, triL, maskP, maskN], axis=1))
    iota = np.ascontiguousarray(np.broadcast_to(np.arange(1024, dtype=np.float32), (128, 1024)))
    tidc = np.zeros((128, NTL, NE, 5), np.float32)
    tidc[:, :, :, 0] = np.arange(NTL)[None, :, None]
    tidc[:, :, :, 1] = np.arange(128)[:, None, None]
    t = np.arange(NL)
    row = (t // 64).astype(np.float32)
    colp = (t % 64).astype(np.float32)
    inv = np.power(np.float32(10000.0), -(np.arange(16, dtype=np.float32) / np.float32(16))).astype(np.float32)
    ang = np.concatenate([row[:, None] * inv[None, :], colp[:, None] * inv[None, :]], axis=-1).astype(np.float32)
    cos = np.cos(ang).astype(np.float32)
    sin = np.sin(ang).astype(np.float32)
    C = np.ones((128, NT), np.float32)
    S = np.zeros((128, NT), np.float32)
    for r in range(128):
        i = r % 64
        f = i % 32
        C[r, :NL] = cos[:, f]
        S[r, :NL] = sin[:, f] * (-1.0 if i < 32 else 1.0)
    return cm, iota, tidc, C, S


def _swap_halves(cols):
    cols = np.asarray(cols)
    return (cols // 64) * 64 + ((cols % 64) + 32) % 64


def prep_inputs(inp):
    L = inp['w_ada'].shape[0]
    cm, iota, tidc, C, S = _consts()
    f = lambda a: np.ascontiguousarray(a, dtype=np.float32)
    w_in = inp['w_in']
    kA, vA, kB, vB = np.arange(0, 128), np.arange(128, 256), np.arange(256, 384), np.arange(384, 512)
    qa0, qb0, cv0, gt0 = 512, 1024, 1536, 3072
    qorder = np.concatenate([np.concatenate([np.arange(j * 64, j * 64 + 64), np.arange((4 + j) * 64, (4 + j) * 64 + 64)]) for j in range(4)])
    c1 = np.concatenate([kA, kB, _swap_halves(kA), _swap_halves(kB), vA, vB])
    c2 = np.concatenate([qa0 + qorder, qa0 + _swap_halves(qorder), qb0 + qorder, qb0 + _swap_halves(qorder)])
    ccx = np.concatenate([np.arange(cv0 + 512, cv0 + 1024), np.arange(cv0 + 1024, cv0 + 1536)])
    c3 = np.concatenate([np.arange(cv0, cv0 + 512), np.arange(gt0, gt0 + 3072)])
    shared = {}
    shared['w_ada'] = f(np.stack([_colmajor(inp['w_ada'][l]) for l in range(L)]))
    bcol = np.stack([inp['b_ada'][l].reshape(48, 128).T for l in range(L)])
    shared['b_col2'] = f(np.repeat(bcol, 2, axis=2))
    sel = [slice(2048, 3072), slice(3072, 4096), slice(4096, 5120), slice(5120, 6144)]
    shared['b_bc'] = f(np.stack([np.stack([np.broadcast_to(inp['b_ada'][l][s], (128, 1024)) for s in sel], axis=1) for l in range(L)]))
    gc = np.zeros((L, 128, 2, 8, 2), np.float32)
    for l in range(L):
        gc[l, :, 0, :, :] = inp['g_mix'][l].reshape(8, 128).T[:, :, None]
        gc[l, :, 1, :, :] = inp['g_ffn'][l].reshape(8, 128).T[:, :, None]
    shared['gcol2'] = gc
    shared['gffn_bc'] = f(np.stack([np.broadcast_to(inp['g_ffn'][l], (128, 1024)) for l in range(L)]))
    shared['w1'] = f(np.stack([_colmajor(w_in[l][:, c1]) for l in range(L)]))
    shared['w2'] = f(np.stack([_colmajor(w_in[l][:, c2]) for l in range(L)]))
    shared['wcx'] = f(np.stack([_colmajor(w_in[l][:, ccx]) for l in range(L)]))
    shared['w3'] = f(np.stack([_colmajor(w_in[l][:, c3]) for l in range(L)]))
    wbr = np.zeros((L, 128, 12, 1024), np.float32)
    for l in range(L):
        for br in range(3):
            wb = inp['w_branch'][l, br]
            if br < 2:
                wb = wb[qorder]
            wbr[l, :, br * 4:(br + 1) * 4, :] = wb.reshape(4, 128, 1024).transpose(1, 0, 2)
    shared['wbr'] = wbr
    shared['wout'] = f(np.stack([_colmajor(inp['w_out'][l]) for l in range(L)]))
    hgv = np.zeros((L, 128, 8), np.float32)
    sw = (np.arange(64) + 32) % 64
    for l in range(L):
        for i, g in enumerate((inp['qg_a'][l], inp['kg_a'][l], inp['qg_b'][l], inp['kg_b'][l])):
            hgv[l, :, 2 * i] = np.tile(g, 2)
            hgv[l, :, 2 * i + 1] = np.tile(g[sw], 2)
    shared['hg'] = hgv
    shared['sink'] = f(np.stack([np.broadcast_to(inp['sink_b'][l], (128, 8)) for l in range(L)]))
    shared['convw'] = f(np.stack([inp['conv_w'][l].reshape(3, 4, 128).transpose(2, 1, 0) for l in range(L)]))
    shared['wr'] = f(np.stack([_colmajor(inp['w_router'][l]) for l in range(L)]))
    for nm, key in (('weg', 'w_e_gate'), ('weu', 'w_e_up'), ('wed', 'w_e_down')):
        w = inp[key]
        shared[nm] = f(w.reshape(L, NE, 8, 128, 1024).transpose(0, 1, 3, 2, 4))
    shared['ropeC'] = C
    shared['ropeS'] = S
    shared['cmisc'] = cm
    shared['iota'] = iota
    shared['comb'] = tidc
    maps = []
    B = inp['x'].shape[0]
    for b in range(B):
        m = dict(shared)
        m['xin'] = f(np.concatenate([inp['x'][b], inp['ctx'][b]], axis=0))
        cv = np.zeros((128, 8, 2), np.float32)
        cv[:, :, 0] = inp['c'][b].reshape(8, 128).T
        cv[:, :, 1] = inp['c_ctx'].reshape(8, 128).T
        m['cvec'] = cv
        maps.append(m)
    return maps


_NC_CACHE = {}
_PER_LAYER = ('w_ada', 'b_col2', 'b_bc', 'gcol2', 'gffn_bc', 'w1', 'w2', 'wcx', 'w3', 'wbr', 'wout', 'hg', 'sink', 'convw', 'wr',
              'weg', 'weu', 'wed')
FUSED = True


def _layer_slice(m, l):
    out = {}
    for k_, v in m.items():
        out[k_] = np.ascontiguousarray(v[l:l + 1]) if k_ in _PER_LAYER else v
    return out


def kernel(**inputs):
    inp = {k_: np.asarray(v) for k_, v in inputs.items()}
    maps = prep_inputs(inp)
    L = inp['w_ada'].shape[0]
    cores = list(range(len(maps)))
    if FUSED:
        if 'nc' not in _NC_CACHE:
            _NC_CACHE['nc'] = build(nlayers=L)
        res = run_bass_kernel_spmd(_NC_CACHE['nc'], maps, core_ids=cores)
        xs = [np.asarray(r["xs"]) for r in res.results]
    else:
        xs = [m['xin'] for m in maps]
        for l in range(L):
            key = ('layer', l == L - 1)
            if key not in _NC_CACHE:
                _NC_CACHE[key] = build(nlayers=1, force_ctx=(l != L - 1))
            lm = []
            for m, x_ in zip(maps, xs):
                d = _layer_slice(m, l)
                d['xin'] = np.ascontiguousarray(x_, dtype=np.float32)
                lm.append(d)
            res = run_bass_kernel_spmd(_NC_CACHE[key], lm, core_ids=cores)
            xs = [np.asarray(r["xs"]) for r in res.results]
    out = np.stack([x_[:NL] for x_ in xs], axis=0)
    return out.astype(np.float32)
```

```python
import numpy as np
from contextlib import ExitStack
import concourse.bass as bass
import concourse.mybir as mybir
from concourse.bass_utils import run_bass_kernel_spmd

F32 = mybir.dt.float32
BF16 = mybir.dt.bfloat16
I32 = mybir.dt.int32
AF = mybir.ActivationFunctionType
ALU = mybir.AluOpType
AX = mybir.AxisListType

NL, NCX, NT, D = 8192, 256, 8448, 1024
NTL = NT // 128
EPS = 1e-6
NE = 16
N_CORES = 4


class K:
    def __init__(self, nc, stack):
        self.nc = nc
        self.stack = stack
        self.eng = {'pe': nc.tensor, 'act': nc.scalar, 'dve': nc.vector, 'pool': nc.gpsimd, 'sp': nc.sync}
        self.sem = {n: stack.enter_context(nc.semaphore("s_" + n)) for n in self.eng}
        self.cnt = {n: 0 for n in self.eng}
        self.seen = {n: {} for n in self.eng}
        self.dsem = {}
        self.dval = {}
        self.lastw = {}
        self.readers = {}
        self.nwaits = 0
        self.nins = 0
        self.drr = {}

    def _key(self, t):
        if isinstance(t, (str, int)):
            return t
        if isinstance(t, tuple):
            return tuple(self._key(x) for x in t)
        return ('id', id(t))

    def _deps(self, R, W):
        deps = []
        for t in list(R) + list(W):
            d = self.lastw.get(self._key(t))
            if d is not None:
                deps.append(d)
        for t in W:
            deps.extend(self.readers.get(self._key(t), {}).items())
        return deps

    def _wait(self, e, deps):
        h = self.eng[e]
        seen = self.seen[e]
        need = {}
        for key, val in deps:
            if key == ('E', 'pe') and e == 'pe':
                continue
            if seen.get(key, 0) >= val:
                continue
            if need.get(key, 0) < val:
                need[key] = val
        for key, val in need.items():
            s = self.sem[key[1]] if key[0] == 'E' else self.dsem[key[1]]
            h.wait_ge(s, val)
            seen[key] = val
            self.nwaits += 1

    def _record(self, tok, R, W):
        key, val = tok
        for t in R:
            r = self.readers.setdefault(self._key(t), {})
            if r.get(key, 0) < val:
                r[key] = val
        for t in W:
            self.lastw[self._key(t)] = tok
            self.readers[self._key(t)] = {}

    def op(self, e, fn, R=(), W=()):
        self._wait(e, self._deps(R, W))
        ins = fn(self.eng[e])
        ins.then_inc(self.sem[e], 1)
        self.cnt[e] += 1
        self.nins += 1
        self._record((('E', e), self.cnt[e]), R, W)
        return ins

    NDS = 8

    def dma(self, e, stream, fn, R=(), W=()):
        i = self.drr.get(e, 0)
        self.drr[e] = i + 1
        stream = "%s%d" % (e, i % self.NDS)
        if stream not in self.dsem:
            self.dsem[stream] = self.stack.enter_context(self.nc.semaphore("d_" + stream))
            self.dval[stream] = 0
        deps = self._deps(R, W)
        if self.dval[stream] > 0:
            deps.append((('D', stream), self.dval[stream]))
        self._wait(e, deps)
        ins = fn(self.eng[e])
        ins.then_inc(self.dsem[stream], 16)
        self.dval[stream] += 16
        self.nins += 1
        self._record((('D', stream), self.dval[stream]), R, W)
        return ins

    def barrier(self, engines=None):
        deps = [(('E', n), c) for n, c in self.cnt.items() if c > 0]
        deps += [(('D', s), v) for s, v in self.dval.items() if v > 0]
        for e in (engines or self.eng):
            self._wait(e, deps)


def build(nlayers=2, debug=False, stop=None, cut=99, force_ctx=False):
    nc = bass.Bass("TRN2", target_bir_lowering=False)
    st = ExitStack()
    with st:
        k = K(nc, st)

        def din(name, shape, dt=F32):
            return nc.dram_tensor(name, list(shape), dt, kind="ExternalInput")

        def dsc(name, shape, dt, out=False):
            return nc.dram_tensor(name, list(shape), dt, kind="ExternalOutput" if (out or debug) else "Internal")

        L = nlayers
        xin = din("xin", [NT, D])
        cvec = din("cvec", [128, 8, 2])
        w_ada = din("w_ada", [L, 128, 8, 6144])
        b_col2 = din("b_col2", [L, 128, 96])
        b_bc = din("b_bc", [L, 128, 4, D])
        gcol2 = din("gcol2", [L, 128, 2, 8, 2])
        gffn_bc = din("gffn_bc", [L, 128, D])
        w1d = din("w1", [L, 128, 8, 768])
        w2d = din("w2", [L, 128, 8, 2048])
        wcxd = din("wcx", [L, 128, 8, 1024])
        w3d = din("w3", [L, 128, 8, 3584])
        wbrd = din("wbr", [L, 128, 12, D])
        woutd = din("wout", [L, 128, 8, D])
        hgd = din("hg", [L, 128, 8])
        sinkd = din("sink", [L, 128, 8])
        convwd = din("convw", [L, 128, 4, 3])
        wrd = din("wr", [L, 128, 8, NE])
        wegd = din("weg", [L, NE, 128, 8, D])
        weud = din("weu", [L, NE, 128, 8, D])
        wedd = din("wed", [L, NE, 128, 8, D])
        ropeCd = din("ropeC", [128, NT])
        ropeSd = din("ropeS", [128, NT])
        cmisc = din("cmisc", [128, 5, 128])
        iotad = din("iota", [128, 1024])
        combd = din("comb", [128, NTL, NE, 5])
        xs = dsc("xs", [NT, D], F32, out=True)
        hTd = dsc("hTd", [128, 8, NT], BF16)
        uTd = dsc("uTd", [128, 4, NT + 4], F32)
        oaTd = dsc("oaTd", [128, 4, NT], BF16)
        obTd = dsc("obTd", [128, 4, NT], BF16)
        mTd = dsc("mTd", [128, 8, NT], BF16)
        h2d = dsc("h2d", [NT, D], BF16)
        affd = dsc("affd", [NT, NE], F32)
        bcd = dsc("bcd", [2, 4, 128, D], F32)
        xgd = dsc("xgd", [NE * 9 * 128, D], BF16)
        Yd = dsc("Yd", [NE * 9 * 128, D], F32)

        def sb(name, shape, dt, stack=None):
            return (stack or st).enter_context(nc.sbuf_tensor(name, list(shape), dt))

        PS = [st.enter_context(nc.psum_tensor("ps%d" % i, [128, 512], F32)) for i in range(7)]
        PSB = st.enter_context(nc.psum_tensor("psb", [128, 1024], BF16))
        psrr = {}

        def ps_get(pool, banks):
            i = psrr.get(pool, 0)
            psrr[pool] = i + 1
            return PS[banks[i % len(banks)]]

        ident = sb("ident", [128, 128], F32)
        blockones = sb("blockones", [128, 128], F32)
        ones32 = sb("ones32", [128, 128], F32)
        onesb = sb("onesb", [128, 128], BF16)
        triL = sb("triL", [128, 128], BF16)
        maskP = sb("maskP", [128, 128], BF16)
        maskN = sb("maskN", [128, 128], BF16)
        identb = sb("identb", [128, 128], BF16)
        sc = sb("sc", [128, 8, 2], F32)
        lbc = sb("lbc", [128, 8, 2, 128], F32)
        modcol = sb("modcol", [128, 48, 2], F32)
        A1c = sb("A1c", [128, 8, 2], F32)
        hg = sb("hgs", [128, 8], F32)
        esink = sb("esink", [128, 8], F32)
        cw = sb("cw", [128, 4, 3], F32)
        ss = sb("ss", [128, 4], F32)
        rsd = sb("rsd", [128, 4], F32)

        g = nc.gpsimd
        lr1 = st.enter_context(g.register("lr1"))
        lr2 = st.enter_context(g.register("lr2"))
        licur = sb("licur", [128, 1], I32)

        def gather_loop(idx_all, src, dst, xg_, nj, tag):
            s1 = st.enter_context(nc.semaphore("lc" + tag))
            s2 = st.enter_context(nc.semaphore("ld" + tag))
            with g.Fori(0, nj) as j:
                g.tensor_copy(out=licur[:, 0:1], in_=idx_all[:, bass.ds(j, 1)]).then_inc(s1, 1)
                g.reg_mov(lr1, 1)
                g.reg_add(lr1, lr1, j)
                g.wait_ge(s1, lr1)
                g.indirect_dma_start(out=xg_[:, :], out_offset=None, in_=src[:, :], in_offset=bass.IndirectOffsetOnAxis(ap=licur[:, 0:1], axis=0),
                                     bounds_check=NT - 1, oob_is_err=False).then_inc(s2, 16)
                g.reg_mov(lr1, 32)
                g.reg_mul(lr1, lr1, j)
                g.reg_add(lr1, lr1, 16)
                g.wait_ge(s2, lr1)
                g.reg_mov(lr2, 128 * D)
                g.reg_mul(lr2, lr2, j)
                g.dma_start(out=bass.AP(dst, lr2, [[D, 128], [1, D]]), in_=xg_[:, :]).then_inc(s2, 16)
                g.reg_add(lr1, lr1, 16)
                g.wait_ge(s2, lr1)

        def scatter_loop(idx_all, src, dst, yt_, nj, tag):
            s1 = st.enter_context(nc.semaphore("sc" + tag))
            s2 = st.enter_context(nc.semaphore("sd" + tag))
            with g.Fori(0, nj) as j:
                g.tensor_copy(out=licur[:, 0:1], in_=idx_all[:, bass.ds(j, 1)]).then_inc(s1, 1)
                g.reg_mov(lr1, 1)
                g.reg_add(lr1, lr1, j)
                g.wait_ge(s1, lr1)
                g.reg_mov(lr2, 128 * D)
                g.reg_mul(lr2, lr2, j)
                g.dma_start(out=yt_[:, :], in_=bass.AP(src, lr2, [[D, 128], [1, D]])).then_inc(s2, 16)
                g.reg_mov(lr1, 32)
                g.reg_mul(lr1, lr1, j)
                g.reg_add(lr1, lr1, 16)
                g.wait_ge(s2, lr1)
                g.indirect_dma_start(out=dst[:, :], out_offset=bass.IndirectOffsetOnAxis(ap=licur[:, 0:1], axis=0), in_=yt_[:, :], in_offset=None,
                                     bounds_check=NT - 1, oob_is_err=False, compute_op=ALU.add).then_inc(s2, 16)
                g.reg_add(lr1, lr1, 16)
                g.wait_ge(s2, lr1)

        def ld(dst, src, eng='sp', stream='misc', R=(), W=None, slow=False):
            if slow:
                return k.dma(eng, stream, lambda e: e.dma_start(out=dst, in_=src, allow_slow_non_contiguous=True), R=R, W=W)
            return k.dma(eng, stream, lambda e: e.dma_start(out=dst, in_=src), R=R, W=W)

        ld(ident[:], cmisc[:, 0, :], W=[ident])
        ld(blockones[:], cmisc[:, 1, :], W=[blockones])
        ld(triL[:], cmisc[:, 2, :], eng='pool', stream='miscp', W=[triL])
        ld(maskP[:], cmisc[:, 3, :], eng='pool', stream='miscp', W=[maskP])
        ld(maskN[:], cmisc[:, 4, :], eng='pool', stream='miscp', W=[maskN])
        ld(identb[:], cmisc[:, 0, :], eng='pool', stream='miscp', W=[identb])
        ld(sc[:], cvec.ap(), W=[sc])
        k.op('dve', lambda e: e.memset(ones32[:], 1.0), W=[ones32])
        k.op('dve', lambda e: e.memset(onesb[:], 1.0), W=[onesb])
        for i in range(4):
            r0, r1 = i * (NT // 4), (i + 1) * (NT // 4)
            ld(xs[r0:r1, :], xin[r0:r1, :], stream='xcopy', W=[('xs', 'init')])
        k.op('act', lambda e: e.activation(out=sc[:], in_=sc[:], func=AF.Silu), R=[sc], W=[sc])
        for kc in range(8):
            for v in range(2):
                k.op('dve', lambda e, kc=kc, v=v: e.tensor_scalar(out=lbc[:, kc, v, :], in0=ones32[:], scalar1=sc[:, kc, v:v + 1],
                                                                   scalar2=None, op0=ALU.mult), R=[sc, ones32], W=[lbc])
        k.barrier()

        blocks = [(i * 512, 512, 0) for i in range(16)] + [(NL, NCX, 1)]

        def rstd_of(xt, junk, col):
            k.op('pool', lambda e: e.memset(ss[:, col:col + 1], 0.0), W=[('ss', col)])
            k.op('act', lambda e: e.activation(out=junk[:], in_=xt[:], func=AF.Square, accum_out=ss[:, col:col + 1]),
                 R=[xt], W=[junk, ('ss', col)])
            k.op('act', lambda e: e.activation(out=rsd[:, col:col + 1], in_=ss[:, col:col + 1], func=AF.Sqrt, scale=1.0 / D, bias=EPS),
                 R=[('ss', col)], W=[('rsd', col)])
            k.op('dve', lambda e: e.reciprocal(out=rsd[:, col:col + 1], in_=rsd[:, col:col + 1]), R=[('rsd', col)], W=[('rsd', col)])

        for l in range(L):
            last = (l == L - 1) and not force_ctx
            with ExitStack() as p0:
                wa = [sb("wa%d_%d" % (l, i), [128, 8, 512], F32, p0) for i in range(2)]
                bb = sb("bb%d" % l, [128, 4, D], F32, p0)
                gfb = sb("gfb%d" % l, [128, D], F32, p0)
                rowt = sb("rowt%d" % l, [128, D], F32, p0)
                bcol = sb("bcol%d" % l, [128, 96], F32, p0)
                gc2 = sb("gc2%d" % l, [128, 2, 8, 2], F32, p0)
                ld(bb[:], b_bc[l], W=[bb])
                ld(gfb[:], gffn_bc[l], W=[gfb])
                ld(bcol[:], b_col2[l], W=[bcol])
                ld(gc2[:], gcol2[l], W=[gc2])
                ld(hg[:], hgd[l], W=[hg])
                ld(esink[:], sinkd[l], W=[esink])
                ld(cw[:], convwd[l], W=[cw])
                k.op('act', lambda e: e.activation(out=esink[:], in_=esink[:], func=AF.Exp), R=[esink], W=[esink])
                psmod = PS[0]
                rowsel = {4: (0, 0), 5: (0, 1), 6: (1, 0), 7: (1, 1), 8: (2, 0), 9: (2, 1), 10: (3, 0), 11: (3, 1)}
                for ch in range(12):
                    w = wa[ch % 2]
                    ld(w[:], w_ada[l, :, :, ch * 512:(ch + 1) * 512], stream='wa%d' % (ch % 2), W=[w])
                    for sub in range(4):
                        j = ch * 4 + sub
                        for kc in range(8):
                            k.op('pe', lambda e, w=w, j=j, kc=kc, sub=sub: e.matmul(
                                psmod[:, 2 * j:2 * j + 2], lhsT=w[:, kc, sub * 128:(sub + 1) * 128], rhs=sc[:, kc, :],
                                start=(kc == 0), stop=(kc == 7)), R=[w, sc], W=[psmod])
                    if ch in rowsel:
                        vi, half = rowsel[ch]
                        for v in range(2):
                            pr = PS[1 + v]
                            for kc in range(8):
                                k.op('pe', lambda e, w=w, kc=kc, v=v, pr=pr: e.matmul(
                                    pr[:, :], lhsT=lbc[:, kc, v, :], rhs=w[:, kc, :], start=(kc == 0), stop=(kc == 7)),
                                    R=[w, lbc], W=[pr])
                            hs = slice(half * 512, (half + 1) * 512)
                            k.op('dve', lambda e, pr=pr, vi=vi, hs=hs: e.tensor_tensor(
                                out=rowt[:, hs], in0=pr[:, :], in1=bb[:, vi, hs], op=ALU.add), R=[pr, bb], W=[rowt])
                            if vi == 2:
                                k.op('dve', lambda e, hs=hs: e.scalar_tensor_tensor(
                                    out=rowt[:, hs], in0=rowt[:, hs], scalar=1.0, in1=gfb[:, hs], op0=ALU.add, op1=ALU.mult),
                                    R=[rowt, gfb], W=[rowt])
                            ld(bcd[v, vi, :, hs], rowt[:, hs], stream='bcst', R=[rowt], W=[('bcd', v, vi, half)])
                mc = modcol[:].rearrange("p a b -> p (a b)")
                k.op('dve', lambda e: e.tensor_tensor(out=mc, in0=psmod[:, 0:96], in1=bcol[:], op=ALU.add), R=[psmod, bcol], W=[modcol])
                k.op('dve', lambda e: e.scalar_tensor_tensor(out=A1c[:], in0=modcol[:, 8:16, :], scalar=1.0, in1=gc2[:, 0, :, :],
                                                            op0=ALU.add, op1=ALU.mult), R=[modcol, gc2], W=[A1c])
                k.barrier()
            if stop == 'p0':
                break

            with ExitStack() as pkv:
                KTA = sb("KTA%d" % l, [128, NT], BF16, pkv)
                KTB = sb("KTB%d" % l, [128, NT], BF16, pkv)
                VA = sb("VA%d" % l, [128, NTL, 2, 80], BF16, pkv)
                VB = sb("VB%d" % l, [128, NTL, 2, 80], BF16, pkv)
                KT = {'A': KTA, 'B': KTB}
                VV = {'A': VA, 'B': VB}
                k.op('pool', lambda e: e.memset(VA[:], 0.0), W=[VA])
                k.op('pool', lambda e: e.memset(VB[:], 0.0), W=[VB])
                k.op('pool', lambda e: e.memset(VA[:, :, :, 64:65], 1.0), W=[VA])
                k.op('pool', lambda e: e.memset(VB[:, :, :, 64:65], 1.0), W=[VB])

                def normrope(psp, psr, gi, Cb, Sb, out_ap, outres, n, tmp):
                    sq, rt, t1, t2 = tmp
                    k.op('act', lambda e: e.activation(out=sq[:, :n], in_=psp[:, :n], func=AF.Square), R=[psp], W=[sq])
                    pq = ps_get('nr', [6])
                    k.op('pe', lambda e: e.matmul(pq[:, :n], lhsT=blockones[:], rhs=sq[:, :n], start=True, stop=True),
                         R=[sq, blockones], W=[pq])
                    k.op('act', lambda e: e.activation(out=rt[:, :n], in_=pq[:, :n], func=AF.Sqrt, scale=1.0 / 64, bias=EPS),
                         R=[pq], W=[rt])
                    k.op('dve', lambda e: e.reciprocal(out=rt[:, :n], in_=rt[:, :n]), R=[rt], W=[rt])
                    k.op('dve', lambda e: e.scalar_tensor_tensor(out=t1[:, :n], in0=psp[:, :n], scalar=hg[:, gi:gi + 1], in1=Cb[:, :n],
                                                                op0=ALU.mult, op1=ALU.mult), R=[psp, Cb, hg], W=[t1])
                    k.op('dve', lambda e: e.scalar_tensor_tensor(out=t2[:, :n], in0=psr[:, :n], scalar=hg[:, gi + 1:gi + 2], in1=Sb[:, :n],
                                                                op0=ALU.mult, op1=ALU.mult), R=[psr, Sb, hg], W=[t2])
                    k.op('pool', lambda e: e.tensor_tensor(out=t1[:, :n], in0=t1[:, :n], in1=t2[:, :n], op=ALU.add), R=[t1, t2], W=[t1])
                    k.op('pool', lambda e: e.tensor_tensor(out=out_ap, in0=t1[:, :n], in1=rt[:, :n], op=ALU.mult), R=[t1, rt], W=[outres])

                with ExitStack() as p1:
                    W1 = sb("W1_%d" % l, [128, 8, 768], BF16, p1)
                    ld(W1[:], w1d[l], eng='pool', stream='wbig', W=[W1])
                    xt2 = [sb("xt%d_%d" % (l, i), [128, D], F32, p1) for i in range(2)]
                    xn2 = [sb("xn%d_%d" % (l, i), [128, D], F32, p1) for i in range(2)]
                    junk = sb("junk%d" % l, [128, D], F32, p1)
                    hTb2 = [sb("hTb%d_%d" % (l, i), [128, 8, 512], BF16, p1) for i in range(2)]
                    Cb2 = [sb("Cb%d_%d" % (l, i), [128, 512], F32, p1) for i in range(2)]
                    Sb2 = [sb("Sb%d_%d" % (l, i), [128, 512], F32, p1) for i in range(2)]
                    tmps = [[sb("nt%d_%d_%d" % (l, i, j), [128, 512], F32, p1) for j in range(4)] for i in range(2)]
                    ti_glob = 0
                    for bi, (t0, ntok, v) in enumerate(blocks if cut >= 5 else blocks[:1]):
                        if cut < 2:
                            break
                        hTb = hTb2[bi % 2]
                        Cb, Sb = Cb2[bi % 2], Sb2[bi % 2]
                        ld(Cb[:, :ntok], ropeCd[:, t0:t0 + ntok], stream='rope%d' % (bi % 2), W=[Cb])
                        ld(Sb[:, :ntok], ropeSd[:, t0:t0 + ntok], stream='rope%d' % (bi % 2), W=[Sb])
                        for tt in range(ntok // 128):
                            xt = xt2[ti_glob % 2]
                            xn = xn2[ti_glob % 2]
                            col = ti_glob % 2
                            r0 = t0 + tt * 128
                            ld(xt[:], xs[r0:r0 + 128, :], stream='xt%d' % (ti_glob % 2), R=[('xs', 'init')], W=[xt])
                            rstd_of(xt, junk, col)
                            k.op('dve', lambda e, xn=xn, xt=xt, col=col: e.tensor_scalar(out=xn[:], in0=xt[:], scalar1=rsd[:, col:col + 1],
                                                                                      scalar2=None, op0=ALU.mult),
                                 R=[xt, ('rsd', col)], W=[xn])
                            for half in range(2):
                                pt = ps_get('tr', [0, 1])
                                for q in range(4):
                                    kc = half * 4 + q
                                    k.op('pe', lambda e, pt=pt, q=q, kc=kc, xn=xn: e.transpose(pt[:, q * 128:(q + 1) * 128],
                                                                                               xn[:, kc * 128:(kc + 1) * 128], ident[:]),
                                         R=[xn, ident], W=[pt])
                                for q in range(4):
                                    kc = half * 4 + q
                                    eng = 'act' if q % 2 == 0 else 'dve'
                                    dst = hTb[:, kc, tt * 128:(tt + 1) * 128]
                                    if eng == 'act':
                                        k.op('act', lambda e, pt=pt, q=q, kc=kc, dst=dst: e.activation(
                                            out=dst, in_=pt[:, q * 128:(q + 1) * 128], func=AF.Identity,
                                            scale=A1c[:, kc, v:v + 1], bias=modcol[:, kc, v:v + 1]), R=[pt, A1c, modcol], W=[hTb])
                                    else:
                                        k.op('dve', lambda e, pt=pt, q=q, kc=kc, dst=dst: e.tensor_scalar(
                                            out=dst, in0=pt[:, q * 128:(q + 1) * 128], scalar1=A1c[:, kc, v:v + 1],
                                            scalar2=modcol[:, kc, v:v + 1], op0=ALU.mult, op1=ALU.add), R=[pt, A1c, modcol], W=[hTb])
                            ti_glob += 1
                        ld(hTd[:, :, t0:t0 + ntok], hTb[:, :, :ntok], stream='hst%d' % (bi % 2), R=[hTb], W=[('hTd', bi)])
                        for ai, (nm, c0, gi) in enumerate((('A', 0, 2), ('B', 128, 6)) if cut >= 3 else ()):
                            psp = ps_get('kp', [2, 3])
                            psr = ps_get('kr', [4, 5])
                            for kc in range(8):
                                k.op('pe', lambda e, kc=kc, psp=psp, c0=c0: e.matmul(psp[:, :ntok], lhsT=W1[:, kc, c0:c0 + 128],
                                                                                    rhs=hTb[:, kc, :ntok], start=(kc == 0), stop=(kc == 7)),
                                     R=[W1, hTb], W=[psp])
                            for kc in range(8):
                                k.op('pe', lambda e, kc=kc, psr=psr, c0=c0: e.matmul(psr[:, :ntok], lhsT=W1[:, kc, 256 + c0:256 + c0 + 128],
                                                                                    rhs=hTb[:, kc, :ntok], start=(kc == 0), stop=(kc == 7)),
                                     R=[W1, hTb], W=[psr])
                            normrope(psp, psr, gi, Cb, Sb, KT[nm][:, t0:t0 + ntok], KT[nm], ntok, tmps[ai])
                        for tt in range(ntok // 128 if cut >= 4 else 0):
                            ti = (t0 + tt * 128) // 128
                            pv = ps_get('kp', [2, 3]) if cut != 41 else ps_get('tr', [0, 1])
                            for kc in range(8):
                                k.op('pe', lambda e, kc=kc, pv=pv, tt=tt: e.matmul(pv[:, 0:256], lhsT=hTb[:, kc, tt * 128:(tt + 1) * 128],
                                                                                  rhs=W1[:, kc, 512:768], start=(kc == 0), stop=(kc == 7)),
                                     R=[W1, hTb], W=[pv])
                            for g_ in range(2):
                                k.op('act', lambda e, pv=pv, ti=ti, g_=g_: e.copy(out=VA[:, ti, g_, 0:64], in_=pv[:, g_ * 64:(g_ + 1) * 64]),
                                     R=[pv], W=[VA])
                                k.op('dve', lambda e, pv=pv, ti=ti, g_=g_: e.tensor_copy(out=VB[:, ti, g_, 0:64], in_=pv[:, 128 + g_ * 64:128 + (g_ + 1) * 64]),
                                     R=[pv], W=[VB])
                    k.barrier()
                if stop == 'p1':
                    if debug:
                        dk = nc.dram_tensor("dbgKTA", [128, NT], BF16, kind="ExternalOutput")
                        dkb = nc.dram_tensor("dbgKTB", [128, NT], BF16, kind="ExternalOutput")
                        dv = nc.dram_tensor("dbgVA", [128, NTL, 2, 80], BF16, kind="ExternalOutput")
                        ld(dk.ap(), KTA[:], stream='dbg', R=[KTA], W=['dbgo'])
                        ld(dkb.ap(), KTB[:], stream='dbg', R=[KTB], W=['dbgo'])
                        ld(dv.ap(), VA[:], stream='dbg', R=[VA], W=['dbgo'])
                        k.barrier()
                    break

                with ExitStack() as p2:
                    W2 = sb("W2_%d" % l, [128, 8, 2048], BF16, p2)
                    ld(W2[:], w2d[l], eng='pool', stream='wbig', W=[W2])
                    hTb2 = [sb("hTq%d_%d" % (l, i), [128, 8, 512], BF16, p2) for i in range(2)]
                    Cb2 = [sb("Cq%d_%d" % (l, i), [128, 512], F32, p2) for i in range(2)]
                    Sb2 = [sb("Sq%d_%d" % (l, i), [128, 512], F32, p2) for i in range(2)]
                    tmps = [[sb("qt%d_%d_%d" % (l, i, j), [128, 512], F32, p2) for j in range(4)] for i in range(2)]
                    QT = {'A': sb("QTA%d" % l, [128, 4, 512], BF16, p2), 'B': sb("QTB%d" % l, [128, 4, 512], BF16, p2)}
                    OT2 = {'A': [sb("OaT%d_%d" % (l, i), [128, 4, 512], BF16, p2) for i in range(2)],
                           'B': [sb("ObT%d_%d" % (l, i), [128, 4, 512], BF16, p2) for i in range(2)]}
                    PT = [sb("PT%d_%d" % (l, i), [128, 512], BF16, p2) for i in range(4)]
                    rec = [sb("rec%d_%d" % (l, i), [128, 512], F32, p2) for i in range(2)]
                    bcs = [sb("bcs%d_%d" % (l, i), [64, 512], F32, p2) for i in range(2)]
                    ptc = [0]
                    fin = [0]

                    def finalize(pso, n, dst_ap, dstres, sink_col=None):
                        r = rec[fin[0] % 2]
                        bc = bcs[fin[0] % 2]
                        fin[0] += 1
                        if sink_col is not None:
                            k.op('dve', lambda e: e.tensor_scalar(out=r[64:65, :n], in0=pso[64:65, :n], scalar1=esink[64:65, sink_col:sink_col + 1],
                                                                  scalar2=None, op0=ALU.add), R=[pso, esink], W=[r])
                            k.op('dve', lambda e: e.reciprocal(out=r[64:65, :n], in_=r[64:65, :n]), R=[r], W=[r])
                        else:
                            k.op('dve', lambda e: e.reciprocal(out=r[64:65, :n], in_=pso[64:65, :n]), R=[pso], W=[r])
                        pb = ps_get('nr', [6])
                        k.op('pe', lambda e: e.matmul(pb[0:64, :n], lhsT=ones32[64:65, 0:64], rhs=r[64:65, :n], start=True, stop=True),
                             R=[r, ones32], W=[pb])
                        k.op('act', lambda e: e.copy(out=bc[0:64, :n], in_=pb[0:64, :n]), R=[pb], W=[bc])
                        k.op('dve', lambda e: e.tensor_tensor(out=dst_ap, in0=pso[0:64, :n], in1=bc[0:64, :n], op=ALU.mult),
                             R=[pso, bc], W=[dstres])

                    qblocks = blocks if not last else blocks[:16]
                    for bi, (t0, ntok, v) in enumerate(qblocks):
                        latent = (v == 0)
                        hTb = hTb2[bi % 2]
                        Cb, Sb = Cb2[bi % 2], Sb2[bi % 2]
                        ld(hTb[:, :, :ntok], hTd[:, :, t0:t0 + ntok], stream='hq%d' % (bi % 2), R=[('hTd', bi)], W=[hTb])
                        ld(Cb[:, :ntok], ropeCd[:, t0:t0 + ntok], stream='rq%d' % (bi % 2), W=[Cb])
                        ld(Sb[:, :ntok], ropeSd[:, t0:t0 + ntok], stream='rq%d' % (bi % 2), W=[Sb])
                        qi = 0
                        for nm, base, gi in (('A', 0, 0), ('B', 1024, 4)):
                            for j in range(4):
                                psp = ps_get('kp', [2, 3])
                                psr = ps_get('kr', [4, 5])
                                for kc in range(8):
                                    k.op('pe', lambda e, kc=kc, psp=psp, c0=base + j * 128: e.matmul(
                                        psp[:, :ntok], lhsT=W2[:, kc, c0:c0 + 128], rhs=hTb[:, kc, :ntok], start=(kc == 0), stop=(kc == 7)),
                                        R=[W2, hTb], W=[psp])
                                for kc in range(8):
                                    k.op('pe', lambda e, kc=kc, psr=psr, c0=base + 512 + j * 128: e.matmul(
                                        psr[:, :ntok], lhsT=W2[:, kc, c0:c0 + 128], rhs=hTb[:, kc, :ntok], start=(kc == 0), stop=(kc == 7)),
                                        R=[W2, hTb], W=[psr])
                                normrope(psp, psr, gi, Cb, Sb, QT[nm][:, j, :ntok], (QT[nm], j), ntok, tmps[qi % 2])
                                qi += 1
                        OaT = OT2['A'][bi % 2]
                        ObT = OT2['B'][bi % 2]
                        kts = list(range(NTL)) if latent else [64, 65]
                        for j in range(4):
                            for hh in range(2):
                                rows = slice(64 * hh, 64 * hh + 64)
                                pso = ps_get('o', [4, 5])
                                LA = 2

                                def emit_s(kt):
                                    pss = ps_get('s', [0, 1, 2, 3])
                                    k.op('pe', lambda e: e.matmul(pss[:, :ntok], lhsT=KTA[rows, kt * 128:(kt + 1) * 128],
                                                                  rhs=QT['A'][rows, j, :ntok], start=True, stop=True),
                                         R=[KTA, (QT['A'], j)], W=[pss])
                                    return pss
                                Sq = [emit_s(kt) for kt in kts[:LA]]
                                for n_, kt in enumerate(kts):
                                    pss = Sq[n_]
                                    pt = PT[ptc[0] % 4]
                                    ptc[0] += 1
                                    k.op('act', lambda e, pss=pss, pt=pt: e.activation(out=pt[:, :ntok], in_=pss[:, :ntok], func=AF.Exp, scale=0.125),
                                         R=[pss], W=[pt])
                                    if n_ + LA < len(kts):
                                        Sq.append(emit_s(kts[n_ + LA]))
                                    k.op('pe', lambda e, pso=pso, pt=pt, kt=kt, n_=n_: e.matmul(
                                        pso[0:65, :ntok], lhsT=VA[:, kt, hh, 0:65], rhs=pt[:, :ntok], start=(n_ == 0), stop=(n_ == len(kts) - 1)),
                                        R=[VA, pt], W=[pso])
                                finalize(pso, ntok, OaT[rows, j, :ntok], (OaT, j, hh))
                        for j in range(4):
                            for hh in range(2):
                                rows = slice(64 * hh, 64 * hh + 64)
                                head = 4 * hh + j
                                pso = ps_get('o', [4, 5])
                                for qt in range(ntok // 128):
                                    cols = slice(qt * 128, (qt + 1) * 128)
                                    I = (t0 + qt * 128) // 128
                                    kl = [(64, None), (65, None)]
                                    if latent:
                                        if I - 1 >= 0:
                                            kl.append((I - 1, maskP))
                                        kl.append((I, None))
                                        if I + 1 < 64:
                                            kl.append((I + 1, maskN))
                                    LA = 2

                                    def emit_sb(kt):
                                        pss = ps_get('s', [0, 1, 2, 3])
                                        k.op('pe', lambda e: e.matmul(pss[:, 0:128], lhsT=KTB[rows, kt * 128:(kt + 1) * 128],
                                                                      rhs=QT['B'][rows, j, cols], start=True, stop=True),
                                             R=[KTB, (QT['B'], j)], W=[pss])
                                        return pss
                                    Sq = [emit_sb(kt) for (kt, _m) in kl[:LA]]
                                    for n_, (kt, msk) in enumerate(kl):
                                        pss = Sq[n_]
                                        pt = PT[ptc[0] % 4]
                                        ptc[0] += 1
                                        k.op('act', lambda e, pss=pss, pt=pt: e.activation(out=pt[:, 0:128], in_=pss[:, 0:128], func=AF.Exp, scale=0.125),
                                             R=[pss], W=[pt])
                                        if n_ + LA < len(kl):
                                            Sq.append(emit_sb(kl[n_ + LA][0]))
                                        if msk is not None:
                                            k.op('pool', lambda e, pt=pt, msk=msk: e.tensor_tensor(out=pt[:, 0:128], in0=pt[:, 0:128], in1=msk[:],
                                                                                                    op=ALU.mult), R=[pt, msk], W=[pt])
                                        k.op('pe', lambda e, pso=pso, pt=pt, kt=kt, n_=n_, kl=kl: e.matmul(
                                            pso[0:65, cols], lhsT=VB[:, kt, hh, 0:65], rhs=pt[:, 0:128], start=(n_ == 0), stop=(n_ == len(kl) - 1)),
                                            R=[VB, pt], W=[pso])
                                finalize(pso, ntok, ObT[rows, j, :ntok], (ObT, j, hh), sink_col=head)
                        ld(oaTd[:, :, t0:t0 + ntok], OaT[:, :, :ntok], stream='ost%d' % (bi % 2), R=[(OaT, j, hh) for j in range(4) for hh in range(2)],
                           W=[('oaTd', bi)])
                        ld(obTd[:, :, t0:t0 + ntok], ObT[:, :, :ntok], stream='ost%d' % (bi % 2), R=[(ObT, j, hh) for j in range(4) for hh in range(2)],
                           W=[('obTd', bi)])
                    k.barrier()
            if stop == 'p2':
                break
            mblocks = blocks if not last else blocks[:16]

            def uoff(t0):
                return t0 + 1 if t0 < NL else t0 + 3

            with ExitStack() as p3a:
                Wcx = sb("Wcx%d" % l, [128, 8, 1024], BF16, p3a)
                ld(Wcx[:], wcxd[l], eng='pool', stream='wbig', W=[Wcx])
                hTb2 = [sb("hTu%d_%d" % (l, i), [128, 8, 512], BF16, p3a) for i in range(2)]
                ub2 = [sb("ub%d_%d" % (l, i), [128, 4, 512], F32, p3a) for i in range(2)]
                cs2 = [sb("cs%d_%d" % (l, i), [128, 512], F32, p3a) for i in range(2)]
                zt = sb("zt%d" % l, [128, 4, 2], F32, p3a)
                k.op('dve', lambda e: e.memset(zt[:], 0.0), W=[zt])
                ld(uTd[:, :, 0:1], zt[:, :, 0:1], stream='zp', R=[zt], W=['uzp'], slow=True)
                ld(uTd[:, :, NL + 1:NL + 3], zt[:, :, 0:2], stream='zp', R=[zt], W=['uzp'], slow=True)
                ld(uTd[:, :, NT + 3:NT + 4], zt[:, :, 0:1], stream='zp', R=[zt], W=['uzp'], slow=True)
                cc = 0
                for bi, (t0, ntok, v) in enumerate(mblocks):
                    hTb = hTb2[bi % 2]
                    ub = ub2[bi % 2]
                    ld(hTb[:, :, :ntok], hTd[:, :, t0:t0 + ntok], stream='hu%d' % (bi % 2), W=[hTb])
                    for c in range(4):
                        psc = ps_get('kp', [2, 3])
                        psx = ps_get('kr', [4, 5])
                        cs = cs2[cc % 2]
                        cc += 1
                        for kc in range(8):
                            k.op('pe', lambda e, kc=kc, psc=psc, c=c: e.matmul(psc[:, :ntok], lhsT=Wcx[:, kc, c * 128:(c + 1) * 128],
                                                                              rhs=hTb[:, kc, :ntok], start=(kc == 0), stop=(kc == 7)),
                                 R=[Wcx, hTb], W=[psc])
                        for kc in range(8):
                            k.op('pe', lambda e, kc=kc, psx=psx, c=c: e.matmul(psx[:, :ntok], lhsT=Wcx[:, kc, 512 + c * 128:512 + (c + 1) * 128],
                                                                              rhs=hTb[:, kc, :ntok], start=(kc == 0), stop=(kc == 7)),
                                 R=[Wcx, hTb], W=[psx])
                        k.op('act', lambda e, cs=cs, psc=psc: e.copy(out=cs[:, :ntok], in_=psc[:, :ntok]), R=[psc], W=[cs])
                        k.op('dve', lambda e, cs=cs, psx=psx, c=c, ub=ub: e.tensor_tensor(out=ub[:, c, :ntok], in0=psx[:, :ntok], in1=cs[:, :ntok],
                                                                                       op=ALU.mult), R=[psx, cs], W=[(ub, c)])
                    o0 = uoff(t0)
                    ld(uTd[:, :, o0:o0 + ntok], ub[:, :, :ntok], stream='ust%d' % (bi % 2), R=[(ub, c) for c in range(4)], W=[('uTd', bi)])
                k.barrier()

            with ExitStack() as p3b:
                W3 = sb("W3_%d" % l, [128, 8, 3584], BF16, p3b)
                Wbr = sb("Wbr%d" % l, [128, 12, D], BF16, p3b)
                ld(W3[:], w3d[l], eng='pool', stream='wbig', W=[W3])
                ld(Wbr[:], wbrd[l], eng='pool', stream='wbig', W=[Wbr])
                hTb2 = [sb("hTm%d_%d" % (l, i), [128, 8, 512], BF16, p3b) for i in range(2)]
                Oa2 = [sb("Oam%d_%d" % (l, i), [128, 4, 512], BF16, p3b) for i in range(2)]
                Ob2 = [sb("Obm%d_%d" % (l, i), [128, 4, 512], BF16, p3b) for i in range(2)]
                u2 = [sb("um%d_%d" % (l, i), [128, 4, 514], F32, p3b) for i in range(2)]
                OcT = sb("OcT%d" % l, [128, 4, 512], BF16, p3b)
                mT2 = [sb("mT%d_%d" % (l, i), [128, 8, 512], BF16, p3b) for i in range(2)]
                cva = [sb("cva%d_%d" % (l, i), [128, 512], F32, p3b) for i in range(2)]
                cvb = [sb("cvb%d_%d" % (l, i), [128, 512], F32, p3b) for i in range(2)]
                sg2 = [sb("sg%d_%d" % (l, i), [128, 512], F32, p3b) for i in range(3)]
                acc2 = [sb("acc%d_%d" % (l, i), [128, 512], F32, p3b) for i in range(2)]
                tm2 = [sb("tm%d_%d" % (l, i), [128, 512], F32, p3b) for i in range(2)]
                cnt = [0, 0, 0]
                for bi, (t0, ntok, v) in enumerate(mblocks):
                    hTb, Oa, Ob, ub, mT = hTb2[bi % 2], Oa2[bi % 2], Ob2[bi % 2], u2[bi % 2], mT2[bi % 2]
                    o0 = uoff(t0)
                    ld(hTb[:, :, :ntok], hTd[:, :, t0:t0 + ntok], stream='hm%d' % (bi % 2), W=[hTb])
                    ld(Oa[:, :, :ntok], oaTd[:, :, t0:t0 + ntok], stream='hm%d' % (bi % 2), W=[Oa])
                    ld(Ob[:, :, :ntok], obTd[:, :, t0:t0 + ntok], stream='hm%d' % (bi % 2), W=[Ob])
                    ld(ub[:, :, :ntok + 2], uTd[:, :, o0 - 1:o0 + ntok + 1], stream='hm%d' % (bi % 2), W=[ub])
                    for c in range(4):
                        psb = ps_get('kp', [2, 3])
                        for kc in range(8):
                            k.op('pe', lambda e, kc=kc, psb=psb, c=c: e.matmul(psb[:, :ntok], lhsT=W3[:, kc, c * 128:(c + 1) * 128],
                                                                              rhs=hTb[:, kc, :ntok], start=(kc == 0), stop=(kc == 7)),
                                 R=[W3, hTb], W=[psb])
                        a = cva[cnt[0] % 2]
                        cnt[0] += 1
                        k.op('pool', lambda e, a=a, c=c: e.tensor_scalar(out=a[:, :ntok], in0=ub[:, c, 0:ntok], scalar1=cw[:, c, 0:1], scalar2=None,
                                                                        op0=ALU.mult), R=[ub, cw], W=[a])
                        a2 = cvb[cnt[0] % 2]
                        for tap in (1, 2):
                            k.op('pool', lambda e, a2=a2, c=c, tap=tap: e.tensor_scalar(out=a2[:, :ntok], in0=ub[:, c, tap:ntok + tap], scalar1=cw[:, c, tap:tap + 1],
                                                                                       scalar2=None, op0=ALU.mult), R=[ub, cw], W=[a2])
                            k.op('pool', lambda e, a=a, a2=a2: e.tensor_tensor(out=a[:, :ntok], in0=a[:, :ntok], in1=a2[:, :ntok], op=ALU.add), R=[a, a2], W=[a])
                        k.op('dve', lambda e, a=a, c=c, psb=psb: e.tensor_tensor(out=OcT[:, c, :ntok], in0=psb[:, :ntok], in1=a[:, :ntok], op=ALU.mult),
                             R=[psb, a], W=[(OcT, c)])
                    Obr = [Oa, Ob, OcT]
                    for m in range(8):
                        acc = acc2[m % 2]
                        for br in range(3):
                            psg = ps_get('s', [0, 1])
                            psr = ps_get('kr', [4, 5])
                            sg = sg2[cnt[1] % 3]
                            cnt[1] += 1
                            c0 = 512 + br * 1024 + m * 128
                            for kc in range(8):
                                k.op('pe', lambda e, kc=kc, psg=psg, c0=c0: e.matmul(psg[:, :ntok], lhsT=W3[:, kc, c0:c0 + 128],
                                                                                    rhs=hTb[:, kc, :ntok], start=(kc == 0), stop=(kc == 7)),
                                     R=[W3, hTb], W=[psg])
                            k.op('act', lambda e, sg=sg, psg=psg: e.activation(out=sg[:, :ntok], in_=psg[:, :ntok], func=AF.Sigmoid), R=[psg], W=[sg])
                            Rr = [Wbr] + ([Oa] if br == 0 else [Ob] if br == 1 else [(OcT, c) for c in range(4)])
                            for c in range(4):
                                k.op('pe', lambda e, c=c, psr=psr, br=br, m=m: e.matmul(psr[:, :ntok], lhsT=Wbr[:, br * 4 + c, m * 128:(m + 1) * 128],
                                                                                       rhs=Obr[br][:, c, :ntok], start=(c == 0), stop=(c == 3)),
                                     R=Rr, W=[psr])
                            if br == 0:
                                k.op('dve', lambda e, psr=psr, sg=sg, acc=acc: e.tensor_tensor(out=acc[:, :ntok], in0=psr[:, :ntok], in1=sg[:, :ntok],
                                                                                            op=ALU.mult), R=[psr, sg], W=[acc])
                            else:
                                tm = tm2[cnt[2] % 2]
                                cnt[2] += 1
                                k.op('dve', lambda e, psr=psr, sg=sg, tm=tm: e.tensor_tensor(out=tm[:, :ntok], in0=psr[:, :ntok], in1=sg[:, :ntok],
                                                                                          op=ALU.mult), R=[psr, sg], W=[tm])
                                if br == 1:
                                    k.op('pool', lambda e, tm=tm, acc=acc: e.tensor_tensor(out=acc[:, :ntok], in0=acc[:, :ntok], in1=tm[:, :ntok],
                                                                                        op=ALU.add), R=[acc, tm], W=[acc])
                                else:
                                    k.op('pool', lambda e, tm=tm, acc=acc, m=m, mT=mT: e.tensor_tensor(out=mT[:, m, :ntok], in0=acc[:, :ntok], in1=tm[:, :ntok],
                                                                                                    op=ALU.add), R=[acc, tm], W=[(mT, m)])
                    ld(mTd[:, :, t0:t0 + ntok], mT[:, :, :ntok], stream='mst%d' % (bi % 2), R=[(mT, m) for m in range(8)], W=[('mTd', bi)])
                k.barrier()
            if stop == 'p3b':
                break

            pmoe = ExitStack()
            aff = sb("aff%d" % l, [128, NTL, NE], F32, pmoe)
            with ExitStack() as p3c:
                Wout = sb("Wout%d" % l, [128, 8, D], BF16, p3c)
                ld(Wout[:], woutd[l], eng='pool', stream='wbig', W=[Wout])
                Wr = sb("Wr%d" % l, [128, 8, NE], F32, p3c)
                ld(Wr[:], wrd[l], W=[Wr])
                bct = {}
                for vv in range(2):
                    for vi, nm in ((0, 'gt1'), (1, 'sh2'), (2, 'A2')):
                        t_ = sb("bc_%s%d_%d" % (nm, vv, l), [128, D], F32, p3c)
                        ld(t_[:], bcd[vv, vi], W=[t_])
                        bct[(vv, nm)] = t_
                mT2 = [sb("mTo%d_%d" % (l, i), [128, 8, 512], BF16, p3c) for i in range(2)]
                xt2 = [sb("xo%d_%d" % (l, i), [128, D], F32, p3c) for i in range(2)]
                xw2 = [sb("xw%d_%d" % (l, i), [128, D], F32, p3c) for i in range(2)]
                h22 = [sb("h2%d_%d" % (l, i), [128, D], F32, p3c) for i in range(2)]
                h2b2 = [sb("h2b%d_%d" % (l, i), [128, D], BF16, p3c) for i in range(2)]
                junk = sb("junk3%d" % l, [128, D], F32, p3c)
                h2T = [sb("h2T%d_%d" % (l, i), [128, 8, 128], F32, p3c) for i in range(2)]
                ex2 = [sb("ex%d_%d" % (l, i), [128, NE], F32, p3c) for i in range(2)]
                sm = sb("sm%d" % l, [128, 8], F32, p3c)
                tg = 0
                for bi, (t0, ntok, v) in enumerate(mblocks):
                    mT = mT2[bi % 2]
                    ld(mT[:, :, :ntok], mTd[:, :, t0:t0 + ntok], stream='mo%d' % (bi % 2), W=[mT])
                    for tt in range(ntok // 128):
                        i2 = tg % 2
                        tg += 1
                        xt, xw, h2, h2b, hT_, ex = xt2[i2], xw2[i2], h22[i2], h2b2[i2], h2T[i2], ex2[i2]
                        r0 = t0 + tt * 128
                        ti = r0 // 128
                        ld(xt[:], xs[r0:r0 + 128, :], stream='xo%d' % i2, W=[xt])
                        for half in range(2):
                            hs = slice(half * 512, (half + 1) * 512)
                            po = ps_get('kp', [2, 3])
                            for m in range(8):
                                k.op('pe', lambda e, m=m, po=po, hs=hs, tt=tt: e.matmul(po[:, :], lhsT=mT[:, m, tt * 128:(tt + 1) * 128], rhs=Wout[:, m, hs],
                                                                                       start=(m == 0), stop=(m == 7)), R=[Wout, mT], W=[po])
                            k.op('dve', lambda e, po=po, hs=hs, xw=xw: e.tensor_tensor(out=xw[:, hs], in0=po[:, :], in1=bct[(v, 'gt1')][:, hs], op=ALU.mult),
                                 R=[po, bct[(v, 'gt1')]], W=[(xw, half)])
                            k.op('pool', lambda e, hs=hs, xw=xw, xt=xt: e.tensor_tensor(out=xw[:, hs], in0=xw[:, hs], in1=xt[:, hs], op=ALU.add),
                                 R=[(xw, half), xt], W=[(xw, half)])
                        ld(xs[r0:r0 + 128, :], xw[:], stream='xst%d' % i2, R=[(xw, 0), (xw, 1)], W=[('xs', ti)])
                        col = 2 + i2
                        k.op('pool', lambda e, col=col: e.memset(ss[:, col:col + 1], 0.0), W=[('ss', col)])
                        k.op('act', lambda e, xw=xw, col=col: e.activation(out=junk[:], in_=xw[:], func=AF.Square, accum_out=ss[:, col:col + 1]),
                             R=[(xw, 0), (xw, 1)], W=[junk, ('ss', col)])
                        k.op('act', lambda e, col=col: e.activation(out=rsd[:, col:col + 1], in_=ss[:, col:col + 1], func=AF.Sqrt, scale=1.0 / D, bias=EPS),
                             R=[('ss', col)], W=[('rsd', col)])
                        k.op('dve', lambda e, col=col: e.reciprocal(out=rsd[:, col:col + 1], in_=rsd[:, col:col + 1]), R=[('rsd', col)], W=[('rsd', col)])
                        k.op('dve', lambda e, xw=xw, h2=h2, col=col: e.scalar_tensor_tensor(out=h2[:], in0=xw[:], scalar=rsd[:, col:col + 1], in1=bct[(v, 'A2')][:],
                                                                                         op0=ALU.mult, op1=ALU.mult),
                             R=[(xw, 0), (xw, 1), ('rsd', col), bct[(v, 'A2')]], W=[h2])
                        k.op('pool', lambda e, h2=h2: e.tensor_tensor(out=h2[:], in0=h2[:], in1=bct[(v, 'sh2')][:], op=ALU.add), R=[h2, bct[(v, 'sh2')]], W=[h2])
                        k.op('act', lambda e, h2=h2, h2b=h2b: e.copy(out=h2b[:], in_=h2[:]), R=[h2], W=[h2b])
                        ld(h2d[r0:r0 + 128, :], h2b[:], stream='h2st%d' % i2, R=[h2b], W=[('h2d', ti)])
                        for half in range(2):
                            pt = ps_get('tr', [0, 1])
                            for q in range(4):
                                kc = half * 4 + q
                                k.op('pe', lambda e, pt=pt, q=q, kc=kc, h2=h2: e.transpose(pt[:, q * 128:(q + 1) * 128], h2[:, kc * 128:(kc + 1) * 128], ident[:]),
                                     R=[h2, ident], W=[pt])
                            eng = 'act' if half == 0 else 'dve'
                            src = pt[:, :]
                            dst = hT_[:].rearrange("p a b -> p (a b)")[:, half * 512:(half + 1) * 512]
                            if eng == 'act':
                                k.op('act', lambda e, dst=dst, src=src: e.copy(out=dst, in_=src), R=[pt], W=[(hT_, half)])
                            else:
                                k.op('dve', lambda e, dst=dst, src=src: e.tensor_copy(out=dst, in_=src), R=[pt], W=[(hT_, half)])
                        pl = ps_get('nr', [6])
                        for kc in range(8):
                            k.op('pe', lambda e, kc=kc, pl=pl, hT_=hT_: e.matmul(pl[:, 0:NE], lhsT=hT_[:, kc, :], rhs=Wr[:, kc, :], start=(kc == 0), stop=(kc == 7)),
                                 R=[(hT_, 0), (hT_, 1), Wr], W=[pl])
                        c2 = 4 * i2
                        k.op('dve', lambda e, pl=pl, c2=c2: e.tensor_reduce(out=sm[:, c2:c2 + 1], in_=pl[:, 0:NE], axis=AX.X, op=ALU.max), R=[pl], W=[('sm', i2)])
                        k.op('dve', lambda e, c2=c2: e.tensor_scalar(out=sm[:, c2 + 1:c2 + 2], in0=sm[:, c2:c2 + 1], scalar1=-1.0, scalar2=None, op0=ALU.mult),
                             R=[('sm', i2)], W=[('sm', i2)])
                        k.op('pool', lambda e, c2=c2: e.memset(sm[:, c2 + 2:c2 + 3], 0.0), R=[('sm', i2)], W=[('sm', i2)])
                        k.op('act', lambda e, pl=pl, ex=ex, c2=c2: e.activation(out=ex[:], in_=pl[:, 0:NE], func=AF.Exp, bias=sm[:, c2 + 1:c2 + 2],
                                                                              accum_out=sm[:, c2 + 2:c2 + 3]), R=[pl, ('sm', i2)], W=[ex, ('sm', i2)])
                        k.op('dve', lambda e, c2=c2: e.reciprocal(out=sm[:, c2 + 3:c2 + 4], in_=sm[:, c2 + 2:c2 + 3]), R=[('sm', i2)], W=[('sm', i2)])
                        k.op('dve', lambda e, ex=ex, ti=ti, c2=c2: e.tensor_scalar(out=aff[:, ti, :], in0=ex[:], scalar1=sm[:, c2 + 3:c2 + 4], scalar2=None, op0=ALU.mult),
                             R=[ex, ('sm', i2)], W=[(aff, ti)])
                        ld(affd[r0:r0 + 128, :], aff[:, ti, :], stream='afst%d' % i2, R=[(aff, ti)], W=[('affd', ti)])
                k.barrier()
            if stop == 'p3c':
                pmoe.close()
                break

            with pmoe:
                gt2 = [sb("gt2_%d_%d" % (l, vv), [128, D], F32, pmoe) for vv in range(2)]
                for vv in range(2):
                    ld(gt2[vv][:], bcd[vv, 3], W=[gt2[vv]])
                NSC = 9
                NJ = NE * NSC
                idx_all = sb("idx_all%d" % l, [128, NJ], I32, pmoe)
                gate_all = sb("gate_all%d" % l, [128, NJ], F32, pmoe)
                k.op('pool', lambda e: e.memset(idx_all[:], 1 << 20), W=[idx_all])
                k.op('pool', lambda e: e.memset(gate_all[:], 0.0), W=[gate_all])
                sets = [(0, 64, 1024, 0)] + ([] if last else [(64, 2, 32, 1)])
                nst = 8 if last else 9
                with ExitStack() as pth:
                    iota = sb("iota%d" % l, [128, 1024], F32, pth)
                    ld(iota[:], iotad.ap(), W=[iota])
                    comb = sb("comb%d" % l, [128, NTL, NE, 5], BF16, pth)
                    ld(comb[:], combd.ap(), eng='pool', W=[comb])
                    posm = sb("posm%d" % l, [128, NTL, NE], F32, pth)
                    lo = sb("lo%d" % l, [128, NE], F32, pth)
                    hi = sb("hi%d" % l, [128, NE], F32, pth)
                    mid = sb("mid%d" % l, [128, NE], F32, pth)
                    ge = sb("ge%d" % l, [128, NE], F32, pth)
                    g2 = sb("g2%d" % l, [128, NE], F32, pth)
                    cntp = sb("cntp%d" % l, [128, NE], F32, pth)
                    cmp_ = sb("cmp%d" % l, [128, 64, NE], F32, pth)
                    maskb = sb("maskb%d" % l, [128, 64, NE], BF16, pth)
                    inc = [sb("inc%d_%d" % (l, i), [128, 64, NE], F32, pth) for i in range(2)]
                    tot = sb("tot%d" % l, [128, 64, NE], F32, pth)
                    r1 = sb("r1_%d" % l, [128, NTL, NE], F32, pth)
                    r2 = sb("r2_%d" % l, [128, NTL, NE], F32, pth)
                    k.op('pool', lambda e: e.tensor_copy(out=comb[:, :, :, 2], in_=aff[:]), R=[aff], W=[comb])
                    k.op('dve', lambda e: e.tensor_tensor(out=r1[:], in0=aff[:], in1=comb[:, :, :, 2], op=ALU.subtract), R=[aff, comb], W=[r1])
                    k.op('pool', lambda e: e.tensor_copy(out=comb[:, :, :, 3], in_=r1[:]), R=[r1], W=[comb])
                    k.op('dve', lambda e: e.tensor_tensor(out=r2[:], in0=r1[:], in1=comb[:, :, :, 3], op=ALU.subtract), R=[r1, comb], W=[r2])
                    k.op('pool', lambda e: e.tensor_copy(out=comb[:, :, :, 4], in_=r2[:]), R=[r2], W=[comb])
                    for (ti0, T, cap, vv) in sets:
                        affs = aff[:, ti0:ti0 + T, :]
                        k.op('dve', lambda e: e.memset(lo[:], 0.0), W=[lo])
                        k.op('dve', lambda e: e.memset(hi[:], 1.0), W=[hi])
                        for it in range(32):
                            k.op('dve', lambda e: e.tensor_tensor(out=mid[:], in0=lo[:], in1=hi[:], op=ALU.add), R=[lo, hi], W=[mid])
                            k.op('dve', lambda e: e.tensor_scalar(out=mid[:], in0=mid[:], scalar1=0.5, scalar2=None, op0=ALU.mult), R=[mid], W=[mid])
                            k.op('dve', lambda e, T=T, affs=affs: e.tensor_tensor(out=cmp_[:, :T, :], in0=affs, in1=mid[:].unsqueeze(1).to_broadcast([128, T, NE]),
                                                                                 op=ALU.is_ge), R=[aff, mid], W=[cmp_])
                            k.op('dve', lambda e, T=T: e.tensor_reduce(out=cntp[:], in_=cmp_[:, :T, :].rearrange("p t e -> p e t"), axis=AX.X, op=ALU.add),
                                 R=[cmp_], W=[cntp])
                            pc = ps_get('nr', [6])
                            k.op('pe', lambda e, pc=pc: e.matmul(pc[:, 0:NE], lhsT=ones32[:], rhs=cntp[:], start=True, stop=True), R=[ones32, cntp], W=[pc])
                            k.op('dve', lambda e, pc=pc, cap=cap: e.tensor_scalar(out=ge[:], in0=pc[:, 0:NE], scalar1=float(cap) - 0.5, scalar2=None, op0=ALU.is_ge),
                                 R=[pc], W=[ge])
                            k.op('dve', lambda e: e.tensor_tensor(out=g2[:], in0=ge[:], in1=mid[:], op=ALU.mult), R=[ge, mid], W=[g2])
                            k.op('dve', lambda e: e.tensor_tensor(out=lo[:], in0=lo[:], in1=g2[:], op=ALU.max), R=[lo, g2], W=[lo])
                            k.op('dve', lambda e: e.scalar_tensor_tensor(out=g2[:], in0=ge[:], scalar=2.0, in1=mid[:], op0=ALU.mult, op1=ALU.add),
                                 R=[ge, mid, g2], W=[g2])
                            k.op('dve', lambda e: e.tensor_tensor(out=hi[:], in0=hi[:], in1=g2[:], op=ALU.min), R=[hi, g2], W=[hi])
                        k.op('dve', lambda e, T=T, affs=affs: e.tensor_tensor(out=cmp_[:, :T, :], in0=affs, in1=lo[:].unsqueeze(1).to_broadcast([128, T, NE]),
                                                                             op=ALU.is_ge), R=[aff, lo], W=[cmp_])
                        k.op('pool', lambda e, T=T: e.tensor_copy(out=maskb[:, :T, :], in_=cmp_[:, :T, :]), R=[cmp_], W=[maskb])
                        ncol = T * NE
                        mb = maskb[:].rearrange("p t e -> p (t e)")
                        totf = tot[:].rearrange("p t e -> p (t e)")
                        pp = [PS[2], PS[3]]
                        pq = [PS[4], PS[5]]
                        nhf = (ncol + 511) // 512
                        for hf in range(nhf):
                            n_ = min(512, ncol - hf * 512)
                            k.op('pe', lambda e, hf=hf, n_=n_: e.matmul(pp[hf][:, :n_], lhsT=triL[:], rhs=mb[:, hf * 512:hf * 512 + n_], start=True, stop=True),
                                 R=[triL, maskb], W=[pp[hf]])
                            k.op('pe', lambda e, hf=hf, n_=n_: e.matmul(pq[hf][:, :n_], lhsT=onesb[:], rhs=mb[:, hf * 512:hf * 512 + n_], start=True, stop=True),
                                 R=[onesb, maskb], W=[pq[hf]])
                            k.op('act', lambda e, hf=hf, n_=n_: e.copy(out=totf[:, hf * 512:hf * 512 + n_], in_=pq[hf][:, :n_]), R=[pq[hf]], W=[tot])
                        k.op('pool', lambda e, T=T: e.tensor_copy(out=inc[0][:, :T, :], in_=tot[:, :T, :]), R=[tot], W=[inc[0]])
                        cur = 0
                        s_ = 1
                        while s_ < T:
                            a_, b_ = inc[cur], inc[1 - cur]
                            k.op('dve', lambda e, a_=a_, b_=b_, s_=s_, T=T: e.tensor_tensor(out=b_[:, s_:T, :], in0=a_[:, s_:T, :], in1=a_[:, 0:T - s_, :], op=ALU.add),
                                 R=[a_], W=[b_])
                            k.op('pool', lambda e, a_=a_, b_=b_, s_=s_: e.tensor_copy(out=b_[:, 0:s_, :], in_=a_[:, 0:s_, :]), R=[a_], W=[b_])
                            cur = 1 - cur
                            s_ *= 2
                        incf = inc[cur]
                        oth = inc[1 - cur]
                        othf = oth[:].rearrange("p t e -> p (t e)")
                        k.op('dve', lambda e, T=T: e.tensor_tensor(out=oth[:, :T, :], in0=incf[:, :T, :], in1=tot[:, :T, :], op=ALU.subtract), R=[incf, tot], W=[oth])
                        for hf in range(nhf):
                            n_ = min(512, ncol - hf * 512)
                            k.op('dve', lambda e, hf=hf, n_=n_: e.tensor_tensor(out=othf[:, hf * 512:hf * 512 + n_], in0=othf[:, hf * 512:hf * 512 + n_],
                                                                              in1=pp[hf][:, :n_], op=ALU.add), R=[oth, pp[hf]], W=[oth])
                        k.op('dve', lambda e, T=T, ti0=ti0: e.scalar_tensor_tensor(out=posm[:, ti0:ti0 + T, :], in0=oth[:, :T, :], scalar=1.0, in1=cmp_[:, :T, :],
                                                                                  op0=ALU.add, op1=ALU.mult), R=[oth, cmp_], W=[posm])
                        k.op('dve', lambda e, T=T, ti0=ti0: e.tensor_scalar(out=posm[:, ti0:ti0 + T, :], in0=posm[:, ti0:ti0 + T, :], scalar1=-1.0, scalar2=None,
                                                                           op0=ALU.add), R=[posm], W=[posm])
                    k.barrier()
                    if debug and stop == 'p4a':
                        dpos = nc.dram_tensor("dbgposm", [128, NTL, NE], F32, kind="ExternalOutput")
                        ld(dpos.ap(), posm[:], R=[posm], W=['dbgo'])
                        k.barrier()
                        break

                    Sb_ = [sb("Sone%d_%d" % (l, i), [128, 1024], BF16, pth) for i in range(3)]
                    rows5 = sb("rows5_%d" % l, [8, 1056], F32, pth)
                    t5 = sb("t5_%d" % l, [128, 48], F32, pth)
                    idxf = sb("idxf%d" % l, [128, NSC], F32, pth)
                    g1 = sb("g1_%d" % l, [128, NSC], F32, pth)
                    rr = 0
                    for e_ in range(NE):
                        for (ti0, T, cap, vv) in sets:
                            off = 0 if vv == 0 else 1024
                            nh = (cap + 511) // 512
                            pi = [PS[0], PS[1]]
                            for i_ in range(T):
                                S_ = Sb_[rr % 3]
                                rr += 1
                                ti = ti0 + i_
                                k.op('dve', lambda e, S_=S_, ti=ti, e_=e_, cap=cap: e.tensor_scalar(out=S_[:, :cap], in0=iota[:, :cap], scalar1=posm[:, ti, e_:e_ + 1],
                                                                                                   scalar2=None, op0=ALU.is_equal), R=[iota, posm], W=[S_])
                                for hf in range(nh):
                                    n_ = min(512, cap - hf * 512)
                                    k.op('pe', lambda e, S_=S_, ti=ti, hf=hf, n_=n_, i_=i_, T=T, e_=e_: e.matmul(pi[hf][0:5, :n_], lhsT=comb[:, ti, e_, :],
                                                                                                              rhs=S_[:, hf * 512:hf * 512 + n_],
                                                                                                              start=(i_ == 0), stop=(i_ == T - 1)), R=[comb, S_], W=[pi[hf]])
                            for hf in range(nh):
                                n_ = min(512, cap - hf * 512)
                                k.op('act', lambda e, hf=hf, n_=n_, off=off: e.copy(out=rows5[0:5, off + hf * 512:off + hf * 512 + n_], in_=pi[hf][0:5, :n_]),
                                     R=[pi[hf]], W=[rows5])
                        ptx = ps_get('nr', [6])
                        for s in range(nst):
                            prow = 128 if s < 8 else 32
                            k.op('pe', lambda e, s=s, prow=prow: e.transpose(ptx[0:prow, 5 * s:5 * s + 5], rows5[0:5, s * 128:s * 128 + prow], ident[0:5, 0:5]),
                                 R=[rows5, ident], W=[ptx])
                        k.op('act', lambda e: e.copy(out=t5[:, 0:40], in_=ptx[:, 0:40]), R=[ptx], W=[t5])
                        if nst == 9:
                            k.op('act', lambda e: e.copy(out=t5[0:32, 40:45], in_=ptx[0:32, 40:45]), R=[ptx], W=[t5])
                        for (c0, c1, prow) in ((0, 8, 128),) + (((8, 9, 32),) if nst == 9 else ()):
                            n5 = slice(5 * c0, 5 * c1, 5)
                            k.op('dve', lambda e, c0=c0, c1=c1, prow=prow: e.scalar_tensor_tensor(
                                out=idxf[0:prow, c0:c1], in0=t5[0:prow, 5 * c0:5 * c1:5], scalar=128.0, in1=t5[0:prow, 5 * c0 + 1:5 * c1:5], op0=ALU.mult, op1=ALU.add),
                                R=[t5], W=[idxf])
                            k.op('dve', lambda e, c0=c0, c1=c1, prow=prow, e_=e_: e.tensor_copy(out=idx_all[0:prow, e_ * NSC + c0:e_ * NSC + c1], in_=idxf[0:prow, c0:c1]),
                                 R=[idxf], W=[idx_all])
                            k.op('dve', lambda e, c0=c0, c1=c1, prow=prow: e.tensor_tensor(out=g1[0:prow, c0:c1], in0=t5[0:prow, 5 * c0 + 2:5 * c1:5],
                                                                                         in1=t5[0:prow, 5 * c0 + 3:5 * c1:5], op=ALU.add), R=[t5], W=[g1])
                            k.op('dve', lambda e, c0=c0, c1=c1, prow=prow, e_=e_: e.tensor_tensor(out=gate_all[0:prow, e_ * NSC + c0:e_ * NSC + c1], in0=g1[0:prow, c0:c1],
                                                                                               in1=t5[0:prow, 5 * c0 + 4:5 * c1:5], op=ALU.add), R=[g1, t5], W=[gate_all])
                    k.barrier()
                if debug and stop == 'p4a':
                    break
                if debug and stop == 'p4b':
                    di = nc.dram_tensor("dbgidx", [128, NJ], I32, kind="ExternalOutput")
                    dg = nc.dram_tensor("dbggate", [128, NJ], F32, kind="ExternalOutput")
                    ld(di.ap(), idx_all[:], R=[idx_all], W=['dbgo'])
                    ld(dg.ap(), gate_all[:], R=[gate_all], W=['dbgo'])
                    k.barrier()
                    break

                with ExitStack() as pgl:
                    xgt = sb("xgt%d" % l, [128, D], BF16, pgl)
                    k.op('pool', lambda e: e.memset(xgt[:], 0.0), W=[xgt])
                    k.barrier()
                    gather_loop(idx_all, h2d, xgd, xgt, NJ, "g%d" % l)
                    k.op('pool', lambda e: e.memset(xgt[:, 0:2], 0.0), W=[xgt])
                    k.barrier()

                with ExitStack() as pex:
                    Wg2 = [sb("Wg%d_%d" % (l, i), [128, 8, D], BF16, pex) for i in range(2)]
                    Wu2 = [sb("Wu%d_%d" % (l, i), [128, 8, D], BF16, pex) for i in range(2)]
                    Wd2 = [sb("Wd%d_%d" % (l, i), [128, 8, D], BF16, pex) for i in range(2)]
                    xg = sb("xg%d" % l, [128, NSC, D], BF16, pex)
                    xsT = sb("xsT%d" % l, [128, 8, 1056], BF16, pex)
                    hd = sb("hd%d" % l, [128, 8, 1056], BF16, pex)
                    sa2 = [sb("sa%d_%d" % (l, i), [128, 512], F32, pex) for i in range(2)]
                    yo2 = [sb("yo%d_%d" % (l, i), [128, D], F32, pex) for i in range(2)]
                    cnt_ = [0]

                    def load_expert(e_, slot):
                        ld(Wg2[slot][:], wegd[l, e_], eng='pool', W=[Wg2[slot]])
                        ld(Wu2[slot][:], weud[l, e_], eng='pool', W=[Wu2[slot]])
                        ld(Wd2[slot][:], wedd[l, e_], eng='pool', W=[Wd2[slot]])

                    load_expert(0, 0)
                    groups = [(0, 512), (512, 512)] + ([(1024, 32)] if nst == 9 else [])
                    for e_ in range(NE):
                        slot = e_ % 2
                        if e_ + 1 < NE:
                            load_expert(e_ + 1, 1 - slot)
                        Wg, Wu, Wd = Wg2[slot], Wu2[slot], Wd2[slot]
                        j0 = e_ * NSC
                        ld(xg[:, 0:nst, :], xgd[j0 * 128:(j0 + nst) * 128, :].rearrange("(s p) d -> p s d", p=128), W=[xg])
                        for kc in range(8):
                            for s in range(8):
                                k.op('pe', lambda e, s=s, kc=kc: e.transpose(PSB[:, s * 128:(s + 1) * 128], xg[:, s, kc * 128:(kc + 1) * 128], identb[:]),
                                     R=[xg, identb], W=[PSB])
                            if kc % 2 == 0:
                                k.op('act', lambda e, kc=kc: e.copy(out=xsT[:, kc, 0:1024], in_=PSB[:, 0:1024]), R=[PSB], W=[(xsT, kc)])
                            else:
                                k.op('dve', lambda e, kc=kc: e.tensor_copy(out=xsT[:, kc, 0:1024], in_=PSB[:, 0:1024]), R=[PSB], W=[(xsT, kc)])
                        if nst == 9:
                            for kc in range(8):
                                k.op('pe', lambda e, kc=kc: e.transpose(PSB[:, kc * 32:(kc + 1) * 32], xg[0:32, 8, kc * 128:(kc + 1) * 128], identb[0:32, 0:32]),
                                     R=[xg, identb], W=[PSB])
                            for kc in range(8):
                                k.op('act', lambda e, kc=kc: e.copy(out=xsT[:, kc, 1024:1056], in_=PSB[:, kc * 32:(kc + 1) * 32]), R=[PSB], W=[(xsT, kc)])
                        xr = [(xsT, kc) for kc in range(8)]
                        for fc in range(8):
                            for (c0, n_) in groups:
                                pa = ps_get('s', [0, 1])
                                pu = ps_get('kr', [4, 5])
                                sa = sa2[cnt_[0] % 2]
                                cnt_[0] += 1
                                for kc in range(8):
                                    k.op('pe', lambda e, kc=kc, fc=fc, c0=c0, n_=n_, pa=pa: e.matmul(pa[:, :n_], lhsT=Wg[:, kc, fc * 128:(fc + 1) * 128],
                                                                                                    rhs=xsT[:, kc, c0:c0 + n_], start=(kc == 0), stop=(kc == 7)),
                                         R=[Wg] + xr, W=[pa])
                                for kc in range(8):
                                    k.op('pe', lambda e, kc=kc, fc=fc, c0=c0, n_=n_, pu=pu: e.matmul(pu[:, :n_], lhsT=Wu[:, kc, fc * 128:(fc + 1) * 128],
                                                                                                    rhs=xsT[:, kc, c0:c0 + n_], start=(kc == 0), stop=(kc == 7)),
                                         R=[Wu] + xr, W=[pu])
                                k.op('act', lambda e, sa=sa, pa=pa, n_=n_: e.activation(out=sa[:, :n_], in_=pa[:, :n_], func=AF.Silu), R=[pa], W=[sa])
                                k.op('dve', lambda e, sa=sa, pu=pu, n_=n_, fc=fc, c0=c0: e.tensor_tensor(out=hd[:, fc, c0:c0 + n_], in0=pu[:, :n_], in1=sa[:, :n_],
                                                                                                      op=ALU.mult), R=[pu, sa], W=[(hd, fc)])
                        hr = [(hd, fc) for fc in range(8)]
                        for s in range(nst):
                            prow = 128 if s < 8 else 32
                            vv = 0 if s < 8 else 1
                            yo = yo2[s % 2]
                            for half in range(2):
                                hs = slice(half * 512, (half + 1) * 512)
                                py = ps_get('kp', [2, 3]) if half == 0 else ps_get('nr', [6])
                                for fc in range(8):
                                    k.op('pe', lambda e, fc=fc, s=s, py=py, hs=hs, prow=prow: e.matmul(py[0:prow, :], lhsT=hd[:, fc, s * 128:s * 128 + prow], rhs=Wd[:, fc, hs],
                                                                                                      start=(fc == 0), stop=(fc == 7)), R=[Wd] + hr, W=[py])
                                k.op('dve', lambda e, py=py, yo=yo, hs=hs, s=s, vv=vv, prow=prow, j0=j0: e.scalar_tensor_tensor(
                                    out=yo[0:prow, hs], in0=py[0:prow, :], scalar=gate_all[0:prow, j0 + s:j0 + s + 1], in1=gt2[vv][0:prow, hs], op0=ALU.mult, op1=ALU.mult),
                                    R=[py, gate_all, gt2[vv]], W=[(yo, half)])
                            ld(Yd[(j0 + s) * 128:(j0 + s) * 128 + prow, :], yo[0:prow, :], R=[(yo, 0), (yo, 1)], W=[('Yd', j0 + s)])
                    k.barrier()

                with ExitStack() as psl:
                    yt = sb("yt%d" % l, [128, D], F32, psl)
                    k.op('pool', lambda e: e.memset(yt[:], 0.0), W=[yt])
                    k.barrier()
                    scatter_loop(idx_all, Yd, xs, yt, NJ, "s%d" % l)
                    k.op('pool', lambda e: e.memset(yt[:, 0:2], 0.0), W=[yt])
                    k.barrier()
        k.barrier()
    return nc


def _colmajor(w):
    return np.ascontiguousarray(w.reshape(8, 128, -1).transpose(1, 0, 2))


def _consts():
    ident = np.eye(128, dtype=np.float32)
    blk = (np.arange(128)[:, None] // 64 == np.arange(128)[None, :] // 64).astype(np.float32)
    p = np.arange(128)
    triL = (p[:, None] < p[None, :]).astype(np.float32)
    maskP = (p[:, None] >= p[None, :]).astype(np.float32)
    maskN = (p[:, None] <= p[None, :]).astype(np.float32)
    cm = np.ascontiguousarray(np.stack([ident, blk, triL, maskP, maskN], axis=1))
    iota = np.ascontiguousarray(np.broadcast_to(np.arange(1024, dtype=np.float32), (128, 1024)))
    tidc = np.zeros((128, NTL, NE, 5), np.float32)
    tidc[:, :, :, 0] = np.arange(NTL)[None, :, None]
    tidc[:, :, :, 1] = np.arange(128)[:, None, None]
    t = np.arange(NL)
    row = (t // 64).astype(np.float32)
    colp = (t % 64).astype(np.float32)
    inv = np.power(np.float32(10000.0), -(np.arange(16, dtype=np.float32) / np.float32(16))).astype(np.float32)
    ang = np.concatenate([row[:, None] * inv[None, :], colp[:, None] * inv[None, :]], axis=-1).astype(np.float32)
    cos = np.cos(ang).astype(np.float32)
    sin = np.sin(ang).astype(np.float32)
    C = np.ones((128, NT), np.float32)
    S = np.zeros((128, NT), np.float32)
    for r in range(128):
        i = r % 64
        f = i % 32
        C[r, :NL] = cos[:, f]
        S[r, :NL] = sin[:, f] * (-1.0 if i < 32 else 1.0)
    return cm, iota, tidc, C, S


def _swap_halves(cols):
    cols = np.asarray(cols)
    return (cols // 64) * 64 + ((cols % 64) + 32) % 64


def prep_inputs(inp):
    L = inp['w_ada'].shape[0]
    cm, iota, tidc, C, S = _consts()
    f = lambda a: np.ascontiguousarray(a, dtype=np.float32)
    w_in = inp['w_in']
    kA, vA, kB, vB = np.arange(0, 128), np.arange(128, 256), np.arange(256, 384), np.arange(384, 512)
    qa0, qb0, cv0, gt0 = 512, 1024, 1536, 3072
    qorder = np.concatenate([np.concatenate([np.arange(j * 64, j * 64 + 64), np.arange((4 + j) * 64, (4 + j) * 64 + 64)]) for j in range(4)])
    c1 = np.concatenate([kA, kB, _swap_halves(kA), _swap_halves(kB), vA, vB])
    c2 = np.concatenate([qa0 + qorder, qa0 + _swap_halves(qorder), qb0 + qorder, qb0 + _swap_halves(qorder)])
    ccx = np.concatenate([np.arange(cv0 + 512, cv0 + 1024), np.arange(cv0 + 1024, cv0 + 1536)])
    c3 = np.concatenate([np.arange(cv0, cv0 + 512), np.arange(gt0, gt0 + 3072)])
    shared = {}
    shared['w_ada'] = f(np.stack([_colmajor(inp['w_ada'][l]) for l in range(L)]))
    bcol = np.stack([inp['b_ada'][l].reshape(48, 128).T for l in range(L)])
    shared['b_col2'] = f(np.repeat(bcol, 2, axis=2))
    sel = [slice(2048, 3072), slice(3072, 4096), slice(4096, 5120), slice(5120, 6144)]
    shared['b_bc'] = f(np.stack([np.stack([np.broadcast_to(inp['b_ada'][l][s], (128, 1024)) for s in sel], axis=1) for l in range(L)]))
    gc = np.zeros((L, 128, 2, 8, 2), np.float32)
    for l in range(L):
        gc[l, :, 0, :, :] = inp['g_mix'][l].reshape(8, 128).T[:, :, None]
        gc[l, :, 1, :, :] = inp['g_ffn'][l].reshape(8, 128).T[:, :, None]
    shared['gcol2'] = gc
    shared['gffn_bc'] = f(np.stack([np.broadcast_to(inp['g_ffn'][l], (128, 1024)) for l in range(L)]))
    shared['w1'] = f(np.stack([_colmajor(w_in[l][:, c1]) for l in range(L)]))
    shared['w2'] = f(np.stack([_colmajor(w_in[l][:, c2]) for l in range(L)]))
    shared['wcx'] = f(np.stack([_colmajor(w_in[l][:, ccx]) for l in range(L)]))
    shared['w3'] = f(np.stack([_colmajor(w_in[l][:, c3]) for l in range(L)]))
    wbr = np.zeros((L, 128, 12, 1024), np.float32)
    for l in range(L):
        for br in range(3):
            wb = inp['w_branch'][l, br]
            if br < 2:
                wb = wb[qorder]
            wbr[l, :, br * 4:(br + 1) * 4, :] = wb.reshape(4, 128, 1024).transpose(1, 0, 2)
    shared['wbr'] = wbr
    shared['wout'] = f(np.stack([_colmajor(inp['w_out'][l]) for l in range(L)]))
    hgv = np.zeros((L, 128, 8), np.float32)
    sw = (np.arange(64) + 32) % 64
    for l in range(L):
        for i, g in enumerate((inp['qg_a'][l], inp['kg_a'][l], inp['qg_b'][l], inp['kg_b'][l])):
            hgv[l, :, 2 * i] = np.tile(g, 2)
            hgv[l, :, 2 * i + 1] = np.tile(g[sw], 2)
    shared['hg'] = hgv
    shared['sink'] = f(np.stack([np.broadcast_to(inp['sink_b'][l], (128, 8)) for l in range(L)]))
    shared['convw'] = f(np.stack([inp['conv_w'][l].reshape(3, 4, 128).transpose(2, 1, 0) for l in range(L)]))
    shared['wr'] = f(np.stack([_colmajor(inp['w_router'][l]) for l in range(L)]))
    for nm, key in (('weg', 'w_e_gate'), ('weu', 'w_e_up'), ('wed', 'w_e_down')):
        w = inp[key]
        shared[nm] = f(w.reshape(L, NE, 8, 128, 1024).transpose(0, 1, 3, 2, 4))
    shared['ropeC'] = C
    shared['ropeS'] = S
    shared['cmisc'] = cm
    shared['iota'] = iota
    shared['comb'] = tidc
    maps = []
    B = inp['x'].shape[0]
    for b in range(B):
        m = dict(shared)
        m['xin'] = f(np.concatenate([inp['x'][b], inp['ctx'][b]], axis=0))
        cv = np.zeros((128, 8, 2), np.float32)
        cv[:, :, 0] = inp['c'][b].reshape(8, 128).T
        cv[:, :, 1] = inp['c_ctx'].reshape(8, 128).T
        m['cvec'] = cv
        maps.append(m)
    return maps


_NC_CACHE = {}
_PER_LAYER = ('w_ada', 'b_col2', 'b_bc', 'gcol2', 'gffn_bc', 'w1', 'w2', 'wcx', 'w3', 'wbr', 'wout', 'hg', 'sink', 'convw', 'wr',
              'weg', 'weu', 'wed')
FUSED = True


def _layer_slice(m, l):
    out = {}
    for k_, v in m.items():
        out[k_] = np.ascontiguousarray(v[l:l + 1]) if k_ in _PER_LAYER else v
    return out


def kernel(**inputs):
    inp = {k_: np.asarray(v) for k_, v in inputs.items()}
    maps = prep_inputs(inp)
    L = inp['w_ada'].shape[0]
    cores = list(range(len(maps)))
    if FUSED:
        if 'nc' not in _NC_CACHE:
            _NC_CACHE['nc'] = build(nlayers=L)
        res = run_bass_kernel_spmd(_NC_CACHE['nc'], maps, core_ids=cores)
        xs = [np.asarray(r["xs"]) for r in res.results]
    else:
        xs = [m['xin'] for m in maps]
        for l in range(L):
            key = ('layer', l == L - 1)
            if key not in _NC_CACHE:
                _NC_CACHE[key] = build(nlayers=1, force_ctx=(l != L - 1))
            lm = []
            for m, x_ in zip(maps, xs):
                d = _layer_slice(m, l)
                d['xin'] = np.ascontiguousarray(x_, dtype=np.float32)
                lm.append(d)
            res = run_bass_kernel_spmd(_NC_CACHE[key], lm, core_ids=cores)
            xs = [np.asarray(r["xs"]) for r in res.results]
    out = np.stack([x_[:NL] for x_ in xs], axis=0)
    return out.astype(np.float32)
```

```python
import numpy as np
from contextlib import ExitStack
import concourse.bass as bass
import concourse.mybir as mybir
from concourse.bass_utils import run_bass_kernel_spmd

F32 = mybir.dt.float32
BF16 = mybir.dt.bfloat16
I32 = mybir.dt.int32
AF = mybir.ActivationFunctionType
ALU = mybir.AluOpType
AX = mybir.AxisListType

NL, NCX, NT, D = 8192, 256, 8448, 1024
NTL = NT // 128
EPS = 1e-6
NE = 16
N_CORES = 4


class K:
    def __init__(self, nc, stack):
        self.nc = nc
        self.stack = stack
        self.eng = {'pe': nc.tensor, 'act': nc.scalar, 'dve': nc.vector, 'pool': nc.gpsimd, 'sp': nc.sync}
        self.sem = {n: stack.enter_context(nc.semaphore("s_" + n)) for n in self.eng}
        self.cnt = {n: 0 for n in self.eng}
        self.seen = {n: {} for n in self.eng}
        self.dsem = {}
        self.dval = {}
        self.lastw = {}
        self.readers = {}
        self.nwaits = 0
        self.nins = 0
        self.drr = {}

    def _key(self, t):
        if isinstance(t, (str, int)):
            return t
        if isinstance(t, tuple):
            return tuple(self._key(x) for x in t)
        return ('id', id(t))

    def _deps(self, R, W):
        deps = []
        for t in list(R) + list(W):
            d = self.lastw.get(self._key(t))
            if d is not None:
                deps.append(d)
        for t in W:
            deps.extend(self.readers.get(self._key(t), {}).items())
        return deps

    def _wait(self, e, deps):
        h = self.eng[e]
        seen = self.seen[e]
        need = {}
        for key, val in deps:
            if key == ('E', 'pe') and e == 'pe':
                continue
            if seen.get(key, 0) >= val:
                continue
            if need.get(key, 0) < val:
                need[key] = val
        for key, val in need.items():
            s = self.sem[key[1]] if key[0] == 'E' else self.dsem[key[1]]
            h.wait_ge(s, val)
            seen[key] = val
            self.nwaits += 1

    def _record(self, tok, R, W):
        key, val = tok
        for t in R:
            r = self.readers.setdefault(self._key(t), {})
            if r.get(key, 0) < val:
                r[key] = val
        for t in W:
            self.lastw[self._key(t)] = tok
            self.readers[self._key(t)] = {}

    def op(self, e, fn, R=(), W=()):
        self._wait(e, self._deps(R, W))
        ins = fn(self.eng[e])
        ins.then_inc(self.sem[e], 1)
        self.cnt[e] += 1
        self.nins += 1
        self._record((('E', e), self.cnt[e]), R, W)
        return ins

    NDS = 8

    def dma(self, e, stream, fn, R=(), W=()):
        i = self.drr.get(e, 0)
        self.drr[e] = i + 1
        stream = "%s%d" % (e, i % self.NDS)
        if stream not in self.dsem:
            self.dsem[stream] = self.stack.enter_context(self.nc.semaphore("d_" + stream))
            self.dval[stream] = 0
        deps = self._deps(R, W)
        if self.dval[stream] > 0:
            deps.append((('D', stream), self.dval[stream]))
        self._wait(e, deps)
        ins = fn(self.eng[e])
        ins.then_inc(self.dsem[stream], 16)
        self.dval[stream] += 16
        self.nins += 1
        self._record((('D', stream), self.dval[stream]), R, W)
        return ins

    def barrier(self, engines=None):
        deps = [(('E', n), c) for n, c in self.cnt.items() if c > 0]
        deps += [(('D', s), v) for s, v in self.dval.items() if v > 0]
        for e in (engines or self.eng):
            self._wait(e, deps)


def build(nlayers=2, debug=False, stop=None, cut=99, force_ctx=False):
    nc = bass.Bass("TRN2", target_bir_lowering=False)
    st = ExitStack()
    with st:
        k = K(nc, st)

        def din(name, shape, dt=F32):
            return nc.dram_tensor(name, list(shape), dt, kind="ExternalInput")

        def dsc(name, shape, dt, out=False):
            return nc.dram_tensor(name, list(shape), dt, kind="ExternalOutput" if (out or debug) else "Internal")

        L = nlayers
        xin = din("xin", [NT, D])
        cvec = din("cvec", [128, 8, 2])
        w_ada = din("w_ada", [L, 128, 8, 6144])
        b_col2 = din("b_col2", [L, 128, 96])
        b_bc = din("b_bc", [L, 128, 4, D])
        gcol2 = din("gcol2", [L, 128, 2, 8, 2])
        gffn_bc = din("gffn_bc", [L, 128, D])
        w1d = din("w1", [L, 128, 8, 768])
        w2d = din("w2", [L, 128, 8, 2048])
        wcxd = din("wcx", [L, 128, 8, 1024])
        w3d = din("w3", [L, 128, 8, 3584])
        wbrd = din("wbr", [L, 128, 12, D])
        woutd = din("wout", [L, 128, 8, D])
        hgd = din("hg", [L, 128, 8])
        sinkd = din("sink", [L, 128, 8])
        convwd = din("convw", [L, 128, 4, 3])
        wrd = din("wr", [L, 128, 8, NE])
        wegd = din("weg", [L, NE, 128, 8, D])
        weud = din("weu", [L, NE, 128, 8, D])
        wedd = din("wed", [L, NE, 128, 8, D])
        ropeCd = din("ropeC", [128, NT])
        ropeSd = din("ropeS", [128, NT])
        cmisc = din("cmisc", [128, 5, 128])
        iotad = din("iota", [128, 1024])
        combd = din("comb", [128, NTL, NE, 5])
        xs = dsc("xs", [NT, D], F32, out=True)
        hTd = dsc("hTd", [128, 8, NT], BF16)
        uTd = dsc("uTd", [128, 4, NT + 4], F32)
        oaTd = dsc("oaTd", [128, 4, NT], BF16)
        obTd = dsc("obTd", [128, 4, NT], BF16)
        mTd = dsc("mTd", [128, 8, NT], BF16)
        h2d = dsc("h2d", [NT, D], BF16)
        affd = dsc("affd", [NT, NE], F32)
        bcd = dsc("bcd", [2, 4, 128, D], F32)
        xgd = dsc("xgd", [NE * 9 * 128, D], BF16)
        Yd = dsc("Yd", [NE * 9 * 128, D], F32)

        def sb(name, shape, dt, stack=None):
            return (stack or st).enter_context(nc.sbuf_tensor(name, list(shape), dt))

        PS = [st.enter_context(nc.psum_tensor("ps%d" % i, [128, 512], F32)) for i in range(7)]
        PSB = st.enter_context(nc.psum_tensor("psb", [128, 1024], BF16))
        psrr = {}

        def ps_get(pool, banks):
            i = psrr.get(pool, 0)
            psrr[pool] = i + 1
            return PS[banks[i % len(banks)]]

        ident = sb("ident", [128, 128], F32)
        blockones = sb("blockones", [128, 128], F32)
        ones32 = sb("ones32", [128, 128], F32)
        onesb = sb("onesb", [128, 128], BF16)
        triL = sb("triL", [128, 128], BF16)
        maskP = sb("maskP", [128, 128], BF16)
        maskN = sb("maskN", [128, 128], BF16)
        identb = sb("identb", [128, 128], BF16)
        sc = sb("sc", [128, 8, 2], F32)
        lbc = sb("lbc", [128, 8, 2, 128], F32)
        modcol = sb("modcol", [128, 48, 2], F32)
        A1c = sb("A1c", [128, 8, 2], F32)
        hg = sb("hgs", [128, 8], F32)
        esink = sb("esink", [128, 8], F32)
        cw = sb("cw", [128, 4, 3], F32)
        ss = sb("ss", [128, 4], F32)
        rsd = sb("rsd", [128, 4], F32)

        g = nc.gpsimd
        lr1 = st.enter_context(g.register("lr1"))
        lr2 = st.enter_context(g.register("lr2"))
        licur = sb("licur", [128, 1], I32)

        def gather_loop(idx_all, src, dst, xg_, nj, tag):
            s1 = st.enter_context(nc.semaphore("lc" + tag))
            s2 = st.enter_context(nc.semaphore("ld" + tag))
            with g.Fori(0, nj) as j:
                g.tensor_copy(out=licur[:, 0:1], in_=idx_all[:, bass.ds(j, 1)]).then_inc(s1, 1)
                g.reg_mov(lr1, 1)
                g.reg_add(lr1, lr1, j)
                g.wait_ge(s1, lr1)
                g.indirect_dma_start(out=xg_[:, :], out_offset=None, in_=src[:, :], in_offset=bass.IndirectOffsetOnAxis(ap=licur[:, 0:1], axis=0),
                                     bounds_check=NT - 1, oob_is_err=False).then_inc(s2, 16)
                g.reg_mov(lr1, 32)
                g.reg_mul(lr1, lr1, j)
                g.reg_add(lr1, lr1, 16)
                g.wait_ge(s2, lr1)
                g.reg_mov(lr2, 128 * D)
                g.reg_mul(lr2, lr2, j)
                g.dma_start(out=bass.AP(dst, lr2, [[D, 128], [1, D]]), in_=xg_[:, :]).then_inc(s2, 16)
                g.reg_add(lr1, lr1, 16)
                g.wait_ge(s2, lr1)

        def scatter_loop(idx_all, src, dst, yt_, nj, tag):
            s1 = st.enter_context(nc.semaphore("sc" + tag))
            s2 = st.enter_context(nc.semaphore("sd" + tag))
            with g.Fori(0, nj) as j:
                g.tensor_copy(out=licur[:, 0:1], in_=idx_all[:, bass.ds(j, 1)]).then_inc(s1, 1)
                g.reg_mov(lr1, 1)
                g.reg_add(lr1, lr1, j)
                g.wait_ge(s1, lr1)
                g.reg_mov(lr2, 128 * D)
                g.reg_mul(lr2, lr2, j)
                g.dma_start(out=yt_[:, :], in_=bass.AP(src, lr2, [[D, 128], [1, D]])).then_inc(s2, 16)
                g.reg_mov(lr1, 32)
                g.reg_mul(lr1, lr1, j)
                g.reg_add(lr1, lr1, 16)
                g.wait_ge(s2, lr1)
                g.indirect_dma_start(out=dst[:, :], out_offset=bass.IndirectOffsetOnAxis(ap=licur[:, 0:1], axis=0), in_=yt_[:, :], in_offset=None,
                                     bounds_check=NT - 1, oob_is_err=False, compute_op=ALU.add).then_inc(s2, 16)
                g.reg_add(lr1, lr1, 16)
                g.wait_ge(s2, lr1)

        def ld(dst, src, eng='sp', stream='misc', R=(), W=None, slow=False):
            if slow:
                return k.dma(eng, stream, lambda e: e.dma_start(out=dst, in_=src, allow_slow_non_contiguous=True), R=R, W=W)
            return k.dma(eng, stream, lambda e: e.dma_start(out=dst, in_=src), R=R, W=W)

        ld(ident[:], cmisc[:, 0, :], W=[ident])
        ld(blockones[:], cmisc[:, 1, :], W=[blockones])
        ld(triL[:], cmisc[:, 2, :], eng='pool', stream='miscp', W=[triL])
        ld(maskP[:], cmisc[:, 3, :], eng='pool', stream='miscp', W=[maskP])
        ld(maskN[:], cmisc[:, 4, :], eng='pool', stream='miscp', W=[maskN])
        ld(identb[:], cmisc[:, 0, :], eng='pool', stream='miscp', W=[identb])
        ld(sc[:], cvec.ap(), W=[sc])
        k.op('dve', lambda e: e.memset(ones32[:], 1.0), W=[ones32])
        k.op('dve', lambda e: e.memset(onesb[:], 1.0), W=[onesb])
        for i in range(4):
            r0, r1 = i * (NT // 4), (i + 1) * (NT // 4)
            ld(xs[r0:r1, :], xin[r0:r1, :], stream='xcopy', W=[('xs', 'init')])
        k.op('act', lambda e: e.activation(out=sc[:], in_=sc[:], func=AF.Silu), R=[sc], W=[sc])
        for kc in range(8):
            for v in range(2):
                k.op('dve', lambda e, kc=kc, v=v: e.tensor_scalar(out=lbc[:, kc, v, :], in0=ones32[:], scalar1=sc[:, kc, v:v + 1],
                                                                   scalar2=None, op0=ALU.mult), R=[sc, ones32], W=[lbc])
        k.barrier()

        blocks = [(i * 512, 512, 0) for i in range(16)] + [(NL, NCX, 1)]

        def rstd_of(xt, junk, col):
            k.op('pool', lambda e: e.memset(ss[:, col:col + 1], 0.0), W=[('ss', col)])
            k.op('act', lambda e: e.activation(out=junk[:], in_=xt[:], func=AF.Square, accum_out=ss[:, col:col + 1]),
                 R=[xt], W=[junk, ('ss', col)])
            k.op('act', lambda e: e.activation(out=rsd[:, col:col + 1], in_=ss[:, col:col + 1], func=AF.Sqrt, scale=1.0 / D, bias=EPS),
                 R=[('ss', col)], W=[('rsd', col)])
            k.op('dve', lambda e: e.reciprocal(out=rsd[:, col:col + 1], in_=rsd[:, col:col + 1]), R=[('rsd', col)], W=[('rsd', col)])

        for l in range(L):
            last = (l == L - 1) and not force_ctx
            with ExitStack() as p0:
                wa = [sb("wa%d_%d" % (l, i), [128, 8, 512], F32, p0) for i in range(2)]
                bb = sb("bb%d" % l, [128, 4, D], F32, p0)
                gfb = sb("gfb%d" % l, [128, D], F32, p0)
                rowt = sb("rowt%d" % l, [128, D], F32, p0)
                bcol = sb("bcol%d" % l, [128, 96], F32, p0)
                gc2 = sb("gc2%d" % l, [128, 2, 8, 2], F32, p0)
                ld(bb[:], b_bc[l], W=[bb])
                ld(gfb[:], gffn_bc[l], W=[gfb])
                ld(bcol[:], b_col2[l], W=[bcol])
                ld(gc2[:], gcol2[l], W=[gc2])
                ld(hg[:], hgd[l], W=[hg])
                ld(esink[:], sinkd[l], W=[esink])
                ld(cw[:], convwd[l], W=[cw])
                k.op('act', lambda e: e.activation(out=esink[:], in_=esink[:], func=AF.Exp), R=[esink], W=[esink])
                psmod = PS[0]
                rowsel = {4: (0, 0), 5: (0, 1), 6: (1, 0), 7: (1, 1), 8: (2, 0), 9: (2, 1), 10: (3, 0), 11: (3, 1)}
                for ch in range(12):
                    w = wa[ch % 2]
                    ld(w[:], w_ada[l, :, :, ch * 512:(ch + 1) * 512], stream='wa%d' % (ch % 2), W=[w])
                    for sub in range(4):
                        j = ch * 4 + sub
                        for kc in range(8):
                            k.op('pe', lambda e, w=w, j=j, kc=kc, sub=sub: e.matmul(
                                psmod[:, 2 * j:2 * j + 2], lhsT=w[:, kc, sub * 128:(sub + 1) * 128], rhs=sc[:, kc, :],
                                start=(kc == 0), stop=(kc == 7)), R=[w, sc], W=[psmod])
                    if ch in rowsel:
                        vi, half = rowsel[ch]
                        for v in range(2):
                            pr = PS[1 + v]
                            for kc in range(8):
                                k.op('pe', lambda e, w=w, kc=kc, v=v, pr=pr: e.matmul(
                                    pr[:, :], lhsT=lbc[:, kc, v, :], rhs=w[:, kc, :], start=(kc == 0), stop=(kc == 7)),
                                    R=[w, lbc], W=[pr])
                            hs = slice(half * 512, (half + 1) * 512)
                            k.op('dve', lambda e, pr=pr, vi=vi, hs=hs: e.tensor_tensor(
                                out=rowt[:, hs], in0=pr[:, :], in1=bb[:, vi, hs], op=ALU.add), R=[pr, bb], W=[rowt])
                            if vi == 2:
                                k.op('dve', lambda e, hs=hs: e.scalar_tensor_tensor(
                                    out=rowt[:, hs], in0=rowt[:, hs], scalar=1.0, in1=gfb[:, hs], op0=ALU.add, op1=ALU.mult),
                                    R=[rowt, gfb], W=[rowt])
                            ld(bcd[v, vi, :, hs], rowt[:, hs], stream='bcst', R=[rowt], W=[('bcd', v, vi, half)])
                mc = modcol[:].rearrange("p a b -> p (a b)")
                k.op('dve', lambda e: e.tensor_tensor(out=mc, in0=psmod[:, 0:96], in1=bcol[:], op=ALU.add), R=[psmod, bcol], W=[modcol])
                k.op('dve', lambda e: e.scalar_tensor_tensor(out=A1c[:], in0=modcol[:, 8:16, :], scalar=1.0, in1=gc2[:, 0, :, :],
                                                            op0=ALU.add, op1=ALU.mult), R=[modcol, gc2], W=[A1c])
                k.barrier()
            if stop == 'p0':
                break

            with ExitStack() as pkv:
                KTA = sb("KTA%d" % l, [128, NT], BF16, pkv)
                KTB = sb("KTB%d" % l, [128, NT], BF16, pkv)
                VA = sb("VA%d" % l, [128, NTL + 1, 2, 80], BF16, pkv)
                VB = sb("VB%d" % l, [128, NTL + 1, 2, 80], BF16, pkv)
                KT = {'A': KTA, 'B': KTB}
                VAf = VA[:].rearrange("p t g c -> p (t g c)")
                VBf = VB[:].rearrange("p t g c -> p (t g c)")
                VV = {'A': VA, 'B': VB}
                k.op('pool', lambda e: e.memset(VA[:], 0.0), W=[VA])
                k.op('pool', lambda e: e.memset(VB[:], 0.0), W=[VB])
                k.op('pool', lambda e: e.memset(VA[:, :, :, 64:65], 1.0), W=[VA])
                k.op('pool', lambda e: e.memset(VB[:, :, :, 64:65], 1.0), W=[VB])

                def normrope(psp, psr, gi, Cb, Sb, out_ap, outres, n, tmp, split=None):
                    sq, rt, t1, t2 = tmp
                    k.op('act', lambda e: e.activation(out=sq[:, :n], in_=psp[:, :n], func=AF.Square), R=[psp], W=[sq])
                    pq = ps_get('nr', [6])
                    k.op('pe', lambda e: e.matmul(pq[:, :n], lhsT=blockones[:], rhs=sq[:, :n], start=True, stop=True),
                         R=[sq, blockones], W=[pq])
                    k.op('act', lambda e: e.activation(out=rt[:, :n], in_=pq[:, :n], func=AF.Sqrt, scale=1.0 / 64, bias=EPS),
                         R=[pq], W=[rt])
                    k.op('dve', lambda e: e.reciprocal(out=rt[:, :n], in_=rt[:, :n]), R=[rt], W=[rt])
                    k.op('dve', lambda e: e.scalar_tensor_tensor(out=t1[:, :n], in0=psp[:, :n], scalar=hg[:, gi:gi + 1], in1=Cb[:, :n],
                                                                op0=ALU.mult, op1=ALU.mult), R=[psp, Cb, hg], W=[t1])
                    k.op('dve', lambda e: e.scalar_tensor_tensor(out=t2[:, :n], in0=psr[:, :n], scalar=hg[:, gi + 1:gi + 2], in1=Sb[:, :n],
                                                                op0=ALU.mult, op1=ALU.mult), R=[psr, Sb, hg], W=[t2])
                    k.op('pool', lambda e: e.tensor_tensor(out=t1[:, :n], in0=t1[:, :n], in1=t2[:, :n], op=ALU.add), R=[t1, t2], W=[t1])
                    if split is None:
                        k.op('pool', lambda e: e.tensor_tensor(out=out_ap, in0=t1[:, :n], in1=rt[:, :n], op=ALU.mult), R=[t1, rt], W=[outres])
                    else:
                        k.op('pool', lambda e: e.tensor_tensor(out=split[0], in0=t1[0:64, :n], in1=rt[0:64, :n], op=ALU.mult), R=[t1, rt], W=[outres])
                        k.op('pool', lambda e: e.tensor_tensor(out=split[1], in0=t1[64:128, :n], in1=rt[64:128, :n], op=ALU.mult), R=[t1, rt], W=[outres])

                with ExitStack() as p1:
                    W1 = sb("W1_%d" % l, [128, 8, 768], BF16, p1)
                    ld(W1[:], w1d[l], eng='pool', stream='wbig', W=[W1])
                    xt2 = [sb("xt%d_%d" % (l, i), [128, D], F32, p1) for i in range(2)]
                    xn2 = [sb("xn%d_%d" % (l, i), [128, D], F32, p1) for i in range(2)]
                    junk = sb("junk%d" % l, [128, D], F32, p1)
                    hTb2 = [sb("hTb%d_%d" % (l, i), [128, 8, 512], BF16, p1) for i in range(2)]
                    Cb2 = [sb("Cb%d_%d" % (l, i), [128, 512], F32, p1) for i in range(2)]
                    Sb2 = [sb("Sb%d_%d" % (l, i), [128, 512], F32, p1) for i in range(2)]
                    tmps = [[sb("nt%d_%d_%d" % (l, i, j), [128, 512], F32, p1) for j in range(4)] for i in range(2)]
                    ti_glob = 0
                    for bi, (t0, ntok, v) in enumerate(blocks if cut >= 5 else blocks[:1]):
                        if cut < 2:
                            break
                        hTb = hTb2[bi % 2]
                        Cb, Sb = Cb2[bi % 2], Sb2[bi % 2]
                        ld(Cb[:, :ntok], ropeCd[:, t0:t0 + ntok], stream='rope%d' % (bi % 2), W=[Cb])
                        ld(Sb[:, :ntok], ropeSd[:, t0:t0 + ntok], stream='rope%d' % (bi % 2), W=[Sb])
                        for tt in range(ntok // 128):
                            xt = xt2[ti_glob % 2]
                            xn = xn2[ti_glob % 2]
                            col = ti_glob % 2
                            r0 = t0 + tt * 128
                            ld(xt[:], xs[r0:r0 + 128, :], stream='xt%d' % (ti_glob % 2), R=[('xs', 'init')], W=[xt])
                            rstd_of(xt, junk, col)
                            k.op('dve', lambda e, xn=xn, xt=xt, col=col: e.tensor_scalar(out=xn[:], in0=xt[:], scalar1=rsd[:, col:col + 1],
                                                                                      scalar2=None, op0=ALU.mult),
                                 R=[xt, ('rsd', col)], W=[xn])
                            for half in range(2):
                                pt = ps_get('tr', [0, 1])
                                for q in range(4):
                                    kc = half * 4 + q
                                    k.op('pe', lambda e, pt=pt, q=q, kc=kc, xn=xn: e.transpose(pt[:, q * 128:(q + 1) * 128],
                                                                                               xn[:, kc * 128:(kc + 1) * 128], ident[:]),
                                         R=[xn, ident], W=[pt])
                                for q in range(4):
                                    kc = half * 4 + q
                                    eng = 'act' if q % 2 == 0 else 'dve'
                                    dst = hTb[:, kc, tt * 128:(tt + 1) * 128]
                                    if eng == 'act':
                                        k.op('act', lambda e, pt=pt, q=q, kc=kc, dst=dst: e.activation(
                                            out=dst, in_=pt[:, q * 128:(q + 1) * 128], func=AF.Identity,
                                            scale=A1c[:, kc, v:v + 1], bias=modcol[:, kc, v:v + 1]), R=[pt, A1c, modcol], W=[hTb])
                                    else:
                                        k.op('dve', lambda e, pt=pt, q=q, kc=kc, dst=dst: e.tensor_scalar(
                                            out=dst, in0=pt[:, q * 128:(q + 1) * 128], scalar1=A1c[:, kc, v:v + 1],
                                            scalar2=modcol[:, kc, v:v + 1], op0=ALU.mult, op1=ALU.add), R=[pt, A1c, modcol], W=[hTb])
                            ti_glob += 1
                        ld(hTd[:, :, t0:t0 + ntok], hTb[:, :, :ntok], stream='hst%d' % (bi % 2), R=[hTb], W=[('hTd', bi)])
                        for ai, (nm, c0, gi) in enumerate((('A', 0, 2), ('B', 128, 6)) if cut >= 3 else ()):
                            psp = ps_get('kp', [2, 3])
                            psr = ps_get('kr', [4, 5])
                            for kc in range(8):
                                k.op('pe', lambda e, kc=kc, psp=psp, c0=c0: e.matmul(psp[:, :ntok], lhsT=W1[:, kc, c0:c0 + 128],
                                                                                    rhs=hTb[:, kc, :ntok], start=(kc == 0), stop=(kc == 7)),
                                     R=[W1, hTb], W=[psp])
                            for kc in range(8):
                                k.op('pe', lambda e, kc=kc, psr=psr, c0=c0: e.matmul(psr[:, :ntok], lhsT=W1[:, kc, 256 + c0:256 + c0 + 128],
                                                                                    rhs=hTb[:, kc, :ntok], start=(kc == 0), stop=(kc == 7)),
                                     R=[W1, hTb], W=[psr])
                            normrope(psp, psr, gi, Cb, Sb, KT[nm][:, t0:t0 + ntok], KT[nm], ntok, tmps[ai])
                        for tt in range(ntok // 128 if cut >= 4 else 0):
                            ti = (t0 + tt * 128) // 128
                            pv = ps_get('kp', [2, 3]) if cut != 41 else ps_get('tr', [0, 1])
                            for kc in range(8):
                                k.op('pe', lambda e, kc=kc, pv=pv, tt=tt: e.matmul(pv[:, 0:256], lhsT=hTb[:, kc, tt * 128:(tt + 1) * 128],
                                                                                  rhs=W1[:, kc, 512:768], start=(kc == 0), stop=(kc == 7)),
                                     R=[W1, hTb], W=[pv])
                            for g_ in range(2):
                                k.op('act', lambda e, pv=pv, ti=ti, g_=g_: e.copy(out=VA[:, ti, g_, 0:64], in_=pv[:, g_ * 64:(g_ + 1) * 64]),
                                     R=[pv], W=[VA])
                                k.op('dve', lambda e, pv=pv, ti=ti, g_=g_: e.tensor_copy(out=VB[:, ti, g_, 0:64], in_=pv[:, 128 + g_ * 64:128 + (g_ + 1) * 64]),
                                     R=[pv], W=[VB])
                    k.barrier()
                if stop == 'p1':
                    if debug:
                        dk = nc.dram_tensor("dbgKTA", [128, NT], BF16, kind="ExternalOutput")
                        dkb = nc.dram_tensor("dbgKTB", [128, NT], BF16, kind="ExternalOutput")
                        dv = nc.dram_tensor("dbgVA", [128, NTL + 1, 2, 80], BF16, kind="ExternalOutput")
                        ld(dk.ap(), KTA[:], stream='dbg', R=[KTA], W=['dbgo'])
                        ld(dkb.ap(), KTB[:], stream='dbg', R=[KTB], W=['dbgo'])
                        ld(dv.ap(), VA[:], stream='dbg', R=[VA], W=['dbgo'])
                        k.barrier()
                    break

                with ExitStack() as p2:
                    W2 = sb("W2_%d" % l, [128, 8, 2048], BF16, p2)
                    ld(W2[:], w2d[l], eng='pool', stream='wbig', W=[W2])
                    hTb2 = [sb("hTq%d_%d" % (l, i), [128, 8, 512], BF16, p2) for i in range(2)]
                    Cb2 = [sb("Cq%d_%d" % (l, i), [128, 512], F32, p2) for i in range(2)]
                    Sb2 = [sb("Sq%d_%d" % (l, i), [128, 512], F32, p2) for i in range(2)]
                    tmps = [[sb("qt%d_%d_%d" % (l, i, j), [128, 512], F32, p2) for j in range(4)] for i in range(2)]
                    QT = {nm: [[sb("QT%s%d_%d_%d" % (nm, l, j, hh), [128, 512], BF16, p2) for hh in range(2)] for j in range(4)] for nm in ('A', 'B')}
                    for nm in ('A', 'B'):
                        for j in range(4):
                            for hh in range(2):
                                k.op('pool', lambda e, t_=QT[nm][j][hh]: e.memset(t_[:], 0.0), W=[(QT[nm], j)])
                    OT2 = {'A': [sb("OaT%d_%d" % (l, i), [128, 4, 512], BF16, p2) for i in range(2)],
                           'B': [sb("ObT%d_%d" % (l, i), [128, 4, 512], BF16, p2) for i in range(2)]}
                    PT = [sb("PT%d_%d" % (l, i), [128, 512], BF16, p2) for i in range(4)]
                    rec = [sb("rec%d_%d" % (l, i), [128, 512], F32, p2) for i in range(2)]
                    bcs = [sb("bcs%d_%d" % (l, i), [64, 512], F32, p2) for i in range(2)]
                    ptc = [0]
                    fin = [0]

                    def finalize(pso, n, dst_ap, dstres, sink_col=None):
                        r = rec[fin[0] % 2]
                        bc = bcs[fin[0] % 2]
                        fin[0] += 1
                        if sink_col is not None:
                            k.op('dve', lambda e: e.tensor_scalar(out=r[64:65, :n], in0=pso[64:65, :n], scalar1=esink[64:65, sink_col:sink_col + 1],
                                                                  scalar2=None, op0=ALU.add), R=[pso, esink], W=[r])
                            k.op('dve', lambda e: e.reciprocal(out=r[64:65, :n], in_=r[64:65, :n]), R=[r], W=[r])
                        else:
                            k.op('dve', lambda e: e.reciprocal(out=r[64:65, :n], in_=pso[64:65, :n]), R=[pso], W=[r])
                        pb = ps_get('nr', [6])
                        k.op('pe', lambda e: e.matmul(pb[0:64, :n], lhsT=ones32[64:65, 0:64], rhs=r[64:65, :n], start=True, stop=True),
                             R=[r, ones32], W=[pb])
                        k.op('act', lambda e: e.copy(out=bc[0:64, :n], in_=pb[0:64, :n]), R=[pb], W=[bc])
                        k.op('dve', lambda e: e.tensor_tensor(out=dst_ap, in0=pso[0:64, :n], in1=bc[0:64, :n], op=ALU.mult),
                             R=[pso, bc], W=[dstres])

                    qblocks = blocks if not last else blocks[:16]
                    for bi, (t0, ntok, v) in enumerate(qblocks):
                        latent = (v == 0)
                        hTb = hTb2[bi % 2]
                        Cb, Sb = Cb2[bi % 2], Sb2[bi % 2]
                        ld(hTb[:, :, :ntok], hTd[:, :, t0:t0 + ntok], stream='hq%d' % (bi % 2), R=[('hTd', bi)], W=[hTb])
                        ld(Cb[:, :ntok], ropeCd[:, t0:t0 + ntok], stream='rq%d' % (bi % 2), W=[Cb])
                        ld(Sb[:, :ntok], ropeSd[:, t0:t0 + ntok], stream='rq%d' % (bi % 2), W=[Sb])
                        qi = 0
                        for nm, base, gi in (('A', 0, 0), ('B', 1024, 4)):
                            for j in range(4):
                                psp = ps_get('kp', [2, 3])
                                psr = ps_get('kr', [4, 5])
                                for kc in range(8):
                                    k.op('pe', lambda e, kc=kc, psp=psp, c0=base + j * 128: e.matmul(
                                        psp[:, :ntok], lhsT=W2[:, kc, c0:c0 + 128], rhs=hTb[:, kc, :ntok], start=(kc == 0), stop=(kc == 7)),
                                        R=[W2, hTb], W=[psp])
                                for kc in range(8):
                                    k.op('pe', lambda e, kc=kc, psr=psr, c0=base + 512 + j * 128: e.matmul(
                                        psr[:, :ntok], lhsT=W2[:, kc, c0:c0 + 128], rhs=hTb[:, kc, :ntok], start=(kc == 0), stop=(kc == 7)),
                                        R=[W2, hTb], W=[psr])
                                normrope(psp, psr, gi, Cb, Sb, None, (QT[nm], j), ntok, tmps[qi % 2],
                                         split=(QT[nm][j][0][0:64, :ntok], QT[nm][j][1][64:128, :ntok]))
                                qi += 1
                        OaT = OT2['A'][bi % 2]
                        ObT = OT2['B'][bi % 2]
                        kts = list(range(NTL)) if latent else [64, 65]
                        for j in range(4):
                            for hh in range(2):
                                rows = slice(64 * hh, 64 * hh + 64)
                                pso = ps_get('o', [4, 5])
                                LA = 2

                                def emit_s(kt):
                                    pss = ps_get('s', [0, 1, 2, 3])
                                    k.op('pe', lambda e: e.matmul(pss[:, :ntok], lhsT=KTA[:, kt * 128:(kt + 1) * 128],
                                                                  rhs=QT['A'][j][hh][:, :ntok], start=True, stop=True),
                                         R=[KTA, (QT['A'], j)], W=[pss])
                                    return pss
                                Sq = [emit_s(kt) for kt in kts[:LA]]
                                for n_, kt in enumerate(kts):
                                    pss = Sq[n_]
                                    pt = PT[ptc[0] % 4]
                                    ptc[0] += 1
                                    k.op('act', lambda e, pss=pss, pt=pt: e.activation(out=pt[:, :ntok], in_=pss[:, :ntok], func=AF.Exp, scale=0.125),
                                         R=[pss], W=[pt])
                                    if n_ + LA < len(kts):
                                        Sq.append(emit_s(kts[n_ + LA]))
                                    k.op('pe', lambda e, pso=pso, pt=pt, kt=kt, n_=n_: e.matmul(
                                        pso[:, :ntok], lhsT=VAf[:, kt * 160 + hh * 80:kt * 160 + hh * 80 + 128], rhs=pt[:, :ntok], start=(n_ == 0), stop=(n_ == len(kts) - 1)),
                                        R=[VA, pt], W=[pso])
                                finalize(pso, ntok, OaT[rows, j, :ntok], (OaT, j, hh))
                        for j in range(4):
                            for hh in range(2):
                                rows = slice(64 * hh, 64 * hh + 64)
                                head = 4 * hh + j
                                pso = ps_get('o', [4, 5])
                                for qt in range(ntok // 128):
                                    cols = slice(qt * 128, (qt + 1) * 128)
                                    I = (t0 + qt * 128) // 128
                                    kl = [(64, None), (65, None)]
                                    if latent:
                                        if I - 1 >= 0:
                                            kl.append((I - 1, maskP))
                                        kl.append((I, None))
                                        if I + 1 < 64:
                                            kl.append((I + 1, maskN))
                                    LA = 2

                                    def emit_sb(kt):
                                        pss = ps_get('s', [0, 1, 2, 3])
                                        k.op('pe', lambda e: e.matmul(pss[:, 0:128], lhsT=KTB[:, kt * 128:(kt + 1) * 128],
                                                                      rhs=QT['B'][j][hh][:, cols], start=True, stop=True),
                                             R=[KTB, (QT['B'], j)], W=[pss])
                                        return pss
                                    Sq = [emit_sb(kt) for (kt, _m) in kl[:LA]]
                                    for n_, (kt, msk) in enumerate(kl):
                                        pss = Sq[n_]
                                        pt = PT[ptc[0] % 4]
                                        ptc[0] += 1
                                        k.op('act', lambda e, pss=pss, pt=pt: e.activation(out=pt[:, 0:128], in_=pss[:, 0:128], func=AF.Exp, scale=0.125),
                                             R=[pss], W=[pt])
                                        if n_ + LA < len(kl):
                                            Sq.append(emit_sb(kl[n_ + LA][0]))
                                        if msk is not None:
                                            k.op('pool', lambda e, pt=pt, msk=msk: e.tensor_tensor(out=pt[:, 0:128], in0=pt[:, 0:128], in1=msk[:],
                                                                                                    op=ALU.mult), R=[pt, msk], W=[pt])
                                        k.op('pe', lambda e, pso=pso, pt=pt, kt=kt, n_=n_, kl=kl: e.matmul(
                                            pso[:, cols], lhsT=VBf[:, kt * 160 + hh * 80:kt * 160 + hh * 80 + 128], rhs=pt[:, 0:128], start=(n_ == 0), stop=(n_ == len(kl) - 1)),
                                            R=[VB, pt], W=[pso])
                                finalize(pso, ntok, ObT[rows, j, :ntok], (ObT, j, hh), sink_col=head)
                        ld(oaTd[:, :, t0:t0 + ntok], OaT[:, :, :ntok], stream='ost%d' % (bi % 2), R=[(OaT, j, hh) for j in range(4) for hh in range(2)],
                           W=[('oaTd', bi)])
                        ld(obTd[:, :, t0:t0 + ntok], ObT[:, :, :ntok], stream='ost%d' % (bi % 2), R=[(ObT, j, hh) for j in range(4) for hh in range(2)],
                           W=[('obTd', bi)])
                    k.barrier()
            if stop == 'p2':
                break
            mblocks = blocks if not last else blocks[:16]

            def uoff(t0):
                return t0 + 1 if t0 < NL else t0 + 3

            with ExitStack() as p3a:
                Wcx = sb("Wcx%d" % l, [128, 8, 1024], BF16, p3a)
                ld(Wcx[:], wcxd[l], eng='pool', stream='wbig', W=[Wcx])
                hTb2 = [sb("hTu%d_%d" % (l, i), [128, 8, 512], BF16, p3a) for i in range(2)]
                ub2 = [sb("ub%d_%d" % (l, i), [128, 4, 512], F32, p3a) for i in range(2)]
                cs2 = [sb("cs%d_%d" % (l, i), [128, 512], F32, p3a) for i in range(2)]
                zt = sb("zt%d" % l, [128, 4, 2], F32, p3a)
                k.op('dve', lambda e: e.memset(zt[:], 0.0), W=[zt])
                ld(uTd[:, :, 0:1], zt[:, :, 0:1], stream='zp', R=[zt], W=['uzp'], slow=True)
                ld(uTd[:, :, NL + 1:NL + 3], zt[:, :, 0:2], stream='zp', R=[zt], W=['uzp'], slow=True)
                ld(uTd[:, :, NT + 3:NT + 4], zt[:, :, 0:1], stream='zp', R=[zt], W=['uzp'], slow=True)
                cc = 0
                for bi, (t0, ntok, v) in enumerate(mblocks):
                    hTb = hTb2[bi % 2]
                    ub = ub2[bi % 2]
                    ld(hTb[:, :, :ntok], hTd[:, :, t0:t0 + ntok], stream='hu%d' % (bi % 2), W=[hTb])
                    for c in range(4):
                        psc = ps_get('kp', [2, 3])
                        psx = ps_get('kr', [4, 5])
                        cs = cs2[cc % 2]
                        cc += 1
                        for kc in range(8):
                            k.op('pe', lambda e, kc=kc, psc=psc, c=c: e.matmul(psc[:, :ntok], lhsT=Wcx[:, kc, c * 128:(c + 1) * 128],
                                                                              rhs=hTb[:, kc, :ntok], start=(kc == 0), stop=(kc == 7)),
                                 R=[Wcx, hTb], W=[psc])
                        for kc in range(8):
                            k.op('pe', lambda e, kc=kc, psx=psx, c=c: e.matmul(psx[:, :ntok], lhsT=Wcx[:, kc, 512 + c * 128:512 + (c + 1) * 128],
                                                                              rhs=hTb[:, kc, :ntok], start=(kc == 0), stop=(kc == 7)),
                                 R=[Wcx, hTb], W=[psx])
                        k.op('act', lambda e, cs=cs, psc=psc: e.copy(out=cs[:, :ntok], in_=psc[:, :ntok]), R=[psc], W=[cs])
                        k.op('dve', lambda e, cs=cs, psx=psx, c=c, ub=ub: e.tensor_tensor(out=ub[:, c, :ntok], in0=psx[:, :ntok], in1=cs[:, :ntok],
                                                                                       op=ALU.mult), R=[psx, cs], W=[(ub, c)])
                    o0 = uoff(t0)
                    ld(uTd[:, :, o0:o0 + ntok], ub[:, :, :ntok], stream='ust%d' % (bi % 2), R=[(ub, c) for c in range(4)], W=[('uTd', bi)])
                k.barrier()

            with ExitStack() as p3b:
                W3 = sb("W3_%d" % l, [128, 8, 3584], BF16, p3b)
                Wbr = sb("Wbr%d" % l, [128, 12, D], BF16, p3b)
                ld(W3[:], w3d[l], eng='pool', stream='wbig', W=[W3])
                ld(Wbr[:], wbrd[l], eng='pool', stream='wbig', W=[Wbr])
                hTb2 = [sb("hTm%d_%d" % (l, i), [128, 8, 512], BF16, p3b) for i in range(2)]
                Oa2 = [sb("Oam%d_%d" % (l, i), [128, 4, 512], BF16, p3b) for i in range(2)]
                Ob2 = [sb("Obm%d_%d" % (l, i), [128, 4, 512], BF16, p3b) for i in range(2)]
                u2 = [sb("um%d_%d" % (l, i), [128, 4, 514], F32, p3b) for i in range(2)]
                OcT = sb("OcT%d" % l, [128, 4, 512], BF16, p3b)
                mT2 = [sb("mT%d_%d" % (l, i), [128, 8, 512], BF16, p3b) for i in range(2)]
                cva = [sb("cva%d_%d" % (l, i), [128, 512], F32, p3b) for i in range(2)]
                cvb = [sb("cvb%d_%d" % (l, i), [128, 512], F32, p3b) for i in range(2)]
                sg2 = [sb("sg%d_%d" % (l, i), [128, 512], F32, p3b) for i in range(3)]
                acc2 = [sb("acc%d_%d" % (l, i), [128, 512], F32, p3b) for i in range(2)]
                tm2 = [sb("tm%d_%d" % (l, i), [128, 512], F32, p3b) for i in range(2)]
                cnt = [0, 0, 0]
                for bi, (t0, ntok, v) in enumerate(mblocks):
                    hTb, Oa, Ob, ub, mT = hTb2[bi % 2], Oa2[bi % 2], Ob2[bi % 2], u2[bi % 2], mT2[bi % 2]
                    o0 = uoff(t0)
                    ld(hTb[:, :, :ntok], hTd[:, :, t0:t0 + ntok], stream='hm%d' % (bi % 2), W=[hTb])
                    ld(Oa[:, :, :ntok], oaTd[:, :, t0:t0 + ntok], stream='hm%d' % (bi % 2), W=[Oa])
                    ld(Ob[:, :, :ntok], obTd[:, :, t0:t0 + ntok], stream='hm%d' % (bi % 2), W=[Ob])
                    ld(ub[:, :, :ntok + 2], uTd[:, :, o0 - 1:o0 + ntok + 1], stream='hm%d' % (bi % 2), W=[ub])
                    for c in range(4):
                        psb = ps_get('kp', [2, 3])
                        for kc in range(8):
                            k.op('pe', lambda e, kc=kc, psb=psb, c=c: e.matmul(psb[:, :ntok], lhsT=W3[:, kc, c * 128:(c + 1) * 128],
                                                                              rhs=hTb[:, kc, :ntok], start=(kc == 0), stop=(kc == 7)),
                                 R=[W3, hTb], W=[psb])
                        a = cva[cnt[0] % 2]
                        cnt[0] += 1
                        k.op('pool', lambda e, a=a, c=c: e.tensor_scalar(out=a[:, :ntok], in0=ub[:, c, 0:ntok], scalar1=cw[:, c, 0:1], scalar2=None,
                                                                        op0=ALU.mult), R=[ub, cw], W=[a])
                        a2 = cvb[cnt[0] % 2]
                        for tap in (1, 2):
                            k.op('pool', lambda e, a2=a2, c=c, tap=tap: e.tensor_scalar(out=a2[:, :ntok], in0=ub[:, c, tap:ntok + tap], scalar1=cw[:, c, tap:tap + 1],
                                                                                       scalar2=None, op0=ALU.mult), R=[ub, cw], W=[a2])
                            k.op('pool', lambda e, a=a, a2=a2: e.tensor_tensor(out=a[:, :ntok], in0=a[:, :ntok], in1=a2[:, :ntok], op=ALU.add), R=[a, a2], W=[a])
                        k.op('dve', lambda e, a=a, c=c, psb=psb: e.tensor_tensor(out=OcT[:, c, :ntok], in0=psb[:, :ntok], in1=a[:, :ntok], op=ALU.mult),
                             R=[psb, a], W=[(OcT, c)])
                    Obr = [Oa, Ob, OcT]
                    for m in range(8):
                        acc = acc2[m % 2]
                        for br in range(3):
                            psg = ps_get('s', [0, 1])
                            psr = ps_get('kr', [4, 5])
                            sg = sg2[cnt[1] % 3]
                            cnt[1] += 1
                            c0 = 512 + br * 1024 + m * 128
                            for kc in range(8):
                                k.op('pe', lambda e, kc=kc, psg=psg, c0=c0: e.matmul(psg[:, :ntok], lhsT=W3[:, kc, c0:c0 + 128],
                                                                                    rhs=hTb[:, kc, :ntok], start=(kc == 0), stop=(kc == 7)),
                                     R=[W3, hTb], W=[psg])
                            k.op('act', lambda e, sg=sg, psg=psg: e.activation(out=sg[:, :ntok], in_=psg[:, :ntok], func=AF.Sigmoid), R=[psg], W=[sg])
                            Rr = [Wbr] + ([Oa] if br == 0 else [Ob] if br == 1 else [(OcT, c) for c in range(4)])
                            for c in range(4):
                                k.op('pe', lambda e, c=c, psr=psr, br=br, m=m: e.matmul(psr[:, :ntok], lhsT=Wbr[:, br * 4 + c, m * 128:(m + 1) * 128],
                                                                                       rhs=Obr[br][:, c, :ntok], start=(c == 0), stop=(c == 3)),
                                     R=Rr, W=[psr])
                            if br == 0:
                                k.op('dve', lambda e, psr=psr, sg=sg, acc=acc: e.tensor_tensor(out=acc[:, :ntok], in0=psr[:, :ntok], in1=sg[:, :ntok],
                                                                                            op=ALU.mult), R=[psr, sg], W=[acc])
                            else:
                                tm = tm2[cnt[2] % 2]
                                cnt[2] += 1
                                k.op('dve', lambda e, psr=psr, sg=sg, tm=tm: e.tensor_tensor(out=tm[:, :ntok], in0=psr[:, :ntok], in1=sg[:, :ntok],
                                                                                          op=ALU.mult), R=[psr, sg], W=[tm])
                                if br == 1:
                                    k.op('pool', lambda e, tm=tm, acc=acc: e.tensor_tensor(out=acc[:, :ntok], in0=acc[:, :ntok], in1=tm[:, :ntok],
                                                                                        op=ALU.add), R=[acc, tm], W=[acc])
                                else:
                                    k.op('pool', lambda e, tm=tm, acc=acc, m=m, mT=mT: e.tensor_tensor(out=mT[:, m, :ntok], in0=acc[:, :ntok], in1=tm[:, :ntok],
                                                                                                    op=ALU.add), R=[acc, tm], W=[(mT, m)])
                    ld(mTd[:, :, t0:t0 + ntok], mT[:, :, :ntok], stream='mst%d' % (bi % 2), R=[(mT, m) for m in range(8)], W=[('mTd', bi)])
                k.barrier()
            if stop == 'p3b':
                break

            pmoe = ExitStack()
            aff = sb("aff%d" % l, [128, NTL, NE], F32, pmoe)
            with ExitStack() as p3c:
                Wout = sb("Wout%d" % l, [128, 8, D], BF16, p3c)
                ld(Wout[:], woutd[l], eng='pool', stream='wbig', W=[Wout])
                Wr = sb("Wr%d" % l, [128, 8, NE], F32, p3c)
                ld(Wr[:], wrd[l], W=[Wr])
                bct = {}
                for vv in range(2):
                    for vi, nm in ((0, 'gt1'), (1, 'sh2'), (2, 'A2')):
                        t_ = sb("bc_%s%d_%d" % (nm, vv, l), [128, D], F32, p3c)
                        ld(t_[:], bcd[vv, vi], W=[t_])
                        bct[(vv, nm)] = t_
                mT2 = [sb("mTo%d_%d" % (l, i), [128, 8, 512], BF16, p3c) for i in range(2)]
                xt2 = [sb("xo%d_%d" % (l, i), [128, D], F32, p3c) for i in range(2)]
                xw2 = [sb("xw%d_%d" % (l, i), [128, D], F32, p3c) for i in range(2)]
                h22 = [sb("h2%d_%d" % (l, i), [128, D], F32, p3c) for i in range(2)]
                h2b2 = [sb("h2b%d_%d" % (l, i), [128, D], BF16, p3c) for i in range(2)]
                junk = sb("junk3%d" % l, [128, D], F32, p3c)
                h2T = [sb("h2T%d_%d" % (l, i), [128, 8, 128], F32, p3c) for i in range(2)]
                ex2 = [sb("ex%d_%d" % (l, i), [128, NE], F32, p3c) for i in range(2)]
                sm = sb("sm%d" % l, [128, 8], F32, p3c)
                tg = 0
                for bi, (t0, ntok, v) in enumerate(mblocks):
                    mT = mT2[bi % 2]
                    ld(mT[:, :, :ntok], mTd[:, :, t0:t0 + ntok], stream='mo%d' % (bi % 2), W=[mT])
                    for tt in range(ntok // 128):
                        i2 = tg % 2
                        tg += 1
                        xt, xw, h2, h2b, hT_, ex = xt2[i2], xw2[i2], h22[i2], h2b2[i2], h2T[i2], ex2[i2]
                        r0 = t0 + tt * 128
                        ti = r0 // 128
                        ld(xt[:], xs[r0:r0 + 128, :], stream='xo%d' % i2, W=[xt])
                        for half in range(2):
                            hs = slice(half * 512, (half + 1) * 512)
                            po = ps_get('kp', [2, 3])
                            for m in range(8):
                                k.op('pe', lambda e, m=m, po=po, hs=hs, tt=tt: e.matmul(po[:, :], lhsT=mT[:, m, tt * 128:(tt + 1) * 128], rhs=Wout[:, m, hs],
                                                                                       start=(m == 0), stop=(m == 7)), R=[Wout, mT], W=[po])
                            k.op('dve', lambda e, po=po, hs=hs, xw=xw: e.tensor_tensor(out=xw[:, hs], in0=po[:, :], in1=bct[(v, 'gt1')][:, hs], op=ALU.mult),
                                 R=[po, bct[(v, 'gt1')]], W=[(xw, half)])
                            k.op('pool', lambda e, hs=hs, xw=xw, xt=xt: e.tensor_tensor(out=xw[:, hs], in0=xw[:, hs], in1=xt[:, hs], op=ALU.add),
                                 R=[(xw, half), xt], W=[(xw, half)])
                        ld(xs[r0:r0 + 128, :], xw[:], stream='xst%d' % i2, R=[(xw, 0), (xw, 1)], W=[('xs', ti)])
                        col = 2 + i2
                        k.op('pool', lambda e, col=col: e.memset(ss[:, col:col + 1], 0.0), W=[('ss', col)])
                        k.op('act', lambda e, xw=xw, col=col: e.activation(out=junk[:], in_=xw[:], func=AF.Square, accum_out=ss[:, col:col + 1]),
                             R=[(xw, 0), (xw, 1)], W=[junk, ('ss', col)])
                        k.op('act', lambda e, col=col: e.activation(out=rsd[:, col:col + 1], in_=ss[:, col:col + 1], func=AF.Sqrt, scale=1.0 / D, bias=EPS),
                             R=[('ss', col)], W=[('rsd', col)])
                        k.op('dve', lambda e, col=col: e.reciprocal(out=rsd[:, col:col + 1], in_=rsd[:, col:col + 1]), R=[('rsd', col)], W=[('rsd', col)])
                        k.op('dve', lambda e, xw=xw, h2=h2, col=col: e.scalar_tensor_tensor(out=h2[:], in0=xw[:], scalar=rsd[:, col:col + 1], in1=bct[(v, 'A2')][:],
                                                                                         op0=ALU.mult, op1=ALU.mult),
                             R=[(xw, 0), (xw, 1), ('rsd', col), bct[(v, 'A2')]], W=[h2])
                        k.op('pool', lambda e, h2=h2: e.tensor_tensor(out=h2[:], in0=h2[:], in1=bct[(v, 'sh2')][:], op=ALU.add), R=[h2, bct[(v, 'sh2')]], W=[h2])
                        k.op('act', lambda e, h2=h2, h2b=h2b: e.copy(out=h2b[:], in_=h2[:]), R=[h2], W=[h2b])
                        ld(h2d[r0:r0 + 128, :], h2b[:], stream='h2st%d' % i2, R=[h2b], W=[('h2d', ti)])
                        for half in range(2):
                            pt = ps_get('tr', [0, 1])
                            for q in range(4):
                                kc = half * 4 + q
                                k.op('pe', lambda e, pt=pt, q=q, kc=kc, h2=h2: e.transpose(pt[:, q * 128:(q + 1) * 128], h2[:, kc * 128:(kc + 1) * 128], ident[:]),
                                     R=[h2, ident], W=[pt])
                            eng = 'act' if half == 0 else 'dve'
                            src = pt[:, :]
                            dst = hT_[:].rearrange("p a b -> p (a b)")[:, half * 512:(half + 1) * 512]
                            if eng == 'act':
                                k.op('act', lambda e, dst=dst, src=src: e.copy(out=dst, in_=src), R=[pt], W=[(hT_, half)])
                            else:
                                k.op('dve', lambda e, dst=dst, src=src: e.tensor_copy(out=dst, in_=src), R=[pt], W=[(hT_, half)])
                        pl = ps_get('nr', [6])
                        for kc in range(8):
                            k.op('pe', lambda e, kc=kc, pl=pl, hT_=hT_: e.matmul(pl[:, 0:NE], lhsT=hT_[:, kc, :], rhs=Wr[:, kc, :], start=(kc == 0), stop=(kc == 7)),
                                 R=[(hT_, 0), (hT_, 1), Wr], W=[pl])
                        c2 = 4 * i2
                        k.op('dve', lambda e, pl=pl, c2=c2: e.tensor_reduce(out=sm[:, c2:c2 + 1], in_=pl[:, 0:NE], axis=AX.X, op=ALU.max), R=[pl], W=[('sm', i2)])
                        k.op('dve', lambda e, c2=c2: e.tensor_scalar(out=sm[:, c2 + 1:c2 + 2], in0=sm[:, c2:c2 + 1], scalar1=-1.0, scalar2=None, op0=ALU.mult),
                             R=[('sm', i2)], W=[('sm', i2)])
                        k.op('pool', lambda e, c2=c2: e.memset(sm[:, c2 + 2:c2 + 3], 0.0), R=[('sm', i2)], W=[('sm', i2)])
                        k.op('act', lambda e, pl=pl, ex=ex, c2=c2: e.activation(out=ex[:], in_=pl[:, 0:NE], func=AF.Exp, bias=sm[:, c2 + 1:c2 + 2],
                                                                              accum_out=sm[:, c2 + 2:c2 + 3]), R=[pl, ('sm', i2)], W=[ex, ('sm', i2)])
                        k.op('dve', lambda e, c2=c2: e.reciprocal(out=sm[:, c2 + 3:c2 + 4], in_=sm[:, c2 + 2:c2 + 3]), R=[('sm', i2)], W=[('sm', i2)])
                        k.op('dve', lambda e, ex=ex, ti=ti, c2=c2: e.tensor_scalar(out=aff[:, ti, :], in0=ex[:], scalar1=sm[:, c2 + 3:c2 + 4], scalar2=None, op0=ALU.mult),
                             R=[ex, ('sm', i2)], W=[(aff, ti)])
                        ld(affd[r0:r0 + 128, :], aff[:, ti, :], stream='afst%d' % i2, R=[(aff, ti)], W=[('affd', ti)])
                k.barrier()
            if stop == 'p3c':
                pmoe.close()
                break

            with pmoe:
                gt2 = [sb("gt2_%d_%d" % (l, vv), [128, D], F32, pmoe) for vv in range(2)]
                for vv in range(2):
                    ld(gt2[vv][:], bcd[vv, 3], W=[gt2[vv]])
                NSC = 9
                NJ = NE * NSC
                idx_all = sb("idx_all%d" % l, [128, NJ], I32, pmoe)
                gate_all = sb("gate_all%d" % l, [128, NJ], F32, pmoe)
                k.op('pool', lambda e: e.memset(idx_all[:], 1 << 20), W=[idx_all])
                k.op('pool', lambda e: e.memset(gate_all[:], 0.0), W=[gate_all])
                sets = [(0, 64, 1024, 0)] + ([] if last else [(64, 2, 32, 1)])
                nst = 8 if last else 9
                with ExitStack() as pth:
                    iota = sb("iota%d" % l, [128, 1024], F32, pth)
                    ld(iota[:], iotad.ap(), W=[iota])
                    comb = sb("comb%d" % l, [128, NTL, NE, 5], BF16, pth)
                    ld(comb[:], combd.ap(), eng='pool', W=[comb])
                    posm = sb("posm%d" % l, [128, NTL, NE], F32, pth)
                    lo = sb("lo%d" % l, [128, NE], F32, pth)
                    hi = sb("hi%d" % l, [128, NE], F32, pth)
                    mid = sb("mid%d" % l, [128, NE], F32, pth)
                    ge = sb("ge%d" % l, [128, NE], F32, pth)
                    g2 = sb("g2%d" % l, [128, NE], F32, pth)
                    cntp = sb("cntp%d" % l, [128, NE], F32, pth)
                    cmp_ = sb("cmp%d" % l, [128, 64, NE], F32, pth)
                    maskb = sb("maskb%d" % l, [128, 64, NE], BF16, pth)
                    inc = [sb("inc%d_%d" % (l, i), [128, 64, NE], F32, pth) for i in range(2)]
                    tot = sb("tot%d" % l, [128, 64, NE], F32, pth)
                    r1 = sb("r1_%d" % l, [128, NTL, NE], F32, pth)
                    r2 = sb("r2_%d" % l, [128, NTL, NE], F32, pth)
                    k.op('pool', lambda e: e.tensor_copy(out=comb[:, :, :, 2], in_=aff[:]), R=[aff], W=[comb])
                    k.op('dve', lambda e: e.tensor_tensor(out=r1[:], in0=aff[:], in1=comb[:, :, :, 2], op=ALU.subtract), R=[aff, comb], W=[r1])
                    k.op('pool', lambda e: e.tensor_copy(out=comb[:, :, :, 3], in_=r1[:]), R=[r1], W=[comb])
                    k.op('dve', lambda e: e.tensor_tensor(out=r2[:], in0=r1[:], in1=comb[:, :, :, 3], op=ALU.subtract), R=[r1, comb], W=[r2])
                    k.op('pool', lambda e: e.tensor_copy(out=comb[:, :, :, 4], in_=r2[:]), R=[r2], W=[comb])
                    for (ti0, T, cap, vv) in sets:
                        affs = aff[:, ti0:ti0 + T, :]
                        k.op('dve', lambda e: e.memset(lo[:], 0.0), W=[lo])
                        k.op('dve', lambda e: e.memset(hi[:], 1.0), W=[hi])
                        for it in range(32):
                            k.op('dve', lambda e: e.tensor_tensor(out=mid[:], in0=lo[:], in1=hi[:], op=ALU.add), R=[lo, hi], W=[mid])
                            k.op('dve', lambda e: e.tensor_scalar(out=mid[:], in0=mid[:], scalar1=0.5, scalar2=None, op0=ALU.mult), R=[mid], W=[mid])
                            k.op('dve', lambda e, T=T, affs=affs: e.tensor_tensor(out=cmp_[:, :T, :], in0=affs, in1=mid[:].unsqueeze(1).to_broadcast([128, T, NE]),
                                                                                 op=ALU.is_ge), R=[aff, mid], W=[cmp_])
                            k.op('dve', lambda e, T=T: e.tensor_reduce(out=cntp[:], in_=cmp_[:, :T, :].rearrange("p t e -> p e t"), axis=AX.X, op=ALU.add),
                                 R=[cmp_], W=[cntp])
                            pc = ps_get('nr', [6])
                            k.op('pe', lambda e, pc=pc: e.matmul(pc[:, 0:NE], lhsT=ones32[:], rhs=cntp[:], start=True, stop=True), R=[ones32, cntp], W=[pc])
                            k.op('dve', lambda e, pc=pc, cap=cap: e.tensor_scalar(out=ge[:], in0=pc[:, 0:NE], scalar1=float(cap) - 0.5, scalar2=None, op0=ALU.is_ge),
                                 R=[pc], W=[ge])
                            k.op('dve', lambda e: e.tensor_tensor(out=g2[:], in0=ge[:], in1=mid[:], op=ALU.mult), R=[ge, mid], W=[g2])
                            k.op('dve', lambda e: e.tensor_tensor(out=lo[:], in0=lo[:], in1=g2[:], op=ALU.max), R=[lo, g2], W=[lo])
                            k.op('dve', lambda e: e.scalar_tensor_tensor(out=g2[:], in0=ge[:], scalar=2.0, in1=mid[:], op0=ALU.mult, op1=ALU.add),
                                 R=[ge, mid, g2], W=[g2])
                            k.op('dve', lambda e: e.tensor_tensor(out=hi[:], in0=hi[:], in1=g2[:], op=ALU.min), R=[hi, g2], W=[hi])
                        k.op('dve', lambda e, T=T, affs=affs: e.tensor_tensor(out=cmp_[:, :T, :], in0=affs, in1=lo[:].unsqueeze(1).to_broadcast([128, T, NE]),
                                                                             op=ALU.is_ge), R=[aff, lo], W=[cmp_])
                        k.op('pool', lambda e, T=T: e.tensor_copy(out=maskb[:, :T, :], in_=cmp_[:, :T, :]), R=[cmp_], W=[maskb])
                        ncol = T * NE
                        mb = maskb[:].rearrange("p t e -> p (t e)")
                        totf = tot[:].rearrange("p t e -> p (t e)")
                        pp = [PS[2], PS[3]]
                        pq = [PS[4], PS[5]]
                        nhf = (ncol + 511) // 512
                        for hf in range(nhf):
                            n_ = min(512, ncol - hf * 512)
                            k.op('pe', lambda e, hf=hf, n_=n_: e.matmul(pp[hf][:, :n_], lhsT=triL[:], rhs=mb[:, hf * 512:hf * 512 + n_], start=True, stop=True),
                                 R=[triL, maskb], W=[pp[hf]])
                            k.op('pe', lambda e, hf=hf, n_=n_: e.matmul(pq[hf][:, :n_], lhsT=onesb[:], rhs=mb[:, hf * 512:hf * 512 + n_], start=True, stop=True),
                                 R=[onesb, maskb], W=[pq[hf]])
                            k.op('act', lambda e, hf=hf, n_=n_: e.copy(out=totf[:, hf * 512:hf * 512 + n_], in_=pq[hf][:, :n_]), R=[pq[hf]], W=[tot])
                        k.op('pool', lambda e, T=T: e.tensor_copy(out=inc[0][:, :T, :], in_=tot[:, :T, :]), R=[tot], W=[inc[0]])
                        cur = 0
                        s_ = 1
                        while s_ < T:
                            a_, b_ = inc[cur], inc[1 - cur]
                            k.op('dve', lambda e, a_=a_, b_=b_, s_=s_, T=T: e.tensor_tensor(out=b_[:, s_:T, :], in0=a_[:, s_:T, :], in1=a_[:, 0:T - s_, :], op=ALU.add),
                                 R=[a_], W=[b_])
                            k.op('pool', lambda e, a_=a_, b_=b_, s_=s_: e.tensor_copy(out=b_[:, 0:s_, :], in_=a_[:, 0:s_, :]), R=[a_], W=[b_])
                            cur = 1 - cur
                            s_ *= 2
                        incf = inc[cur]
                        oth = inc[1 - cur]
                        othf = oth[:].rearrange("p t e -> p (t e)")
                        k.op('dve', lambda e, T=T: e.tensor_tensor(out=oth[:, :T, :], in0=incf[:, :T, :], in1=tot[:, :T, :], op=ALU.subtract), R=[incf, tot], W=[oth])
                        for hf in range(nhf):
                            n_ = min(512, ncol - hf * 512)
                            k.op('dve', lambda e, hf=hf, n_=n_: e.tensor_tensor(out=othf[:, hf * 512:hf * 512 + n_], in0=othf[:, hf * 512:hf * 512 + n_],
                                                                              in1=pp[hf][:, :n_], op=ALU.add), R=[oth, pp[hf]], W=[oth])
                        k.op('dve', lambda e, T=T, ti0=ti0: e.scalar_tensor_tensor(out=posm[:, ti0:ti0 + T, :], in0=oth[:, :T, :], scalar=1.0, in1=cmp_[:, :T, :],
                                                                                  op0=ALU.add, op1=ALU.mult), R=[oth, cmp_], W=[posm])
                        k.op('dve', lambda e, T=T, ti0=ti0: e.tensor_scalar(out=posm[:, ti0:ti0 + T, :], in0=posm[:, ti0:ti0 + T, :], scalar1=-1.0, scalar2=None,
                                                                           op0=ALU.add), R=[posm], W=[posm])
                    k.barrier()
                    if debug and stop == 'p4a':
                        dpos = nc.dram_tensor("dbgposm", [128, NTL, NE], F32, kind="ExternalOutput")
                        ld(dpos.ap(), posm[:], R=[posm], W=['dbgo'])
                        k.barrier()
                        break

                    Sb_ = [sb("Sone%d_%d" % (l, i), [128, 1024], BF16, pth) for i in range(3)]
                    rows5 = sb("rows5_%d" % l, [8, 1056], F32, pth)
                    t5 = sb("t5_%d" % l, [128, 48], F32, pth)
                    idxf = sb("idxf%d" % l, [128, NSC], F32, pth)
                    g1 = sb("g1_%d" % l, [128, NSC], F32, pth)
                    rr = 0
                    for e_ in range(NE):
                        for (ti0, T, cap, vv) in sets:
                            off = 0 if vv == 0 else 1024
                            nh = (cap + 511) // 512
                            pi = [PS[0], PS[1]]
                            for i_ in range(T):
                                S_ = Sb_[rr % 3]
                                rr += 1
                                ti = ti0 + i_
                                k.op('dve', lambda e, S_=S_, ti=ti, e_=e_, cap=cap: e.tensor_scalar(out=S_[:, :cap], in0=iota[:, :cap], scalar1=posm[:, ti, e_:e_ + 1],
                                                                                                   scalar2=None, op0=ALU.is_equal), R=[iota, posm], W=[S_])
                                for hf in range(nh):
                                    n_ = min(512, cap - hf * 512)
                                    k.op('pe', lambda e, S_=S_, ti=ti, hf=hf, n_=n_, i_=i_, T=T, e_=e_: e.matmul(pi[hf][0:5, :n_], lhsT=comb[:, ti, e_, :],
                                                                                                              rhs=S_[:, hf * 512:hf * 512 + n_],
                                                                                                              start=(i_ == 0), stop=(i_ == T - 1)), R=[comb, S_], W=[pi[hf]])
                            for hf in range(nh):
                                n_ = min(512, cap - hf * 512)
                                k.op('act', lambda e, hf=hf, n_=n_, off=off: e.copy(out=rows5[0:5, off + hf * 512:off + hf * 512 + n_], in_=pi[hf][0:5, :n_]),
                                     R=[pi[hf]], W=[rows5])
                        ptx = ps_get('nr', [6])
                        for s in range(nst):
                            prow = 128 if s < 8 else 32
                            k.op('pe', lambda e, s=s, prow=prow: e.transpose(ptx[0:prow, 5 * s:5 * s + 5], rows5[0:5, s * 128:s * 128 + prow], ident[0:5, 0:5]),
                                 R=[rows5, ident], W=[ptx])
                        k.op('act', lambda e: e.copy(out=t5[:, 0:40], in_=ptx[:, 0:40]), R=[ptx], W=[t5])
                        if nst == 9:
                            k.op('act', lambda e: e.copy(out=t5[0:32, 40:45], in_=ptx[0:32, 40:45]), R=[ptx], W=[t5])
                        for (c0, c1, prow) in ((0, 8, 128),) + (((8, 9, 32),) if nst == 9 else ()):
                            n5 = slice(5 * c0, 5 * c1, 5)
                            k.op('dve', lambda e, c0=c0, c1=c1, prow=prow: e.scalar_tensor_tensor(
                                out=idxf[0:prow, c0:c1], in0=t5[0:prow, 5 * c0:5 * c1:5], scalar=128.0, in1=t5[0:prow, 5 * c0 + 1:5 * c1:5], op0=ALU.mult, op1=ALU.add),
                                R=[t5], W=[idxf])
                            k.op('dve', lambda e, c0=c0, c1=c1, prow=prow, e_=e_: e.tensor_copy(out=idx_all[0:prow, e_ * NSC + c0:e_ * NSC + c1], in_=idxf[0:prow, c0:c1]),
                                 R=[idxf], W=[idx_all])
                            k.op('dve', lambda e, c0=c0, c1=c1, prow=prow: e.tensor_tensor(out=g1[0:prow, c0:c1], in0=t5[0:prow, 5 * c0 + 2:5 * c1:5],
                                                                                         in1=t5[0:prow, 5 * c0 + 3:5 * c1:5], op=ALU.add), R=[t5], W=[g1])
                            k.op('dve', lambda e, c0=c0, c1=c1, prow=prow, e_=e_: e.tensor_tensor(out=gate_all[0:prow, e_ * NSC + c0:e_ * NSC + c1], in0=g1[0:prow, c0:c1],
                                                                                               in1=t5[0:prow, 5 * c0 + 4:5 * c1:5], op=ALU.add), R=[g1, t5], W=[gate_all])
                    k.barrier()
                if debug and stop == 'p4a':
                    break
                if debug and stop == 'p4b':
                    di = nc.dram_tensor("dbgidx", [128, NJ], I32, kind="ExternalOutput")
                    dg = nc.dram_tensor("dbggate", [128, NJ], F32, kind="ExternalOutput")
                    ld(di.ap(), idx_all[:], R=[idx_all], W=['dbgo'])
                    ld(dg.ap(), gate_all[:], R=[gate_all], W=['dbgo'])
                    k.barrier()
                    break

                with ExitStack() as pgl:
                    xgt = sb("xgt%d" % l, [128, D], BF16, pgl)
                    k.op('pool', lambda e: e.memset(xgt[:], 0.0), W=[xgt])
                    k.barrier()
                    gather_loop(idx_all, h2d, xgd, xgt, NJ, "g%d" % l)
                    k.op('pool', lambda e: e.memset(xgt[:, 0:2], 0.0), W=[xgt])
                    k.barrier()

                with ExitStack() as pex:
                    Wg2 = [sb("Wg%d_%d" % (l, i), [128, 8, D], BF16, pex) for i in range(2)]
                    Wu2 = [sb("Wu%d_%d" % (l, i), [128, 8, D], BF16, pex) for i in range(2)]
                    Wd2 = [sb("Wd%d_%d" % (l, i), [128, 8, D], BF16, pex) for i in range(2)]
                    xg = sb("xg%d" % l, [128, NSC, D], BF16, pex)
                    xsT = sb("xsT%d" % l, [128, 8, 1056], BF16, pex)
                    hd = sb("hd%d" % l, [128, 8, 1056], BF16, pex)
                    sa2 = [sb("sa%d_%d" % (l, i), [128, 512], F32, pex) for i in range(2)]
                    yo2 = [sb("yo%d_%d" % (l, i), [128, D], F32, pex) for i in range(2)]
                    cnt_ = [0]

                    def load_expert(e_, slot):
                        ld(Wg2[slot][:], wegd[l, e_], eng='pool', W=[Wg2[slot]])
                        ld(Wu2[slot][:], weud[l, e_], eng='pool', W=[Wu2[slot]])
                        ld(Wd2[slot][:], wedd[l, e_], eng='pool', W=[Wd2[slot]])

                    load_expert(0, 0)
                    groups = [(0, 512), (512, 512)] + ([(1024, 32)] if nst == 9 else [])
                    for e_ in range(NE):
                        slot = e_ % 2
                        if e_ + 1 < NE:
                            load_expert(e_ + 1, 1 - slot)
                        Wg, Wu, Wd = Wg2[slot], Wu2[slot], Wd2[slot]
                        j0 = e_ * NSC
                        ld(xg[:, 0:nst, :], xgd[j0 * 128:(j0 + nst) * 128, :].rearrange("(s p) d -> p s d", p=128), W=[xg])
                        for kc in range(8):
                            for s in range(8):
                                k.op('pe', lambda e, s=s, kc=kc: e.transpose(PSB[:, s * 128:(s + 1) * 128], xg[:, s, kc * 128:(kc + 1) * 128], identb[:]),
                                     R=[xg, identb], W=[PSB])
                            if kc % 2 == 0:
                                k.op('act', lambda e, kc=kc: e.copy(out=xsT[:, kc, 0:1024], in_=PSB[:, 0:1024]), R=[PSB], W=[(xsT, kc)])
                            else:
                                k.op('dve', lambda e, kc=kc: e.tensor_copy(out=xsT[:, kc, 0:1024], in_=PSB[:, 0:1024]), R=[PSB], W=[(xsT, kc)])
                        if nst == 9:
                            for kc in range(8):
                                k.op('pe', lambda e, kc=kc: e.transpose(PSB[:, kc * 32:(kc + 1) * 32], xg[0:32, 8, kc * 128:(kc + 1) * 128], identb[0:32, 0:32]),
                                     R=[xg, identb], W=[PSB])
                            for kc in range(8):
                                k.op('act', lambda e, kc=kc: e.copy(out=xsT[:, kc, 1024:1056], in_=PSB[:, kc * 32:(kc + 1) * 32]), R=[PSB], W=[(xsT, kc)])
                        xr = [(xsT, kc) for kc in range(8)]
                        for fc in range(8):
                            for (c0, n_) in groups:
                                pa = ps_get('s', [0, 1])
                                pu = ps_get('kr', [4, 5])
                                sa = sa2[cnt_[0] % 2]
                                cnt_[0] += 1
                                for kc in range(8):
                                    k.op('pe', lambda e, kc=kc, fc=fc, c0=c0, n_=n_, pa=pa: e.matmul(pa[:, :n_], lhsT=Wg[:, kc, fc * 128:(fc + 1) * 128],
                                                                                                    rhs=xsT[:, kc, c0:c0 + n_], start=(kc == 0), stop=(kc == 7)),
                                         R=[Wg] + xr, W=[pa])
                                for kc in range(8):
                                    k.op('pe', lambda e, kc=kc, fc=fc, c0=c0, n_=n_, pu=pu: e.matmul(pu[:, :n_], lhsT=Wu[:, kc, fc * 128:(fc + 1) * 128],
                                                                                                    rhs=xsT[:, kc, c0:c0 + n_], start=(kc == 0), stop=(kc == 7)),
                                         R=[Wu] + xr, W=[pu])
                                k.op('act', lambda e, sa=sa, pa=pa, n_=n_: e.activation(out=sa[:, :n_], in_=pa[:, :n_], func=AF.Silu), R=[pa], W=[sa])
                                k.op('dve', lambda e, sa=sa, pu=pu, n_=n_, fc=fc, c0=c0: e.tensor_tensor(out=hd[:, fc, c0:c0 + n_], in0=pu[:, :n_], in1=sa[:, :n_],
                                                                                                      op=ALU.mult), R=[pu, sa], W=[(hd, fc)])
                        hr = [(hd, fc) for fc in range(8)]
                        for s in range(nst):
                            prow = 128 if s < 8 else 32
                            vv = 0 if s < 8 else 1
                            yo = yo2[s % 2]
                            for half in range(2):
                                hs = slice(half * 512, (half + 1) * 512)
                                py = ps_get('kp', [2, 3]) if half == 0 else ps_get('nr', [6])
                                for fc in range(8):
                                    k.op('pe', lambda e, fc=fc, s=s, py=py, hs=hs, prow=prow: e.matmul(py[0:prow, :], lhsT=hd[:, fc, s * 128:s * 128 + prow], rhs=Wd[:, fc, hs],
                                                                                                      start=(fc == 0), stop=(fc == 7)), R=[Wd] + hr, W=[py])
                                k.op('dve', lambda e, py=py, yo=yo, hs=hs, s=s, vv=vv, prow=prow, j0=j0: e.scalar_tensor_tensor(
                                    out=yo[0:prow, hs], in0=py[0:prow, :], scalar=gate_all[0:prow, j0 + s:j0 + s + 1], in1=gt2[vv][0:prow, hs], op0=ALU.mult, op1=ALU.mult),
                                    R=[py, gate_all, gt2[vv]], W=[(yo, half)])
                            ld(Yd[(j0 + s) * 128:(j0 + s) * 128 + prow, :], yo[0:prow, :], R=[(yo, 0), (yo, 1)], W=[('Yd', j0 + s)])
                    k.barrier()

                with ExitStack() as psl:
                    yt = sb("yt%d" % l, [128, D], F32, psl)
                    k.op('pool', lambda e: e.memset(yt[:], 0.0), W=[yt])
                    k.barrier()
                    scatter_loop(idx_all, Yd, xs, yt, NJ, "s%d" % l)
                    k.op('pool', lambda e: e.memset(yt[:, 0:2], 0.0), W=[yt])
                    k.barrier()
        k.barrier()
    return nc


def _colmajor(w):
    return np.ascontiguousarray(w.reshape(8, 128, -1).transpose(1, 0, 2))


def _consts():
    ident = np.eye(128, dtype=np.float32)
    blk = (np.arange(128)[:, None] // 64 == np.arange(128)[None, :] // 64).astype(np.float32)
    p = np.arange(128)
    triL = (p[:, None] < p[None, :]).astype(np.float32)
    maskP = (p[:, None] >= p[None, :]).astype(np.float32)
    maskN = (p[:, None] <= p[None, :]).astype(np.float32)
    cm = np.ascontiguousarray(np.stack([ident, blk, triL, maskP, maskN], axis=1))
    iota = np.ascontiguousarray(np.broadcast_to(np.arange(1024, dtype=np.float32), (128, 1024)))
    tidc = np.zeros((128, NTL, NE, 5), np.float32)
    tidc[:, :, :, 0] = np.arange(NTL)[None, :, None]
    tidc[:, :, :, 1] = np.arange(128)[:, None, None]
    t = np.arange(NL)
    row = (t // 64).astype(np.float32)
    colp = (t % 64).astype(np.float32)
    inv = np.power(np.float32(10000.0), -(np.arange(16, dtype=np.float32) / np.float32(16))).astype(np.float32)
    ang = np.concatenate([row[:, None] * inv[None, :], colp[:, None] * inv[None, :]], axis=-1).astype(np.float32)
    cos = np.cos(ang).astype(np.float32)
    sin = np.sin(ang).astype(np.float32)
    C = np.ones((128, NT), np.float32)
    S = np.zeros((128, NT), np.float32)
    for r in range(128):
        i = r % 64
        f = i % 32
        C[r, :NL] = cos[:, f]
        S[r, :NL] = sin[:, f] * (-1.0 if i < 32 else 1.0)
    return cm, iota, tidc, C, S


def _swap_halves(cols):
    cols = np.asarray(cols)
    return (cols // 64) * 64 + ((cols % 64) + 32) % 64


def prep_inputs(inp):
    L = inp['w_ada'].shape[0]
    cm, iota, tidc, C, S = _consts()
    f = lambda a: np.ascontiguousarray(a, dtype=np.float32)
    w_in = inp['w_in']
    kA, vA, kB, vB = np.arange(0, 128), np.arange(128, 256), np.arange(256, 384), np.arange(384, 512)
    qa0, qb0, cv0, gt0 = 512, 1024, 1536, 3072
    qorder = np.concatenate([np.concatenate([np.arange(j * 64, j * 64 + 64), np.arange((4 + j) * 64, (4 + j) * 64 + 64)]) for j in range(4)])
    c1 = np.concatenate([kA, kB, _swap_halves(kA), _swap_halves(kB), vA, vB])
    c2 = np.concatenate([qa0 + qorder, qa0 + _swap_halves(qorder), qb0 + qorder, qb0 + _swap_halves(qorder)])
    ccx = np.concatenate([np.arange(cv0 + 512, cv0 + 1024), np.arange(cv0 + 1024, cv0 + 1536)])
    c3 = np.concatenate([np.arange(cv0, cv0 + 512), np.arange(gt0, gt0 + 3072)])
    shared = {}
    shared['w_ada'] = f(np.stack([_colmajor(inp['w_ada'][l]) for l in range(L)]))
    bcol = np.stack([inp['b_ada'][l].reshape(48, 128).T for l in range(L)])
    shared['b_col2'] = f(np.repeat(bcol, 2, axis=2))
    sel = [slice(2048, 3072), slice(3072, 4096), slice(4096, 5120), slice(5120, 6144)]
    shared['b_bc'] = f(np.stack([np.stack([np.broadcast_to(inp['b_ada'][l][s], (128, 1024)) for s in sel], axis=1) for l in range(L)]))
    gc = np.zeros((L, 128, 2, 8, 2), np.float32)
    for l in range(L):
        gc[l, :, 0, :, :] = inp['g_mix'][l].reshape(8, 128).T[:, :, None]
        gc[l, :, 1, :, :] = inp['g_ffn'][l].reshape(8, 128).T[:, :, None]
    shared['gcol2'] = gc
    shared['gffn_bc'] = f(np.stack([np.broadcast_to(inp['g_ffn'][l], (128, 1024)) for l in range(L)]))
    shared['w1'] = f(np.stack([_colmajor(w_in[l][:, c1]) for l in range(L)]))
    shared['w2'] = f(np.stack([_colmajor(w_in[l][:, c2]) for l in range(L)]))
    shared['wcx'] = f(np.stack([_colmajor(w_in[l][:, ccx]) for l in range(L)]))
    shared['w3'] = f(np.stack([_colmajor(w_in[l][:, c3]) for l in range(L)]))
    wbr = np.zeros((L, 128, 12, 1024), np.float32)
    for l in range(L):
        for br in range(3):
            wb = inp['w_branch'][l, br]
            if br < 2:
                wb = wb[qorder]
            wbr[l, :, br * 4:(br + 1) * 4, :] = wb.reshape(4, 128, 1024).transpose(1, 0, 2)
    shared['wbr'] = wbr
    shared['wout'] = f(np.stack([_colmajor(inp['w_out'][l]) for l in range(L)]))
    hgv = np.zeros((L, 128, 8), np.float32)
    sw = (np.arange(64) + 32) % 64
    for l in range(L):
        for i, g in enumerate((inp['qg_a'][l], inp['kg_a'][l], inp['qg_b'][l], inp['kg_b'][l])):
            hgv[l, :, 2 * i] = np.tile(g, 2)
            hgv[l, :, 2 * i + 1] = np.tile(g[sw], 2)
    shared['hg'] = hgv
    shared['sink'] = f(np.stack([np.broadcast_to(inp['sink_b'][l], (128, 8)) for l in range(L)]))
    shared['convw'] = f(np.stack([inp['conv_w'][l].reshape(3, 4, 128).transpose(2, 1, 0) for l in range(L)]))
    shared['wr'] = f(np.stack([_colmajor(inp['w_router'][l]) for l in range(L)]))
    for nm, key in (('weg', 'w_e_gate'), ('weu', 'w_e_up'), ('wed', 'w_e_down')):
        w = inp[key]
        shared[nm] = f(w.reshape(L, NE, 8, 128, 1024).transpose(0, 1, 3, 2, 4))
    shared['ropeC'] = C
    shared['ropeS'] = S
    shared['cmisc'] = cm
    shared['iota'] = iota
    shared['comb'] = tidc
    maps = []
    B = inp['x'].shape[0]
    for b in range(B):
        m = dict(shared)
        m['xin'] = f(np.concatenate([inp['x'][b], inp['ctx'][b]], axis=0))
        cv = np.zeros((128, 8, 2), np.float32)
        cv[:, :, 0] = inp['c'][b].reshape(8, 128).T
        cv[:, :, 1] = inp['c_ctx'].reshape(8, 128).T
        m['cvec'] = cv
        maps.append(m)
    return maps


_NC_CACHE = {}
_PER_LAYER = ('w_ada', 'b_col2', 'b_bc', 'gcol2', 'gffn_bc', 'w1', 'w2', 'wcx', 'w3', 'wbr', 'wout', 'hg', 'sink', 'convw', 'wr',
              'weg', 'weu', 'wed')
FUSED = True


def _layer_slice(m, l):
    out = {}
    for k_, v in m.items():
        out[k_] = np.ascontiguousarray(v[l:l + 1]) if k_ in _PER_LAYER else v
    return out


def kernel(**inputs):
    inp = {k_: np.asarray(v) for k_, v in inputs.items()}
    maps = prep_inputs(inp)
    L = inp['w_ada'].shape[0]
    cores = list(range(len(maps)))
    if FUSED:
        if 'nc' not in _NC_CACHE:
            _NC_CACHE['nc'] = build(nlayers=L)
        res = run_bass_kernel_spmd(_NC_CACHE['nc'], maps, core_ids=cores)
        xs = [np.asarray(r["xs"]) for r in res.results]
    else:
        xs = [m['xin'] for m in maps]
        for l in range(L):
            key = ('layer', l == L - 1)
            if key not in _NC_CACHE:
                _NC_CACHE[key] = build(nlayers=1, force_ctx=(l != L - 1))
            lm = []
            for m, x_ in zip(maps, xs):
                d = _layer_slice(m, l)
                d['xin'] = np.ascontiguousarray(x_, dtype=np.float32)
                lm.append(d)
            res = run_bass_kernel_spmd(_NC_CACHE[key], lm, core_ids=cores)
            xs = [np.asarray(r["xs"]) for r in res.results]
    out = np.stack([x_[:NL] for x_ in xs], axis=0)
    return out.astype(np.float32)
```

```python
import numpy as np
from contextlib import ExitStack
import concourse.bass as bass
import concourse.mybir as mybir
from concourse.bass_utils import run_bass_kernel_spmd

F32 = mybir.dt.float32
BF16 = mybir.dt.bfloat16
I32 = mybir.dt.int32
AF = mybir.ActivationFunctionType
ALU = mybir.AluOpType
AX = mybir.AxisListType

NL, NCX, NT, D = 8192, 256, 8448, 1024
NTL = NT // 128
EPS = 1e-6
NE = 16
N_CORES = 4


class K:
    def __init__(self, nc, stack):
        self.nc = nc
        self.stack = stack
        self.eng = {'pe': nc.tensor, 'act': nc.scalar, 'dve': nc.vector, 'pool': nc.gpsimd, 'sp': nc.sync}
        self.sem = {n: stack.enter_context(nc.semaphore("s_" + n)) for n in self.eng}
        self.cnt = {n: 0 for n in self.eng}
        self.seen = {n: {} for n in self.eng}
        self.dsem = {}
        self.dval = {}
        self.lastw = {}
        self.readers = {}
        self.nwaits = 0
        self.nins = 0
        self.drr = {}

    def _key(self, t):
        if isinstance(t, (str, int)):
            return t
        if isinstance(t, tuple):
            return tuple(self._key(x) for x in t)
        return ('id', id(t))

    def _deps(self, R, W):
        deps = []
        for t in list(R) + list(W):
            d = self.lastw.get(self._key(t))
            if d is not None:
                deps.append(d)
        for t in W:
            deps.extend(self.readers.get(self._key(t), {}).items())
        return deps

    def _wait(self, e, deps):
        h = self.eng[e]
        seen = self.seen[e]
        need = {}
        for key, val in deps:
            if key == ('E', 'pe') and e == 'pe':
                continue
            if seen.get(key, 0) >= val:
                continue
            if need.get(key, 0) < val:
                need[key] = val
        for key, val in need.items():
            s = self.sem[key[1]] if key[0] == 'E' else self.dsem[key[1]]
            h.wait_ge(s, val)
            seen[key] = val
            self.nwaits += 1

    def _record(self, tok, R, W):
        key, val = tok
        for t in R:
            r = self.readers.setdefault(self._key(t), {})
            if r.get(key, 0) < val:
                r[key] = val
        for t in W:
            self.lastw[self._key(t)] = tok
            self.readers[self._key(t)] = {}

    def op(self, e, fn, R=(), W=()):
        self._wait(e, self._deps(R, W))
        ins = fn(self.eng[e])
        ins.then_inc(self.sem[e], 1)
        self.cnt[e] += 1
        self.nins += 1
        self._record((('E', e), self.cnt[e]), R, W)
        return ins

    NDS = 8

    def dma(self, e, stream, fn, R=(), W=()):
        i = self.drr.get(e, 0)
        self.drr[e] = i + 1
        stream = "%s%d" % (e, i % self.NDS)
        if stream not in self.dsem:
            self.dsem[stream] = self.stack.enter_context(self.nc.semaphore("d_" + stream))
            self.dval[stream] = 0
        deps = self._deps(R, W)
        if self.dval[stream] > 0:
            deps.append((('D', stream), self.dval[stream]))
        self._wait(e, deps)
        ins = fn(self.eng[e])
        ins.then_inc(self.dsem[stream], 16)
        self.dval[stream] += 16
        self.nins += 1
        self._record((('D', stream), self.dval[stream]), R, W)
        return ins

    def barrier(self, engines=None):
        deps = [(('E', n), c) for n, c in self.cnt.items() if c > 0]
        deps += [(('D', s), v) for s, v in self.dval.items() if v > 0]
        for e in (engines or self.eng):
            self._wait(e, deps)


def build(nlayers=2, debug=False, stop=None, cut=99, force_ctx=False):
    nc = bass.Bass("TRN2", target_bir_lowering=False)
    st = ExitStack()
    with st:
        k = K(nc, st)

        def din(name, shape, dt=F32):
            return nc.dram_tensor(name, list(shape), dt, kind="ExternalInput")

        def dsc(name, shape, dt, out=False):
            return nc.dram_tensor(name, list(shape), dt, kind="ExternalOutput" if (out or debug) else "Internal")

        L = nlayers
        xin = din("xin", [NT, D])
        cvec = din("cvec", [128, 8, 2])
        w_ada = din("w_ada", [L, 128, 8, 6144])
        b_col2 = din("b_col2", [L, 128, 96])
        b_bc = din("b_bc", [L, 128, 4, D])
        gcol2 = din("gcol2", [L, 128, 2, 8, 2])
        gffn_bc = din("gffn_bc", [L, 128, D])
        w1d = din("w1", [L, 128, 8, 768])
        w2d = din("w2", [L, 128, 8, 2048])
        wcxd = din("wcx", [L, 128, 8, 1024])
        w3d = din("w3", [L, 128, 8, 3584])
        wbrd = din("wbr", [L, 128, 12, D])
        woutd = din("wout", [L, 128, 8, D])
        hgd = din("hg", [L, 128, 8])
        sinkd = din("sink", [L, 128, 8])
        convwd = din("convw", [L, 128, 4, 3])
        wrd = din("wr", [L, 128, 8, NE])
        wegd = din("weg", [L, NE, 128, 8, D])
        weud = din("weu", [L, NE, 128, 8, D])
        wedd = din("wed", [L, NE, 128, 8, D])
        ropeCd = din("ropeC", [128, NT])
        ropeSd = din("ropeS", [128, NT])
        cmisc = din("cmisc", [128, 5, 128])
        iotad = din("iota", [128, 1024])
        combd = din("comb", [128, NTL, NE, 5])
        xs = dsc("xs", [NT, D], F32, out=True)
        hTd = dsc("hTd", [128, 8, NT], BF16)
        uTd = dsc("uTd", [128, 4, NT + 4], F32)
        oaTd = dsc("oaTd", [128, 4, NT], BF16)
        obTd = dsc("obTd", [128, 4, NT], BF16)
        mTd = dsc("mTd", [128, 8, NT], BF16)
        h2d = dsc("h2d", [NT, D], BF16)
        affd = dsc("affd", [NT, NE], F32)
        bcd = dsc("bcd", [2, 4, 128, D], F32)
        xgd = dsc("xgd", [NE * 9 * 128, D], BF16)
        Yd = dsc("Yd", [NE * 9 * 128, D], F32)

        def sb(name, shape, dt, stack=None):
            return (stack or st).enter_context(nc.sbuf_tensor(name, list(shape), dt))

        PS = [st.enter_context(nc.psum_tensor("ps%d" % i, [128, 512], F32)) for i in range(7)]
        PSB = st.enter_context(nc.psum_tensor("psb", [128, 1024], BF16))
        psrr = {}

        def ps_get(pool, banks):
            i = psrr.get(pool, 0)
            psrr[pool] = i + 1
            return PS[banks[i % len(banks)]]

        ident = sb("ident", [128, 128], F32)
        blockones = sb("blockones", [128, 128], F32)
        ones32 = sb("ones32", [128, 128], F32)
        onesb = sb("onesb", [128, 128], BF16)
        triL = sb("triL", [128, 128], BF16)
        maskP = sb("maskP", [128, 128], BF16)
        maskN = sb("maskN", [128, 128], BF16)
        identb = sb("identb", [128, 128], BF16)
        sc = sb("sc", [128, 8, 2], F32)
        lbc = sb("lbc", [128, 8, 2, 128], F32)
        modcol = sb("modcol", [128, 48, 2], F32)
        A1c = sb("A1c", [128, 8, 2], F32)
        hg = sb("hgs", [128, 8], F32)
        esink = sb("esink", [128, 8], F32)
        cw = sb("cw", [128, 4, 3], F32)
        ss = sb("ss", [128, 4], F32)
        rsd = sb("rsd", [128, 4], F32)

        g = nc.gpsimd
        lr1 = st.enter_context(g.register("lr1"))
        lr2 = st.enter_context(g.register("lr2"))
        licur = sb("licur", [128, 1], I32)

        def gather_loop(idx_all, src, dst, xg_, nj, tag):
            s1 = st.enter_context(nc.semaphore("lc" + tag))
            s2 = st.enter_context(nc.semaphore("ld" + tag))
            with g.Fori(0, nj) as j:
                g.tensor_copy(out=licur[:, 0:1], in_=idx_all[:, bass.ds(j, 1)]).then_inc(s1, 1)
                g.reg_mov(lr1, 1)
                g.reg_add(lr1, lr1, j)
                g.wait_ge(s1, lr1)
                g.indirect_dma_start(out=xg_[:, :], out_offset=None, in_=src[:, :], in_offset=bass.IndirectOffsetOnAxis(ap=licur[:, 0:1], axis=0),
                                     bounds_check=NT - 1, oob_is_err=False).then_inc(s2, 16)
                g.reg_mov(lr1, 32)
                g.reg_mul(lr1, lr1, j)
                g.reg_add(lr1, lr1, 16)
                g.wait_ge(s2, lr1)
                g.reg_mov(lr2, 128 * D)
                g.reg_mul(lr2, lr2, j)
                g.dma_start(out=bass.AP(dst, lr2, [[D, 128], [1, D]]), in_=xg_[:, :]).then_inc(s2, 16)
                g.reg_add(lr1, lr1, 16)
                g.wait_ge(s2, lr1)

        def scatter_loop(idx_all, src, dst, yt_, nj, tag):
            s1 = st.enter_context(nc.semaphore("sc" + tag))
            s2 = st.enter_context(nc.semaphore("sd" + tag))
            with g.Fori(0, nj) as j:
                g.tensor_copy(out=licur[:, 0:1], in_=idx_all[:, bass.ds(j, 1)]).then_inc(s1, 1)
                g.reg_mov(lr1, 1)
                g.reg_add(lr1, lr1, j)
                g.wait_ge(s1, lr1)
                g.reg_mov(lr2, 128 * D)
                g.reg_mul(lr2, lr2, j)
                g.dma_start(out=yt_[:, :], in_=bass.AP(src, lr2, [[D, 128], [1, D]])).then_inc(s2, 16)
                g.reg_mov(lr1, 32)
                g.reg_mul(lr1, lr1, j)
                g.reg_add(lr1, lr1, 16)
                g.wait_ge(s2, lr1)
                g.indirect_dma_start(out=dst[:, :], out_offset=bass.IndirectOffsetOnAxis(ap=licur[:, 0:1], axis=0), in_=yt_[:, :], in_offset=None,
                                     bounds_check=NT - 1, oob_is_err=False, compute_op=ALU.add).then_inc(s2, 16)
                g.reg_add(lr1, lr1, 16)
                g.wait_ge(s2, lr1)

        def ld(dst, src, eng='sp', stream='misc', R=(), W=None, slow=False):
            if slow:
                return k.dma(eng, stream, lambda e: e.dma_start(out=dst, in_=src, allow_slow_non_contiguous=True), R=R, W=W)
            return k.dma(eng, stream, lambda e: e.dma_start(out=dst, in_=src), R=R, W=W)

        ld(ident[:], cmisc[:, 0, :], W=[ident])
        ld(blockones[:], cmisc[:, 1, :], W=[blockones])
        ld(triL[:], cmisc[:, 2, :], eng='pool', stream='miscp', W=[triL])
        ld(maskP[:], cmisc[:, 3, :], eng='pool', stream='miscp', W=[maskP])
        ld(maskN[:], cmisc[:, 4, :], eng='pool', stream='miscp', W=[maskN])
        ld(identb[:], cmisc[:, 0, :], eng='pool', stream='miscp', W=[identb])
        ld(sc[:], cvec.ap(), W=[sc])
        k.op('dve', lambda e: e.memset(ones32[:], 1.0), W=[ones32])
        k.op('dve', lambda e: e.memset(onesb[:], 1.0), W=[onesb])
        for i in range(4):
            r0, r1 = i * (NT // 4), (i + 1) * (NT // 4)
            ld(xs[r0:r1, :], xin[r0:r1, :], stream='xcopy', W=[('xs', 'init')])
        k.op('act', lambda e: e.activation(out=sc[:], in_=sc[:], func=AF.Silu), R=[sc], W=[sc])
        for kc in range(8):
            for v in range(2):
                k.op('dve', lambda e, kc=kc, v=v: e.tensor_scalar(out=lbc[:, kc, v, :], in0=ones32[:], scalar1=sc[:, kc, v:v + 1],
                                                                   scalar2=None, op0=ALU.mult), R=[sc, ones32], W=[lbc])
        k.barrier()

        blocks = [(i * 512, 512, 0) for i in range(16)] + [(NL, NCX, 1)]

        def rstd_of(xt, junk, col):
            k.op('pool', lambda e: e.memset(ss[:, col:col + 1], 0.0), W=[('ss', col)])
            k.op('act', lambda e: e.activation(out=junk[:], in_=xt[:], func=AF.Square, accum_out=ss[:, col:col + 1]),
                 R=[xt], W=[junk, ('ss', col)])
            k.op('act', lambda e: e.activation(out=rsd[:, col:col + 1], in_=ss[:, col:col + 1], func=AF.Sqrt, scale=1.0 / D, bias=EPS),
                 R=[('ss', col)], W=[('rsd', col)])
            k.op('dve', lambda e: e.reciprocal(out=rsd[:, col:col + 1], in_=rsd[:, col:col + 1]), R=[('rsd', col)], W=[('rsd', col)])

        for l in range(L):
            last = (l == L - 1) and not force_ctx
            with ExitStack() as p0:
                wa = [sb("wa%d_%d" % (l, i), [128, 8, 512], F32, p0) for i in range(2)]
                bb = sb("bb%d" % l, [128, 4, D], F32, p0)
                gfb = sb("gfb%d" % l, [128, D], F32, p0)
                rowt = sb("rowt%d" % l, [128, D], F32, p0)
                bcol = sb("bcol%d" % l, [128, 96], F32, p0)
                gc2 = sb("gc2%d" % l, [128, 2, 8, 2], F32, p0)
                ld(bb[:], b_bc[l], W=[bb])
                ld(gfb[:], gffn_bc[l], W=[gfb])
                ld(bcol[:], b_col2[l], W=[bcol])
                ld(gc2[:], gcol2[l], W=[gc2])
                ld(hg[:], hgd[l], W=[hg])
                ld(esink[:], sinkd[l], W=[esink])
                ld(cw[:], convwd[l], W=[cw])
                k.op('act', lambda e: e.activation(out=esink[:], in_=esink[:], func=AF.Exp), R=[esink], W=[esink])
                psmod = PS[0]
                rowsel = {4: (0, 0), 5: (0, 1), 6: (1, 0), 7: (1, 1), 8: (2, 0), 9: (2, 1), 10: (3, 0), 11: (3, 1)}
                for ch in range(12):
                    w = wa[ch % 2]
                    ld(w[:], w_ada[l, :, :, ch * 512:(ch + 1) * 512], stream='wa%d' % (ch % 2), W=[w])
                    for sub in range(4):
                        j = ch * 4 + sub
                        for kc in range(8):
                            k.op('pe', lambda e, w=w, j=j, kc=kc, sub=sub: e.matmul(
                                psmod[:, 2 * j:2 * j + 2], lhsT=w[:, kc, sub * 128:(sub + 1) * 128], rhs=sc[:, kc, :],
                                start=(kc == 0), stop=(kc == 7)), R=[w, sc], W=[psmod])
                    if ch in rowsel:
                        vi, half = rowsel[ch]
                        for v in range(2):
                            pr = PS[1 + v]
                            for kc in range(8):
                                k.op('pe', lambda e, w=w, kc=kc, v=v, pr=pr: e.matmul(
                                    pr[:, :], lhsT=lbc[:, kc, v, :], rhs=w[:, kc, :], start=(kc == 0), stop=(kc == 7)),
                                    R=[w, lbc], W=[pr])
                            hs = slice(half * 512, (half + 1) * 512)
                            k.op('dve', lambda e, pr=pr, vi=vi, hs=hs: e.tensor_tensor(
                                out=rowt[:, hs], in0=pr[:, :], in1=bb[:, vi, hs], op=ALU.add), R=[pr, bb], W=[rowt])
                            if vi == 2:
                                k.op('dve', lambda e, hs=hs: e.scalar_tensor_tensor(
                                    out=rowt[:, hs], in0=rowt[:, hs], scalar=1.0, in1=gfb[:, hs], op0=ALU.add, op1=ALU.mult),
                                    R=[rowt, gfb], W=[rowt])
                            ld(bcd[v, vi, :, hs], rowt[:, hs], stream='bcst', R=[rowt], W=[('bcd', v, vi, half)])
                mc = modcol[:].rearrange("p a b -> p (a b)")
                k.op('dve', lambda e: e.tensor_tensor(out=mc, in0=psmod[:, 0:96], in1=bcol[:], op=ALU.add), R=[psmod, bcol], W=[modcol])
                k.op('dve', lambda e: e.scalar_tensor_tensor(out=A1c[:], in0=modcol[:, 8:16, :], scalar=1.0, in1=gc2[:, 0, :, :],
                                                            op0=ALU.add, op1=ALU.mult), R=[modcol, gc2], W=[A1c])
                k.barrier()
            if stop == 'p0':
                break

            with ExitStack() as pkv:
                KTA = sb("KTA%d" % l, [128, NT], BF16, pkv)
                KTB = sb("KTB%d" % l, [128, NT], BF16, pkv)
                VA = sb("VA%d" % l, [128, NTL + 1, 2, 80], BF16, pkv)
                VB = sb("VB%d" % l, [128, NTL + 1, 2, 80], BF16, pkv)
                KT = {'A': KTA, 'B': KTB}
                VAf = VA[:].rearrange("p t g c -> p (t g c)")
                VBf = VB[:].rearrange("p t g c -> p (t g c)")
                VV = {'A': VA, 'B': VB}
                k.op('pool', lambda e: e.memset(VA[:], 0.0), W=[VA])
                k.op('pool', lambda e: e.memset(VB[:], 0.0), W=[VB])
                k.op('pool', lambda e: e.memset(VA[:, :, :, 64:65], 1.0), W=[VA])
                k.op('pool', lambda e: e.memset(VB[:, :, :, 64:65], 1.0), W=[VB])

                def normrope(psp, psr, gi, Cb, Sb, out_ap, outres, n, tmp, split=None):
                    sq, rt, t1, t2 = tmp
                    k.op('act', lambda e: e.activation(out=sq[:, :n], in_=psp[:, :n], func=AF.Square), R=[psp], W=[sq])
                    pq = ps_get('nr', [6])
                    k.op('pe', lambda e: e.matmul(pq[:, :n], lhsT=blockones[:], rhs=sq[:, :n], start=True, stop=True),
                         R=[sq, blockones], W=[pq])
                    k.op('act', lambda e: e.activation(out=rt[:, :n], in_=pq[:, :n], func=AF.Sqrt, scale=1.0 / 64, bias=EPS),
                         R=[pq], W=[rt])
                    k.op('dve', lambda e: e.reciprocal(out=rt[:, :n], in_=rt[:, :n]), R=[rt], W=[rt])
                    k.op('dve', lambda e: e.scalar_tensor_tensor(out=t1[:, :n], in0=psp[:, :n], scalar=hg[:, gi:gi + 1], in1=Cb[:, :n],
                                                                op0=ALU.mult, op1=ALU.mult), R=[psp, Cb, hg], W=[t1])
                    k.op('dve', lambda e: e.scalar_tensor_tensor(out=t2[:, :n], in0=psr[:, :n], scalar=hg[:, gi + 1:gi + 2], in1=Sb[:, :n],
                                                                op0=ALU.mult, op1=ALU.mult), R=[psr, Sb, hg], W=[t2])
                    k.op('pool', lambda e: e.tensor_tensor(out=t1[:, :n], in0=t1[:, :n], in1=t2[:, :n], op=ALU.add), R=[t1, t2], W=[t1])
                    if split is None:
                        k.op('pool', lambda e: e.tensor_tensor(out=out_ap, in0=t1[:, :n], in1=rt[:, :n], op=ALU.mult), R=[t1, rt], W=[outres])
                    else:
                        k.op('pool', lambda e: e.tensor_tensor(out=split[0], in0=t1[0:64, :n], in1=rt[0:64, :n], op=ALU.mult), R=[t1, rt], W=[outres])
                        k.op('pool', lambda e: e.tensor_tensor(out=split[1], in0=t1[64:128, :n], in1=rt[64:128, :n], op=ALU.mult), R=[t1, rt], W=[outres])

                with ExitStack() as p1:
                    W1 = sb("W1_%d" % l, [128, 8, 768], BF16, p1)
                    ld(W1[:], w1d[l], eng='pool', stream='wbig', W=[W1])
                    xt2 = [sb("xt%d_%d" % (l, i), [128, D], F32, p1) for i in range(2)]
                    xn2 = [sb("xn%d_%d" % (l, i), [128, D], F32, p1) for i in range(2)]
                    junk = sb("junk%d" % l, [128, D], F32, p1)
                    hTb2 = [sb("hTb%d_%d" % (l, i), [128, 8, 512], BF16, p1) for i in range(2)]
                    Cb2 = [sb("Cb%d_%d" % (l, i), [128, 512], F32, p1) for i in range(2)]
                    Sb2 = [sb("Sb%d_%d" % (l, i), [128, 512], F32, p1) for i in range(2)]
                    tmps = [[sb("nt%d_%d_%d" % (l, i, j), [128, 512], F32, p1) for j in range(4)] for i in range(2)]
                    ti_glob = 0
                    for bi, (t0, ntok, v) in enumerate(blocks if cut >= 5 else blocks[:1]):
                        if cut < 2:
                            break
                        hTb = hTb2[bi % 2]
                        Cb, Sb = Cb2[bi % 2], Sb2[bi % 2]
                        ld(Cb[:, :ntok], ropeCd[:, t0:t0 + ntok], stream='rope%d' % (bi % 2), W=[Cb])
                        ld(Sb[:, :ntok], ropeSd[:, t0:t0 + ntok], stream='rope%d' % (bi % 2), W=[Sb])
                        for tt in range(ntok // 128):
                            xt = xt2[ti_glob % 2]
                            xn = xn2[ti_glob % 2]
                            col = ti_glob % 2
                            r0 = t0 + tt * 128
                            ld(xt[:], xs[r0:r0 + 128, :], stream='xt%d' % (ti_glob % 2), R=[('xs', 'init')], W=[xt])
                            rstd_of(xt, junk, col)
                            k.op('dve', lambda e, xn=xn, xt=xt, col=col: e.tensor_scalar(out=xn[:], in0=xt[:], scalar1=rsd[:, col:col + 1],
                                                                                      scalar2=None, op0=ALU.mult),
                                 R=[xt, ('rsd', col)], W=[xn])
                            for half in range(2):
                                pt = ps_get('tr', [0, 1])
                                for q in range(4):
                                    kc = half * 4 + q
                                    k.op('pe', lambda e, pt=pt, q=q, kc=kc, xn=xn: e.transpose(pt[:, q * 128:(q + 1) * 128],
                                                                                               xn[:, kc * 128:(kc + 1) * 128], ident[:]),
                                         R=[xn, ident], W=[pt])
                                for q in range(4):
                                    kc = half * 4 + q
                                    eng = 'act' if q % 2 == 0 else 'dve'
                                    dst = hTb[:, kc, tt * 128:(tt + 1) * 128]
                                    if eng == 'act':
                                        k.op('act', lambda e, pt=pt, q=q, kc=kc, dst=dst: e.activation(
                                            out=dst, in_=pt[:, q * 128:(q + 1) * 128], func=AF.Identity,
                                            scale=A1c[:, kc, v:v + 1], bias=modcol[:, kc, v:v + 1]), R=[pt, A1c, modcol], W=[hTb])
                                    else:
                                        k.op('dve', lambda e, pt=pt, q=q, kc=kc, dst=dst: e.tensor_scalar(
                                            out=dst, in0=pt[:, q * 128:(q + 1) * 128], scalar1=A1c[:, kc, v:v + 1],
                                            scalar2=modcol[:, kc, v:v + 1], op0=ALU.mult, op1=ALU.add), R=[pt, A1c, modcol], W=[hTb])
                            ti_glob += 1
                        ld(hTd[:, :, t0:t0 + ntok], hTb[:, :, :ntok], stream='hst%d' % (bi % 2), R=[hTb], W=[('hTd', bi)])
                        for ai, (nm, c0, gi) in enumerate((('A', 0, 2), ('B', 128, 6)) if cut >= 3 else ()):
                            psp = ps_get('kp', [2, 3])
                            psr = ps_get('kr', [4, 5])
                            for kc in range(8):
                                k.op('pe', lambda e, kc=kc, psp=psp, c0=c0: e.matmul(psp[:, :ntok], lhsT=W1[:, kc, c0:c0 + 128],
                                                                                    rhs=hTb[:, kc, :ntok], start=(kc == 0), stop=(kc == 7)),
                                     R=[W1, hTb], W=[psp])
                            for kc in range(8):
                                k.op('pe', lambda e, kc=kc, psr=psr, c0=c0: e.matmul(psr[:, :ntok], lhsT=W1[:, kc, 256 + c0:256 + c0 + 128],
                                                                                    rhs=hTb[:, kc, :ntok], start=(kc == 0), stop=(kc == 7)),
                                     R=[W1, hTb], W=[psr])
                            normrope(psp, psr, gi, Cb, Sb, KT[nm][:, t0:t0 + ntok], KT[nm], ntok, tmps[ai])
                        for tt in range(ntok // 128 if cut >= 4 else 0):
                            ti = (t0 + tt * 128) // 128
                            pv = ps_get('kp', [2, 3]) if cut != 41 else ps_get('tr', [0, 1])
                            for kc in range(8):
                                k.op('pe', lambda e, kc=kc, pv=pv, tt=tt: e.matmul(pv[:, 0:256], lhsT=hTb[:, kc, tt * 128:(tt + 1) * 128],
                                                                                  rhs=W1[:, kc, 512:768], start=(kc == 0), stop=(kc == 7)),
                                     R=[W1, hTb], W=[pv])
                            for g_ in range(2):
                                k.op('act', lambda e, pv=pv, ti=ti, g_=g_: e.copy(out=VA[:, ti, g_, 0:64], in_=pv[:, g_ * 64:(g_ + 1) * 64]),
                                     R=[pv], W=[VA])
                                k.op('dve', lambda e, pv=pv, ti=ti, g_=g_: e.tensor_copy(out=VB[:, ti, g_, 0:64], in_=pv[:, 128 + g_ * 64:128 + (g_ + 1) * 64]),
                                     R=[pv], W=[VB])
                    k.barrier()
                if stop == 'p1':
                    if debug:
                        dk = nc.dram_tensor("dbgKTA", [128, NT], BF16, kind="ExternalOutput")
                        dkb = nc.dram_tensor("dbgKTB", [128, NT], BF16, kind="ExternalOutput")
                        dv = nc.dram_tensor("dbgVA", [128, NTL + 1, 2, 80], BF16, kind="ExternalOutput")
                        ld(dk.ap(), KTA[:], stream='dbg', R=[KTA], W=['dbgo'])
                        ld(dkb.ap(), KTB[:], stream='dbg', R=[KTB], W=['dbgo'])
                        ld(dv.ap(), VA[:], stream='dbg', R=[VA], W=['dbgo'])
                        k.barrier()
                    break

                with ExitStack() as p2:
                    W2 = sb("W2_%d" % l, [128, 8, 2048], BF16, p2)
                    ld(W2[:], w2d[l], eng='pool', stream='wbig', W=[W2])
                    hTb2 = [sb("hTq%d_%d" % (l, i), [128, 8, 512], BF16, p2) for i in range(2)]
                    Cb2 = [sb("Cq%d_%d" % (l, i), [128, 512], F32, p2) for i in range(2)]
                    Sb2 = [sb("Sq%d_%d" % (l, i), [128, 512], F32, p2) for i in range(2)]
                    tmps = [[sb("qt%d_%d_%d" % (l, i, j), [128, 512], F32, p2) for j in range(4)] for i in range(2)]
                    QT = {nm: [[sb("QT%s%d_%d_%d" % (nm, l, j, hh), [128, 512], BF16, p2) for hh in range(2)] for j in range(4)] for nm in ('A', 'B')}
                    for nm in ('A', 'B'):
                        for j in range(4):
                            for hh in range(2):
                                k.op('pool', lambda e, t_=QT[nm][j][hh]: e.memset(t_[:], 0.0), W=[(QT[nm], j)])
                    OT2 = {'A': [sb("OaT%d_%d" % (l, i), [128, 4, 512], BF16, p2) for i in range(2)],
                           'B': [sb("ObT%d_%d" % (l, i), [128, 4, 512], BF16, p2) for i in range(2)]}
                    PT = [sb("PT%d_%d" % (l, i), [128, 512], BF16, p2) for i in range(4)]
                    rec = [sb("rec%d_%d" % (l, i), [128, 512], F32, p2) for i in range(2)]
                    bcs = [sb("bcs%d_%d" % (l, i), [64, 512], F32, p2) for i in range(2)]
                    ptc = [0]
                    fin = [0]

                    def finalize(pso, n, dst_ap, dstres, sink_col=None):
                        r = rec[fin[0] % 2]
                        bc = bcs[fin[0] % 2]
                        fin[0] += 1
                        if sink_col is not None:
                            k.op('dve', lambda e: e.tensor_scalar(out=r[64:65, :n], in0=pso[64:65, :n], scalar1=esink[64:65, sink_col:sink_col + 1],
                                                                  scalar2=None, op0=ALU.add), R=[pso, esink], W=[r])
                            k.op('dve', lambda e: e.reciprocal(out=r[64:65, :n], in_=r[64:65, :n]), R=[r], W=[r])
                        else:
                            k.op('dve', lambda e: e.reciprocal(out=r[64:65, :n], in_=pso[64:65, :n]), R=[pso], W=[r])
                        pb = ps_get('nr', [6])
                        k.op('pe', lambda e: e.matmul(pb[0:64, :n], lhsT=ones32[64:65, 0:64], rhs=r[64:65, :n], start=True, stop=True),
                             R=[r, ones32], W=[pb])
                        k.op('act', lambda e: e.copy(out=bc[0:64, :n], in_=pb[0:64, :n]), R=[pb], W=[bc])
                        k.op('dve', lambda e: e.tensor_tensor(out=dst_ap, in0=pso[0:64, :n], in1=bc[0:64, :n], op=ALU.mult),
                             R=[pso, bc], W=[dstres])

                    qblocks = blocks if not last else blocks[:16]
                    for bi, (t0, ntok, v) in enumerate(qblocks):
                        latent = (v == 0)
                        hTb = hTb2[bi % 2]
                        Cb, Sb = Cb2[bi % 2], Sb2[bi % 2]
                        ld(hTb[:, :, :ntok], hTd[:, :, t0:t0 + ntok], stream='hq%d' % (bi % 2), R=[('hTd', bi)], W=[hTb])
                        ld(Cb[:, :ntok], ropeCd[:, t0:t0 + ntok], stream='rq%d' % (bi % 2), W=[Cb])
                        ld(Sb[:, :ntok], ropeSd[:, t0:t0 + ntok], stream='rq%d' % (bi % 2), W=[Sb])
                        qi = 0
                        for nm, base, gi in (('A', 0, 0), ('B', 1024, 4)):
                            for j in range(4):
                                psp = ps_get('kp', [2, 3])
                                psr = ps_get('kr', [4, 5])
                                for kc in range(8):
                                    k.op('pe', lambda e, kc=kc, psp=psp, c0=base + j * 128: e.matmul(
                                        psp[:, :ntok], lhsT=W2[:, kc, c0:c0 + 128], rhs=hTb[:, kc, :ntok], start=(kc == 0), stop=(kc == 7)),
                                        R=[W2, hTb], W=[psp])
                                for kc in range(8):
                                    k.op('pe', lambda e, kc=kc, psr=psr, c0=base + 512 + j * 128: e.matmul(
                                        psr[:, :ntok], lhsT=W2[:, kc, c0:c0 + 128], rhs=hTb[:, kc, :ntok], start=(kc == 0), stop=(kc == 7)),
                                        R=[W2, hTb], W=[psr])
                                normrope(psp, psr, gi, Cb, Sb, None, (QT[nm], j), ntok, tmps[qi % 2],
                                         split=(QT[nm][j][0][0:64, :ntok], QT[nm][j][1][64:128, :ntok]))
                                qi += 1
                        OaT = OT2['A'][bi % 2]
                        ObT = OT2['B'][bi % 2]
                        kts = list(range(NTL)) if latent else [64, 65]
                        for j in range(4):
                            for hh in range(2):
                                rows = slice(64 * hh, 64 * hh + 64)
                                pso = ps_get('o', [4, 5])
                                LA = 3

                                def emit_s(kt):
                                    pss = ps_get('s', [0, 1, 2, 3])
                                    k.op('pe', lambda e: e.matmul(pss[:, :ntok], lhsT=KTA[:, kt * 128:(kt + 1) * 128],
                                                                  rhs=QT['A'][j][hh][:, :ntok], start=True, stop=True),
                                         R=[KTA, (QT['A'], j)], W=[pss])
                                    return pss
                                Sq = [emit_s(kt) for kt in kts[:LA]]
                                for n_, kt in enumerate(kts):
                                    pss = Sq[n_]
                                    pt = PT[ptc[0] % 4]
                                    ptc[0] += 1
                                    k.op('act', lambda e, pss=pss, pt=pt: e.activation(out=pt[:, :ntok], in_=pss[:, :ntok], func=AF.Exp, scale=0.125),
                                         R=[pss], W=[pt])
                                    if n_ + LA < len(kts):
                                        Sq.append(emit_s(kts[n_ + LA]))
                                    k.op('pe', lambda e, pso=pso, pt=pt, kt=kt, n_=n_: e.matmul(
                                        pso[:, :ntok], lhsT=VAf[:, kt * 160 + hh * 80:kt * 160 + hh * 80 + 128], rhs=pt[:, :ntok], start=(n_ == 0), stop=(n_ == len(kts) - 1)),
                                        R=[VA, pt], W=[pso])
                                finalize(pso, ntok, OaT[rows, j, :ntok], (OaT, j, hh))
                        for j in range(4):
                            for hh in range(2):
                                rows = slice(64 * hh, 64 * hh + 64)
                                head = 4 * hh + j
                                pso = ps_get('o', [4, 5])
                                for qt in range(ntok // 128):
                                    cols = slice(qt * 128, (qt + 1) * 128)
                                    I = (t0 + qt * 128) // 128
                                    kl = [(64, None), (65, None)]
                                    if latent:
                                        if I - 1 >= 0:
                                            kl.append((I - 1, maskP))
                                        kl.append((I, None))
                                        if I + 1 < 64:
                                            kl.append((I + 1, maskN))
                                    LA = 2

                                    def emit_sb(kt):
                                        pss = ps_get('s', [0, 1, 2, 3])
                                        k.op('pe', lambda e: e.matmul(pss[:, 0:128], lhsT=KTB[:, kt * 128:(kt + 1) * 128],
                                                                      rhs=QT['B'][j][hh][:, cols], start=True, stop=True),
                                             R=[KTB, (QT['B'], j)], W=[pss])
                                        return pss
                                    Sq = [emit_sb(kt) for (kt, _m) in kl[:LA]]
                                    for n_, (kt, msk) in enumerate(kl):
                                        pss = Sq[n_]
                                        pt = PT[ptc[0] % 4]
                                        ptc[0] += 1
                                        k.op('act', lambda e, pss=pss, pt=pt: e.activation(out=pt[:, 0:128], in_=pss[:, 0:128], func=AF.Exp, scale=0.125),
                                             R=[pss], W=[pt])
                                        if n_ + LA < len(kl):
                                            Sq.append(emit_sb(kl[n_ + LA][0]))
                                        if msk is not None:
                                            k.op('pool', lambda e, pt=pt, msk=msk: e.tensor_tensor(out=pt[:, 0:128], in0=pt[:, 0:128], in1=msk[:],
                                                                                                    op=ALU.mult), R=[pt, msk], W=[pt])
                                        k.op('pe', lambda e, pso=pso, pt=pt, kt=kt, n_=n_, kl=kl: e.matmul(
                                            pso[:, cols], lhsT=VBf[:, kt * 160 + hh * 80:kt * 160 + hh * 80 + 128], rhs=pt[:, 0:128], start=(n_ == 0), stop=(n_ == len(kl) - 1)),
                                            R=[VB, pt], W=[pso])
                                finalize(pso, ntok, ObT[rows, j, :ntok], (ObT, j, hh), sink_col=head)
                        ld(oaTd[:, :, t0:t0 + ntok], OaT[:, :, :ntok], stream='ost%d' % (bi % 2), R=[(OaT, j, hh) for j in range(4) for hh in range(2)],
                           W=[('oaTd', bi)])
                        ld(obTd[:, :, t0:t0 + ntok], ObT[:, :, :ntok], stream='ost%d' % (bi % 2), R=[(ObT, j, hh) for j in range(4) for hh in range(2)],
                           W=[('obTd', bi)])
                    k.barrier()
            if stop == 'p2':
                break
            mblocks = blocks if not last else blocks[:16]

            def uoff(t0):
                return t0 + 1 if t0 < NL else t0 + 3

            with ExitStack() as p3a:
                Wcx = sb("Wcx%d" % l, [128, 8, 1024], BF16, p3a)
                ld(Wcx[:], wcxd[l], eng='pool', stream='wbig', W=[Wcx])
                hTb2 = [sb("hTu%d_%d" % (l, i), [128, 8, 512], BF16, p3a) for i in range(2)]
                ub2 = [sb("ub%d_%d" % (l, i), [128, 4, 512], F32, p3a) for i in range(2)]
                cs2 = [sb("cs%d_%d" % (l, i), [128, 512], F32, p3a) for i in range(2)]
                zt = sb("zt%d" % l, [128, 4, 2], F32, p3a)
                k.op('dve', lambda e: e.memset(zt[:], 0.0), W=[zt])
                ld(uTd[:, :, 0:1], zt[:, :, 0:1], stream='zp', R=[zt], W=['uzp'], slow=True)
                ld(uTd[:, :, NL + 1:NL + 3], zt[:, :, 0:2], stream='zp', R=[zt], W=['uzp'], slow=True)
                ld(uTd[:, :, NT + 3:NT + 4], zt[:, :, 0:1], stream='zp', R=[zt], W=['uzp'], slow=True)
                cc = 0
                for bi, (t0, ntok, v) in enumerate(mblocks):
                    hTb = hTb2[bi % 2]
                    ub = ub2[bi % 2]
                    ld(hTb[:, :, :ntok], hTd[:, :, t0:t0 + ntok], stream='hu%d' % (bi % 2), W=[hTb])
                    for c in range(4):
                        psc = ps_get('kp', [2, 3])
                        psx = ps_get('kr', [4, 5])
                        cs = cs2[cc % 2]
                        cc += 1
                        for kc in range(8):
                            k.op('pe', lambda e, kc=kc, psc=psc, c=c: e.matmul(psc[:, :ntok], lhsT=Wcx[:, kc, c * 128:(c + 1) * 128],
                                                                              rhs=hTb[:, kc, :ntok], start=(kc == 0), stop=(kc == 7)),
                                 R=[Wcx, hTb], W=[psc])
                        for kc in range(8):
                            k.op('pe', lambda e, kc=kc, psx=psx, c=c: e.matmul(psx[:, :ntok], lhsT=Wcx[:, kc, 512 + c * 128:512 + (c + 1) * 128],
                                                                              rhs=hTb[:, kc, :ntok], start=(kc == 0), stop=(kc == 7)),
                                 R=[Wcx, hTb], W=[psx])
                        k.op('act', lambda e, cs=cs, psc=psc: e.copy(out=cs[:, :ntok], in_=psc[:, :ntok]), R=[psc], W=[cs])
                        k.op('dve', lambda e, cs=cs, psx=psx, c=c, ub=ub: e.tensor_tensor(out=ub[:, c, :ntok], in0=psx[:, :ntok], in1=cs[:, :ntok],
                                                                                       op=ALU.mult), R=[psx, cs], W=[(ub, c)])
                    o0 = uoff(t0)
                    ld(uTd[:, :, o0:o0 + ntok], ub[:, :, :ntok], stream='ust%d' % (bi % 2), R=[(ub, c) for c in range(4)], W=[('uTd', bi)])
                k.barrier()

            with ExitStack() as p3b:
                W3 = sb("W3_%d" % l, [128, 8, 3584], BF16, p3b)
                Wbr = sb("Wbr%d" % l, [128, 12, D], BF16, p3b)
                ld(W3[:], w3d[l], eng='pool', stream='wbig', W=[W3])
                ld(Wbr[:], wbrd[l], eng='pool', stream='wbig', W=[Wbr])
                hTb2 = [sb("hTm%d_%d" % (l, i), [128, 8, 512], BF16, p3b) for i in range(2)]
                Oa2 = [sb("Oam%d_%d" % (l, i), [128, 4, 512], BF16, p3b) for i in range(2)]
                Ob2 = [sb("Obm%d_%d" % (l, i), [128, 4, 512], BF16, p3b) for i in range(2)]
                u2 = [sb("um%d_%d" % (l, i), [128, 4, 514], F32, p3b) for i in range(2)]
                OcT = sb("OcT%d" % l, [128, 4, 512], BF16, p3b)
                mT2 = [sb("mT%d_%d" % (l, i), [128, 8, 512], BF16, p3b) for i in range(2)]
                cva = [sb("cva%d_%d" % (l, i), [128, 512], F32, p3b) for i in range(2)]
                cvb = [sb("cvb%d_%d" % (l, i), [128, 512], F32, p3b) for i in range(2)]
                sg2 = [sb("sg%d_%d" % (l, i), [128, 512], F32, p3b) for i in range(3)]
                acc2 = [sb("acc%d_%d" % (l, i), [128, 512], F32, p3b) for i in range(2)]
                tm2 = [sb("tm%d_%d" % (l, i), [128, 512], F32, p3b) for i in range(2)]
                cnt = [0, 0, 0]
                for bi, (t0, ntok, v) in enumerate(mblocks):
                    hTb, Oa, Ob, ub, mT = hTb2[bi % 2], Oa2[bi % 2], Ob2[bi % 2], u2[bi % 2], mT2[bi % 2]
                    o0 = uoff(t0)
                    ld(hTb[:, :, :ntok], hTd[:, :, t0:t0 + ntok], stream='hm%d' % (bi % 2), W=[hTb])
                    ld(Oa[:, :, :ntok], oaTd[:, :, t0:t0 + ntok], stream='hm%d' % (bi % 2), W=[Oa])
                    ld(Ob[:, :, :ntok], obTd[:, :, t0:t0 + ntok], stream='hm%d' % (bi % 2), W=[Ob])
                    ld(ub[:, :, :ntok + 2], uTd[:, :, o0 - 1:o0 + ntok + 1], stream='hm%d' % (bi % 2), W=[ub])
                    for c in range(4):
                        psb = ps_get('kp', [2, 3])
                        for kc in range(8):
                            k.op('pe', lambda e, kc=kc, psb=psb, c=c: e.matmul(psb[:, :ntok], lhsT=W3[:, kc, c * 128:(c + 1) * 128],
                                                                              rhs=hTb[:, kc, :ntok], start=(kc == 0), stop=(kc == 7)),
                                 R=[W3, hTb], W=[psb])
                        a = cva[cnt[0] % 2]
                        cnt[0] += 1
                        k.op('pool', lambda e, a=a, c=c: e.tensor_scalar(out=a[:, :ntok], in0=ub[:, c, 0:ntok], scalar1=cw[:, c, 0:1], scalar2=None,
                                                                        op0=ALU.mult), R=[ub, cw], W=[a])
                        a2 = cvb[cnt[0] % 2]
                        for tap in (1, 2):
                            k.op('pool', lambda e, a2=a2, c=c, tap=tap: e.tensor_scalar(out=a2[:, :ntok], in0=ub[:, c, tap:ntok + tap], scalar1=cw[:, c, tap:tap + 1],
                                                                                       scalar2=None, op0=ALU.mult), R=[ub, cw], W=[a2])
                            k.op('pool', lambda e, a=a, a2=a2: e.tensor_tensor(out=a[:, :ntok], in0=a[:, :ntok], in1=a2[:, :ntok], op=ALU.add), R=[a, a2], W=[a])
                        k.op('dve', lambda e, a=a, c=c, psb=psb: e.tensor_tensor(out=OcT[:, c, :ntok], in0=psb[:, :ntok], in1=a[:, :ntok], op=ALU.mult),
                             R=[psb, a], W=[(OcT, c)])
                    Obr = [Oa, Ob, OcT]
                    for m in range(8):
                        acc = acc2[m % 2]
                        for br in range(3):
                            psg = ps_get('s', [0, 1])
                            psr = ps_get('kr', [4, 5])
                            sg = sg2[cnt[1] % 3]
                            cnt[1] += 1
                            c0 = 512 + br * 1024 + m * 128
                            for kc in range(8):
                                k.op('pe', lambda e, kc=kc, psg=psg, c0=c0: e.matmul(psg[:, :ntok], lhsT=W3[:, kc, c0:c0 + 128],
                                                                                    rhs=hTb[:, kc, :ntok], start=(kc == 0), stop=(kc == 7)),
                                     R=[W3, hTb], W=[psg])
                            k.op('act', lambda e, sg=sg, psg=psg: e.activation(out=sg[:, :ntok], in_=psg[:, :ntok], func=AF.Sigmoid), R=[psg], W=[sg])
                            Rr = [Wbr] + ([Oa] if br == 0 else [Ob] if br == 1 else [(OcT, c) for c in range(4)])
                            for c in range(4):
                                k.op('pe', lambda e, c=c, psr=psr, br=br, m=m: e.matmul(psr[:, :ntok], lhsT=Wbr[:, br * 4 + c, m * 128:(m + 1) * 128],
                                                                                       rhs=Obr[br][:, c, :ntok], start=(c == 0), stop=(c == 3)),
                                     R=Rr, W=[psr])
                            if br == 0:
                                k.op('dve', lambda e, psr=psr, sg=sg, acc=acc: e.tensor_tensor(out=acc[:, :ntok], in0=psr[:, :ntok], in1=sg[:, :ntok],
                                                                                            op=ALU.mult), R=[psr, sg], W=[acc])
                            else:
                                tm = tm2[cnt[2] % 2]
                                cnt[2] += 1
                                k.op('dve', lambda e, psr=psr, sg=sg, tm=tm: e.tensor_tensor(out=tm[:, :ntok], in0=psr[:, :ntok], in1=sg[:, :ntok],
                                                                                          op=ALU.mult), R=[psr, sg], W=[tm])
                                if br == 1:
                                    k.op('pool', lambda e, tm=tm, acc=acc: e.tensor_tensor(out=acc[:, :ntok], in0=acc[:, :ntok], in1=tm[:, :ntok],
                                                                                        op=ALU.add), R=[acc, tm], W=[acc])
                                else:
                                    k.op('pool', lambda e, tm=tm, acc=acc, m=m, mT=mT: e.tensor_tensor(out=mT[:, m, :ntok], in0=acc[:, :ntok], in1=tm[:, :ntok],
                                                                                                    op=ALU.add), R=[acc, tm], W=[(mT, m)])
                    ld(mTd[:, :, t0:t0 + ntok], mT[:, :, :ntok], stream='mst%d' % (bi % 2), R=[(mT, m) for m in range(8)], W=[('mTd', bi)])
                k.barrier()
            if stop == 'p3b':
                break

            pmoe = ExitStack()
            aff = sb("aff%d" % l, [128, NTL, NE], F32, pmoe)
            with ExitStack() as p3c:
                Wout = sb("Wout%d" % l, [128, 8, D], BF16, p3c)
                ld(Wout[:], woutd[l], eng='pool', stream='wbig', W=[Wout])
                Wr = sb("Wr%d" % l, [128, 8, NE], F32, p3c)
                ld(Wr[:], wrd[l], W=[Wr])
                bct = {}
                for vv in range(2):
                    for vi, nm in ((0, 'gt1'), (1, 'sh2'), (2, 'A2')):
                        t_ = sb("bc_%s%d_%d" % (nm, vv, l), [128, D], F32, p3c)
                        ld(t_[:], bcd[vv, vi], W=[t_])
                        bct[(vv, nm)] = t_
                mT2 = [sb("mTo%d_%d" % (l, i), [128, 8, 512], BF16, p3c) for i in range(2)]
                xt2 = [sb("xo%d_%d" % (l, i), [128, D], F32, p3c) for i in range(2)]
                xw2 = [sb("xw%d_%d" % (l, i), [128, D], F32, p3c) for i in range(2)]
                h22 = [sb("h2%d_%d" % (l, i), [128, D], F32, p3c) for i in range(2)]
                h2b2 = [sb("h2b%d_%d" % (l, i), [128, D], BF16, p3c) for i in range(2)]
                junk = sb("junk3%d" % l, [128, D], F32, p3c)
                h2T = [sb("h2T%d_%d" % (l, i), [128, 8, 128], F32, p3c) for i in range(2)]
                ex2 = [sb("ex%d_%d" % (l, i), [128, NE], F32, p3c) for i in range(2)]
                sm = sb("sm%d" % l, [128, 8], F32, p3c)
                tg = 0
                for bi, (t0, ntok, v) in enumerate(mblocks):
                    mT = mT2[bi % 2]
                    ld(mT[:, :, :ntok], mTd[:, :, t0:t0 + ntok], stream='mo%d' % (bi % 2), W=[mT])
                    for tt in range(ntok // 128):
                        i2 = tg % 2
                        tg += 1
                        xt, xw, h2, h2b, hT_, ex = xt2[i2], xw2[i2], h22[i2], h2b2[i2], h2T[i2], ex2[i2]
                        r0 = t0 + tt * 128
                        ti = r0 // 128
                        ld(xt[:], xs[r0:r0 + 128, :], stream='xo%d' % i2, W=[xt])
                        for half in range(2):
                            hs = slice(half * 512, (half + 1) * 512)
                            po = ps_get('kp', [2, 3])
                            for m in range(8):
                                k.op('pe', lambda e, m=m, po=po, hs=hs, tt=tt: e.matmul(po[:, :], lhsT=mT[:, m, tt * 128:(tt + 1) * 128], rhs=Wout[:, m, hs],
                                                                                       start=(m == 0), stop=(m == 7)), R=[Wout, mT], W=[po])
                            k.op('dve', lambda e, po=po, hs=hs, xw=xw: e.tensor_tensor(out=xw[:, hs], in0=po[:, :], in1=bct[(v, 'gt1')][:, hs], op=ALU.mult),
                                 R=[po, bct[(v, 'gt1')]], W=[(xw, half)])
                            k.op('pool', lambda e, hs=hs, xw=xw, xt=xt: e.tensor_tensor(out=xw[:, hs], in0=xw[:, hs], in1=xt[:, hs], op=ALU.add),
                                 R=[(xw, half), xt], W=[(xw, half)])
                        ld(xs[r0:r0 + 128, :], xw[:], stream='xst%d' % i2, R=[(xw, 0), (xw, 1)], W=[('xs', ti)])
                        col = 2 + i2
                        k.op('pool', lambda e, col=col: e.memset(ss[:, col:col + 1], 0.0), W=[('ss', col)])
                        k.op('act', lambda e, xw=xw, col=col: e.activation(out=junk[:], in_=xw[:], func=AF.Square, accum_out=ss[:, col:col + 1]),
                             R=[(xw, 0), (xw, 1)], W=[junk, ('ss', col)])
                        k.op('act', lambda e, col=col: e.activation(out=rsd[:, col:col + 1], in_=ss[:, col:col + 1], func=AF.Sqrt, scale=1.0 / D, bias=EPS),
                             R=[('ss', col)], W=[('rsd', col)])
                        k.op('dve', lambda e, col=col: e.reciprocal(out=rsd[:, col:col + 1], in_=rsd[:, col:col + 1]), R=[('rsd', col)], W=[('rsd', col)])
                        k.op('dve', lambda e, xw=xw, h2=h2, col=col: e.scalar_tensor_tensor(out=h2[:], in0=xw[:], scalar=rsd[:, col:col + 1], in1=bct[(v, 'A2')][:],
                                                                                         op0=ALU.mult, op1=ALU.mult),
                             R=[(xw, 0), (xw, 1), ('rsd', col), bct[(v, 'A2')]], W=[h2])
                        k.op('pool', lambda e, h2=h2: e.tensor_tensor(out=h2[:], in0=h2[:], in1=bct[(v, 'sh2')][:], op=ALU.add), R=[h2, bct[(v, 'sh2')]], W=[h2])
                        k.op('act', lambda e, h2=h2, h2b=h2b: e.copy(out=h2b[:], in_=h2[:]), R=[h2], W=[h2b])
                        ld(h2d[r0:r0 + 128, :], h2b[:], stream='h2st%d' % i2, R=[h2b], W=[('h2d', ti)])
                        for half in range(2):
                            pt = ps_get('tr', [0, 1])
                            for q in range(4):
                                kc = half * 4 + q
                                k.op('pe', lambda e, pt=pt, q=q, kc=kc, h2=h2: e.transpose(pt[:, q * 128:(q + 1) * 128], h2[:, kc * 128:(kc + 1) * 128], ident[:]),
                                     R=[h2, ident], W=[pt])
                            eng = 'act' if half == 0 else 'dve'
                            src = pt[:, :]
                            dst = hT_[:].rearrange("p a b -> p (a b)")[:, half * 512:(half + 1) * 512]
                            if eng == 'act':
                                k.op('act', lambda e, dst=dst, src=src: e.copy(out=dst, in_=src), R=[pt], W=[(hT_, half)])
                            else:
                                k.op('dve', lambda e, dst=dst, src=src: e.tensor_copy(out=dst, in_=src), R=[pt], W=[(hT_, half)])
                        pl = ps_get('nr', [6])
                        for kc in range(8):
                            k.op('pe', lambda e, kc=kc, pl=pl, hT_=hT_: e.matmul(pl[:, 0:NE], lhsT=hT_[:, kc, :], rhs=Wr[:, kc, :], start=(kc == 0), stop=(kc == 7)),
                                 R=[(hT_, 0), (hT_, 1), Wr], W=[pl])
                        c2 = 4 * i2
                        k.op('dve', lambda e, pl=pl, c2=c2: e.tensor_reduce(out=sm[:, c2:c2 + 1], in_=pl[:, 0:NE], axis=AX.X, op=ALU.max), R=[pl], W=[('sm', i2)])
                        k.op('dve', lambda e, c2=c2: e.tensor_scalar(out=sm[:, c2 + 1:c2 + 2], in0=sm[:, c2:c2 + 1], scalar1=-1.0, scalar2=None, op0=ALU.mult),
                             R=[('sm', i2)], W=[('sm', i2)])
                        k.op('pool', lambda e, c2=c2: e.memset(sm[:, c2 + 2:c2 + 3], 0.0), R=[('sm', i2)], W=[('sm', i2)])
                        k.op('act', lambda e, pl=pl, ex=ex, c2=c2: e.activation(out=ex[:], in_=pl[:, 0:NE], func=AF.Exp, bias=sm[:, c2 + 1:c2 + 2],
                                                                              accum_out=sm[:, c2 + 2:c2 + 3]), R=[pl, ('sm', i2)], W=[ex, ('sm', i2)])
                        k.op('dve', lambda e, c2=c2: e.reciprocal(out=sm[:, c2 + 3:c2 + 4], in_=sm[:, c2 + 2:c2 + 3]), R=[('sm', i2)], W=[('sm', i2)])
                        k.op('dve', lambda e, ex=ex, ti=ti, c2=c2: e.tensor_scalar(out=aff[:, ti, :], in0=ex[:], scalar1=sm[:, c2 + 3:c2 + 4], scalar2=None, op0=ALU.mult),
                             R=[ex, ('sm', i2)], W=[(aff, ti)])
                        ld(affd[r0:r0 + 128, :], aff[:, ti, :], stream='afst%d' % i2, R=[(aff, ti)], W=[('affd', ti)])
                k.barrier()
            if stop == 'p3c':
                pmoe.close()
                break

            with pmoe:
                gt2 = [sb("gt2_%d_%d" % (l, vv), [128, D], F32, pmoe) for vv in range(2)]
                for vv in range(2):
                    ld(gt2[vv][:], bcd[vv, 3], W=[gt2[vv]])
                NSC = 9
                NJ = NE * NSC
                idx_all = sb("idx_all%d" % l, [128, NJ], I32, pmoe)
                gate_all = sb("gate_all%d" % l, [128, NJ], F32, pmoe)
                k.op('pool', lambda e: e.memset(idx_all[:], 1 << 20), W=[idx_all])
                k.op('pool', lambda e: e.memset(gate_all[:], 0.0), W=[gate_all])
                sets = [(0, 64, 1024, 0)] + ([] if last else [(64, 2, 32, 1)])
                nst = 8 if last else 9
                with ExitStack() as pth:
                    iota = sb("iota%d" % l, [128, 1024], F32, pth)
                    ld(iota[:], iotad.ap(), W=[iota])
                    comb = sb("comb%d" % l, [128, NTL, NE, 5], BF16, pth)
                    ld(comb[:], combd.ap(), eng='pool', W=[comb])
                    posm = sb("posm%d" % l, [128, NTL, NE], F32, pth)
                    lo = sb("lo%d" % l, [128, NE], F32, pth)
                    hi = sb("hi%d" % l, [128, NE], F32, pth)
                    mid = sb("mid%d" % l, [128, NE], F32, pth)
                    ge = sb("ge%d" % l, [128, NE], F32, pth)
                    g2 = sb("g2%d" % l, [128, NE], F32, pth)
                    cntp = sb("cntp%d" % l, [128, NE], F32, pth)
                    cmp_ = sb("cmp%d" % l, [128, 64, NE], F32, pth)
                    maskb = sb("maskb%d" % l, [128, 64, NE], BF16, pth)
                    inc = [sb("inc%d_%d" % (l, i), [128, 64, NE], F32, pth) for i in range(2)]
                    tot = sb("tot%d" % l, [128, 64, NE], F32, pth)
                    r1 = sb("r1_%d" % l, [128, NTL, NE], F32, pth)
                    r2 = sb("r2_%d" % l, [128, NTL, NE], F32, pth)
                    k.op('pool', lambda e: e.tensor_copy(out=comb[:, :, :, 2], in_=aff[:]), R=[aff], W=[comb])
                    k.op('dve', lambda e: e.tensor_tensor(out=r1[:], in0=aff[:], in1=comb[:, :, :, 2], op=ALU.subtract), R=[aff, comb], W=[r1])
                    k.op('pool', lambda e: e.tensor_copy(out=comb[:, :, :, 3], in_=r1[:]), R=[r1], W=[comb])
                    k.op('dve', lambda e: e.tensor_tensor(out=r2[:], in0=r1[:], in1=comb[:, :, :, 3], op=ALU.subtract), R=[r1, comb], W=[r2])
                    k.op('pool', lambda e: e.tensor_copy(out=comb[:, :, :, 4], in_=r2[:]), R=[r2], W=[comb])
                    for (ti0, T, cap, vv) in sets:
                        affs = aff[:, ti0:ti0 + T, :]
                        k.op('dve', lambda e: e.memset(lo[:], 0.0), W=[lo])
                        k.op('dve', lambda e: e.memset(hi[:], 1.0), W=[hi])
                        for it in range(32):
                            k.op('dve', lambda e: e.tensor_tensor(out=mid[:], in0=lo[:], in1=hi[:], op=ALU.add), R=[lo, hi], W=[mid])
                            k.op('dve', lambda e: e.tensor_scalar(out=mid[:], in0=mid[:], scalar1=0.5, scalar2=None, op0=ALU.mult), R=[mid], W=[mid])
                            k.op('dve', lambda e, T=T, affs=affs: e.tensor_tensor(out=cmp_[:, :T, :], in0=affs, in1=mid[:].unsqueeze(1).to_broadcast([128, T, NE]),
                                                                                 op=ALU.is_ge), R=[aff, mid], W=[cmp_])
                            k.op('dve', lambda e, T=T: e.tensor_reduce(out=cntp[:], in_=cmp_[:, :T, :].rearrange("p t e -> p e t"), axis=AX.X, op=ALU.add),
                                 R=[cmp_], W=[cntp])
                            pc = ps_get('nr', [6])
                            k.op('pe', lambda e, pc=pc: e.matmul(pc[:, 0:NE], lhsT=ones32[:], rhs=cntp[:], start=True, stop=True), R=[ones32, cntp], W=[pc])
                            k.op('dve', lambda e, pc=pc, cap=cap: e.tensor_scalar(out=ge[:], in0=pc[:, 0:NE], scalar1=float(cap) - 0.5, scalar2=None, op0=ALU.is_ge),
                                 R=[pc], W=[ge])
                            k.op('dve', lambda e: e.tensor_tensor(out=g2[:], in0=ge[:], in1=mid[:], op=ALU.mult), R=[ge, mid], W=[g2])
                            k.op('dve', lambda e: e.tensor_tensor(out=lo[:], in0=lo[:], in1=g2[:], op=ALU.max), R=[lo, g2], W=[lo])
                            k.op('dve', lambda e: e.scalar_tensor_tensor(out=g2[:], in0=ge[:], scalar=2.0, in1=mid[:], op0=ALU.mult, op1=ALU.add),
                                 R=[ge, mid, g2], W=[g2])
                            k.op('dve', lambda e: e.tensor_tensor(out=hi[:], in0=hi[:], in1=g2[:], op=ALU.min), R=[hi, g2], W=[hi])
                        k.op('dve', lambda e, T=T, affs=affs: e.tensor_tensor(out=cmp_[:, :T, :], in0=affs, in1=lo[:].unsqueeze(1).to_broadcast([128, T, NE]),
                                                                             op=ALU.is_ge), R=[aff, lo], W=[cmp_])
                        k.op('pool', lambda e, T=T: e.tensor_copy(out=maskb[:, :T, :], in_=cmp_[:, :T, :]), R=[cmp_], W=[maskb])
                        ncol = T * NE
                        mb = maskb[:].rearrange("p t e -> p (t e)")
                        totf = tot[:].rearrange("p t e -> p (t e)")
                        pp = [PS[2], PS[3]]
                        pq = [PS[4], PS[5]]
                        nhf = (ncol + 511) // 512
                        for hf in range(nhf):
                            n_ = min(512, ncol - hf * 512)
                            k.op('pe', lambda e, hf=hf, n_=n_: e.matmul(pp[hf][:, :n_], lhsT=triL[:], rhs=mb[:, hf * 512:hf * 512 + n_], start=True, stop=True),
                                 R=[triL, maskb], W=[pp[hf]])
                            k.op('pe', lambda e, hf=hf, n_=n_: e.matmul(pq[hf][:, :n_], lhsT=onesb[:], rhs=mb[:, hf * 512:hf * 512 + n_], start=True, stop=True),
                                 R=[onesb, maskb], W=[pq[hf]])
                            k.op('act', lambda e, hf=hf, n_=n_: e.copy(out=totf[:, hf * 512:hf * 512 + n_], in_=pq[hf][:, :n_]), R=[pq[hf]], W=[tot])
                        k.op('pool', lambda e, T=T: e.tensor_copy(out=inc[0][:, :T, :], in_=tot[:, :T, :]), R=[tot], W=[inc[0]])
                        cur = 0
                        s_ = 1
                        while s_ < T:
                            a_, b_ = inc[cur], inc[1 - cur]
                            k.op('dve', lambda e, a_=a_, b_=b_, s_=s_, T=T: e.tensor_tensor(out=b_[:, s_:T, :], in0=a_[:, s_:T, :], in1=a_[:, 0:T - s_, :], op=ALU.add),
                                 R=[a_], W=[b_])
                            k.op('pool', lambda e, a_=a_, b_=b_, s_=s_: e.tensor_copy(out=b_[:, 0:s_, :], in_=a_[:, 0:s_, :]), R=[a_], W=[b_])
                            cur = 1 - cur
                            s_ *= 2
                        incf = inc[cur]
                        oth = inc[1 - cur]
                        othf = oth[:].rearrange("p t e -> p (t e)")
                        k.op('dve', lambda e, T=T: e.tensor_tensor(out=oth[:, :T, :], in0=incf[:, :T, :], in1=tot[:, :T, :], op=ALU.subtract), R=[incf, tot], W=[oth])
                        for hf in range(nhf):
                            n_ = min(512, ncol - hf * 512)
                            k.op('dve', lambda e, hf=hf, n_=n_: e.tensor_tensor(out=othf[:, hf * 512:hf * 512 + n_], in0=othf[:, hf * 512:hf * 512 + n_],
                                                                              in1=pp[hf][:, :n_], op=ALU.add), R=[oth, pp[hf]], W=[oth])
                        k.op('dve', lambda e, T=T, ti0=ti0: e.scalar_tensor_tensor(out=posm[:, ti0:ti0 + T, :], in0=oth[:, :T, :], scalar=1.0, in1=cmp_[:, :T, :],
                                                                                  op0=ALU.add, op1=ALU.mult), R=[oth, cmp_], W=[posm])
                        k.op('dve', lambda e, T=T, ti0=ti0: e.tensor_scalar(out=posm[:, ti0:ti0 + T, :], in0=posm[:, ti0:ti0 + T, :], scalar1=-1.0, scalar2=None,
                                                                           op0=ALU.add), R=[posm], W=[posm])
                    k.barrier()
                    if debug and stop == 'p4a':
                        dpos = nc.dram_tensor("dbgposm", [128, NTL, NE], F32, kind="ExternalOutput")
                        ld(dpos.ap(), posm[:], R=[posm], W=['dbgo'])
                        k.barrier()
                        break

                    Sb_ = [sb("Sone%d_%d" % (l, i), [128, 1024], BF16, pth) for i in range(3)]
                    rows5 = sb("rows5_%d" % l, [8, 1056], F32, pth)
                    t5 = sb("t5_%d" % l, [128, 48], F32, pth)
                    idxf = sb("idxf%d" % l, [128, NSC], F32, pth)
                    g1 = sb("g1_%d" % l, [128, NSC], F32, pth)
                    rr = 0
                    for e_ in range(NE):
                        for (ti0, T, cap, vv) in sets:
                            off = 0 if vv == 0 else 1024
                            nh = (cap + 511) // 512
                            pi = [PS[0], PS[1]]
                            for i_ in range(T):
                                S_ = Sb_[rr % 3]
                                rr += 1
                                ti = ti0 + i_
                                k.op('dve', lambda e, S_=S_, ti=ti, e_=e_, cap=cap: e.tensor_scalar(out=S_[:, :cap], in0=iota[:, :cap], scalar1=posm[:, ti, e_:e_ + 1],
                                                                                                   scalar2=None, op0=ALU.is_equal), R=[iota, posm], W=[S_])
                                for hf in range(nh):
                                    n_ = min(512, cap - hf * 512)
                                    k.op('pe', lambda e, S_=S_, ti=ti, hf=hf, n_=n_, i_=i_, T=T, e_=e_: e.matmul(pi[hf][0:5, :n_], lhsT=comb[:, ti, e_, :],
                                                                                                              rhs=S_[:, hf * 512:hf * 512 + n_],
                                                                                                              start=(i_ == 0), stop=(i_ == T - 1)), R=[comb, S_], W=[pi[hf]])
                            for hf in range(nh):
                                n_ = min(512, cap - hf * 512)
                                k.op('act', lambda e, hf=hf, n_=n_, off=off: e.copy(out=rows5[0:5, off + hf * 512:off + hf * 512 + n_], in_=pi[hf][0:5, :n_]),
                                     R=[pi[hf]], W=[rows5])
                        ptx = ps_get('nr', [6])
                        for s in range(nst):
                            prow = 128 if s < 8 else 32
                            k.op('pe', lambda e, s=s, prow=prow: e.transpose(ptx[0:prow, 5 * s:5 * s + 5], rows5[0:5, s * 128:s * 128 + prow], ident[0:5, 0:5]),
                                 R=[rows5, ident], W=[ptx])
                        k.op('act', lambda e: e.copy(out=t5[:, 0:40], in_=ptx[:, 0:40]), R=[ptx], W=[t5])
                        if nst == 9:
                            k.op('act', lambda e: e.copy(out=t5[0:32, 40:45], in_=ptx[0:32, 40:45]), R=[ptx], W=[t5])
                        for (c0, c1, prow) in ((0, 8, 128),) + (((8, 9, 32),) if nst == 9 else ()):
                            n5 = slice(5 * c0, 5 * c1, 5)
                            k.op('dve', lambda e, c0=c0, c1=c1, prow=prow: e.scalar_tensor_tensor(
                                out=idxf[0:prow, c0:c1], in0=t5[0:prow, 5 * c0:5 * c1:5], scalar=128.0, in1=t5[0:prow, 5 * c0 + 1:5 * c1:5], op0=ALU.mult, op1=ALU.add),
                                R=[t5], W=[idxf])
                            k.op('dve', lambda e, c0=c0, c1=c1, prow=prow, e_=e_: e.tensor_copy(out=idx_all[0:prow, e_ * NSC + c0:e_ * NSC + c1], in_=idxf[0:prow, c0:c1]),
                                 R=[idxf], W=[idx_all])
                            k.op('dve', lambda e, c0=c0, c1=c1, prow=prow: e.tensor_tensor(out=g1[0:prow, c0:c1], in0=t5[0:prow, 5 * c0 + 2:5 * c1:5],
                                                                                         in1=t5[0:prow, 5 * c0 + 3:5 * c1:5], op=ALU.add), R=[t5], W=[g1])
                            k.op('dve', lambda e, c0=c0, c1=c1, prow=prow, e_=e_: e.tensor_tensor(out=gate_all[0:prow, e_ * NSC + c0:e_ * NSC + c1], in0=g1[0:prow, c0:c1],
                                                                                               in1=t5[0:prow, 5 * c0 + 4:5 * c1:5], op=ALU.add), R=[g1, t5], W=[gate_all])
                    k.barrier()
                if debug and stop == 'p4a':
                    break
                if debug and stop == 'p4b':
                    di = nc.dram_tensor("dbgidx", [128, NJ], I32, kind="ExternalOutput")
                    dg = nc.dram_tensor("dbggate", [128, NJ], F32, kind="ExternalOutput")
                    ld(di.ap(), idx_all[:], R=[idx_all], W=['dbgo'])
                    ld(dg.ap(), gate_all[:], R=[gate_all], W=['dbgo'])
                    k.barrier()
                    break

                with ExitStack() as pgl:
                    xgt = sb("xgt%d" % l, [128, D], BF16, pgl)
                    k.op('pool', lambda e: e.memset(xgt[:], 0.0), W=[xgt])
                    k.barrier()
                    gather_loop(idx_all, h2d, xgd, xgt, NJ, "g%d" % l)
                    k.op('pool', lambda e: e.memset(xgt[:, 0:2], 0.0), W=[xgt])
                    k.barrier()

                with ExitStack() as pex:
                    Wg2 = [sb("Wg%d_%d" % (l, i), [128, 8, D], BF16, pex) for i in range(2)]
                    Wu2 = [sb("Wu%d_%d" % (l, i), [128, 8, D], BF16, pex) for i in range(2)]
                    Wd2 = [sb("Wd%d_%d" % (l, i), [128, 8, D], BF16, pex) for i in range(2)]
                    xg = sb("xg%d" % l, [128, NSC, D], BF16, pex)
                    xsT = sb("xsT%d" % l, [128, 8, 1056], BF16, pex)
                    hd = sb("hd%d" % l, [128, 8, 1056], BF16, pex)
                    sa2 = [sb("sa%d_%d" % (l, i), [128, 512], F32, pex) for i in range(2)]
                    yo2 = [sb("yo%d_%d" % (l, i), [128, D], F32, pex) for i in range(2)]
                    cnt_ = [0]

                    def load_expert(e_, slot):
                        ld(Wg2[slot][:], wegd[l, e_], eng='pool', W=[Wg2[slot]])
                        ld(Wu2[slot][:], weud[l, e_], eng='pool', W=[Wu2[slot]])
                        ld(Wd2[slot][:], wedd[l, e_], eng='pool', W=[Wd2[slot]])

                    load_expert(0, 0)
                    groups = [(0, 512), (512, 512)] + ([(1024, 32)] if nst == 9 else [])
                    for e_ in range(NE):
                        slot = e_ % 2
                        if e_ + 1 < NE:
                            load_expert(e_ + 1, 1 - slot)
                        Wg, Wu, Wd = Wg2[slot], Wu2[slot], Wd2[slot]
                        j0 = e_ * NSC
                        ld(xg[:, 0:nst, :], xgd[j0 * 128:(j0 + nst) * 128, :].rearrange("(s p) d -> p s d", p=128), W=[xg])
                        for kc in range(8):
                            for s in range(8):
                                k.op('pe', lambda e, s=s, kc=kc: e.transpose(PSB[:, s * 128:(s + 1) * 128], xg[:, s, kc * 128:(kc + 1) * 128], identb[:]),
                                     R=[xg, identb], W=[PSB])
                            if kc % 2 == 0:
                                k.op('act', lambda e, kc=kc: e.copy(out=xsT[:, kc, 0:1024], in_=PSB[:, 0:1024]), R=[PSB], W=[(xsT, kc)])
                            else:
                                k.op('dve', lambda e, kc=kc: e.tensor_copy(out=xsT[:, kc, 0:1024], in_=PSB[:, 0:1024]), R=[PSB], W=[(xsT, kc)])
                        if nst == 9:
                            for kc in range(8):
                                k.op('pe', lambda e, kc=kc: e.transpose(PSB[:, kc * 32:(kc + 1) * 32], xg[0:32, 8, kc * 128:(kc + 1) * 128], identb[0:32, 0:32]),
                                     R=[xg, identb], W=[PSB])
                            for kc in range(8):
                                k.op('act', lambda e, kc=kc: e.copy(out=xsT[:, kc, 1024:1056], in_=PSB[:, kc * 32:(kc + 1) * 32]), R=[PSB], W=[(xsT, kc)])
                        xr = [(xsT, kc) for kc in range(8)]
                        for fc in range(8):
                            for (c0, n_) in groups:
                                pa = ps_get('s', [0, 1])
                                pu = ps_get('kr', [4, 5])
                                sa = sa2[cnt_[0] % 2]
                                cnt_[0] += 1
                                for kc in range(8):
                                    k.op('pe', lambda e, kc=kc, fc=fc, c0=c0, n_=n_, pa=pa: e.matmul(pa[:, :n_], lhsT=Wg[:, kc, fc * 128:(fc + 1) * 128],
                                                                                                    rhs=xsT[:, kc, c0:c0 + n_], start=(kc == 0), stop=(kc == 7)),
                                         R=[Wg] + xr, W=[pa])
                                for kc in range(8):
                                    k.op('pe', lambda e, kc=kc, fc=fc, c0=c0, n_=n_, pu=pu: e.matmul(pu[:, :n_], lhsT=Wu[:, kc, fc * 128:(fc + 1) * 128],
                                                                                                    rhs=xsT[:, kc, c0:c0 + n_], start=(kc == 0), stop=(kc == 7)),
                                         R=[Wu] + xr, W=[pu])
                                k.op('act', lambda e, sa=sa, pa=pa, n_=n_: e.activation(out=sa[:, :n_], in_=pa[:, :n_], func=AF.Silu), R=[pa], W=[sa])
                                k.op('dve', lambda e, sa=sa, pu=pu, n_=n_, fc=fc, c0=c0: e.tensor_tensor(out=hd[:, fc, c0:c0 + n_], in0=pu[:, :n_], in1=sa[:, :n_],
                                                                                                      op=ALU.mult), R=[pu, sa], W=[(hd, fc)])
                        hr = [(hd, fc) for fc in range(8)]
                        for s in range(nst):
                            prow = 128 if s < 8 else 32
                            vv = 0 if s < 8 else 1
                            yo = yo2[s % 2]
                            for half in range(2):
                                hs = slice(half * 512, (half + 1) * 512)
                                py = ps_get('kp', [2, 3]) if half == 0 else ps_get('nr', [6])
                                for fc in range(8):
                                    k.op('pe', lambda e, fc=fc, s=s, py=py, hs=hs, prow=prow: e.matmul(py[0:prow, :], lhsT=hd[:, fc, s * 128:s * 128 + prow], rhs=Wd[:, fc, hs],
                                                                                                      start=(fc == 0), stop=(fc == 7)), R=[Wd] + hr, W=[py])
                                k.op('dve', lambda e, py=py, yo=yo, hs=hs, s=s, vv=vv, prow=prow, j0=j0: e.scalar_tensor_tensor(
                                    out=yo[0:prow, hs], in0=py[0:prow, :], scalar=gate_all[0:prow, j0 + s:j0 + s + 1], in1=gt2[vv][0:prow, hs], op0=ALU.mult, op1=ALU.mult),
                                    R=[py, gate_all, gt2[vv]], W=[(yo, half)])
                            ld(Yd[(j0 + s) * 128:(j0 + s) * 128 + prow, :], yo[0:prow, :], R=[(yo, 0), (yo, 1)], W=[('Yd', j0 + s)])
                    k.barrier()

                with ExitStack() as psl:
                    yt = sb("yt%d" % l, [128, D], F32, psl)
                    k.op('pool', lambda e: e.memset(yt[:], 0.0), W=[yt])
                    k.barrier()
                    scatter_loop(idx_all, Yd, xs, yt, NJ, "s%d" % l)
                    k.op('pool', lambda e: e.memset(yt[:, 0:2], 0.0), W=[yt])
                    k.barrier()
        k.barrier()
    return nc


def _colmajor(w):
    return np.ascontiguousarray(w.reshape(8, 128, -1).transpose(1, 0, 2))


def _consts():
    ident = np.eye(128, dtype=np.float32)
    blk = (np.arange(128)[:, None] // 64 == np.arange(128)[None, :] // 64).astype(np.float32)
    p = np.arange(128)
    triL = (p[:, None] < p[None, :]).astype(np.float32)
    maskP = (p[:, None] >= p[None, :]).astype(np.float32)
    maskN = (p[:, None] <= p[None, :]).astype(np.float32)
    cm = np.ascontiguousarray(np.stack([ident, blk, triL, maskP, maskN], axis=1))
    iota = np.ascontiguousarray(np.broadcast_to(np.arange(1024, dtype=np.float32), (128, 1024)))
    tidc = np.zeros((128, NTL, NE, 5), np.float32)
    tidc[:, :, :, 0] = np.arange(NTL)[None, :, None]
    tidc[:, :, :, 1] = np.arange(128)[:, None, None]
    t = np.arange(NL)
    row = (t // 64).astype(np.float32)
    colp = (t % 64).astype(np.float32)
    inv = np.power(np.float32(10000.0), -(np.arange(16, dtype=np.float32) / np.float32(16))).astype(np.float32)
    ang = np.concatenate([row[:, None] * inv[None, :], colp[:, None] * inv[None, :]], axis=-1).astype(np.float32)
    cos = np.cos(ang).astype(np.float32)
    sin = np.sin(ang).astype(np.float32)
    C = np.ones((128, NT), np.float32)
    S = np.zeros((128, NT), np.float32)
    for r in range(128):
        i = r % 64
        f = i % 32
        C[r, :NL] = cos[:, f]
        S[r, :NL] = sin[:, f] * (-1.0 if i < 32 else 1.0)
    return cm, iota, tidc, C, S


def _swap_halves(cols):
    cols = np.asarray(cols)
    return (cols // 64) * 64 + ((cols % 64) + 32) % 64


def prep_inputs(inp):
    L = inp['w_ada'].shape[0]
    cm, iota, tidc, C, S = _consts()
    f = lambda a: np.ascontiguousarray(a, dtype=np.float32)
    w_in = inp['w_in']
    kA, vA, kB, vB = np.arange(0, 128), np.arange(128, 256), np.arange(256, 384), np.arange(384, 512)
    qa0, qb0, cv0, gt0 = 512, 1024, 1536, 3072
    qorder = np.concatenate([np.concatenate([np.arange(j * 64, j * 64 + 64), np.arange((4 + j) * 64, (4 + j) * 64 + 64)]) for j in range(4)])
    c1 = np.concatenate([kA, kB, _swap_halves(kA), _swap_halves(kB), vA, vB])
    c2 = np.concatenate([qa0 + qorder, qa0 + _swap_halves(qorder), qb0 + qorder, qb0 + _swap_halves(qorder)])
    ccx = np.concatenate([np.arange(cv0 + 512, cv0 + 1024), np.arange(cv0 + 1024, cv0 + 1536)])
    c3 = np.concatenate([np.arange(cv0, cv0 + 512), np.arange(gt0, gt0 + 3072)])
    shared = {}
    shared['w_ada'] = f(np.stack([_colmajor(inp['w_ada'][l]) for l in range(L)]))
    bcol = np.stack([inp['b_ada'][l].reshape(48, 128).T for l in range(L)])
    shared['b_col2'] = f(np.repeat(bcol, 2, axis=2))
    sel = [slice(2048, 3072), slice(3072, 4096), slice(4096, 5120), slice(5120, 6144)]
    shared['b_bc'] = f(np.stack([np.stack([np.broadcast_to(inp['b_ada'][l][s], (128, 1024)) for s in sel], axis=1) for l in range(L)]))
    gc = np.zeros((L, 128, 2, 8, 2), np.float32)
    for l in range(L):
        gc[l, :, 0, :, :] = inp['g_mix'][l].reshape(8, 128).T[:, :, None]
        gc[l, :, 1, :, :] = inp['g_ffn'][l].reshape(8, 128).T[:, :, None]
    shared['gcol2'] = gc
    shared['gffn_bc'] = f(np.stack([np.broadcast_to(inp['g_ffn'][l], (128, 1024)) for l in range(L)]))
    shared['w1'] = f(np.stack([_colmajor(w_in[l][:, c1]) for l in range(L)]))
    shared['w2'] = f(np.stack([_colmajor(w_in[l][:, c2]) for l in range(L)]))
    shared['wcx'] = f(np.stack([_colmajor(w_in[l][:, ccx]) for l in range(L)]))
    shared['w3'] = f(np.stack([_colmajor(w_in[l][:, c3]) for l in range(L)]))
    wbr = np.zeros((L, 128, 12, 1024), np.float32)
    for l in range(L):
        for br in range(3):
            wb = inp['w_branch'][l, br]
            if br < 2:
                wb = wb[qorder]
            wbr[l, :, br * 4:(br + 1) * 4, :] = wb.reshape(4, 128, 1024).transpose(1, 0, 2)
    shared['wbr'] = wbr
    shared['wout'] = f(np.stack([_colmajor(inp['w_out'][l]) for l in range(L)]))
    hgv = np.zeros((L, 128, 8), np.float32)
    sw = (np.arange(64) + 32) % 64
    for l in range(L):
        for i, g in enumerate((inp['qg_a'][l], inp['kg_a'][l], inp['qg_b'][l], inp['kg_b'][l])):
            hgv[l, :, 2 * i] = np.tile(g, 2)
            hgv[l, :, 2 * i + 1] = np.tile(g[sw], 2)
    shared['hg'] = hgv
    shared['sink'] = f(np.stack([np.broadcast_to(inp['sink_b'][l], (128, 8)) for l in range(L)]))
    shared['convw'] = f(np.stack([inp['conv_w'][l].reshape(3, 4, 128).transpose(2, 1, 0) for l in range(L)]))
    shared['wr'] = f(np.stack([_colmajor(inp['w_router'][l]) for l in range(L)]))
    for nm, key in (('weg', 'w_e_gate'), ('weu', 'w_e_up'), ('wed', 'w_e_down')):
        w = inp[key]
        shared[nm] = f(w.reshape(L, NE, 8, 128, 1024).transpose(0, 1, 3, 2, 4))
    shared['ropeC'] = C
    shared['ropeS'] = S
    shared['cmisc'] = cm
    shared['iota'] = iota
    shared['comb'] = tidc
    maps = []
    B = inp['x'].shape[0]
    for b in range(B):
        m = dict(shared)
        m['xin'] = f(np.concatenate([inp['x'][b], inp['ctx'][b]], axis=0))
        cv = np.zeros((128, 8, 2), np.float32)
        cv[:, :, 0] = inp['c'][b].reshape(8, 128).T
        cv[:, :, 1] = inp['c_ctx'].reshape(8, 128).T
        m['cvec'] = cv
        maps.append(m)
    return maps


_NC_CACHE = {}
_PER_LAYER = ('w_ada', 'b_col2', 'b_bc', 'gcol2', 'gffn_bc', 'w1', 'w2', 'wcx', 'w3', 'wbr', 'wout', 'hg', 'sink', 'convw', 'wr',
              'weg', 'weu', 'wed')
FUSED = True


def _layer_slice(m, l):
    out = {}
    for k_, v in m.items():
        out[k_] = np.ascontiguousarray(v[l:l + 1]) if k_ in _PER_LAYER else v
    return out


def kernel(**inputs):
    inp = {k_: np.asarray(v) for k_, v in inputs.items()}
    maps = prep_inputs(inp)
    L = inp['w_ada'].shape[0]
    cores = list(range(len(maps)))
    if FUSED:
        if 'nc' not in _NC_CACHE:
            _NC_CACHE['nc'] = build(nlayers=L)
        res = run_bass_kernel_spmd(_NC_CACHE['nc'], maps, core_ids=cores)
        xs = [np.asarray(r["xs"]) for r in res.results]
    else:
        xs = [m['xin'] for m in maps]
        for l in range(L):
            key = ('layer', l == L - 1)
            if key not in _NC_CACHE:
                _NC_CACHE[key] = build(nlayers=1, force_ctx=(l != L - 1))
            lm = []
            for m, x_ in zip(maps, xs):
                d = _layer_slice(m, l)
                d['xin'] = np.ascontiguousarray(x_, dtype=np.float32)
                lm.append(d)
            res = run_bass_kernel_spmd(_NC_CACHE[key], lm, core_ids=cores)
            xs = [np.asarray(r["xs"]) for r in res.results]
    out = np.stack([x_[:NL] for x_ in xs], axis=0)
    return out.astype(np.float32)
```
